# Optimizing a Trainium2 kernel written in Bass

```python
import jax, jax.numpy as jnp
from jax import lax
import numpy as np

D_MODEL = 1024
BATCH = 2
SEQ = 8192
DEPTH = 1

MLA_HEADS = 8
MLA_Q_RANK = 384
MLA_KV_RANK = 256
MLA_NOPE = 64
MLA_ROPE = 32
MLA_V = 64
ATTN_QBLOCK = 128
RET_HEADS = 4
RET_QK = 128
RET_V = 256
RET_CHUNK = 128
ROPE_THETA = 10000.0
N_EXPERTS = 64
TOP_K = 8
N_GROUPS = 8
TOPK_GROUPS = 4
D_EXPERT = 256
D_SHARED = 256
ROUTED_SCALE = 2.5
MOE_BLOCK = 128
RMS_EPS = 1e-6
GN_EPS = 1e-5

IN_SPLITS = [MLA_Q_RANK, MLA_KV_RANK, MLA_ROPE,
             RET_HEADS * RET_QK, RET_HEADS * RET_QK, RET_HEADS * RET_V, RET_HEADS * RET_V,
             D_MODEL, D_MODEL]
D_IN = int(sum(IN_SPLITS))
IN_SPLIT_IDX = [int(v) for v in np.cumsum(IN_SPLITS)[:-1]]

kernel_name = "hybrid_mla_retention_moe_adaln"


def rmsnorm(x, g):
    xf = x.astype(jnp.float32)
    y = xf * lax.rsqrt(jnp.mean(xf * xf, axis=-1, keepdims=True) + RMS_EPS)
    return y.astype(x.dtype) * g


def rope_tables(positions, dim, dtype):
    inv = 1.0 / (ROPE_THETA ** (jnp.arange(0, dim, 2, dtype=jnp.float32) / dim))
    ang = positions.astype(jnp.float32)[..., None] * inv
    return jnp.cos(ang).astype(dtype), jnp.sin(ang).astype(dtype)


def rope(x, cos, sin):
    half = x.shape[-1] // 2
    x1, x2 = x[..., :half], x[..., half:]
    return jnp.concatenate([x1 * cos - x2 * sin, x2 * cos + x1 * sin], axis=-1)


def mla_branch(cq, ckv, krope, positions, g_cq, w_uq, g_ckv, w_ukv):
    B, S, _ = cq.shape
    H = MLA_HEADS
    cos, sin = rope_tables(positions, MLA_ROPE, cq.dtype)
    q = (rmsnorm(cq, g_cq) @ w_uq).reshape(B, S, H, MLA_NOPE + MLA_ROPE)
    q_nope, q_pe = q[..., :MLA_NOPE], q[..., MLA_NOPE:]
    q_pe = rope(q_pe, cos[:, :, None, :], sin[:, :, None, :])
    kv = (rmsnorm(ckv, g_ckv) @ w_ukv).reshape(B, S, H, MLA_NOPE + MLA_V)
    k_nope, v = kv[..., :MLA_NOPE], kv[..., MLA_NOPE:]
    k_pe = rope(krope, cos, sin)[:, :, None, :]
    q = jnp.concatenate([q_nope, q_pe], axis=-1)
    k = jnp.concatenate([k_nope, jnp.broadcast_to(k_pe, (B, S, H, MLA_ROPE))], axis=-1)
    scale = (MLA_NOPE + MLA_ROPE) ** -0.5
    n_blocks = S // ATTN_QBLOCK
    key_idx = jnp.arange(S)

    def attend_block(i):
        qb = lax.dynamic_slice_in_dim(q, i * ATTN_QBLOCK, ATTN_QBLOCK, axis=1)
        s = jnp.einsum('bqhd,bkhd->bhqk', qb, k).astype(jnp.float32) * scale
        q_idx = i * ATTN_QBLOCK + jnp.arange(ATTN_QBLOCK)
        causal = q_idx[:, None] >= key_idx[None, :]
        s = jnp.where(causal[None, None], s, -jnp.inf)
        p = jax.nn.softmax(s, axis=-1).astype(v.dtype)
        return jnp.einsum('bhqk,bkhd->bqhd', p, v)

    o = lax.map(attend_block, jnp.arange(n_blocks))
    o = jnp.moveaxis(o, 0, 1).reshape(B, S, H * MLA_V)
    return o


def retention_branch(rq, rk, rv, rg, positions, g_ret):
    B, S, _ = rq.shape
    H, C = RET_HEADS, RET_CHUNK
    N = S // C
    dtype = rq.dtype
    cos, sin = rope_tables(positions, RET_QK, dtype)
    q = rope(rq.reshape(B, S, H, RET_QK), cos[:, :, None, :], sin[:, :, None, :])
    k = rope(rk.reshape(B, S, H, RET_QK), cos[:, :, None, :], sin[:, :, None, :]) * (RET_QK ** -0.5)
    v = rv.reshape(B, S, H, RET_V)

    def to_chunks(t):
        return t.astype(jnp.float32).reshape(B, N, C, H, t.shape[-1]).transpose(0, 3, 1, 2, 4)

    q, k, v = to_chunks(q), to_chunks(k), to_chunks(v)
    gamma = 1.0 - jnp.exp2(-5.0 - jnp.arange(H, dtype=jnp.float32))
    log_g = jnp.log(gamma)
    idx = jnp.arange(C, dtype=jnp.float32)
    diff = idx[:, None] - idx[None, :]
    decay = jnp.where(diff[None] >= 0, jnp.exp(jnp.maximum(diff, 0.0)[None] * log_g[:, None, None]), 0.0)
    scores = jnp.einsum('bhncd,bhnmd->bhncm', q, k) * decay[None, :, None]
    inner = jnp.einsum('bhncm,bhnme->bhnce', scores, v)
    zeta = jnp.exp((C - 1 - idx)[None, :] * log_g[:, None])
    kv = jnp.einsum('bhnmd,bhnme->bhnde', k * zeta[None, :, None, :, None], v)
    g_chunk = jnp.exp(C * log_g)[None, :, None, None]

    def step(state, kv_n):
        return g_chunk * state + kv_n, state

    _, s_prev = lax.scan(step, jnp.zeros((B, H, RET_QK, RET_V), jnp.float32), jnp.moveaxis(kv, 2, 0))
    s_prev = jnp.moveaxis(s_prev, 0, 2)
    xi = jnp.exp((idx + 1.0)[None, :] * log_g[:, None])
    cross = jnp.einsum('bhncd,bhnde->bhnce', q, s_prev) * xi[None, :, None, :, None]
    o = (inner + cross).transpose(0, 2, 3, 1, 4).reshape(B, S, H, RET_V)
    mu = jnp.mean(o, axis=-1, keepdims=True)
    var = jnp.mean(jnp.square(o - mu), axis=-1, keepdims=True)
    o = ((o - mu) * lax.rsqrt(var + GN_EPS)).reshape(B, S, H * RET_V).astype(dtype) * g_ret
    return jax.nn.silu(rg) * o


def moe_ffn(h, w_router, b_router, w_exp_gate, w_exp_up, w_exp_down, w_sh_gate, w_sh_up, w_sh_down):
    T, D = h.shape
    E, G = N_EXPERTS, N_GROUPS
    s = jax.nn.sigmoid((h @ w_router).astype(jnp.float32))
    biased = s + b_router.astype(jnp.float32)
    grp_score = lax.top_k(biased.reshape(T, G, E // G), 2)[0].sum(-1)
    _, grp_idx = lax.top_k(grp_score, TOPK_GROUPS)
    grp_mask = jax.nn.one_hot(grp_idx, G, dtype=jnp.float32).sum(1)
    exp_mask = jnp.repeat(grp_mask, E // G, axis=1) > 0
    _, top_idx = lax.top_k(jnp.where(exp_mask, biased, -jnp.inf), TOP_K)
    w = jnp.take_along_axis(s, top_idx, axis=1)
    w = w / jnp.sum(w, axis=-1, keepdims=True) * ROUTED_SCALE
    combine = jnp.sum(jax.nn.one_hot(top_idx, E, dtype=jnp.float32) * w[..., None], axis=1).astype(h.dtype)
    nb = T // MOE_BLOCK

    def expert_block(args):
        hb, cb = args
        g = jnp.einsum('td,edf->tef', hb, w_exp_gate)
        u = jnp.einsum('td,edf->tef', hb, w_exp_up)
        a = jax.nn.silu(g) * u * cb[:, :, None]
        return jnp.einsum('tef,efd->td', a, w_exp_down)

    routed = lax.map(expert_block, (h.reshape(nb, MOE_BLOCK, D), combine.reshape(nb, MOE_BLOCK, E))).reshape(T, D)
    shared = (jax.nn.silu(h @ w_sh_gate) * (h @ w_sh_up)) @ w_sh_down
    return routed + shared


def setup_inputs(seed: int = 0) -> dict:
    key = jax.random.key(seed)
    ks = jax.random.split(key, 32)
    D = D_MODEL
    f32 = jnp.float32

    def nrm(k, shape, fan_in, mult=1.0):
        return jax.random.normal(k, shape, f32) * (fan_in ** -0.5) * mult

    def gain(k, n):
        return 1.0 + 0.05 * jax.random.normal(k, (n,), f32)

    offsets = jax.random.randint(ks[2], (BATCH, 1), 0, 4096, dtype=jnp.int32)
    positions = offsets + jnp.arange(SEQ, dtype=jnp.int32)[None, :]
    return {
        "x": jax.random.normal(ks[0], (BATCH, SEQ, D), f32),
        "c": jax.random.normal(ks[1], (BATCH, D), f32),
        "positions": positions,
        "w_ada": nrm(ks[3], (D, 6 * D), D, 0.5),
        "b_ada": 0.02 * jax.random.normal(ks[4], (6 * D,), f32),
        "g_norm1": gain(ks[5], D),
        "w_in": nrm(ks[6], (D, D_IN), D),
        "g_cq": gain(ks[7], MLA_Q_RANK),
        "w_uq": nrm(ks[8], (MLA_Q_RANK, MLA_HEADS * (MLA_NOPE + MLA_ROPE)), MLA_Q_RANK),
        "g_ckv": gain(ks[9], MLA_KV_RANK),
        "w_ukv": nrm(ks[10], (MLA_KV_RANK, MLA_HEADS * (MLA_NOPE + MLA_V)), MLA_KV_RANK),
        "g_ret": gain(ks[11], RET_HEADS * RET_V),
        "w_o_mla": nrm(ks[12], (MLA_HEADS * MLA_V, D), MLA_HEADS * MLA_V),
        "w_o_ret": nrm(ks[13], (RET_HEADS * RET_V, D), RET_HEADS * RET_V),
        "w_out": nrm(ks[14], (D, D), D),
        "g_norm2": gain(ks[15], D),
        "w_router": nrm(ks[16], (D, N_EXPERTS), D),
        "b_router": 0.01 * jax.random.normal(ks[17], (N_EXPERTS,), f32),
        "w_exp_gate": nrm(ks[18], (N_EXPERTS, D, D_EXPERT), D),
        "w_exp_up": nrm(ks[19], (N_EXPERTS, D, D_EXPERT), D),
        "w_exp_down": nrm(ks[20], (N_EXPERTS, D_EXPERT, D), D_EXPERT),
        "w_sh_gate": nrm(ks[21], (D, D_SHARED), D),
        "w_sh_up": nrm(ks[22], (D, D_SHARED), D),
        "w_sh_down": nrm(ks[23], (D_SHARED, D), D_SHARED),
        "g_final": gain(ks[24], D),
    }


def reference(x, c, positions, w_ada, b_ada, g_norm1, w_in, g_cq, w_uq, g_ckv, w_ukv, g_ret,
              w_o_mla, w_o_ret, w_out, g_norm2, w_router, b_router, w_exp_gate, w_exp_up,
              w_exp_down, w_sh_gate, w_sh_up, w_sh_down, g_final):
    B, S, D = x.shape
    for _ in range(DEPTH):
        mod = jax.nn.silu(c) @ w_ada + b_ada
        sh1, sc1, gt1, sh2, sc2, gt2 = [m[:, None, :] for m in jnp.split(mod, 6, axis=-1)]
        h = rmsnorm(x, g_norm1) * (1.0 + sc1) + sh1
        z = h @ w_in
        cq, ckv, krope, rq, rk, rv, rg, ga, gb = jnp.split(z, IN_SPLIT_IDX, axis=-1)
        branch_a = mla_branch(cq, ckv, krope, positions, g_cq, w_uq, g_ckv, w_ukv) @ w_o_mla
        branch_b = retention_branch(rq, rk, rv, rg, positions, g_ret) @ w_o_ret
        merged = jax.nn.sigmoid(ga) * branch_a + jax.nn.sigmoid(gb) * branch_b
        x = x + gt1 * (merged @ w_out)
        h2 = rmsnorm(x, g_norm2) * (1.0 + sc2) + sh2
        ffn = moe_ffn(h2.reshape(B * S, D), w_router, b_router, w_exp_gate, w_exp_up, w_exp_down,
                      w_sh_gate, w_sh_up, w_sh_down).reshape(B, S, D)
        x = x + gt2 * ffn
    return rmsnorm(x, g_final)
```

```python
import math
from contextlib import ExitStack

import numpy as np
import ml_dtypes

import concourse.bass as bass
import concourse.mybir as mybir
from concourse.bass_utils import run_bass_kernel_spmd

F32 = mybir.dt.float32
BF16 = mybir.dt.bfloat16
I32 = mybir.dt.int32
AF = mybir.ActivationFunctionType
ALU = mybir.AluOpType
AX = mybir.AxisListType

D = 1024
S = 8192
NT = 64
NO = 16
D_IN = 5792
RMS_EPS = 1e-6
GN_EPS = 1e-5
TWO_PI = 2.0 * math.pi
MAGIC = 12582912.0
C1 = 6.28125
C2 = float(np.float32(TWO_PI - 6.28125))
C3 = float(TWO_PI - 6.28125 - float(np.float32(TWO_PI - 6.28125)))
SCALE_MLA = 96.0 ** -0.5
GAMMA = [1.0 - 2.0 ** (-5.0 - h) for h in range(4)]
NEXP = 64

O_CQ, O_CKV, O_KR, O_RQ, O_RK, O_RV, O_RG, O_GA, O_GB = 0, 384, 640, 672, 1184, 1696, 2720, 3744, 4768


class Ctx:
    def __init__(self, nc):
        self.nc = nc
        self.stacks = [ExitStack()]
        self.eng = {"pe": nc.tensor, "act": nc.scalar, "dve": nc.vector, "pool": nc.gpsimd, "sp": nc.sync}
        self.sem, self.cnt = {}, {}
        for e in ("pe", "act", "dve", "pool"):
            self.sem[e] = self.stacks[0].enter_context(nc.semaphore("s_" + e))
            self.cnt[e] = 0
        self.dsem, self.dcnt = {}, {}
        self.waited = {e: {} for e in self.eng}
        self.last_w, self.readers, self.owner = {}, {}, {}
        self.nops = 0
        self.uid = 0

    def sbuf(self, name, shape, dtype):
        self.uid += 1
        return self.stacks[-1].enter_context(self.nc.sbuf_tensor(f"{name}_{self.uid}", list(shape), dtype))

    def psum(self, name, shape, dtype):
        self.uid += 1
        return self.stacks[-1].enter_context(self.nc.psum_tensor(f"{name}_{self.uid}", list(shape), dtype))

    def push(self):
        self.stacks.append(ExitStack())

    def pop(self):
        self.barrier()
        self.stacks.pop().close()

    def W(self, name, t=0, n=1):
        k = (name, t % n)
        self.owner[k] = t
        return k

    def R(self, name, t=0, n=1):
        k = (name, t % n)
        assert self.owner.get(k) == t, f"stale read {name} tile {t} owner {self.owner.get(k)}"
        return k

    PSUM_NAMES = {"pmod", "pc", "pgt", "pT", "pz", "ptA", "pkv", "pcq", "ptQ", "pq", "ptQ2", "pk", "ps", "po", "ptC",
                  "ptD", "pg", "py"}

    def _split(self, reads, writes):
        rd, wr = [], list(writes)
        for r in reads:
            base = r[0] if isinstance(r, tuple) else r
            if base in self.PSUM_NAMES:
                if r not in wr:
                    wr.append(r)
            else:
                rd.append(r)
        return rd, wr

    def _deps(self, reads, writes):
        deps = []
        for r in reads:
            t = self.last_w.get(r)
            if t is not None:
                deps.append(t)
        for w in writes:
            t = self.last_w.get(w)
            if t is not None:
                deps.append(t)
            deps.extend(self.readers.get(w, ()))
        return deps

    def _wait(self, eng, deps):
        best = {}
        for (skey, sem, val) in deps:
            if val > best.get(skey, (None, 0))[1]:
                best[skey] = (sem, val)
        w = self.waited[eng]
        for skey, (sem, val) in best.items():
            if w.get(skey, 0) >= val:
                continue
            self.eng[eng].wait_ge(sem, val)
            w[skey] = val

    def _commit(self, ticket, reads, writes):
        for r in reads:
            self.readers.setdefault(r, []).append(ticket)
        for w in writes:
            self.last_w[w] = ticket
            self.readers[w] = []

    def op(self, eng, fn, reads=(), writes=()):
        reads, writes = self._split(reads, writes)
        self._wait(eng, self._deps(reads, writes))
        ins = fn(self.eng[eng])
        self.cnt[eng] += 1
        ins.then_inc(self.sem[eng], 1)
        t = (eng, self.sem[eng], self.cnt[eng])
        self._commit(t, reads, writes)
        self.nops += 1
        return t

    def dma(self, queue, out, in_, reads=(), writes=(), key=None):
        if key not in self.dsem:
            self.dsem[key] = self.stacks[0].enter_context(self.nc.semaphore("d_" + str(key)))
            self.dcnt[key] = 0
        self._wait(queue, self._deps(reads, writes))
        ins = self.eng[queue].dma_start(out=out, in_=in_)
        self.dcnt[key] += 16
        ins.then_inc(self.dsem[key], 16)
        t = ("d_" + str(key), self.dsem[key], self.dcnt[key])
        self._commit(t, reads, writes)
        return t

    def barrier(self):
        tickets = [(e, self.sem[e], self.cnt[e]) for e in self.sem if self.cnt[e] > 0]
        tickets += [("d_" + str(k), self.dsem[k], self.dcnt[k]) for k in self.dsem]
        for e in self.eng:
            self._wait(e, tickets)
        self.last_w, self.readers = {}, {}

    def close(self):
        self.barrier()
        while self.stacks:
            self.stacks.pop().close()


def pipeline(n, stages):
    ns = len(stages)
    for s in range(n + ns - 1):
        for k in range(ns - 1, -1, -1):
            t = s - k
            if 0 <= t < n:
                stages[k](t)


def build(stop=None, debug=False):
    nc = bass.Bass("TRN2", target_bir_lowering=False)
    dbg_out = {}

    def dbg(name, ap, shape, dt):
        if not debug:
            return
        d_ = nc.dram_tensor("dbg_" + name, list(shape), dt, kind="ExternalOutput").ap()
        dbg_out[name] = d_
        c.barrier()
        c.dma("sp", d_, ap, key="dbg_" + name)
        c.barrier()

    def finish():
        c.close()
        return nc

    def din(name, shape, dt=F32):
        return nc.dram_tensor(name, list(shape), dt, kind="ExternalInput").ap()

    xall = din("xall", [S, D])
    xown = din("xown", [NO * 128, D])
    posall = din("posall", [128, NT], I32)
    posown = din("posown", [128, NO], I32)
    cvec = din("cvec", [128, 8])
    w_ada = din("w_ada", [D, 6 * D])
    bada_row = din("bada_row", [1, 6 * D])
    g1c = din("g1c", [128, 8])
    g2c = din("g2c", [128, 8])
    w_in = din("w_in", [D, D_IN])
    gcq = din("gcq", [128, 3])
    w_uq = din("w_uq", [384, 768])
    gckv = din("gckv", [128, 2])
    w_ukv = din("w_ukv", [256, 1024])
    gret = din("gret", [128, 8])
    w_o_mla = din("w_o_mla", [512, 1024])
    w_o_ret = din("w_o_ret", [1024, 1024])
    w_out = din("w_out", [1024, 1024])
    w_router = din("w_router", [1024, 64])
    brout = din("brout", [128, 64])
    w_eg = din("w_exp_gate", [NEXP, 1024, 256])
    w_eu = din("w_exp_up", [NEXP, 1024, 256])
    w_ed = din("w_exp_down", [NEXP, 256, 1024])
    w_sg = din("w_sh_gate", [1024, 256])
    w_su = din("w_sh_up", [1024, 256])
    w_sd = din("w_sh_down", [256, 1024])
    gfin = din("gfin", [128, 1024])
    identb_d = din("identb", [128, 128], BF16)
    identf_d = din("identf", [128, 128])
    tri_d = din("tri", [128, 128])
    amask_d = din("amask", [128, 4, 128], BF16)
    invf_d = din("invf", [128, 80])
    kdecA_d = din("kdecA", [128, 4, 128])
    kdecC_d = din("kdecC", [128, 4, 128])
    qdec_d = din("qdec", [128, 4, 128])
    rcoef_d = din("rcoef", [128, 16])
    out_d = nc.dram_tensor("out", [NO * 128, D], F32, kind="ExternalOutput").ap()
    sown_d = nc.dram_tensor("sown_s", [NO, 128, 1024], BF16, kind="ExternalOutput").ap()
    bb_d = nc.dram_tensor("bb_s", [NO, 128, 1024], BF16, kind="ExternalOutput").ap()
    x1_d = nc.dram_tensor("x1_s", [NO, 128, 1024], F32, kind="ExternalOutput").ap()
    ot_d = nc.dram_tensor("ot_s", [8, 64, NO * 128], BF16, kind="ExternalOutput").ap()

    c = Ctx(nc)

    identb = c.sbuf("identb", [128, 128], BF16)
    identf = c.sbuf("identf", [128, 128], F32)
    tri = c.sbuf("tri", [128, 128], F32)
    amask = c.sbuf("amask", [128, 4, 128], BF16)
    invf = c.sbuf("invf", [128, 80], F32)
    rcoef = c.sbuf("rcoef", [128, 16], F32)
    nhalf = c.sbuf("nhalf", [128, 64], F32)
    onesf = c.sbuf("onesf", [128, 128], F32)
    AB = c.sbuf("AB", [128, 32], F32)
    GT1 = c.sbuf("GT1", [128, 1024], F32)
    GT2 = c.sbuf("GT2", [128, 1024], F32)
    gfin_t = c.sbuf("gfin_t", [128, 1024], F32)
    brout_t = c.sbuf("brout_t", [128, 64], F32)
    posf = c.sbuf("posf", [128, NT + NO], F32)
    junk = c.sbuf("junk", [128, 1024], BF16)
    comb = c.sbuf("comb", [128, NO, 64], F32)

    for (t_, d_, k_) in [(identb, identb_d, "c0"), (identf, identf_d, "c1"), (tri, tri_d, "c2"),
                         (amask, amask_d, "c3"), (invf, invf_d, "c4"), (rcoef, rcoef_d, "c5"),
                         (gfin_t, gfin, "c6"), (brout_t, brout, "c7")]:
        c.dma("sp", t_[:], d_, writes=["const"], key=k_)
    c.op("pool", lambda e: e.memset(nhalf[:], -0.5), writes=["const"])
    c.op("pool", lambda e: e.memset(onesf[:], 1.0), writes=["const"])
    c.barrier()
    if stop == "c":
        return finish()

    def rsqrt_cols(src_ap, dst_ap, ncols, rd, wr):
        c.op("pool", lambda e: e.tensor_tensor(out=dst_ap, in0=src_ap, in1=nhalf[:, 0:ncols], op=ALU.pow),
             reads=rd, writes=wr)

    c.push()
    PS = [c.psum(f"p0_{i}", [128, 512], F32) for i in range(8)]
    cv = c.sbuf("cv", [128, 8], F32)
    cs = c.sbuf("cs", [128, 8], F32)
    modrow = c.sbuf("modrow", [1, 6 * D], F32)
    brow = c.sbuf("brow", [1, 6 * D], F32)
    gcols = c.sbuf("gcols", [128, 16], F32)
    wab = [c.sbuf(f"wab{i}", [128, 8, 512], F32) for i in range(2)]
    posi = c.sbuf("posi", [128, NT + NO], I32)
    c.dma("sp", cv[:], cvec, writes=["cv"], key="p0a")
    c.dma("sp", brow[:], bada_row, writes=["brow"], key="p0b")
    c.dma("sp", gcols[:, 0:8], g1c, writes=["gcols"], key="p0c")
    c.dma("sp", gcols[:, 8:16], g2c, writes=["gcols"], key="p0d")
    c.dma("sp", posi[:, 0:NT], posall, writes=["posi"], key="p0e")
    c.dma("sp", posi[:, NT:NT + NO], posown, writes=["posi"], key="p0f")
    c.op("dve", lambda e: e.tensor_copy(out=posf[:], in_=posi[:]), reads=["posi"], writes=["posf"])
    c.op("act", lambda e: e.activation(out=cs[:], in_=cv[:], func=AF.Silu), reads=["cv"], writes=["cs"])
    for n in range(12):
        wt = wab[n % 2]
        c.dma("sp", wt[:], w_ada[:, n * 512:(n + 1) * 512].rearrange("(k p) n -> p k n", p=128),
              writes=[c.W("wab", n, 2)], key=f"wab{n % 2}")
        for k in range(8):
            c.op("pe", lambda e: e.matmul(PS[n % 2][0:1, :], lhsT=cs[:, k:k + 1], rhs=wt[:, k, :],
                                          start=(k == 0), stop=(k == 7)),
                 reads=[c.R("wab", n, 2), "cs"], writes=[("pmod", n % 2)])
        c.op("dve", lambda e: e.tensor_tensor(out=modrow[0:1, n * 512:(n + 1) * 512], in0=PS[n % 2][0:1, :],
                                              in1=brow[0:1, n * 512:(n + 1) * 512], op=ALU.add),
             reads=[("pmod", n % 2), "brow"], writes=["modrow"])
    if stop == "0a":
        dbg("modrow", modrow[:], [1, 6 * D], F32)
        c.pop()
        return finish()
    pc = PS[2]
    for idx, ch in enumerate(list(range(0, 16)) + list(range(24, 40))):
        c.op("pe", lambda e: e.matmul(pc[:, idx:idx + 1], lhsT=modrow[0:1, ch * 128:(ch + 1) * 128],
                                      rhs=onesf[0:1, 0:1], start=True, stop=True),
             reads=["modrow", "const"], writes=["pc"])
    c.op("dve", lambda e: e.scalar_tensor_tensor(out=AB[:, 0:8], in0=pc[:, 8:16], scalar=1.0, in1=gcols[:, 0:8],
                                                 op0=ALU.add, op1=ALU.mult), reads=["pc", "gcols"], writes=["AB"])
    c.op("dve", lambda e: e.tensor_copy(out=AB[:, 8:16], in_=pc[:, 0:8]), reads=["pc"], writes=["AB"])
    c.op("dve", lambda e: e.scalar_tensor_tensor(out=AB[:, 16:24], in0=pc[:, 24:32], scalar=1.0, in1=gcols[:, 8:16],
                                                 op0=ALU.add, op1=ALU.mult), reads=["pc", "gcols"], writes=["AB"])
    c.op("dve", lambda e: e.tensor_copy(out=AB[:, 24:32], in_=pc[:, 16:24]), reads=["pc"], writes=["AB"])
    if stop == "0b":
        c.pop()
        dbg("AB", AB[:], [128, 32], F32)
        return finish()
    for (dst, base, scl, pi) in [(GT1, 2048, 0.5, 4), (GT2, 5120, 1.0, 6)]:
        for hh in range(2):
            c.op("pe", lambda e: e.matmul(PS[pi + hh][:], lhsT=onesf[0:1, 0:128],
                                          rhs=modrow[0:1, base + hh * 512: base + (hh + 1) * 512],
                                          start=True, stop=True), reads=["modrow", "const"], writes=[("pgt", pi + hh)])
            c.op("act", lambda e: e.activation(out=dst[:, hh * 512:(hh + 1) * 512], in_=PS[pi + hh][:],
                                               func=AF.Copy, scale=scl), reads=[("pgt", pi + hh)], writes=["GT"])
    c.pop()
    dbg("AB", AB[:], [128, 32], F32)
    dbg("GT1", GT1[:], [128, 1024], F32)
    if stop == "0":
        return finish()

    A1, B1, A2, B2 = AB[:, 0:8], AB[:, 8:16], AB[:, 16:24], AB[:, 24:32]

    def load_w(dst_tile, src_ap, name, key):
        c.dma("pool", dst_tile, src_ap, writes=[name], key=key)

    def rope_tables(pos_ap, G, sinT, cosT, tmp, lo, hi, tag):
        n = hi - lo
        ang, u, r = (tmp[0][:, 0:G, 0:n], tmp[1][:, 0:G, 0:n], tmp[2][:, 0:G, 0:n])
        c.op("dve", lambda e: e.tensor_tensor(out=ang, in0=invf[:, lo:hi].unsqueeze(1).broadcast_to([128, G, n]),
                                              in1=pos_ap.unsqueeze(2).broadcast_to([128, G, n]), op=ALU.mult),
             reads=["posf", "const"], writes=[tag + "t0"])
        c.op("dve", lambda e: e.tensor_scalar(out=u, in0=ang, scalar1=1.0 / TWO_PI, scalar2=MAGIC,
                                              op0=ALU.mult, op1=ALU.add), reads=[tag + "t0"], writes=[tag + "t1"])
        c.op("dve", lambda e: e.tensor_scalar(out=u, in0=u, scalar1=-MAGIC, scalar2=None, op0=ALU.add),
             reads=[tag + "t1"], writes=[tag + "t1"])
        c.op("dve", lambda e: e.scalar_tensor_tensor(out=r, in0=u, scalar=-C1, in1=ang, op0=ALU.mult, op1=ALU.add),
             reads=[tag + "t1", tag + "t0"], writes=[tag + "t2"])
        c.op("dve", lambda e: e.scalar_tensor_tensor(out=r, in0=u, scalar=-C2, in1=r, op0=ALU.mult, op1=ALU.add),
             reads=[tag + "t1", tag + "t2"], writes=[tag + "t2"])
        c.op("dve", lambda e: e.scalar_tensor_tensor(out=r, in0=u, scalar=-C3, in1=r, op0=ALU.mult, op1=ALU.add),
             reads=[tag + "t1", tag + "t2"], writes=[tag + "t2"])
        c.op("dve", lambda e: e.tensor_scalar(out=r, in0=r, scalar1=-math.pi, scalar2=math.pi, op0=ALU.max, op1=ALU.min),
             reads=[tag + "t2"], writes=[tag + "t2"])
        c.op("act", lambda e: e.activation(out=sinT[:, 0:G, lo:hi], in_=r, func=AF.Sin), reads=[tag + "t2"],
             writes=[tag + "sin"])
        c.op("dve", lambda e: e.scalar_tensor_tensor(out=ang, in0=r, scalar=-1.0, in1=r, op0=ALU.mult, op1=ALU.max),
             reads=[tag + "t2"], writes=[tag + "t0"])
        c.op("dve", lambda e: e.tensor_scalar(out=ang, in0=ang, scalar1=-1.0, scalar2=math.pi / 2, op0=ALU.mult,
                                              op1=ALU.add), reads=[tag + "t0"], writes=[tag + "t0"])
        c.op("act", lambda e: e.activation(out=cosT[:, 0:G, lo:hi], in_=ang, func=AF.Sin), reads=[tag + "t0"],
             writes=[tag + "cos"])

    def rope(out4, z4, cos_b, sin_b, t1, t2, eng2, rd, wr, tmpname):
        c.op("dve", lambda e: e.tensor_tensor(out=t1, in0=z4, in1=cos_b, op=ALU.mult), reads=rd, writes=[tmpname + "1"])
        c.op("dve", lambda e: e.tensor_tensor(out=t2, in0=z4, in1=sin_b, op=ALU.mult), reads=rd, writes=[tmpname + "2"])
        c.op(eng2, lambda e: e.tensor_tensor(out=out4[:, :, 0, :], in0=t1[:, :, 0, :], in1=t2[:, :, 1, :], op=ALU.subtract),
             reads=[tmpname + "1", tmpname + "2"], writes=wr)
        c.op(eng2, lambda e: e.tensor_tensor(out=out4[:, :, 1, :], in0=t1[:, :, 1, :], in1=t2[:, :, 0, :], op=ALU.add),
             reads=[tmpname + "1", tmpname + "2"], writes=wr)

    class HT:
        def __init__(self, src, nb, A, B, pT):
            self.src, self.nb, self.A, self.B, self.pT = src, nb, A, B, pT
            self.xt = [c.sbuf("xt", [128, 1024], F32) for _ in range(nb)]
            self.xs = [c.sbuf("xs", [128, 1024], BF16) for _ in range(2)]
            self.hT = [c.sbuf("hT", [128, 8, 128], BF16) for _ in range(2)]
            self.st = c.sbuf("hst", [128, 3 * 64], F32)

        def load(self, t):
            c.dma("sp", self.xt[t % self.nb][:], self.src[t * 128:(t + 1) * 128, :],
                  writes=[c.W("xt", t, self.nb)], key=f"xt{t % self.nb}")

        def norm(self, t):
            xt, st = self.xt[t % self.nb], self.st
            a, b, r = st[:, t % 64:t % 64 + 1], st[:, 64 + t % 64:65 + t % 64], st[:, 128 + t % 64:129 + t % 64]
            c.op("act", lambda e: e.activation(out=junk[:], in_=xt[:], func=AF.Square, accum_out=a),
                 reads=[c.R("xt", t, self.nb)], writes=["junk", ("hsa", t % 64)])
            c.op("dve", lambda e: e.tensor_scalar(out=b, in0=a, scalar1=1.0 / D, scalar2=RMS_EPS, op0=ALU.mult,
                                                  op1=ALU.add), reads=[("hsa", t % 64)], writes=[("hsb", t % 64)])
            rsqrt_cols(b, r, 1, [("hsb", t % 64)], [("hsr", t % 64)])
            c.op("dve", lambda e: e.tensor_scalar(out=self.xs[t % 2][:], in0=xt[:], scalar1=r, scalar2=None,
                                                  op0=ALU.mult), reads=[c.R("xt", t, self.nb), ("hsr", t % 64)],
                 writes=[c.W("xs", t, 2)])

        def transpose(self, t):
            xs, hT, pT = self.xs[t % 2], self.hT[t % 2], self.pT
            pTb = pT[:].bitcast(BF16)
            for k in range(8):
                c.op("pe", lambda e: e.transpose(out=pTb[:, k * 128:(k + 1) * 128], in_=xs[:, k * 128:(k + 1) * 128],
                                                 identity=identb[:]), reads=[c.R("xs", t, 2), "const"], writes=["pT"])
            c.W("hT", t, 2)
            for k in range(8):
                c.op("act", lambda e: e.activation(out=hT[:, k, :], in_=pTb[:, k * 128:(k + 1) * 128],
                                                   func=AF.Identity, scale=self.A[:, k:k + 1],
                                                   bias=self.B[:, k:k + 1]), reads=["pT", "AB"],
                     writes=[("hT", t % 2)])

        def get(self, t):
            return self.hT[t % 2], c.R("hT", t, 2)

    c.push()
    ckvnT = c.sbuf("ckvnT", [128, 2, S], BF16)
    KT = [c.sbuf(f"KT{i}", [128, S], BF16) for i in range(2)]

    c.push()
    PS = [c.psum(f"pa_{i}", [128, 512], F32) for i in range(8)]
    WA = c.sbuf("WA", [128, 8, 1824], BF16)
    load_w(WA[:, :, 0:288], w_in[:, O_CKV:O_CKV + 288].rearrange("(k p) n -> p k n", p=128), "WA", "wA0")
    load_w(WA[:, :, 288:800], w_in[:, O_RK:O_RK + 512].rearrange("(k p) n -> p k n", p=128), "WA", "wA1")
    load_w(WA[:, :, 800:1824], w_in[:, O_RV:O_RV + 1024].rearrange("(k p) n -> p k n", p=128), "WA", "wA2")
    kdecA = c.sbuf("kdecA", [128, 4, 128], F32)
    c.dma("sp", kdecA[:], kdecA_d, writes=["kdecA"], key="kdA")
    ht = HT(xall, 3, A1, B1, PS[0])
    sinT = [c.sbuf("sinT", [128, 4, 80], F32) for _ in range(2)]
    cosT = [c.sbuf("cosT", [128, 4, 80], F32) for _ in range(2)]
    rtmp = [c.sbuf("rtmp", [128, 4, 80], F32) for _ in range(3)]
    stA = c.sbuf("stA", [128, 3 * 64], F32)
    ckvs = [c.sbuf("ckvs", [128, 256], BF16) for _ in range(2)]
    kst = [c.sbuf("kst", [128, 96], BF16) for _ in range(2)]
    kr1 = c.sbuf("kr1", [128, 32], F32)
    kr2 = c.sbuf("kr2", [128, 32], F32)
    rk1 = c.sbuf("rk1", [128, 512], F32)
    rk2 = c.sbuf("rk2", [128, 512], F32)
    rk3 = c.sbuf("rk3", [128, 512], F32)
    kp = [c.sbuf("kp", [128, 4, 128], BF16) for _ in range(2)]
    vb = [c.sbuf("vb", [128, 1024], BF16) for _ in range(2)]
    Sst = c.sbuf("Sst", [128, 1024], F32)
    Sown = [c.sbuf("Sown", [128, 1024], F32) for _ in range(2)]
    Sownb = [c.sbuf("Sownb", [128, 1024], BF16) for _ in range(2)]
    for i in range(2):
        c.op("pool", lambda e: e.memset(kst[i][:], 0.0), writes=[("kst", i)])
    c.op("pool", lambda e: e.memset(Sst[:], 0.0), writes=["Sst"])

    def A_s0(t):
        if t == 0:
            ht.load(0)
            ht.load(1)
        if t + 2 < NT:
            ht.load(t + 2)
        if t % 4 == 0:
            g = t // 4
            rope_tables(posf[:, t:t + 4], 4, sinT[g % 2], cosT[g % 2], rtmp, 0, 80, f"rtA{g % 2}")
            c.W("tabA", g, 2)
        ht.norm(t)
        ht.transpose(t)

    def A_s1(t):
        hT, hk = ht.get(t)
        for (pi, lo, n) in [(1, 0, 288), (2, 288, 512), (3, 800, 512), (4, 1312, 512)]:
            for k in range(8):
                c.op("pe", lambda e: e.matmul(PS[pi][:, 0:n], lhsT=hT[:, k, :], rhs=WA[:, k, lo:lo + n],
                                              start=(k == 0), stop=(k == 7)), reads=[hk, "WA"], writes=[("pz", pi)])
        m = t % 64
        a, b, r = stA[:, m:m + 1], stA[:, 64 + m:65 + m], stA[:, 128 + m:129 + m]
        c.op("act", lambda e: e.activation(out=junk[:, 0:256], in_=PS[1][:, 0:256], func=AF.Square, accum_out=a),
             reads=[("pz", 1)], writes=["junk", ("sAa", m)])
        c.op("dve", lambda e: e.tensor_scalar(out=b, in0=a, scalar1=1.0 / 256, scalar2=RMS_EPS, op0=ALU.mult,
                                              op1=ALU.add), reads=[("sAa", m)], writes=[("sAb", m)])
        rsqrt_cols(b, r, 1, [("sAb", m)], [("sAr", m)])
        c.op("dve", lambda e: e.tensor_scalar(out=ckvs[t % 2][:], in0=PS[1][:, 0:256], scalar1=r, scalar2=None,
                                              op0=ALU.mult), reads=[("pz", 1), ("sAr", m)], writes=[c.W("ckvs", t, 2)])
        g = t // 4
        tk = c.R("tabA", g, 2)
        sn, cs_ = sinT[g % 2], cosT[g % 2]
        z4 = PS[1][:, 256:288].rearrange("p (h t d) -> p h t d", h=1, t=2)
        cb = cs_[:, t % 4, 0:16].unsqueeze(1).unsqueeze(1).broadcast_to([128, 1, 2, 16])
        sb = sn[:, t % 4, 0:16].unsqueeze(1).unsqueeze(1).broadcast_to([128, 1, 2, 16])
        o4 = kst[t % 2][:, 64:96].rearrange("p (h t d) -> p h t d", h=1, t=2)
        rope(o4, z4, cb, sb, kr1[:].rearrange("p (h t d) -> p h t d", h=1, t=2),
             kr2[:].rearrange("p (h t d) -> p h t d", h=1, t=2), "pool",
             [("pz", 1), f"rtA{g % 2}sin", f"rtA{g % 2}cos"], [c.W("kst", t, 2)], "kr")
        z4 = PS[2][:].rearrange("p (h t d) -> p h t d", h=4, t=2)
        cb = cs_[:, t % 4, 16:80].unsqueeze(1).unsqueeze(1).broadcast_to([128, 4, 2, 64])
        sb = sn[:, t % 4, 16:80].unsqueeze(1).unsqueeze(1).broadcast_to([128, 4, 2, 64])
        o4 = rk3[:].rearrange("p (h t d) -> p h t d", h=4, t=2)
        rope(o4, z4, cb, sb, rk1[:].rearrange("p (h t d) -> p h t d", h=4, t=2),
             rk2[:].rearrange("p (h t d) -> p h t d", h=4, t=2), "pool",
             [("pz", 2), f"rtA{g % 2}sin", f"rtA{g % 2}cos"], ["rk3"], "rk")
        c.op("pool", lambda e: e.tensor_tensor(out=kp[t % 2][:], in0=rk3[:].rearrange("p (h d) -> p h d", h=4),
                                               in1=kdecA[:], op=ALU.mult), reads=["rk3", "kdecA"],
             writes=[c.W("kp", t, 2)])
        c.W("vb", t, 2)
        for hh in range(2):
            c.op("act", lambda e: e.activation(out=vb[t % 2][:, hh * 512:(hh + 1) * 512], in_=PS[3 + hh][:],
                                               func=AF.Copy), reads=[("pz", 3 + hh)], writes=[("vb", t % 2)])

    import os
    _lv = int(os.environ.get("DBG_LV", 9))

    def A_s2(t):
        pt = PS[5][:].bitcast(BF16)
        for k in range(2):
            c.op("pe", lambda e: e.transpose(out=pt[:, k * 128:(k + 1) * 128], in_=ckvs[t % 2][:, k * 128:(k + 1) * 128],
                                             identity=identb[:]), reads=[c.R("ckvs", t, 2), "const"], writes=["ptA"])
        c.op("pe", lambda e: e.transpose(out=pt[0:96, 256:384], in_=kst[t % 2][:], identity=identb[:]),
             reads=[c.R("kst", t, 2), "const"], writes=["ptA"])
        if _lv < 2:
            return
        for k in range(2):
            c.op("act", lambda e: e.activation(out=ckvnT[:, k, t * 128:(t + 1) * 128],
                                               in_=pt[:, k * 128:(k + 1) * 128], func=AF.Copy),
                 reads=["ptA"], writes=["ckvnT"])
        if _lv < 3:
            return
        _kt = os.environ.get("DBG_KT", "both")
        if _kt in ("both", "act"):
            c.op("act", lambda e: e.activation(out=KT[0][64:96, t * 128:(t + 1) * 128], in_=pt[64:96, 256:384],
                                               func=AF.Copy), reads=["ptA"], writes=["KT0r"])
        if _kt in ("both", "dve"):
            c.op("act", lambda e: e.activation(out=KT[1][64:96, t * 128:(t + 1) * 128], in_=pt[64:96, 256:384],
                                               func=AF.Copy), reads=["ptA"], writes=["KT1r"])
        if _lv < 4:
            return
        pkv = [PS[6], PS[7]]
        for h in range(4):
            c.op("pe", lambda e: e.matmul(pkv[h // 2][:, (h % 2) * 256:(h % 2 + 1) * 256], lhsT=kp[t % 2][:, h, :],
                                          rhs=vb[t % 2][:, h * 256:(h + 1) * 256], start=True, stop=True),
                 reads=[c.R("kp", t, 2), c.R("vb", t, 2)], writes=["pkv"])
        if _lv < 5:
            return
        l, g = t % 4, t // 4
        so = Sown[g % 2]
        if l == 0:
            c.W("Sown", g, 2)
        for h in range(4):
            hs = slice(h * 256, (h + 1) * 256)
            pk = pkv[h // 2][:, (h % 2) * 256:(h % 2 + 1) * 256]
            if l == 0:
                c.op("pool", lambda e: e.tensor_scalar(out=so[:, hs], in0=Sst[:, hs], scalar1=rcoef[:, h * 4:h * 4 + 1],
                                                       scalar2=None, op0=ALU.mult), reads=["Sst", "const"],
                     writes=[("Sown", g % 2)])
            if l < 3:
                c.op("dve", lambda e: e.scalar_tensor_tensor(out=so[:, hs], in0=pk, scalar=rcoef[:, h * 4 + 1 + l:h * 4 + 2 + l],
                                                             in1=so[:, hs], op0=ALU.mult, op1=ALU.add),
                     reads=["pkv", ("Sown", g % 2), "const"], writes=[("Sown", g % 2)])
            c.op("dve", lambda e: e.scalar_tensor_tensor(out=Sst[:, hs], in0=Sst[:, hs], scalar=GAMMA[h] ** 128, in1=pk,
                                                         op0=ALU.mult, op1=ALU.add), reads=["pkv", "Sst"], writes=["Sst"])
        if l == 3 and _lv >= 6:
            c.op("act", lambda e: e.activation(out=Sownb[g % 2][:], in_=so[:], func=AF.Copy),
                 reads=[c.R("Sown", g, 2)], writes=[c.W("Sownb", g, 2)])
            c.dma("sp", sown_d[g], Sownb[g % 2][:], reads=[c.R("Sownb", g, 2)], writes=["sown_d"], key=f"so{g % 2}")

    import os
    _na = int(os.environ.get("DBG_NA", NT))
    _ns = int(os.environ.get("DBG_NS", 3))
    pipeline(_na, [A_s0, A_s1, A_s2][:_ns])
    print("sbuf remaining in pass A:", nc.sbuf_bytes_remaining, "ops", c.nops)
    dbg("Sst", Sst[:], [128, 1024], F32)
    c.pop()
    dbg("ckvnT", ckvnT[:], [128, 2, S], BF16)
    dbg("KT0", KT[0][:], [128, S], BF16)
    if stop == "A":
        return finish()

    QT = c.sbuf("QT", [128, 8, NO * 128], BF16)
    c.push()
    PS = [c.psum(f"pq_{i}", [128, 512], F32) for i in range(8)]
    WQ = c.sbuf("WQ", [128, 8, 384], BF16)
    load_w(WQ[:], w_in[:, O_CQ:O_CQ + 384].rearrange("(k p) n -> p k n", p=128), "WQ", "wQ0")
    wuq = c.sbuf("wuq", [128, 3, 768], BF16)
    c.push()
    wuq_f = c.sbuf("wuq_f", [128, 3, 768], F32)
    gq = c.sbuf("gq", [128, 3], F32)
    c.dma("sp", wuq_f[:], w_uq.rearrange("(k p) n -> p k n", p=128), writes=["wuq_f"], key="wQ1")
    c.dma("sp", gq[:], gcq, writes=["gq"], key="wQ2")
    for k in range(3):
        wv = wuq_f[:, k, :].rearrange("p (h d) -> p h d", h=8)
        c.op("dve", lambda e: e.tensor_scalar(out=wuq[:, k, 0:512].rearrange("p (h d) -> p h d", h=8), in0=wv[:, :, 0:64],
                                              scalar1=gq[:, k:k + 1], scalar2=None, op0=ALU.mult),
             reads=["wuq_f", "gq"], writes=["wuq"])
        c.op("dve", lambda e: e.tensor_scalar(out=wuq[:, k, 512:768].rearrange("p (h d) -> p h d", h=8), in0=wv[:, :, 64:96],
                                              scalar1=gq[:, k:k + 1], scalar2=None, op0=ALU.mult),
             reads=["wuq_f", "gq"], writes=["wuq"])
    c.pop()
    ht = HT(xown, 3, A1, B1, PS[0])
    sinQ = c.sbuf("sinQ", [128, NO, 16], F32)
    cosQ = c.sbuf("cosQ", [128, NO, 16], F32)
    c.push()
    rtmpQ = [c.sbuf("rtmpQ", [128, NO, 16], F32) for _ in range(3)]
    rope_tables(posf[:, NT:NT + NO], NO, sinQ, cosQ, rtmpQ, 0, 16, "rtQ")
    c.pop()
    stQ = c.sbuf("stQ", [128, 3 * 64], F32)
    cqs = [c.sbuf("cqs", [128, 384], BF16) for _ in range(2)]
    cqnT = [c.sbuf("cqnT", [128, 3, 128], BF16) for _ in range(2)]
    qsb = [c.sbuf("qsb", [128, 8, 96], BF16) for _ in range(2)]
    qr1 = c.sbuf("qr1", [128, 8, 32], F32)
    qr2 = c.sbuf("qr2", [128, 8, 32], F32)

    def Q_s0(t):
        if t == 0:
            ht.load(0)
            ht.load(1)
        if t + 2 < NO:
            ht.load(t + 2)
        ht.norm(t)
        ht.transpose(t)

    def Q_s1(t):
        hT, hk = ht.get(t)
        for k in range(8):
            c.op("pe", lambda e: e.matmul(PS[1][:, 0:384], lhsT=hT[:, k, :], rhs=WQ[:, k, :], start=(k == 0),
                                          stop=(k == 7)), reads=[hk, "WQ"], writes=["pcq"])
        m = t
        a, b, r = stQ[:, m:m + 1], stQ[:, 64 + m:65 + m], stQ[:, 128 + m:129 + m]
        c.op("act", lambda e: e.activation(out=junk[:, 0:384], in_=PS[1][:, 0:384], func=AF.Square, accum_out=a),
             reads=["pcq"], writes=["junk", ("sQa", m)])
        c.op("dve", lambda e: e.tensor_scalar(out=b, in0=a, scalar1=1.0 / 384, scalar2=RMS_EPS, op0=ALU.mult,
                                              op1=ALU.add), reads=[("sQa", m)], writes=[("sQb", m)])
        rsqrt_cols(b, r, 1, [("sQb", m)], [("sQr", m)])
        c.op("dve", lambda e: e.tensor_scalar(out=cqs[t % 2][:], in0=PS[1][:, 0:384], scalar1=r, scalar2=None,
                                              op0=ALU.mult), reads=["pcq", ("sQr", m)], writes=[c.W("cqs", t, 2)])
        pt = PS[2][:].bitcast(BF16)
        for k in range(3):
            c.op("pe", lambda e: e.transpose(out=pt[:, k * 128:(k + 1) * 128], in_=cqs[t % 2][:, k * 128:(k + 1) * 128],
                                             identity=identb[:]), reads=[c.R("cqs", t, 2), "const"], writes=["ptQ"])
        c.op("act", lambda e: e.activation(out=cqnT[t % 2][:], in_=pt[:, 0:384].rearrange("p (k n) -> p k n", k=3),
                                           func=AF.Copy), reads=["ptQ"], writes=[c.W("cqnT", t, 2)])

    def Q_s2(t):
        for (pi, lo, n) in [(3, 0, 512), (4, 512, 256)]:
            for k in range(3):
                c.op("pe", lambda e: e.matmul(PS[pi][:, 0:n], lhsT=cqnT[t % 2][:, k, :], rhs=wuq[:, k, lo:lo + n],
                                              start=(k == 0), stop=(k == 2)),
                     reads=[c.R("cqnT", t, 2), "wuq"], writes=[("pq", pi)])
        c.W("qsb", t, 2)
        _ql = int(os.environ.get("DBG_QL", 9))
        c.op("act", lambda e: e.activation(out=qsb[t % 2][:, :, 0:64], in_=PS[3][:].rearrange("p (h d) -> p h d", h=8),
                                           func=AF.Copy), reads=[("pq", 3)], writes=[("qsb", t % 2)])
        z4 = PS[4][:, 0:256].rearrange("p (h t d) -> p h t d", h=8, t=2)
        cb = cosQ[:, t, 0:16].unsqueeze(1).unsqueeze(1).broadcast_to([128, 8, 2, 16])
        sb = sinQ[:, t, 0:16].unsqueeze(1).unsqueeze(1).broadcast_to([128, 8, 2, 16])
        o4 = qsb[t % 2][:, :, 64:96].rearrange("p h (t d) -> p h t d", t=2)
        rope(o4, z4, cb, sb, qr1[:].rearrange("p h (t d) -> p h t d", t=2),
             qr2[:].rearrange("p h (t d) -> p h t d", t=2), "pool",
             [("pq", 4), "rtQsin", "rtQcos"], [("qsb", t % 2)], "qr")
        if _ql < 3:
            return
        pt = PS[5][:].bitcast(BF16)
        for h in range(8):
            c.op("pe", lambda e: e.transpose(out=pt[0:96, h * 128:(h + 1) * 128], in_=qsb[t % 2][:, h, :],
                                             identity=identb[:]), reads=[("qsb", t % 2), "const"], writes=["ptQ2"])
        if _ql < 4:
            return
        c.op("act", lambda e: e.activation(out=QT[0:96, :, t * 128:(t + 1) * 128],
                                           in_=pt[0:96, :].rearrange("p (h n) -> p h n", h=8), func=AF.Copy),
             reads=["ptQ2"], writes=["QT"])

    pipeline(int(os.environ.get("DBG_NQ", NO)), [Q_s0, Q_s1, Q_s2][:int(os.environ.get("DBG_QS", 3))])
    print("sbuf remaining in pass Q:", nc.sbuf_bytes_remaining, "ops", c.nops)
    c.pop()
    dbg("QT", QT[:], [128, 8, NO * 128], BF16)
    if stop == "Q":
        return finish()

    c.push()
    PS = [c.psum(f"pt_{i}", [128, 512], F32) for i in range(8)]
    wukv = c.sbuf("wukv", [128, 2, 1024], BF16)
    c.push()
    wukv_f = c.sbuf("wukv_f", [128, 2, 1024], F32)
    gkv = c.sbuf("gkv", [128, 2], F32)
    c.dma("sp", wukv_f[:], w_ukv.rearrange("(k p) n -> p k n", p=128), writes=["wukv_f"], key="wT0")
    c.dma("sp", gkv[:], gckv, writes=["gkv"], key="wT1")
    for k in range(2):
        c.op("dve", lambda e: e.tensor_scalar(out=wukv[:, k, :], in0=wukv_f[:, k, :], scalar1=gkv[:, k:k + 1],
                                              scalar2=None, op0=ALU.mult), reads=["wukv_f", "gkv"], writes=["wukv"])
    c.pop()
    otb = [c.sbuf("otb", [64, 512], BF16) for _ in range(2)]
    Vb = [c.sbuf("Vb", [128, NT, 65], BF16) for _ in range(2)]
    for i in range(2):
        c.op("pool", lambda e: e.memset(Vb[i][:, :, 64:65], 1.0), writes=[("Vb1", i)])
    PT = [c.sbuf("PT", [128, 512], BF16) for _ in range(4)]
    osb = [c.sbuf("osb", [65, 512], F32) for _ in range(2)]
    rec = [c.sbuf("rec", [64, 512], F32) for _ in range(2)]
    step = [0]
    fin = [0]

    for h in range(8):
        kt_buf, v_buf = KT[h % 2], Vb[h % 2]
        c.W("KTn", h, 2)
        for kc in range(16):
            pk = PS[kc % 2]
            for k in range(2):
                c.op("pe", lambda e: e.matmul(pk[0:64, :], lhsT=wukv[:, k, h * 128:h * 128 + 64],
                                              rhs=ckvnT[:, k, kc * 512:(kc + 1) * 512], start=(k == 0), stop=(k == 1)),
                     reads=["wukv", "ckvnT"], writes=[("pk", kc % 2)])
            if kc % 2 == 0:
                c.op("act", lambda e: e.activation(out=kt_buf[0:64, kc * 512:(kc + 1) * 512], in_=pk[0:64, :],
                                                   func=AF.Copy), reads=[("pk", kc % 2)], writes=[("KTn", h % 2)])
            else:
                c.op("dve", lambda e: e.tensor_copy(out=kt_buf[0:64, kc * 512:(kc + 1) * 512], in_=pk[0:64, :]),
                     reads=[("pk", kc % 2)], writes=[("KTn", h % 2)])
        c.W("Vb", h, 2)
        for kb in range(8):
            pv = PS[kb % 2]
            for j8 in range(8):
                kt = kb * 8 + j8
                for k in range(2):
                    c.op("pe", lambda e: e.matmul(pv[:, j8 * 64:(j8 + 1) * 64], lhsT=ckvnT[:, k, kt * 128:(kt + 1) * 128],
                                                  rhs=wukv[:, k, h * 128 + 64:h * 128 + 128], start=(k == 0),
                                                  stop=(k == 1)), reads=["wukv", "ckvnT"], writes=[("pk", kb % 2)])
            src = pv[:].rearrange("p (j d) -> p j d", j=8)
            if kb % 2 == 0:
                c.op("act", lambda e: e.activation(out=v_buf[:, kb * 8:(kb + 1) * 8, 0:64], in_=src, func=AF.Copy),
                     reads=[("pk", kb % 2)], writes=[("Vb", h % 2)])
            else:
                c.op("dve", lambda e: e.tensor_copy(out=v_buf[:, kb * 8:(kb + 1) * 8, 0:64], in_=src),
                     reads=[("pk", kb % 2)], writes=[("Vb", h % 2)])
        ktk, vk = c.R("KTn", h, 2), c.R("Vb", h, 2)
        for qc in range(4):
            po = PS[6 + qc % 2]
            nk = 16 * qc + 16
            for kt in range(nk):
                gk, l = kt // 4, kt % 4
                c0 = (max(gk, 4 * qc) - 4 * qc) * 128
                s = step[0]
                step[0] += 1
                ps, pt_ = PS[2 + s % 4], PT[s % 4]
                c.op("pe", lambda e: e.matmul(ps[:, c0:512], lhsT=kt_buf[0:96, kt * 128:(kt + 1) * 128],
                                              rhs=QT[0:96, h, qc * 512 + c0:(qc + 1) * 512], start=True, stop=True),
                     reads=[ktk, f"KT{h % 2}r", "QT"], writes=[("ps", s % 4)])
                c.op("act", lambda e: e.activation(out=pt_[:, c0:512], in_=ps[:, c0:512], func=AF.Exp, scale=SCALE_MLA),
                     reads=[("ps", s % 4)], writes=[("PT", s % 4)])
                if gk >= 4 * qc:
                    c.op("pool", lambda e: e.tensor_tensor(out=pt_[:, c0:c0 + 128], in0=pt_[:, c0:c0 + 128],
                                                           in1=amask[:, l, :], op=ALU.mult),
                         reads=[("PT", s % 4), "const"], writes=[("PT", s % 4)])
                c.op("pe", lambda e: e.matmul(po[0:65, c0:512], lhsT=v_buf[:, kt, 0:65], rhs=pt_[:, c0:512],
                                              start=(kt == 0), stop=(kt == nk - 1)),
                     reads=[vk, ("Vb1", h % 2), ("PT", s % 4)], writes=[("po", qc % 2)])
            f = fin[0]
            fin[0] += 1
            ob, rc = osb[f % 2], rec[f % 2]
            c.op("act", lambda e: e.activation(out=ob[:], in_=po[0:65, :], func=AF.Copy), reads=[("po", qc % 2)],
                 writes=[("osb", f % 2)])
            pd = PS[f % 2]
            c.op("pe", lambda e: e.matmul(pd[0:64, :], lhsT=onesf[64:65, 0:64], rhs=ob[64:65, :], start=True, stop=True),
                 reads=[("osb", f % 2), "const"], writes=[("pk", f % 2)])
            c.op("dve", lambda e: e.reciprocal(out=rc[:], in_=pd[0:64, :]), reads=[("pk", f % 2)], writes=[("rec", f % 2)])
            c.op("dve", lambda e: e.tensor_tensor(out=otb[f % 2][:], in0=ob[0:64, :], in1=rc[:],
                                                  op=ALU.mult), reads=[("osb", f % 2), ("rec", f % 2)], writes=[("otb", f % 2)])
            c.dma("sp", ot_d[h, :, qc * 512:(qc + 1) * 512], otb[f % 2][:], reads=[("otb", f % 2)], writes=["ot_d"],
                  key=f"otw{f % 2}")
    c.pop()
    c.pop()
    if stop == "T":
        return finish()

    c.push()
    PS = [c.psum(f"pc_{i}", [128, 512], F32) for i in range(8)]
    WC = c.sbuf("WC", [128, 8, 3072], BF16)
    load_w(WC[:, :, 0:1024], w_in[:, O_RQ:O_RQ + 1024].rearrange("(k p) n -> p k n", p=128), "WC", "wC0")
    load_w(WC[:, :, 1024:2048], w_in[:, O_RV:O_RV + 1024].rearrange("(k p) n -> p k n", p=128), "WC", "wC1")
    load_w(WC[:, :, 2048:3072], w_in[:, O_RG:O_RG + 1024].rearrange("(k p) n -> p k n", p=128), "WC", "wC2")
    wor = c.sbuf("wor", [128, 8, 1024], BF16)
    c.push()
    wor_f = c.sbuf("wor_f", [128, 8, 1024], F32)
    gr = c.sbuf("gr", [128, 8], F32)
    c.dma("sp", wor_f[:], w_o_ret.rearrange("(k p) n -> p k n", p=128), writes=["wor_f"], key="wC3")
    c.dma("sp", gr[:], gret, writes=["gr"], key="wC4")
    for k in range(8):
        c.op("dve", lambda e: e.tensor_scalar(out=wor[:, k, :], in0=wor_f[:, k, :], scalar1=gr[:, k:k + 1],
                                              scalar2=None, op0=ALU.mult), reads=["wor_f", "gr"], writes=["wor"])
    c.pop()
    kdecC = c.sbuf("kdecC", [128, 8, 128], F32)
    c.dma("sp", kdecC[:, 0:4, :], qdec_d, writes=["kdecC"], key="wC5")
    c.dma("sp", kdecC[:, 4:8, :], kdecC_d, writes=["kdecC"], key="wC6")
    ht = HT(xown, 3, A1, B1, PS[0])
    sinC = c.sbuf("sinC", [128, NO, 80], F32)
    cosC = c.sbuf("cosC", [128, NO, 80], F32)
    c.push()
    rtmpC = [c.sbuf("rtmpC", [128, NO, 64], F32) for _ in range(3)]
    rope_tables(posf[:, NT:NT + NO], NO, sinC, cosC, rtmpC, 16, 80, "rtC")
    c.pop()
    qk1 = c.sbuf("qk1", [128, 1024], F32)
    qk2 = c.sbuf("qk2", [128, 1024], F32)
    qk3 = c.sbuf("qk3", [128, 1024], F32)
    qkp = [c.sbuf("qkp", [128, 8, 128], BF16) for _ in range(2)]
    qkT = [c.sbuf("qkT", [128, 8, 128], BF16) for _ in range(2)]
    vbc = [c.sbuf("vbc", [128, 1024], BF16) for _ in range(2)]
    sg = [c.sbuf("sg", [128, 1024], BF16) for _ in range(2)]
    scT = [c.sbuf("scT", [128, 4, 128], BF16) for _ in range(2)]
    sob = [c.sbuf("sob", [128, 1024], BF16) for _ in range(2)]
    bnst = c.sbuf("bnst", [128, NO, 4, 6], F32)
    bnag = c.sbuf("bnag", [128, NO, 4, 2], F32)
    bnr = c.sbuf("bnr", [128, NO, 4, 2], F32)
    onr = [c.sbuf("onr", [128, 1024], F32) for _ in range(2)]
    gat = [c.sbuf("gat", [128, 1024], BF16) for _ in range(2)]
    gT = [c.sbuf("gT", [128, 8, 128], BF16) for _ in range(2)]
    bbt = [c.sbuf("bbt", [128, 1024], BF16) for _ in range(2)]

    def C1_s0(t):
        if t == 0:
            ht.load(0)
            ht.load(1)
        if t + 2 < NO:
            ht.load(t + 2)
        c.dma("sp", sob[t % 2][:], sown_d[t], reads=[], writes=[c.W("sob", t, 2)], key=f"sob{t % 2}")
        ht.norm(t)
        ht.transpose(t)

    def C1_s1(t):
        hT, hk = ht.get(t)
        for (pi, lo) in [(1, 0), (2, 512), (3, 1024), (4, 1536), (5, 2048), (6, 2560)]:
            for k in range(8):
                c.op("pe", lambda e: e.matmul(PS[pi][:], lhsT=hT[:, k, :], rhs=WC[:, k, lo:lo + 512], start=(k == 0),
                                              stop=(k == 7)), reads=[hk, "WC"], writes=[("pz", pi)])
        cb = cosC[:, t, 16:80].unsqueeze(1).unsqueeze(1).broadcast_to([128, 4, 2, 64])
        sb = sinC[:, t, 16:80].unsqueeze(1).unsqueeze(1).broadcast_to([128, 4, 2, 64])
        for j in range(2):
            z4 = PS[1 + j][:].rearrange("p (h t d) -> p h t d", h=4, t=2)
            sl = slice(j * 512, (j + 1) * 512)
            rope(qk3[:, sl].rearrange("p (h t d) -> p h t d", h=4, t=2), z4, cb, sb,
                 qk1[:, sl].rearrange("p (h t d) -> p h t d", h=4, t=2),
                 qk2[:, sl].rearrange("p (h t d) -> p h t d", h=4, t=2), "pool",
                 [("pz", 1 + j), "rtCsin", "rtCcos"], [("qk3", j)], f"qk{j}")
        c.op("pool", lambda e: e.tensor_tensor(out=qkp[t % 2][:], in0=qk3[:].rearrange("p (h d) -> p h d", h=8),
                                               in1=kdecC[:], op=ALU.mult), reads=[("qk3", 0), ("qk3", 1), "kdecC"],
             writes=[c.W("qkp", t, 2)])
        c.W("vbc", t, 2)
        c.W("sg", t, 2)
        for hh in range(2):
            c.op("act", lambda e: e.activation(out=vbc[t % 2][:, hh * 512:(hh + 1) * 512], in_=PS[3 + hh][:],
                                               func=AF.Copy), reads=[("pz", 3 + hh)], writes=[("vbc", t % 2)])
            c.op("act", lambda e: e.activation(out=sg[t % 2][:, hh * 512:(hh + 1) * 512], in_=PS[5 + hh][:],
                                               func=AF.Silu), reads=[("pz", 5 + hh)], writes=[("sg", t % 2)])

    def C1_s2(t):
        pt = PS[7][:].bitcast(BF16)
        for j in range(8):
            c.op("pe", lambda e: e.transpose(out=pt[:, j * 128:(j + 1) * 128], in_=qkp[t % 2][:, j, :],
                                             identity=identb[:]), reads=[c.R("qkp", t, 2), "const"], writes=["ptC"])
        c.op("act", lambda e: e.activation(out=qkT[t % 2][:], in_=pt.rearrange("p (j n) -> p j n", j=8), func=AF.Copy),
             reads=["ptC"], writes=[c.W("qkT", t, 2)])
        for h in range(4):
            c.op("pe", lambda e: e.matmul(PS[1][:, h * 128:(h + 1) * 128], lhsT=qkT[t % 2][:, 4 + h, :],
                                          rhs=qkT[t % 2][:, h, :], start=True, stop=True),
                 reads=[c.R("qkT", t, 2)], writes=[("pz", 1)])
        c.op("dve", lambda e: e.tensor_tensor(out=scT[t % 2][:], in0=PS[1][:].rearrange("p (h n) -> p h n", h=4),
                                              in1=tri[:].unsqueeze(1).broadcast_to([128, 4, 128]), op=ALU.mult),
             reads=[("pz", 1), "const"], writes=[c.W("scT", t, 2)])
        for h in range(4):
            po = PS[3 + h // 2][:, (h % 2) * 256:(h % 2 + 1) * 256]
            c.op("pe", lambda e: e.matmul(po, lhsT=scT[t % 2][:, h, :], rhs=vbc[t % 2][:, h * 256:(h + 1) * 256],
                                          start=True, stop=False), reads=[c.R("scT", t, 2), c.R("vbc", t, 2)],
                 writes=[("pz", 3 + h // 2)])
            c.op("pe", lambda e: e.matmul(po, lhsT=qkT[t % 2][:, h, :], rhs=sob[t % 2][:, h * 256:(h + 1) * 256],
                                          start=False, stop=True), reads=[c.R("qkT", t, 2), c.R("sob", t, 2)],
                 writes=[("pz", 3 + h // 2)])
        for h in range(4):
            po = PS[3 + h // 2][:, (h % 2) * 256:(h % 2 + 1) * 256]
            c.op("dve", lambda e: e.bn_stats(out=bnst[:, t, h, :], in_=po), reads=[("pz", 3 + h // 2)],
                 writes=[("bnst", t)])
            c.op("dve", lambda e: e.bn_aggr(out=bnag[:, t, h, :], in_=bnst[:, t, h, :]), reads=[("bnst", t)],
                 writes=[("bnag", t)])
        c.op("dve", lambda e: e.tensor_scalar(out=bnr[:, t, :, 0], in0=bnag[:, t, :, 1], scalar1=GN_EPS, scalar2=None,
                                              op0=ALU.add), reads=[("bnag", t)], writes=[("bnr0", t)])
        rsqrt_cols(bnr[:, t, :, 0], bnr[:, t, :, 1], 4, [("bnr0", t)], [("bnr1", t)])
        c.W("onr", t, 2)
        for h in range(4):
            po = PS[3 + h // 2][:, (h % 2) * 256:(h % 2 + 1) * 256]
            c.op("dve", lambda e: e.tensor_scalar(out=onr[t % 2][:, h * 256:(h + 1) * 256], in0=po,
                                                  scalar1=bnag[:, t, h, 0:1], scalar2=bnr[:, t, h, 1:2],
                                                  op0=ALU.subtract, op1=ALU.mult),
                 reads=[("pz", 3 + h // 2), ("bnag", t), ("bnr1", t)], writes=[("onr", t % 2)])
        c.op("pool", lambda e: e.tensor_tensor(out=gat[t % 2][:], in0=onr[t % 2][:], in1=sg[t % 2][:], op=ALU.mult),
             reads=[("onr", t % 2), c.R("sg", t, 2)], writes=[c.W("gat", t, 2)])

    def C1_s3(t):
        pt = PS[7][:].bitcast(BF16)
        for k in range(8):
            c.op("pe", lambda e: e.transpose(out=pt[:, k * 128:(k + 1) * 128], in_=gat[t % 2][:, k * 128:(k + 1) * 128],
                                             identity=identb[:]), reads=[c.R("gat", t, 2), "const"], writes=["ptC"])
        c.op("act", lambda e: e.activation(out=gT[t % 2][:], in_=pt.rearrange("p (j n) -> p j n", j=8), func=AF.Copy),
             reads=["ptC"], writes=[c.W("gT", t, 2)])
        c.W("bbt", t, 2)
        for hh in range(2):
            for k in range(8):
                c.op("pe", lambda e: e.matmul(PS[5 + hh][:], lhsT=gT[t % 2][:, k, :], rhs=wor[:, k, hh * 512:(hh + 1) * 512],
                                              start=(k == 0), stop=(k == 7)), reads=[c.R("gT", t, 2), "wor"],
                     writes=[("pz", 5 + hh)])
            c.op("act", lambda e: e.activation(out=bbt[t % 2][:, hh * 512:(hh + 1) * 512], in_=PS[5 + hh][:],
                                               func=AF.Copy), reads=[("pz", 5 + hh)], writes=[("bbt", t % 2)])
        c.dma("sp", bb_d[t], bbt[t % 2][:], reads=[("bbt", t % 2)], writes=["bb_d"], key=f"bbw{t % 2}")

    def C1_s123(t):
        C1_s1(t)
        C1_s2(t)
        C1_s3(t)

    pipeline(NO, [C1_s0, C1_s123])
    print("sbuf remaining in pass C1:", nc.sbuf_bytes_remaining, "ops", c.nops)
    c.pop()
    if stop == "C1":
        return finish()

    h2T = c.sbuf("h2T", [128, 8, NO * 128], BF16)
    print("sbuf remaining before C2:", nc.sbuf_bytes_remaining)
    c.push()
    PS = [c.psum(f"pd_{i}", [128, 512], F32) for i in range(8)]
    WG = c.sbuf("WG", [128, 8, 2048], BF16)
    load_w(WG[:], w_in[:, O_GA:O_GA + 2048].rearrange("(k p) n -> p k n", p=128), "WG", "wD0")
    wom = c.sbuf("wom", [64, 8, 1024], BF16)
    load_w(wom[:], w_o_mla.rearrange("(h p) n -> p h n", p=64), "wom", "wD1")
    wout = c.sbuf("wout", [128, 8, 1024], BF16)
    load_w(wout[:], w_out.rearrange("(k p) n -> p k n", p=128), "wout", "wD2")
    wr = c.sbuf("wr", [128, 8, 64], F32)
    c.dma("sp", wr[:], w_router.rearrange("(k p) n -> p k n", p=128), writes=["wr"], key="wD3")
    ht = HT(xown, 3, A1, B1, PS[0])
    tg = [c.sbuf("tg", [128, 2048], BF16) for _ in range(2)]
    ott = [c.sbuf("ott", [64, 8, 128], BF16) for _ in range(2)]
    bbr = [c.sbuf("bbr", [128, 1024], BF16) for _ in range(2)]
    m1 = c.sbuf("m1", [128, 1024], F32)
    m2 = c.sbuf("m2", [128, 1024], F32)
    mg = [c.sbuf("mg", [128, 1024], BF16) for _ in range(2)]
    mgT = [c.sbuf("mgT", [128, 8, 128], BF16) for _ in range(2)]
    ty = c.sbuf("ty", [128, 1024], F32)
    x1 = [c.sbuf("x1", [128, 1024], F32) for _ in range(2)]
    xs2 = [c.sbuf("xs2", [128, 1024], F32) for _ in range(2)]
    h2f = [c.sbuf("h2f", [128, 8, 128], F32) for _ in range(2)]
    st2 = c.sbuf("st2", [128, 3 * 64], F32)
    rt = c.sbuf("rt", [128, 2, 64 * 4 + 64 + 8 * 4], F32)

    def C2_s0(t):
        if t == 0:
            ht.load(0)
            ht.load(1)
        if t + 2 < NO:
            ht.load(t + 2)
        c.dma("sp", bbr[t % 2][:], bb_d[t], reads=["bb_d"], writes=[c.W("bbr", t, 2)], key=f"bbr{t % 2}")
        c.dma("sp", ott[t % 2][:], ot_d[:, :, t * 128:(t + 1) * 128].rearrange("h p n -> p h n"), reads=["ot_d"],
              writes=[c.W("ott", t, 2)], key=f"ott{t % 2}")
        ht.norm(t)
        ht.transpose(t)

    def C2_s1(t):
        hT, hk = ht.get(t)
        xt = ht.xt[t % 3]
        c.W("tg", t, 2)
        for j in range(4):
            for k in range(8):
                c.op("pe", lambda e: e.matmul(PS[1 + j][:], lhsT=hT[:, k, :], rhs=WG[:, k, j * 512:(j + 1) * 512],
                                              start=(k == 0), stop=(k == 7)), reads=[hk, "WG"], writes=[("pz", 1 + j)])
            c.op("act", lambda e: e.activation(out=tg[t % 2][:, j * 512:(j + 1) * 512], in_=PS[1 + j][:], func=AF.Tanh,
                                               scale=0.5), reads=[("pz", 1 + j)], writes=[("tg", t % 2)])
        for hh in range(2):
            for h in range(8):
                c.op("pe", lambda e: e.matmul(PS[5 + hh][:], lhsT=ott[t % 2][:, h, :],
                                              rhs=wom[:, h, hh * 512:(hh + 1) * 512], start=(h == 0), stop=(h == 7)),
                     reads=[c.R("ott", t, 2), "wom"], writes=[("pz", 5 + hh)])
            sl = slice(hh * 512, (hh + 1) * 512)
            c.op("dve", lambda e: e.scalar_tensor_tensor(out=m1[:, sl], in0=tg[t % 2][:, sl], scalar=1.0, in1=PS[5 + hh][:],
                                                         op0=ALU.add, op1=ALU.mult),
                 reads=[("tg", t % 2), ("pz", 5 + hh)], writes=[("m1", hh)])
        c.op("dve", lambda e: e.scalar_tensor_tensor(out=m2[:], in0=tg[t % 2][:, 1024:2048], scalar=1.0, in1=bbr[t % 2][:],
                                                     op0=ALU.add, op1=ALU.mult),
             reads=[("tg", t % 2), c.R("bbr", t, 2)], writes=["m2"])
        c.op("pool", lambda e: e.tensor_tensor(out=mg[t % 2][:], in0=m1[:], in1=m2[:], op=ALU.add),
             reads=[("m1", 0), ("m1", 1), "m2"], writes=[c.W("mg", t, 2)])
        pt = PS[7][:].bitcast(BF16)
        for k in range(8):
            c.op("pe", lambda e: e.transpose(out=pt[:, k * 128:(k + 1) * 128], in_=mg[t % 2][:, k * 128:(k + 1) * 128],
                                             identity=identb[:]), reads=[c.R("mg", t, 2), "const"], writes=["ptD"])
        c.op("act", lambda e: e.activation(out=mgT[t % 2][:], in_=pt.rearrange("p (j n) -> p j n", j=8), func=AF.Copy),
             reads=["ptD"], writes=[c.W("mgT", t, 2)])
        c.W("x1", t, 2)
        for hh in range(2):
            sl = slice(hh * 512, (hh + 1) * 512)
            for k in range(8):
                c.op("pe", lambda e: e.matmul(PS[1 + hh][:], lhsT=mgT[t % 2][:, k, :], rhs=wout[:, k, sl],
                                              start=(k == 0), stop=(k == 7)), reads=[c.R("mgT", t, 2), "wout"],
                     writes=[("pz", 1 + hh)])
            c.op("dve", lambda e: e.tensor_tensor(out=ty[:, sl], in0=PS[1 + hh][:], in1=GT1[:, sl], op=ALU.mult),
                 reads=[("pz", 1 + hh), "GT"], writes=[("ty", hh)])
            c.op("pool", lambda e: e.tensor_tensor(out=x1[t % 2][:, sl], in0=ty[:, sl], in1=xt[:, sl], op=ALU.add),
                 reads=[("ty", hh), c.R("xt", t, 3)], writes=[("x1", t % 2)])
        c.dma("sp", x1_d[t], x1[t % 2][:], reads=[("x1", t % 2)], writes=["x1_d"], key=f"x1w{t % 2}")
        m = t
        a, b, r = st2[:, m:m + 1], st2[:, 64 + m:65 + m], st2[:, 128 + m:129 + m]
        c.op("act", lambda e: e.activation(out=junk[:], in_=x1[t % 2][:], func=AF.Square, accum_out=a),
             reads=[("x1", t % 2)], writes=["junk", ("s2a", m)])
        c.op("dve", lambda e: e.tensor_scalar(out=b, in0=a, scalar1=1.0 / D, scalar2=RMS_EPS, op0=ALU.mult, op1=ALU.add),
             reads=[("s2a", m)], writes=[("s2b", m)])
        rsqrt_cols(b, r, 1, [("s2b", m)], [("s2r", m)])
        c.op("dve", lambda e: e.tensor_scalar(out=xs2[t % 2][:], in0=x1[t % 2][:], scalar1=r, scalar2=None, op0=ALU.mult),
             reads=[("x1", t % 2), ("s2r", m)], writes=[c.W("xs2", t, 2)])

    def C2_s2(t):
        c.W("h2f", t, 2)
        for k in range(8):
            pb = PS[3 + k // 4][:, (k % 4) * 128:(k % 4 + 1) * 128]
            c.op("pe", lambda e: e.transpose(out=pb, in_=xs2[t % 2][:, k * 128:(k + 1) * 128], identity=identf[:]),
                 reads=[c.R("xs2", t, 2), "const"], writes=[("pz", 3 + k // 4)])
        for k in range(8):
            pb = PS[3 + k // 4][:, (k % 4) * 128:(k % 4 + 1) * 128]
            c.op("act", lambda e: e.activation(out=h2f[t % 2][:, k, :], in_=pb, func=AF.Identity, scale=A2[:, k:k + 1],
                                               bias=B2[:, k:k + 1]), reads=[("pz", 3 + k // 4), "AB"],
                 writes=[("h2f", t % 2)])
        c.op("dve", lambda e: e.tensor_copy(out=h2T[:, :, t * 128:(t + 1) * 128], in_=h2f[t % 2][:]),
             reads=[("h2f", t % 2)], writes=["h2T"])
        for k in range(8):
            c.op("pe", lambda e: e.matmul(PS[5][:, 0:64], lhsT=h2f[t % 2][:, k, :], rhs=wr[:, k, :], start=(k == 0),
                                          stop=(k == 7)), reads=[("h2f", t % 2), "wr"], writes=[("pz", 5)])
        R_ = rt[:, t % 2, :]
        s_, bi, mb, sel = R_[:, 0:64], R_[:, 64:128], R_[:, 128:192], R_[:, 192:256]
        m8 = R_[:, 256:320]
        gs, g8, gm, gneg = R_[:, 320:328], R_[:, 328:336], R_[:, 336:344], R_[:, 344:352]
        rk_ = ("rt", t % 2)
        c.op("act", lambda e: e.activation(out=s_, in_=PS[5][:, 0:64], func=AF.Tanh, scale=0.5), reads=[("pz", 5)],
             writes=[rk_])
        c.op("dve", lambda e: e.tensor_scalar(out=s_, in0=s_, scalar1=0.5, scalar2=0.5, op0=ALU.mult, op1=ALU.add),
             reads=[rk_], writes=[rk_])
        c.op("dve", lambda e: e.tensor_tensor(out=bi, in0=s_, in1=brout_t[:], op=ALU.add), reads=[rk_, "const"],
             writes=[rk_])
        for g in range(8):
            c.op("dve", lambda e: e.max(out=m8[:, g * 8:(g + 1) * 8], in_=bi[:, g * 8:(g + 1) * 8]), reads=[rk_],
                 writes=[rk_])
        m83 = m8.rearrange("p (g k) -> p g k", g=8)
        c.op("dve", lambda e: e.tensor_tensor(out=gs, in0=m83[:, :, 0], in1=m83[:, :, 1], op=ALU.add), reads=[rk_],
             writes=[rk_])
        c.op("dve", lambda e: e.max(out=g8, in_=gs), reads=[rk_], writes=[rk_])
        c.op("dve", lambda e: e.tensor_scalar(out=gm, in0=gs, scalar1=g8[:, 3:4], scalar2=None, op0=ALU.is_ge),
             reads=[rk_], writes=[rk_])
        c.op("dve", lambda e: e.tensor_scalar(out=gneg, in0=gm, scalar1=-1.0, scalar2=8.0, op0=ALU.add, op1=ALU.mult),
             reads=[rk_], writes=[rk_])
        bi3, mb3 = bi.rearrange("p (g k) -> p g k", g=8), mb.rearrange("p (g k) -> p g k", g=8)
        c.op("dve", lambda e: e.tensor_tensor(out=mb3, in0=bi3, in1=gm.unsqueeze(2).broadcast_to([128, 8, 8]), op=ALU.mult),
             reads=[rk_], writes=[rk_])
        c.op("dve", lambda e: e.tensor_tensor(out=mb3, in0=mb3, in1=gneg.unsqueeze(2).broadcast_to([128, 8, 8]), op=ALU.add),
             reads=[rk_], writes=[rk_])
        c.op("dve", lambda e: e.max(out=g8, in_=mb), reads=[rk_], writes=[rk_])
        c.op("dve", lambda e: e.tensor_scalar(out=sel, in0=mb, scalar1=g8[:, 7:8], scalar2=None, op0=ALU.is_ge),
             reads=[rk_], writes=[rk_])
        c.op("dve", lambda e: e.tensor_tensor(out=sel, in0=sel, in1=s_, op=ALU.mult), reads=[rk_], writes=[rk_])
        c.op("dve", lambda e: e.tensor_reduce(out=gs[:, 0:1], in_=sel, axis=AX.X, op=ALU.add), reads=[rk_], writes=[rk_])
        c.op("dve", lambda e: e.reciprocal(out=gs[:, 1:2], in_=gs[:, 0:1]), reads=[rk_], writes=[rk_])
        c.op("dve", lambda e: e.tensor_scalar(out=comb[:, t, :], in0=sel, scalar1=gs[:, 1:2], scalar2=2.5, op0=ALU.mult,
                                              op1=ALU.mult), reads=[rk_], writes=["comb"])

    def C2_s12(t):
        C2_s1(t)
        C2_s2(t)

    pipeline(NO, [C2_s0, C2_s12])
    print("sbuf remaining in pass C2:", nc.sbuf_bytes_remaining, "ops", c.nops)
    c.pop()
    dbg("h2T", h2T[:], [128, 8, NO * 128], BF16)
    dbg("comb", comb[:], [128, NO, 64], F32)
    if stop == "C2":
        return finish()

    c.push()
    PG = c.psum("pg", [128, 2048], F32)
    PY = [c.psum(f"py{i}", [128, 1024], F32) for i in range(2)]
    acc = c.sbuf("acc", [128, NO, 1024], F32)
    wgu = [c.sbuf("wgu", [128, 8, 512], BF16) for _ in range(2)]
    wdn = [c.sbuf("wdn", [128, 2, 1024], BF16) for _ in range(2)]
    sgm = [c.sbuf("sgm", [128, 1024], BF16) for _ in range(2)]
    actT = [c.sbuf("actT", [128, 2, 512], BF16) for _ in range(2)]
    cnt = [0, 0]
    for ei in range(NEXP + 1):
        e_ = ei - 1
        sl = ei % 2
        if e_ < 0:
            srcs = (w_sg, w_su, w_sd)
        else:
            srcs = (w_eg[e_], w_eu[e_], w_ed[e_])
        c.W("wexp", ei, 2)
        c.dma("pool", wgu[sl][:, :, 0:256], srcs[0].rearrange("(k p) n -> p k n", p=128), writes=[("wexp", sl)], key=f"we{sl}a")
        c.dma("pool", wgu[sl][:, :, 256:512], srcs[1].rearrange("(k p) n -> p k n", p=128), writes=[("wexp", sl)], key=f"we{sl}b")
        c.dma("pool", wdn[sl][:], srcs[2].rearrange("(k p) n -> p k n", p=128), writes=[("wexp", sl)], key=f"we{sl}c")
        wk = ("wexp", sl)
        for tc_ in range(4):
            for j in range(4):
                for k in range(8):
                    c.op("pe", lambda e: e.matmul(PG[:, j * 512:(j + 1) * 512], lhsT=wgu[sl][:, k, j * 128:(j + 1) * 128],
                                                  rhs=h2T[:, k, tc_ * 512:(tc_ + 1) * 512], start=(k == 0), stop=(k == 7)),
                         reads=[wk, "h2T"], writes=[("pg", j // 2)])
            a_ = cnt[0] % 2
            cnt[0] += 1
            c.op("act", lambda e: e.activation(out=sgm[a_][:], in_=PG[:, 0:1024], func=AF.Silu), reads=[("pg", 0)],
                 writes=[("sgm", a_)])
            c.op("dve", lambda e: e.tensor_tensor(out=actT[a_][:].rearrange("p f n -> p (f n)"), in0=sgm[a_][:],
                                                  in1=PG[:, 1024:2048], op=ALU.mult), reads=[("sgm", a_), ("pg", 1)],
                 writes=[("actT", a_)])
            for tt in range(4):
                i = tc_ * 4 + tt
                y_ = cnt[1] % 2
                cnt[1] += 1
                for hh in range(2):
                    for fc in range(2):
                        c.op("pe", lambda e: e.matmul(PY[y_][:, hh * 512:(hh + 1) * 512], lhsT=actT[a_][:, fc, tt * 128:(tt + 1) * 128],
                                                      rhs=wdn[sl][:, fc, hh * 512:(hh + 1) * 512], start=(fc == 0), stop=(fc == 1)),
                             reads=[("actT", a_), wk], writes=[("py", y_)])
                if e_ < 0:
                    c.op("act", lambda e: e.activation(out=acc[:, i, :], in_=PY[y_][:], func=AF.Copy), reads=[("py", y_)],
                         writes=[("acc", i)])
                else:
                    c.op("dve", lambda e: e.scalar_tensor_tensor(out=acc[:, i, :], in0=PY[y_][:], scalar=comb[:, i, e_:e_ + 1],
                                                                 in1=acc[:, i, :], op0=ALU.mult, op1=ALU.add),
                         reads=[("py", y_), ("acc", i), "comb"], writes=[("acc", i)])
    x1r = [c.sbuf("x1r", [128, 1024], F32) for _ in range(2)]
    xo = [c.sbuf("xo", [128, 1024], F32) for _ in range(2)]
    yo = [c.sbuf("yo", [128, 1024], F32) for _ in range(2)]
    stf = c.sbuf("stf", [128, 3 * 64], F32)
    for t in range(NO):
        c.dma("sp", x1r[t % 2][:], x1_d[t], reads=["x1_d"], writes=[("x1r", t % 2)], key=f"x1r{t % 2}")
        c.op("dve", lambda e: e.tensor_tensor(out=xo[t % 2][:], in0=acc[:, t, :], in1=GT2[:], op=ALU.mult),
             reads=[("acc", t), "GT"], writes=[("xo", t % 2)])
        c.op("pool", lambda e: e.tensor_tensor(out=xo[t % 2][:], in0=xo[t % 2][:], in1=x1r[t % 2][:], op=ALU.add),
             reads=[("xo", t % 2), ("x1r", t % 2)], writes=[("xo", t % 2)])
        a, b, r = stf[:, t:t + 1], stf[:, 64 + t:65 + t], stf[:, 128 + t:129 + t]
        c.op("act", lambda e: e.activation(out=junk[:], in_=xo[t % 2][:], func=AF.Square, accum_out=a),
             reads=[("xo", t % 2)], writes=["junk", ("sfa", t)])
        c.op("dve", lambda e: e.tensor_scalar(out=b, in0=a, scalar1=1.0 / D, scalar2=RMS_EPS, op0=ALU.mult, op1=ALU.add),
             reads=[("sfa", t)], writes=[("sfb", t)])
        rsqrt_cols(b, r, 1, [("sfb", t)], [("sfr", t)])
        c.op("dve", lambda e: e.scalar_tensor_tensor(out=yo[t % 2][:], in0=xo[t % 2][:], scalar=r, in1=gfin_t[:],
                                                     op0=ALU.mult, op1=ALU.mult),
             reads=[("xo", t % 2), ("sfr", t), "const"], writes=[("yo", t % 2)])
        c.dma("sp", out_d[t * 128:(t + 1) * 128, :], yo[t % 2][:], reads=[("yo", t % 2)], writes=["out"], key=f"out{t % 2}")
    c.pop()
    c.close()
    return nc


_NC_CACHE = {}


def _consts(j):
    bf = ml_dtypes.bfloat16
    k = np.arange(128)
    tri = (k[:, None] <= k[None, :]).astype(np.float32)
    amask = np.zeros((128, 4, 128), np.float32)
    for l in range(4):
        if l < j:
            amask[:, l, :] = 1.0
        elif l == j:
            amask[:, l, :] = tri
    inv_mla = 1.0 / (10000.0 ** (np.arange(0, 32, 2, dtype=np.float32) / np.float32(32)))
    inv_ret = 1.0 / (10000.0 ** (np.arange(0, 128, 2, dtype=np.float32) / np.float32(128)))
    invf = np.broadcast_to(np.concatenate([inv_mla, inv_ret]).astype(np.float32)[None, :], (128, 80)).copy()
    g = np.array(GAMMA, np.float64)
    m = np.arange(128, dtype=np.float64)
    kdecA = (g[None, :] ** (127.0 - m[:, None])) * 128.0 ** -0.5
    kdecC = (g[None, :] ** (-m[:, None])) * 128.0 ** -0.5
    qdec = g[None, :] ** m[:, None]
    rep = lambda a: np.repeat(a[:, :, None], 128, axis=2).astype(np.float32)
    G = g ** 128
    rc = np.zeros((128, 16), np.float64)
    for h in range(4):
        rc[:, h * 4 + 0] = g[h] * G[h] ** j
        for l in range(3):
            rc[:, h * 4 + 1 + l] = g[h] * (G[h] ** (j - 1 - l)) if l < j else 0.0
    return dict(identb=np.eye(128).astype(bf), identf=np.eye(128, dtype=np.float32), tri=tri,
                amask=amask.astype(bf), invf=invf, kdecA=rep(kdecA), kdecC=rep(kdecC), qdec=rep(qdec),
                rcoef=rc.astype(np.float32))


def _col(v, nchunk):
    return np.ascontiguousarray(np.asarray(v, np.float32).reshape(nchunk, 128).T)


_BUILD_ARGS = {}


def kernel(x, c, positions, w_ada, b_ada, g_norm1, w_in, g_cq, w_uq, g_ckv, w_ukv, g_ret, w_o_mla, w_o_ret,
           w_out, g_norm2, w_router, b_router, w_exp_gate, w_exp_up, w_exp_down, w_sh_gate, w_sh_up, w_sh_down,
           g_final):
    f = lambda a: np.ascontiguousarray(np.asarray(a, dtype=np.float32))
    x = f(x)
    positions = np.asarray(positions).astype(np.int32)
    if "nc" not in _NC_CACHE:
        _NC_CACHE["nc"] = build(**_BUILD_ARGS)
    nc = _NC_CACHE["nc"]
    shared = dict(
        w_ada=f(w_ada), bada_row=f(b_ada).reshape(1, -1), g1c=_col(g_norm1, 8), g2c=_col(g_norm2, 8), w_in=f(w_in),
        gcq=_col(g_cq, 3), w_uq=f(w_uq), gckv=_col(g_ckv, 2), w_ukv=f(w_ukv), gret=_col(g_ret, 8), w_o_mla=f(w_o_mla),
        w_o_ret=f(w_o_ret), w_out=f(w_out), w_router=f(w_router),
        brout=np.ascontiguousarray(np.broadcast_to(f(b_router)[None, :], (128, 64))),
        w_exp_gate=f(w_exp_gate), w_exp_up=f(w_exp_up), w_exp_down=f(w_exp_down), w_sh_gate=f(w_sh_gate),
        w_sh_up=f(w_sh_up), w_sh_down=f(w_sh_down),
        gfin=np.ascontiguousarray(np.broadcast_to(f(g_final)[None, :], (128, 1024))),
    )
    in_maps = []
    for core in range(8):
        b, j = core // 4, core % 4
        xb = x[b]
        xt = xb.reshape(NT, 128, D)
        pt = positions[b].reshape(NT, 128)
        m = dict(shared)
        m.update(_consts(j))
        m["xall"] = xb
        m["xown"] = np.ascontiguousarray(xt[j::4].reshape(NO * 128, D))
        m["posall"] = np.ascontiguousarray(pt.T)
        m["posown"] = np.ascontiguousarray(pt[j::4].T)
        m["cvec"] = _col(c[b], 8)
        in_maps.append(m)
    res = run_bass_kernel_spmd(nc, in_maps, core_ids=list(range(8)))
    _NC_CACHE["res"] = res
    out = np.empty((2, S, D), np.float32)
    for core in range(8):
        b, j = core // 4, core % 4
        o = np.asarray(res.results[core]["out"]).reshape(NO, 128, D)
        out[b].reshape(NT, 128, D)[j::4] = o
    return out
```

```python
import math
from contextlib import ExitStack

import numpy as np
import ml_dtypes

import concourse.bass as bass
import concourse.mybir as mybir
from concourse.bass_utils import run_bass_kernel_spmd

F32 = mybir.dt.float32
BF16 = mybir.dt.bfloat16
I32 = mybir.dt.int32
AF = mybir.ActivationFunctionType
ALU = mybir.AluOpType
AX = mybir.AxisListType

D = 1024
S = 8192
NT = 64
NO = 16
D_IN = 5792
RMS_EPS = 1e-6
GN_EPS = 1e-5
TWO_PI = 2.0 * math.pi
MAGIC = 12582912.0
C1 = 6.28125
C2 = float(np.float32(TWO_PI - 6.28125))
C3 = float(TWO_PI - 6.28125 - float(np.float32(TWO_PI - 6.28125)))
SCALE_MLA = 96.0 ** -0.5
GAMMA = [1.0 - 2.0 ** (-5.0 - h) for h in range(4)]
NEXP = 64

O_CQ, O_CKV, O_KR, O_RQ, O_RK, O_RV, O_RG, O_GA, O_GB = 0, 384, 640, 672, 1184, 1696, 2720, 3744, 4768


class Ctx:
    def __init__(self, nc):
        self.nc = nc
        self.stacks = [ExitStack()]
        self.eng = {"pe": nc.tensor, "act": nc.scalar, "dve": nc.vector, "pool": nc.gpsimd, "sp": nc.sync}
        self.sem, self.cnt = {}, {}
        for e in ("pe", "act", "dve", "pool"):
            self.sem[e] = self.stacks[0].enter_context(nc.semaphore("s_" + e))
            self.cnt[e] = 0
        self.dsem, self.dcnt = {}, {}
        self.waited = {e: {} for e in self.eng}
        self.last_w, self.readers, self.owner = {}, {}, {}
        self.nops = 0
        self.uid = 0

    def sbuf(self, name, shape, dtype):
        self.uid += 1
        return self.stacks[-1].enter_context(self.nc.sbuf_tensor(f"{name}_{self.uid}", list(shape), dtype))

    def psum(self, name, shape, dtype):
        self.uid += 1
        return self.stacks[-1].enter_context(self.nc.psum_tensor(f"{name}_{self.uid}", list(shape), dtype))

    def push(self):
        self.stacks.append(ExitStack())

    def pop(self):
        self.barrier()
        self.stacks.pop().close()

    def W(self, name, t=0, n=1):
        k = (name, t % n)
        self.owner[k] = t
        return k

    def R(self, name, t=0, n=1):
        k = (name, t % n)
        assert self.owner.get(k) == t, f"stale read {name} tile {t} owner {self.owner.get(k)}"
        return k

    PSUM_NAMES = {"pmod", "pc", "pgt", "pT", "pz", "ptA", "pkv", "pcq", "ptQ", "pq", "ptQ2", "pk", "ps", "po", "ptC",
                  "ptD", "pg", "py"}

    def _split(self, reads, writes):
        rd, wr = [], list(writes)
        for r in reads:
            base = r[0] if isinstance(r, tuple) else r
            if base in self.PSUM_NAMES:
                if r not in wr:
                    wr.append(r)
            else:
                rd.append(r)
        return rd, wr

    def _deps(self, reads, writes):
        deps = []
        for r in reads:
            t = self.last_w.get(r)
            if t is not None:
                deps.append(t)
        for w in writes:
            t = self.last_w.get(w)
            if t is not None:
                deps.append(t)
            deps.extend(self.readers.get(w, ()))
        return deps

    def _wait(self, eng, deps):
        best = {}
        for (skey, sem, val) in deps:
            if eng == "pe" and skey == "pe":
                continue
            if val > best.get(skey, (None, 0))[1]:
                best[skey] = (sem, val)
        w = self.waited[eng]
        for skey, (sem, val) in best.items():
            if w.get(skey, 0) >= val:
                continue
            self.eng[eng].wait_ge(sem, val)
            w[skey] = val

    def _commit(self, ticket, reads, writes):
        for r in reads:
            self.readers.setdefault(r, []).append(ticket)
        for w in writes:
            self.last_w[w] = ticket
            self.readers[w] = []

    def op(self, eng, fn, reads=(), writes=()):
        reads, writes = self._split(reads, writes)
        self._wait(eng, self._deps(reads, writes))
        ins = fn(self.eng[eng])
        self.cnt[eng] += 1
        ins.then_inc(self.sem[eng], 1)
        t = (eng, self.sem[eng], self.cnt[eng])
        self._commit(t, reads, writes)
        self.nops += 1
        return t

    def dma(self, queue, out, in_, reads=(), writes=(), key=None):
        if key not in self.dsem:
            self.dsem[key] = self.stacks[0].enter_context(self.nc.semaphore("d_" + str(key)))
            self.dcnt[key] = 0
        self._wait(queue, self._deps(reads, writes))
        ins = self.eng[queue].dma_start(out=out, in_=in_)
        self.dcnt[key] += 16
        ins.then_inc(self.dsem[key], 16)
        t = ("d_" + str(key), self.dsem[key], self.dcnt[key])
        self._commit(t, reads, writes)
        return t

    def barrier(self):
        tickets = [(e, self.sem[e], self.cnt[e]) for e in self.sem if self.cnt[e] > 0]
        tickets += [("d_" + str(k), self.dsem[k], self.dcnt[k]) for k in self.dsem]
        for e in self.eng:
            self._wait(e, tickets)
        self.last_w, self.readers = {}, {}

    def close(self):
        self.barrier()
        while self.stacks:
            self.stacks.pop().close()


def pipeline(n, stages):
    ns = len(stages)
    for s in range(n + ns - 1):
        for k in range(ns - 1, -1, -1):
            t = s - k
            if 0 <= t < n:
                stages[k](t)


def build(stop=None, debug=False):
    nc = bass.Bass("TRN2", target_bir_lowering=False)
    dbg_out = {}

    def dbg(name, ap, shape, dt):
        if not debug:
            return
        d_ = nc.dram_tensor("dbg_" + name, list(shape), dt, kind="ExternalOutput").ap()
        dbg_out[name] = d_
        c.barrier()
        c.dma("sp", d_, ap, key="dbg_" + name)
        c.barrier()

    def finish():
        c.close()
        return nc

    def din(name, shape, dt=F32):
        return nc.dram_tensor(name, list(shape), dt, kind="ExternalInput").ap()

    xall = din("xall", [S, D])
    xown = din("xown", [NO * 128, D])
    posall = din("posall", [128, NT], I32)
    posown = din("posown", [128, NO], I32)
    cvec = din("cvec", [128, 8])
    w_ada = din("w_ada", [D, 6 * D])
    bada_row = din("bada_row", [1, 6 * D])
    g1c = din("g1c", [128, 8])
    g2c = din("g2c", [128, 8])
    w_in = din("w_in", [D, D_IN])
    gcq = din("gcq", [128, 3])
    w_uq = din("w_uq", [384, 768])
    gckv = din("gckv", [128, 2])
    w_ukv = din("w_ukv", [256, 1024])
    gret = din("gret", [128, 8])
    w_o_mla = din("w_o_mla", [512, 1024])
    w_o_ret = din("w_o_ret", [1024, 1024])
    w_out = din("w_out", [1024, 1024])
    w_router = din("w_router", [1024, 64])
    brout = din("brout", [128, 64])
    w_eg = din("w_exp_gate", [NEXP, 1024, 256])
    w_eu = din("w_exp_up", [NEXP, 1024, 256])
    w_ed = din("w_exp_down", [NEXP, 256, 1024])
    w_sg = din("w_sh_gate", [1024, 256])
    w_su = din("w_sh_up", [1024, 256])
    w_sd = din("w_sh_down", [256, 1024])
    gfin = din("gfin", [128, 1024])
    identb_d = din("identb", [128, 128], BF16)
    identf_d = din("identf", [128, 128])
    tri_d = din("tri", [128, 128])
    amask_d = din("amask", [128, 4, 128], BF16)
    invf_d = din("invf", [128, 80])
    kdecA_d = din("kdecA", [128, 4, 128])
    kdecC_d = din("kdecC", [128, 4, 128])
    qdec_d = din("qdec", [128, 4, 128])
    rcoef_d = din("rcoef", [128, 16])
    out_d = nc.dram_tensor("out", [NO * 128, D], F32, kind="ExternalOutput").ap()
    sown_d = nc.dram_tensor("sown_s", [NO, 128, 1024], BF16, kind="ExternalOutput").ap()
    bb_d = nc.dram_tensor("bb_s", [NO, 128, 1024], BF16, kind="ExternalOutput").ap()
    x1_d = nc.dram_tensor("x1_s", [NO, 128, 1024], F32, kind="ExternalOutput").ap()
    ot_d = nc.dram_tensor("ot_s", [8, 64, NO * 128], BF16, kind="ExternalOutput").ap()

    c = Ctx(nc)

    identb = c.sbuf("identb", [128, 128], BF16)
    identf = c.sbuf("identf", [128, 128], F32)
    tri = c.sbuf("tri", [128, 128], F32)
    amask = c.sbuf("amask", [128, 4, 128], BF16)
    invf = c.sbuf("invf", [128, 80], F32)
    rcoef = c.sbuf("rcoef", [128, 16], F32)
    nhalf = c.sbuf("nhalf", [128, 64], F32)
    onesf = c.sbuf("onesf", [128, 128], F32)
    AB = c.sbuf("AB", [128, 32], F32)
    GT1 = c.sbuf("GT1", [128, 1024], F32)
    GT2 = c.sbuf("GT2", [128, 1024], F32)
    gfin_t = c.sbuf("gfin_t", [128, 1024], F32)
    brout_t = c.sbuf("brout_t", [128, 64], F32)
    posf = c.sbuf("posf", [128, NT + NO], F32)
    junk = c.sbuf("junk", [128, 1024], BF16)
    comb = c.sbuf("comb", [128, NO, 64], F32)

    for (t_, d_, k_) in [(identb, identb_d, "c0"), (identf, identf_d, "c1"), (tri, tri_d, "c2"),
                         (amask, amask_d, "c3"), (invf, invf_d, "c4"), (rcoef, rcoef_d, "c5"),
                         (gfin_t, gfin, "c6"), (brout_t, brout, "c7")]:
        c.dma("sp", t_[:], d_, writes=["const"], key=k_)
    c.op("pool", lambda e: e.memset(nhalf[:], -0.5), writes=["const"])
    c.op("pool", lambda e: e.memset(onesf[:], 1.0), writes=["const"])
    c.barrier()
    if stop == "c":
        return finish()

    def rsqrt_cols(src_ap, dst_ap, ncols, rd, wr):
        c.op("pool", lambda e: e.tensor_tensor(out=dst_ap, in0=src_ap, in1=nhalf[:, 0:ncols], op=ALU.pow),
             reads=rd, writes=wr)

    c.push()
    PS = [c.psum(f"p0_{i}", [128, 512], F32) for i in range(8)]
    cv = c.sbuf("cv", [128, 8], F32)
    cs = c.sbuf("cs", [128, 8], F32)
    modrow = c.sbuf("modrow", [1, 6 * D], F32)
    brow = c.sbuf("brow", [1, 6 * D], F32)
    gcols = c.sbuf("gcols", [128, 16], F32)
    wab = [c.sbuf(f"wab{i}", [128, 8, 512], F32) for i in range(2)]
    posi = c.sbuf("posi", [128, NT + NO], I32)
    c.dma("sp", cv[:], cvec, writes=["cv"], key="p0a")
    c.dma("sp", brow[:], bada_row, writes=["brow"], key="p0b")
    c.dma("sp", gcols[:, 0:8], g1c, writes=["gcols"], key="p0c")
    c.dma("sp", gcols[:, 8:16], g2c, writes=["gcols"], key="p0d")
    c.dma("sp", posi[:, 0:NT], posall, writes=["posi"], key="p0e")
    c.dma("sp", posi[:, NT:NT + NO], posown, writes=["posi"], key="p0f")
    c.op("dve", lambda e: e.tensor_copy(out=posf[:], in_=posi[:]), reads=["posi"], writes=["posf"])
    c.op("act", lambda e: e.activation(out=cs[:], in_=cv[:], func=AF.Silu), reads=["cv"], writes=["cs"])
    for n in range(12):
        wt = wab[n % 2]
        c.dma("sp", wt[:], w_ada[:, n * 512:(n + 1) * 512].rearrange("(k p) n -> p k n", p=128),
              writes=[c.W("wab", n, 2)], key=f"wab{n % 2}")
        for k in range(8):
            c.op("pe", lambda e: e.matmul(PS[n % 2][0:1, :], lhsT=cs[:, k:k + 1], rhs=wt[:, k, :],
                                          start=(k == 0), stop=(k == 7)),
                 reads=[c.R("wab", n, 2), "cs"], writes=[("pmod", n % 2)])
        c.op("dve", lambda e: e.tensor_tensor(out=modrow[0:1, n * 512:(n + 1) * 512], in0=PS[n % 2][0:1, :],
                                              in1=brow[0:1, n * 512:(n + 1) * 512], op=ALU.add),
             reads=[("pmod", n % 2), "brow"], writes=["modrow"])
    if stop == "0a":
        dbg("modrow", modrow[:], [1, 6 * D], F32)
        c.pop()
        return finish()
    pc = PS[2]
    for idx, ch in enumerate(list(range(0, 16)) + list(range(24, 40))):
        c.op("pe", lambda e: e.matmul(pc[:, idx:idx + 1], lhsT=modrow[0:1, ch * 128:(ch + 1) * 128],
                                      rhs=onesf[0:1, 0:1], start=True, stop=True),
             reads=["modrow", "const"], writes=["pc"])
    c.op("dve", lambda e: e.scalar_tensor_tensor(out=AB[:, 0:8], in0=pc[:, 8:16], scalar=1.0, in1=gcols[:, 0:8],
                                                 op0=ALU.add, op1=ALU.mult), reads=["pc", "gcols"], writes=["AB"])
    c.op("dve", lambda e: e.tensor_copy(out=AB[:, 8:16], in_=pc[:, 0:8]), reads=["pc"], writes=["AB"])
    c.op("dve", lambda e: e.scalar_tensor_tensor(out=AB[:, 16:24], in0=pc[:, 24:32], scalar=1.0, in1=gcols[:, 8:16],
                                                 op0=ALU.add, op1=ALU.mult), reads=["pc", "gcols"], writes=["AB"])
    c.op("dve", lambda e: e.tensor_copy(out=AB[:, 24:32], in_=pc[:, 16:24]), reads=["pc"], writes=["AB"])
    if stop == "0b":
        c.pop()
        dbg("AB", AB[:], [128, 32], F32)
        return finish()
    for (dst, base, scl, pi) in [(GT1, 2048, 0.5, 4), (GT2, 5120, 1.0, 6)]:
        for hh in range(2):
            c.op("pe", lambda e: e.matmul(PS[pi + hh][:], lhsT=onesf[0:1, 0:128],
                                          rhs=modrow[0:1, base + hh * 512: base + (hh + 1) * 512],
                                          start=True, stop=True), reads=["modrow", "const"], writes=[("pgt", pi + hh)])
            c.op("act", lambda e: e.activation(out=dst[:, hh * 512:(hh + 1) * 512], in_=PS[pi + hh][:],
                                               func=AF.Copy, scale=scl), reads=[("pgt", pi + hh)], writes=["GT"])
    c.pop()
    dbg("AB", AB[:], [128, 32], F32)
    dbg("GT1", GT1[:], [128, 1024], F32)
    if stop == "0":
        return finish()

    A1, B1, A2, B2 = AB[:, 0:8], AB[:, 8:16], AB[:, 16:24], AB[:, 24:32]

    def load_w(dst_tile, src_ap, name, key):
        c.dma("pool", dst_tile, src_ap, writes=[name], key=key)

    def rope_tables(pos_ap, G, sinT, cosT, tmp, lo, hi, tag):
        n = hi - lo
        ang, u, r = (tmp[0][:, 0:G, 0:n], tmp[1][:, 0:G, 0:n], tmp[2][:, 0:G, 0:n])
        c.op("dve", lambda e: e.tensor_tensor(out=ang, in0=invf[:, lo:hi].unsqueeze(1).broadcast_to([128, G, n]),
                                              in1=pos_ap.unsqueeze(2).broadcast_to([128, G, n]), op=ALU.mult),
             reads=["posf", "const"], writes=[tag + "t0"])
        c.op("dve", lambda e: e.tensor_scalar(out=u, in0=ang, scalar1=1.0 / TWO_PI, scalar2=MAGIC,
                                              op0=ALU.mult, op1=ALU.add), reads=[tag + "t0"], writes=[tag + "t1"])
        c.op("dve", lambda e: e.tensor_scalar(out=u, in0=u, scalar1=-MAGIC, scalar2=None, op0=ALU.add),
             reads=[tag + "t1"], writes=[tag + "t1"])
        c.op("dve", lambda e: e.scalar_tensor_tensor(out=r, in0=u, scalar=-C1, in1=ang, op0=ALU.mult, op1=ALU.add),
             reads=[tag + "t1", tag + "t0"], writes=[tag + "t2"])
        c.op("dve", lambda e: e.scalar_tensor_tensor(out=r, in0=u, scalar=-C2, in1=r, op0=ALU.mult, op1=ALU.add),
             reads=[tag + "t1", tag + "t2"], writes=[tag + "t2"])
        c.op("dve", lambda e: e.scalar_tensor_tensor(out=r, in0=u, scalar=-C3, in1=r, op0=ALU.mult, op1=ALU.add),
             reads=[tag + "t1", tag + "t2"], writes=[tag + "t2"])
        c.op("dve", lambda e: e.tensor_scalar(out=r, in0=r, scalar1=-math.pi, scalar2=math.pi, op0=ALU.max, op1=ALU.min),
             reads=[tag + "t2"], writes=[tag + "t2"])
        c.op("act", lambda e: e.activation(out=sinT[:, 0:G, lo:hi], in_=r, func=AF.Sin), reads=[tag + "t2"],
             writes=[tag + "sin"])
        c.op("dve", lambda e: e.scalar_tensor_tensor(out=ang, in0=r, scalar=-1.0, in1=r, op0=ALU.mult, op1=ALU.max),
             reads=[tag + "t2"], writes=[tag + "t0"])
        c.op("dve", lambda e: e.tensor_scalar(out=ang, in0=ang, scalar1=-1.0, scalar2=math.pi / 2, op0=ALU.mult,
                                              op1=ALU.add), reads=[tag + "t0"], writes=[tag + "t0"])
        c.op("act", lambda e: e.activation(out=cosT[:, 0:G, lo:hi], in_=ang, func=AF.Sin), reads=[tag + "t0"],
             writes=[tag + "cos"])

    def rope(out4, z4, cos_b, sin_b, t1, t2, eng2, rd, wr, tmpname):
        c.op("dve", lambda e: e.tensor_tensor(out=t1, in0=z4, in1=cos_b, op=ALU.mult), reads=rd, writes=[tmpname + "1"])
        c.op("dve", lambda e: e.tensor_tensor(out=t2, in0=z4, in1=sin_b, op=ALU.mult), reads=rd, writes=[tmpname + "2"])
        c.op(eng2, lambda e: e.tensor_tensor(out=out4[:, :, 0, :], in0=t1[:, :, 0, :], in1=t2[:, :, 1, :], op=ALU.subtract),
             reads=[tmpname + "1", tmpname + "2"], writes=wr)
        c.op(eng2, lambda e: e.tensor_tensor(out=out4[:, :, 1, :], in0=t1[:, :, 1, :], in1=t2[:, :, 0, :], op=ALU.add),
             reads=[tmpname + "1", tmpname + "2"], writes=wr)

    class HT:
        def __init__(self, src, nb, A, B, pT):
            self.src, self.nb, self.A, self.B, self.pT = src, nb, A, B, pT
            self.xt = [c.sbuf("xt", [128, 1024], F32) for _ in range(nb)]
            self.xs = [c.sbuf("xs", [128, 1024], BF16) for _ in range(2)]
            self.hT = [c.sbuf("hT", [128, 8, 128], BF16) for _ in range(2)]
            self.st = c.sbuf("hst", [128, 3 * 64], F32)

        def load(self, t):
            c.dma("sp", self.xt[t % self.nb][:], self.src[t * 128:(t + 1) * 128, :],
                  writes=[c.W("xt", t, self.nb)], key=f"xt{t % self.nb}")

        def norm(self, t):
            xt, st = self.xt[t % self.nb], self.st
            a, b, r = st[:, t % 64:t % 64 + 1], st[:, 64 + t % 64:65 + t % 64], st[:, 128 + t % 64:129 + t % 64]
            c.op("act", lambda e: e.activation(out=junk[:], in_=xt[:], func=AF.Square, accum_out=a),
                 reads=[c.R("xt", t, self.nb)], writes=["junk", ("hsa", t % 64)])
            c.op("dve", lambda e: e.tensor_scalar(out=b, in0=a, scalar1=1.0 / D, scalar2=RMS_EPS, op0=ALU.mult,
                                                  op1=ALU.add), reads=[("hsa", t % 64)], writes=[("hsb", t % 64)])
            rsqrt_cols(b, r, 1, [("hsb", t % 64)], [("hsr", t % 64)])
            c.op("dve", lambda e: e.tensor_scalar(out=self.xs[t % 2][:], in0=xt[:], scalar1=r, scalar2=None,
                                                  op0=ALU.mult), reads=[c.R("xt", t, self.nb), ("hsr", t % 64)],
                 writes=[c.W("xs", t, 2)])

        def transpose(self, t):
            xs, hT, pT = self.xs[t % 2], self.hT[t % 2], self.pT
            pTb = pT[:].bitcast(BF16)
            for k in range(8):
                c.op("pe", lambda e: e.transpose(out=pTb[:, k * 128:(k + 1) * 128], in_=xs[:, k * 128:(k + 1) * 128],
                                                 identity=identb[:]), reads=[c.R("xs", t, 2), "const"], writes=["pT"])
            c.W("hT", t, 2)
            for k in range(8):
                c.op("act", lambda e: e.activation(out=hT[:, k, :], in_=pTb[:, k * 128:(k + 1) * 128],
                                                   func=AF.Identity, scale=self.A[:, k:k + 1],
                                                   bias=self.B[:, k:k + 1]), reads=["pT", "AB"],
                     writes=[("hT", t % 2)])

        def get(self, t):
            return self.hT[t % 2], c.R("hT", t, 2)

    c.push()
    ckvnT = c.sbuf("ckvnT", [128, 2, S], BF16)
    KT = [c.sbuf(f"KT{i}", [128, S], BF16) for i in range(2)]

    c.push()
    PS = [c.psum(f"pa_{i}", [128, 512], F32) for i in range(8)]
    WA = c.sbuf("WA", [128, 8, 1824], BF16)
    load_w(WA[:, :, 0:288], w_in[:, O_CKV:O_CKV + 288].rearrange("(k p) n -> p k n", p=128), "WA", "wA0")
    load_w(WA[:, :, 288:800], w_in[:, O_RK:O_RK + 512].rearrange("(k p) n -> p k n", p=128), "WA", "wA1")
    load_w(WA[:, :, 800:1824], w_in[:, O_RV:O_RV + 1024].rearrange("(k p) n -> p k n", p=128), "WA", "wA2")
    kdecA = c.sbuf("kdecA", [128, 4, 128], F32)
    c.dma("sp", kdecA[:], kdecA_d, writes=["kdecA"], key="kdA")
    ht = HT(xall, 3, A1, B1, PS[0])
    sinT = [c.sbuf("sinT", [128, 4, 80], F32) for _ in range(2)]
    cosT = [c.sbuf("cosT", [128, 4, 80], F32) for _ in range(2)]
    rtmp = [c.sbuf("rtmp", [128, 4, 80], F32) for _ in range(3)]
    stA = c.sbuf("stA", [128, 3 * 64], F32)
    ckvs = [c.sbuf("ckvs", [128, 256], BF16) for _ in range(2)]
    kst = [c.sbuf("kst", [128, 96], BF16) for _ in range(2)]
    kr1 = c.sbuf("kr1", [128, 32], F32)
    kr2 = c.sbuf("kr2", [128, 32], F32)
    rk1 = c.sbuf("rk1", [128, 512], F32)
    rk2 = c.sbuf("rk2", [128, 512], F32)
    rk3 = c.sbuf("rk3", [128, 512], F32)
    kp = [c.sbuf("kp", [128, 4, 128], BF16) for _ in range(2)]
    vb = [c.sbuf("vb", [128, 1024], BF16) for _ in range(2)]
    Sst = c.sbuf("Sst", [128, 1024], F32)
    Sown = [c.sbuf("Sown", [128, 1024], F32) for _ in range(2)]
    Sownb = [c.sbuf("Sownb", [128, 1024], BF16) for _ in range(2)]
    for i in range(2):
        c.op("pool", lambda e: e.memset(kst[i][:], 0.0), writes=[("kst", i)])
    c.op("pool", lambda e: e.memset(Sst[:], 0.0), writes=["Sst"])

    def A_s0(t):
        if t == 0:
            ht.load(0)
            ht.load(1)
        if t + 2 < NT:
            ht.load(t + 2)
        if t % 4 == 0:
            g = t // 4
            rope_tables(posf[:, t:t + 4], 4, sinT[g % 2], cosT[g % 2], rtmp, 0, 80, f"rtA{g % 2}")
            c.W("tabA", g, 2)
        ht.norm(t)
        ht.transpose(t)

    def A_s1(t):
        hT, hk = ht.get(t)
        for (pi, lo, n) in [(1, 0, 288), (2, 288, 512), (3, 800, 512), (4, 1312, 512)]:
            for k in range(8):
                c.op("pe", lambda e: e.matmul(PS[pi][:, 0:n], lhsT=hT[:, k, :], rhs=WA[:, k, lo:lo + n],
                                              start=(k == 0), stop=(k == 7)), reads=[hk, "WA"], writes=[("pz", pi)])
        m = t % 64
        a, b, r = stA[:, m:m + 1], stA[:, 64 + m:65 + m], stA[:, 128 + m:129 + m]
        c.op("act", lambda e: e.activation(out=junk[:, 0:256], in_=PS[1][:, 0:256], func=AF.Square, accum_out=a),
             reads=[("pz", 1)], writes=["junk", ("sAa", m)])
        c.op("dve", lambda e: e.tensor_scalar(out=b, in0=a, scalar1=1.0 / 256, scalar2=RMS_EPS, op0=ALU.mult,
                                              op1=ALU.add), reads=[("sAa", m)], writes=[("sAb", m)])
        rsqrt_cols(b, r, 1, [("sAb", m)], [("sAr", m)])
        c.op("dve", lambda e: e.tensor_scalar(out=ckvs[t % 2][:], in0=PS[1][:, 0:256], scalar1=r, scalar2=None,
                                              op0=ALU.mult), reads=[("pz", 1), ("sAr", m)], writes=[c.W("ckvs", t, 2)])
        g = t // 4
        tk = c.R("tabA", g, 2)
        sn, cs_ = sinT[g % 2], cosT[g % 2]
        z4 = PS[1][:, 256:288].rearrange("p (h t d) -> p h t d", h=1, t=2)
        cb = cs_[:, t % 4, 0:16].unsqueeze(1).unsqueeze(1).broadcast_to([128, 1, 2, 16])
        sb = sn[:, t % 4, 0:16].unsqueeze(1).unsqueeze(1).broadcast_to([128, 1, 2, 16])
        o4 = kst[t % 2][:, 64:96].rearrange("p (h t d) -> p h t d", h=1, t=2)
        rope(o4, z4, cb, sb, kr1[:].rearrange("p (h t d) -> p h t d", h=1, t=2),
             kr2[:].rearrange("p (h t d) -> p h t d", h=1, t=2), "pool",
             [("pz", 1), f"rtA{g % 2}sin", f"rtA{g % 2}cos"], [c.W("kst", t, 2)], "kr")
        z4 = PS[2][:].rearrange("p (h t d) -> p h t d", h=4, t=2)
        cb = cs_[:, t % 4, 16:80].unsqueeze(1).unsqueeze(1).broadcast_to([128, 4, 2, 64])
        sb = sn[:, t % 4, 16:80].unsqueeze(1).unsqueeze(1).broadcast_to([128, 4, 2, 64])
        o4 = rk3[:].rearrange("p (h t d) -> p h t d", h=4, t=2)
        rope(o4, z4, cb, sb, rk1[:].rearrange("p (h t d) -> p h t d", h=4, t=2),
             rk2[:].rearrange("p (h t d) -> p h t d", h=4, t=2), "pool",
             [("pz", 2), f"rtA{g % 2}sin", f"rtA{g % 2}cos"], ["rk3"], "rk")
        c.op("pool", lambda e: e.tensor_tensor(out=kp[t % 2][:], in0=rk3[:].rearrange("p (h d) -> p h d", h=4),
                                               in1=kdecA[:], op=ALU.mult), reads=["rk3", "kdecA"],
             writes=[c.W("kp", t, 2)])
        c.W("vb", t, 2)
        for hh in range(2):
            c.op("act", lambda e: e.activation(out=vb[t % 2][:, hh * 512:(hh + 1) * 512], in_=PS[3 + hh][:],
                                               func=AF.Copy), reads=[("pz", 3 + hh)], writes=[("vb", t % 2)])

    import os
    _lv = int(os.environ.get("DBG_LV", 9))

    def A_s2(t):
        pt = PS[5][:].bitcast(BF16)
        for k in range(2):
            c.op("pe", lambda e: e.transpose(out=pt[:, k * 128:(k + 1) * 128], in_=ckvs[t % 2][:, k * 128:(k + 1) * 128],
                                             identity=identb[:]), reads=[c.R("ckvs", t, 2), "const"], writes=["ptA"])
        c.op("pe", lambda e: e.transpose(out=pt[0:96, 256:384], in_=kst[t % 2][:], identity=identb[:]),
             reads=[c.R("kst", t, 2), "const"], writes=["ptA"])
        if _lv < 2:
            return
        for k in range(2):
            c.op("act", lambda e: e.activation(out=ckvnT[:, k, t * 128:(t + 1) * 128],
                                               in_=pt[:, k * 128:(k + 1) * 128], func=AF.Copy),
                 reads=["ptA"], writes=["ckvnT"])
        if _lv < 3:
            return
        _kt = os.environ.get("DBG_KT", "both")
        if _kt in ("both", "act"):
            c.op("act", lambda e: e.activation(out=KT[0][64:96, t * 128:(t + 1) * 128], in_=pt[64:96, 256:384],
                                               func=AF.Copy), reads=["ptA"], writes=["KT0r"])
        if _kt in ("both", "dve"):
            c.op("act", lambda e: e.activation(out=KT[1][64:96, t * 128:(t + 1) * 128], in_=pt[64:96, 256:384],
                                               func=AF.Copy), reads=["ptA"], writes=["KT1r"])
        if _lv < 4:
            return
        pkv = [PS[6], PS[7]]
        for h in range(4):
            c.op("pe", lambda e: e.matmul(pkv[h // 2][:, (h % 2) * 256:(h % 2 + 1) * 256], lhsT=kp[t % 2][:, h, :],
                                          rhs=vb[t % 2][:, h * 256:(h + 1) * 256], start=True, stop=True),
                 reads=[c.R("kp", t, 2), c.R("vb", t, 2)], writes=["pkv"])
        if _lv < 5:
            return
        l, g = t % 4, t // 4
        so = Sown[g % 2]
        if l == 0:
            c.W("Sown", g, 2)
        for h in range(4):
            hs = slice(h * 256, (h + 1) * 256)
            pk = pkv[h // 2][:, (h % 2) * 256:(h % 2 + 1) * 256]
            if l == 0:
                c.op("pool", lambda e: e.tensor_scalar(out=so[:, hs], in0=Sst[:, hs], scalar1=rcoef[:, h * 4:h * 4 + 1],
                                                       scalar2=None, op0=ALU.mult), reads=["Sst", "const"],
                     writes=[("Sown", g % 2)])
            if l < 3:
                c.op("dve", lambda e: e.scalar_tensor_tensor(out=so[:, hs], in0=pk, scalar=rcoef[:, h * 4 + 1 + l:h * 4 + 2 + l],
                                                             in1=so[:, hs], op0=ALU.mult, op1=ALU.add),
                     reads=["pkv", ("Sown", g % 2), "const"], writes=[("Sown", g % 2)])
            c.op("dve", lambda e: e.scalar_tensor_tensor(out=Sst[:, hs], in0=Sst[:, hs], scalar=GAMMA[h] ** 128, in1=pk,
                                                         op0=ALU.mult, op1=ALU.add), reads=["pkv", "Sst"], writes=["Sst"])
        if l == 3 and _lv >= 6:
            c.op("act", lambda e: e.activation(out=Sownb[g % 2][:], in_=so[:], func=AF.Copy),
                 reads=[c.R("Sown", g, 2)], writes=[c.W("Sownb", g, 2)])
            c.dma("sp", sown_d[g], Sownb[g % 2][:], reads=[c.R("Sownb", g, 2)], writes=["sown_d"], key=f"so{g % 2}")

    import os
    _na = int(os.environ.get("DBG_NA", NT))
    _ns = int(os.environ.get("DBG_NS", 3))
    pipeline(_na, [A_s0, A_s1, A_s2][:_ns])
    print("sbuf remaining in pass A:", nc.sbuf_bytes_remaining, "ops", c.nops)
    dbg("Sst", Sst[:], [128, 1024], F32)
    c.pop()
    dbg("ckvnT", ckvnT[:], [128, 2, S], BF16)
    dbg("KT0", KT[0][:], [128, S], BF16)
    if stop == "A":
        return finish()

    QT = c.sbuf("QT", [128, 8, NO * 128], BF16)
    c.push()
    PS = [c.psum(f"pq_{i}", [128, 512], F32) for i in range(8)]
    WQ = c.sbuf("WQ", [128, 8, 384], BF16)
    load_w(WQ[:], w_in[:, O_CQ:O_CQ + 384].rearrange("(k p) n -> p k n", p=128), "WQ", "wQ0")
    wuq = c.sbuf("wuq", [128, 3, 768], BF16)
    c.push()
    wuq_f = c.sbuf("wuq_f", [128, 3, 768], F32)
    gq = c.sbuf("gq", [128, 3], F32)
    c.dma("sp", wuq_f[:], w_uq.rearrange("(k p) n -> p k n", p=128), writes=["wuq_f"], key="wQ1")
    c.dma("sp", gq[:], gcq, writes=["gq"], key="wQ2")
    for k in range(3):
        wv = wuq_f[:, k, :].rearrange("p (h d) -> p h d", h=8)
        c.op("dve", lambda e: e.tensor_scalar(out=wuq[:, k, 0:512].rearrange("p (h d) -> p h d", h=8), in0=wv[:, :, 0:64],
                                              scalar1=gq[:, k:k + 1], scalar2=None, op0=ALU.mult),
             reads=["wuq_f", "gq"], writes=["wuq"])
        c.op("dve", lambda e: e.tensor_scalar(out=wuq[:, k, 512:768].rearrange("p (h d) -> p h d", h=8), in0=wv[:, :, 64:96],
                                              scalar1=gq[:, k:k + 1], scalar2=None, op0=ALU.mult),
             reads=["wuq_f", "gq"], writes=["wuq"])
    c.pop()
    ht = HT(xown, 3, A1, B1, PS[0])
    sinQ = c.sbuf("sinQ", [128, NO, 16], F32)
    cosQ = c.sbuf("cosQ", [128, NO, 16], F32)
    c.push()
    rtmpQ = [c.sbuf("rtmpQ", [128, NO, 16], F32) for _ in range(3)]
    rope_tables(posf[:, NT:NT + NO], NO, sinQ, cosQ, rtmpQ, 0, 16, "rtQ")
    c.pop()
    stQ = c.sbuf("stQ", [128, 3 * 64], F32)
    cqs = [c.sbuf("cqs", [128, 384], BF16) for _ in range(2)]
    cqnT = [c.sbuf("cqnT", [128, 3, 128], BF16) for _ in range(2)]
    qsb = [c.sbuf("qsb", [128, 8, 96], BF16) for _ in range(2)]
    qr1 = c.sbuf("qr1", [128, 8, 32], F32)
    qr2 = c.sbuf("qr2", [128, 8, 32], F32)

    def Q_s0(t):
        if t == 0:
            ht.load(0)
            ht.load(1)
        if t + 2 < NO:
            ht.load(t + 2)
        ht.norm(t)
        ht.transpose(t)

    def Q_s1(t):
        hT, hk = ht.get(t)
        for k in range(8):
            c.op("pe", lambda e: e.matmul(PS[1][:, 0:384], lhsT=hT[:, k, :], rhs=WQ[:, k, :], start=(k == 0),
                                          stop=(k == 7)), reads=[hk, "WQ"], writes=["pcq"])
        m = t
        a, b, r = stQ[:, m:m + 1], stQ[:, 64 + m:65 + m], stQ[:, 128 + m:129 + m]
        c.op("act", lambda e: e.activation(out=junk[:, 0:384], in_=PS[1][:, 0:384], func=AF.Square, accum_out=a),
             reads=["pcq"], writes=["junk", ("sQa", m)])
        c.op("dve", lambda e: e.tensor_scalar(out=b, in0=a, scalar1=1.0 / 384, scalar2=RMS_EPS, op0=ALU.mult,
                                              op1=ALU.add), reads=[("sQa", m)], writes=[("sQb", m)])
        rsqrt_cols(b, r, 1, [("sQb", m)], [("sQr", m)])
        c.op("dve", lambda e: e.tensor_scalar(out=cqs[t % 2][:], in0=PS[1][:, 0:384], scalar1=r, scalar2=None,
                                              op0=ALU.mult), reads=["pcq", ("sQr", m)], writes=[c.W("cqs", t, 2)])
        pt = PS[2][:].bitcast(BF16)
        for k in range(3):
            c.op("pe", lambda e: e.transpose(out=pt[:, k * 128:(k + 1) * 128], in_=cqs[t % 2][:, k * 128:(k + 1) * 128],
                                             identity=identb[:]), reads=[c.R("cqs", t, 2), "const"], writes=["ptQ"])
        c.op("act", lambda e: e.activation(out=cqnT[t % 2][:], in_=pt[:, 0:384].rearrange("p (k n) -> p k n", k=3),
                                           func=AF.Copy), reads=["ptQ"], writes=[c.W("cqnT", t, 2)])

    def Q_s2(t):
        for (pi, lo, n) in [(3, 0, 512), (4, 512, 256)]:
            for k in range(3):
                c.op("pe", lambda e: e.matmul(PS[pi][:, 0:n], lhsT=cqnT[t % 2][:, k, :], rhs=wuq[:, k, lo:lo + n],
                                              start=(k == 0), stop=(k == 2)),
                     reads=[c.R("cqnT", t, 2), "wuq"], writes=[("pq", pi)])
        c.W("qsb", t, 2)
        _ql = int(os.environ.get("DBG_QL", 9))
        c.op("act", lambda e: e.activation(out=qsb[t % 2][:, :, 0:64], in_=PS[3][:].rearrange("p (h d) -> p h d", h=8),
                                           func=AF.Copy), reads=[("pq", 3)], writes=[("qsb", t % 2)])
        z4 = PS[4][:, 0:256].rearrange("p (h t d) -> p h t d", h=8, t=2)
        cb = cosQ[:, t, 0:16].unsqueeze(1).unsqueeze(1).broadcast_to([128, 8, 2, 16])
        sb = sinQ[:, t, 0:16].unsqueeze(1).unsqueeze(1).broadcast_to([128, 8, 2, 16])
        o4 = qsb[t % 2][:, :, 64:96].rearrange("p h (t d) -> p h t d", t=2)
        rope(o4, z4, cb, sb, qr1[:].rearrange("p h (t d) -> p h t d", t=2),
             qr2[:].rearrange("p h (t d) -> p h t d", t=2), "pool",
             [("pq", 4), "rtQsin", "rtQcos"], [("qsb", t % 2)], "qr")
        if _ql < 3:
            return
        pt = PS[5][:].bitcast(BF16)
        for h in range(8):
            c.op("pe", lambda e: e.transpose(out=pt[0:96, h * 128:(h + 1) * 128], in_=qsb[t % 2][:, h, :],
                                             identity=identb[:]), reads=[("qsb", t % 2), "const"], writes=["ptQ2"])
        if _ql < 4:
            return
        c.op("act", lambda e: e.activation(out=QT[0:96, :, t * 128:(t + 1) * 128],
                                           in_=pt[0:96, :].rearrange("p (h n) -> p h n", h=8), func=AF.Copy),
             reads=["ptQ2"], writes=["QT"])

    pipeline(int(os.environ.get("DBG_NQ", NO)), [Q_s0, Q_s1, Q_s2][:int(os.environ.get("DBG_QS", 3))])
    print("sbuf remaining in pass Q:", nc.sbuf_bytes_remaining, "ops", c.nops)
    c.pop()
    dbg("QT", QT[:], [128, 8, NO * 128], BF16)
    if stop == "Q":
        return finish()

    c.push()
    PS = [c.psum(f"pt_{i}", [128, 512], F32) for i in range(8)]
    wukv = c.sbuf("wukv", [128, 2, 1024], BF16)
    c.push()
    wukv_f = c.sbuf("wukv_f", [128, 2, 1024], F32)
    gkv = c.sbuf("gkv", [128, 2], F32)
    c.dma("sp", wukv_f[:], w_ukv.rearrange("(k p) n -> p k n", p=128), writes=["wukv_f"], key="wT0")
    c.dma("sp", gkv[:], gckv, writes=["gkv"], key="wT1")
    for k in range(2):
        c.op("dve", lambda e: e.tensor_scalar(out=wukv[:, k, :], in0=wukv_f[:, k, :], scalar1=gkv[:, k:k + 1],
                                              scalar2=None, op0=ALU.mult), reads=["wukv_f", "gkv"], writes=["wukv"])
    c.pop()
    otb = [c.sbuf("otb", [64, 512], BF16) for _ in range(2)]
    Vb = [c.sbuf("Vb", [128, NT, 65], BF16) for _ in range(2)]
    for i in range(2):
        c.op("pool", lambda e: e.memset(Vb[i][:, :, 64:65], 1.0), writes=[("Vb1", i)])
    PT = [c.sbuf("PT", [128, 512], BF16) for _ in range(4)]
    osb = [c.sbuf("osb", [65, 512], F32) for _ in range(2)]
    rec = [c.sbuf("rec", [64, 512], F32) for _ in range(2)]
    fin = [0]

    def up_units(h):
        kt_buf, v_buf = KT[h % 2], Vb[h % 2]
        units = []

        def k_unit(kc):
            def f():
                pk = PS[kc % 2]
                for k in range(2):
                    c.op("pe", lambda e: e.matmul(pk[0:64, :], lhsT=wukv[:, k, h * 128:h * 128 + 64],
                                                  rhs=ckvnT[:, k, kc * 512:(kc + 1) * 512], start=(k == 0), stop=(k == 1)),
                         reads=["wukv", "ckvnT"], writes=[("pk", kc % 2)])
                c.op("dve", lambda e: e.tensor_copy(out=kt_buf[0:64, kc * 512:(kc + 1) * 512], in_=pk[0:64, :]),
                     reads=[("pk", kc % 2)], writes=[("KTn", h % 2)])
            return f

        def v_unit(kb):
            def f():
                pv = PS[kb % 2]
                for j8 in range(8):
                    kt = kb * 8 + j8
                    for k in range(2):
                        c.op("pe", lambda e: e.matmul(pv[:, j8 * 64:(j8 + 1) * 64], lhsT=ckvnT[:, k, kt * 128:(kt + 1) * 128],
                                                      rhs=wukv[:, k, h * 128 + 64:h * 128 + 128], start=(k == 0),
                                                      stop=(k == 1)), reads=["wukv", "ckvnT"], writes=[("pk", kb % 2)])
                c.op("dve", lambda e: e.tensor_copy(out=v_buf[:, kb * 8:(kb + 1) * 8, 0:64],
                                                    in_=pv[:].rearrange("p (j d) -> p j d", j=8)),
                     reads=[("pk", kb % 2)], writes=[("Vb", h % 2)])
            return f

        for kc in range(16):
            units.append(k_unit(kc))
        for kb in range(8):
            units.append(v_unit(kb))
        return units

    steps = [(h, qc, kt) for h in range(8) for qc in range(4) for kt in range(16 * qc + 16)]
    pending = {}
    c.W("KTn", 0, 2)
    c.W("Vb", 0, 2)
    for u in up_units(0):
        u()

    def att_s0(i):
        h, qc, kt = steps[i]
        kt_buf = KT[h % 2]
        if qc == 0 and kt == 0 and h + 1 < 8:
            pending["units"] = up_units(h + 1)
            pending["local"] = 0
            pending["armed"] = False
        if pending.get("units"):
            pending["local"] += 1
            if pending["local"] >= 4 and (pending["local"] - 4) % 6 == 0:
                if not pending["armed"]:
                    c.W("KTn", h + 1, 2)
                    c.W("Vb", h + 1, 2)
                    pending["armed"] = True
                pending["units"].pop(0)()
        gk, l = kt // 4, kt % 4
        c0 = (max(gk, 4 * qc) - 4 * qc) * 128
        ps, pt_ = PS[2 + i % 4], PT[i % 4]
        c.op("pe", lambda e: e.matmul(ps[:, c0:512], lhsT=kt_buf[0:96, kt * 128:(kt + 1) * 128],
                                      rhs=QT[0:96, h, qc * 512 + c0:(qc + 1) * 512], start=True, stop=True),
             reads=[("KTn", h % 2), f"KT{h % 2}r", "QT"], writes=[("ps", i % 4)])
        c.op("act", lambda e: e.activation(out=pt_[:, c0:512], in_=ps[:, c0:512], func=AF.Exp, scale=SCALE_MLA),
             reads=[("ps", i % 4)], writes=[("PT", i % 4)])
        if gk >= 4 * qc:
            c.op("pool", lambda e: e.tensor_tensor(out=pt_[:, c0:c0 + 128], in0=pt_[:, c0:c0 + 128],
                                                   in1=amask[:, l, :], op=ALU.mult),
                 reads=[("PT", i % 4), "const"], writes=[("PT", i % 4)])

    def att_s1(i):
        pass

    def att_s2(i):
        h, qc, kt = steps[i]
        v_buf = Vb[h % 2]
        nk = 16 * qc + 16
        gk = kt // 4
        c0 = (max(gk, 4 * qc) - 4 * qc) * 128
        po = PS[6 + qc % 2]
        pt_ = PT[i % 4]
        c.op("pe", lambda e: e.matmul(po[0:65, c0:512], lhsT=v_buf[:, kt, 0:65], rhs=pt_[:, c0:512],
                                      start=(kt == 0), stop=(kt == nk - 1)),
             reads=[("Vb", h % 2), ("Vb1", h % 2), ("PT", i % 4)], writes=[("po", qc % 2)])
        if kt == nk - 1:
            f = fin[0]
            fin[0] += 1
            ob, rc = osb[f % 2], rec[f % 2]
            c.op("dve", lambda e: e.tensor_copy(out=ob[:], in_=po[0:65, :]), reads=[("po", qc % 2)],
                 writes=[("osb", f % 2)])
            pd = PS[f % 2]
            c.op("pe", lambda e: e.matmul(pd[0:64, :], lhsT=onesf[64:65, 0:64], rhs=ob[64:65, :], start=True, stop=True),
                 reads=[("osb", f % 2), "const"], writes=[("pk", f % 2)])
            c.op("dve", lambda e: e.reciprocal(out=rc[:], in_=pd[0:64, :]), reads=[("pk", f % 2)], writes=[("rec", f % 2)])
            c.op("dve", lambda e: e.tensor_tensor(out=otb[f % 2][:], in0=ob[0:64, :], in1=rc[:],
                                                  op=ALU.mult), reads=[("osb", f % 2), ("rec", f % 2)], writes=[("otb", f % 2)])
            c.dma("sp", ot_d[h, :, qc * 512:(qc + 1) * 512], otb[f % 2][:], reads=[("otb", f % 2)], writes=["ot_d"],
                  key=f"otw{f % 2}")

    pipeline(len(steps), [att_s0, att_s1, att_s2])
    c.pop()
    c.pop()
    if stop == "T":
        return finish()

    c.push()
    PS = [c.psum(f"pc_{i}", [128, 512], F32) for i in range(8)]
    WC = c.sbuf("WC", [128, 8, 3072], BF16)
    load_w(WC[:, :, 0:1024], w_in[:, O_RQ:O_RQ + 1024].rearrange("(k p) n -> p k n", p=128), "WC", "wC0")
    load_w(WC[:, :, 1024:2048], w_in[:, O_RV:O_RV + 1024].rearrange("(k p) n -> p k n", p=128), "WC", "wC1")
    load_w(WC[:, :, 2048:3072], w_in[:, O_RG:O_RG + 1024].rearrange("(k p) n -> p k n", p=128), "WC", "wC2")
    wor = c.sbuf("wor", [128, 8, 1024], BF16)
    c.push()
    wor_f = c.sbuf("wor_f", [128, 8, 1024], F32)
    gr = c.sbuf("gr", [128, 8], F32)
    c.dma("sp", wor_f[:], w_o_ret.rearrange("(k p) n -> p k n", p=128), writes=["wor_f"], key="wC3")
    c.dma("sp", gr[:], gret, writes=["gr"], key="wC4")
    for k in range(8):
        c.op("dve", lambda e: e.tensor_scalar(out=wor[:, k, :], in0=wor_f[:, k, :], scalar1=gr[:, k:k + 1],
                                              scalar2=None, op0=ALU.mult), reads=["wor_f", "gr"], writes=["wor"])
    c.pop()
    kdecC = c.sbuf("kdecC", [128, 8, 128], F32)
    c.dma("sp", kdecC[:, 0:4, :], qdec_d, writes=["kdecC"], key="wC5")
    c.dma("sp", kdecC[:, 4:8, :], kdecC_d, writes=["kdecC"], key="wC6")
    ht = HT(xown, 3, A1, B1, PS[0])
    sinC = c.sbuf("sinC", [128, NO, 80], F32)
    cosC = c.sbuf("cosC", [128, NO, 80], F32)
    c.push()
    rtmpC = [c.sbuf("rtmpC", [128, NO, 64], F32) for _ in range(3)]
    rope_tables(posf[:, NT:NT + NO], NO, sinC, cosC, rtmpC, 16, 80, "rtC")
    c.pop()
    qk1 = c.sbuf("qk1", [128, 1024], F32)
    qk2 = c.sbuf("qk2", [128, 1024], F32)
    qk3 = c.sbuf("qk3", [128, 1024], F32)
    qkp = [c.sbuf("qkp", [128, 8, 128], BF16) for _ in range(2)]
    qkT = [c.sbuf("qkT", [128, 8, 128], BF16) for _ in range(2)]
    vbc = [c.sbuf("vbc", [128, 1024], BF16) for _ in range(2)]
    sg = [c.sbuf("sg", [128, 1024], BF16) for _ in range(2)]
    scT = [c.sbuf("scT", [128, 4, 128], BF16) for _ in range(2)]
    sob = [c.sbuf("sob", [128, 1024], BF16) for _ in range(2)]
    bnst = c.sbuf("bnst", [128, NO, 4, 6], F32)
    bnag = c.sbuf("bnag", [128, NO, 4, 2], F32)
    bnr = c.sbuf("bnr", [128, NO, 4, 2], F32)
    onr = [c.sbuf("onr", [128, 1024], F32) for _ in range(2)]
    gat = [c.sbuf("gat", [128, 1024], BF16) for _ in range(2)]
    gT = [c.sbuf("gT", [128, 8, 128], BF16) for _ in range(2)]
    bbt = [c.sbuf("bbt", [128, 1024], BF16) for _ in range(2)]

    def C1_s0(t):
        if t == 0:
            ht.load(0)
            ht.load(1)
        if t + 2 < NO:
            ht.load(t + 2)
        c.dma("sp", sob[t % 2][:], sown_d[t], reads=[], writes=[c.W("sob", t, 2)], key=f"sob{t % 2}")
        ht.norm(t)
        ht.transpose(t)

    def C1_s1(t):
        hT, hk = ht.get(t)
        for (pi, lo) in [(1, 0), (2, 512), (3, 1024), (4, 1536), (5, 2048), (6, 2560)]:
            for k in range(8):
                c.op("pe", lambda e: e.matmul(PS[pi][:], lhsT=hT[:, k, :], rhs=WC[:, k, lo:lo + 512], start=(k == 0),
                                              stop=(k == 7)), reads=[hk, "WC"], writes=[("pz", pi)])
        cb = cosC[:, t, 16:80].unsqueeze(1).unsqueeze(1).broadcast_to([128, 4, 2, 64])
        sb = sinC[:, t, 16:80].unsqueeze(1).unsqueeze(1).broadcast_to([128, 4, 2, 64])
        for j in range(2):
            z4 = PS[1 + j][:].rearrange("p (h t d) -> p h t d", h=4, t=2)
            sl = slice(j * 512, (j + 1) * 512)
            rope(qk3[:, sl].rearrange("p (h t d) -> p h t d", h=4, t=2), z4, cb, sb,
                 qk1[:, sl].rearrange("p (h t d) -> p h t d", h=4, t=2),
                 qk2[:, sl].rearrange("p (h t d) -> p h t d", h=4, t=2), "pool",
                 [("pz", 1 + j), "rtCsin", "rtCcos"], [("qk3", j)], f"qk{j}")
        c.op("pool", lambda e: e.tensor_tensor(out=qkp[t % 2][:], in0=qk3[:].rearrange("p (h d) -> p h d", h=8),
                                               in1=kdecC[:], op=ALU.mult), reads=[("qk3", 0), ("qk3", 1), "kdecC"],
             writes=[c.W("qkp", t, 2)])
        c.W("vbc", t, 2)
        c.W("sg", t, 2)
        for hh in range(2):
            c.op("act", lambda e: e.activation(out=vbc[t % 2][:, hh * 512:(hh + 1) * 512], in_=PS[3 + hh][:],
                                               func=AF.Copy), reads=[("pz", 3 + hh)], writes=[("vbc", t % 2)])
            c.op("act", lambda e: e.activation(out=sg[t % 2][:, hh * 512:(hh + 1) * 512], in_=PS[5 + hh][:],
                                               func=AF.Silu), reads=[("pz", 5 + hh)], writes=[("sg", t % 2)])

    def C1_s2(t):
        pt = PS[7][:].bitcast(BF16)
        for j in range(8):
            c.op("pe", lambda e: e.transpose(out=pt[:, j * 128:(j + 1) * 128], in_=qkp[t % 2][:, j, :],
                                             identity=identb[:]), reads=[c.R("qkp", t, 2), "const"], writes=["ptC"])
        c.op("act", lambda e: e.activation(out=qkT[t % 2][:], in_=pt.rearrange("p (j n) -> p j n", j=8), func=AF.Copy),
             reads=["ptC"], writes=[c.W("qkT", t, 2)])
        for h in range(4):
            c.op("pe", lambda e: e.matmul(PS[1][:, h * 128:(h + 1) * 128], lhsT=qkT[t % 2][:, 4 + h, :],
                                          rhs=qkT[t % 2][:, h, :], start=True, stop=True),
                 reads=[c.R("qkT", t, 2)], writes=[("pz", 1)])
        c.op("dve", lambda e: e.tensor_tensor(out=scT[t % 2][:], in0=PS[1][:].rearrange("p (h n) -> p h n", h=4),
                                              in1=tri[:].unsqueeze(1).broadcast_to([128, 4, 128]), op=ALU.mult),
             reads=[("pz", 1), "const"], writes=[c.W("scT", t, 2)])
        for h in range(4):
            po = PS[3 + h // 2][:, (h % 2) * 256:(h % 2 + 1) * 256]
            c.op("pe", lambda e: e.matmul(po, lhsT=scT[t % 2][:, h, :], rhs=vbc[t % 2][:, h * 256:(h + 1) * 256],
                                          start=True, stop=False), reads=[c.R("scT", t, 2), c.R("vbc", t, 2)],
                 writes=[("pz", 3 + h // 2)])
            c.op("pe", lambda e: e.matmul(po, lhsT=qkT[t % 2][:, h, :], rhs=sob[t % 2][:, h * 256:(h + 1) * 256],
                                          start=False, stop=True), reads=[c.R("qkT", t, 2), c.R("sob", t, 2)],
                 writes=[("pz", 3 + h // 2)])
        for h in range(4):
            po = PS[3 + h // 2][:, (h % 2) * 256:(h % 2 + 1) * 256]
            c.op("dve", lambda e: e.bn_stats(out=bnst[:, t, h, :], in_=po), reads=[("pz", 3 + h // 2)],
                 writes=[("bnst", t)])
            c.op("dve", lambda e: e.bn_aggr(out=bnag[:, t, h, :], in_=bnst[:, t, h, :]), reads=[("bnst", t)],
                 writes=[("bnag", t)])
        c.op("dve", lambda e: e.tensor_scalar(out=bnr[:, t, :, 0], in0=bnag[:, t, :, 1], scalar1=GN_EPS, scalar2=None,
                                              op0=ALU.add), reads=[("bnag", t)], writes=[("bnr0", t)])
        rsqrt_cols(bnr[:, t, :, 0], bnr[:, t, :, 1], 4, [("bnr0", t)], [("bnr1", t)])
        c.W("onr", t, 2)
        for h in range(4):
            po = PS[3 + h // 2][:, (h % 2) * 256:(h % 2 + 1) * 256]
            c.op("dve", lambda e: e.tensor_scalar(out=onr[t % 2][:, h * 256:(h + 1) * 256], in0=po,
                                                  scalar1=bnag[:, t, h, 0:1], scalar2=bnr[:, t, h, 1:2],
                                                  op0=ALU.subtract, op1=ALU.mult),
                 reads=[("pz", 3 + h // 2), ("bnag", t), ("bnr1", t)], writes=[("onr", t % 2)])
        c.op("pool", lambda e: e.tensor_tensor(out=gat[t % 2][:], in0=onr[t % 2][:], in1=sg[t % 2][:], op=ALU.mult),
             reads=[("onr", t % 2), c.R("sg", t, 2)], writes=[c.W("gat", t, 2)])

    def C1_s3(t):
        pt = PS[7][:].bitcast(BF16)
        for k in range(8):
            c.op("pe", lambda e: e.transpose(out=pt[:, k * 128:(k + 1) * 128], in_=gat[t % 2][:, k * 128:(k + 1) * 128],
                                             identity=identb[:]), reads=[c.R("gat", t, 2), "const"], writes=["ptC"])
        c.op("act", lambda e: e.activation(out=gT[t % 2][:], in_=pt.rearrange("p (j n) -> p j n", j=8), func=AF.Copy),
             reads=["ptC"], writes=[c.W("gT", t, 2)])
        c.W("bbt", t, 2)
        for hh in range(2):
            for k in range(8):
                c.op("pe", lambda e: e.matmul(PS[5 + hh][:], lhsT=gT[t % 2][:, k, :], rhs=wor[:, k, hh * 512:(hh + 1) * 512],
                                              start=(k == 0), stop=(k == 7)), reads=[c.R("gT", t, 2), "wor"],
                     writes=[("pz", 5 + hh)])
            c.op("act", lambda e: e.activation(out=bbt[t % 2][:, hh * 512:(hh + 1) * 512], in_=PS[5 + hh][:],
                                               func=AF.Copy), reads=[("pz", 5 + hh)], writes=[("bbt", t % 2)])
        c.dma("sp", bb_d[t], bbt[t % 2][:], reads=[("bbt", t % 2)], writes=["bb_d"], key=f"bbw{t % 2}")

    def C1_s123(t):
        C1_s1(t)
        C1_s2(t)
        C1_s3(t)

    pipeline(NO, [C1_s0, C1_s123])
    print("sbuf remaining in pass C1:", nc.sbuf_bytes_remaining, "ops", c.nops)
    c.pop()
    if stop == "C1":
        return finish()

    h2T = c.sbuf("h2T", [128, 8, NO * 128], BF16)
    print("sbuf remaining before C2:", nc.sbuf_bytes_remaining)
    c.push()
    PS = [c.psum(f"pd_{i}", [128, 512], F32) for i in range(8)]
    WG = c.sbuf("WG", [128, 8, 2048], BF16)
    load_w(WG[:], w_in[:, O_GA:O_GA + 2048].rearrange("(k p) n -> p k n", p=128), "WG", "wD0")
    wom = c.sbuf("wom", [64, 8, 1024], BF16)
    load_w(wom[:], w_o_mla.rearrange("(h p) n -> p h n", p=64), "wom", "wD1")
    wout = c.sbuf("wout", [128, 8, 1024], BF16)
    load_w(wout[:], w_out.rearrange("(k p) n -> p k n", p=128), "wout", "wD2")
    wr = c.sbuf("wr", [128, 8, 64], F32)
    c.dma("sp", wr[:], w_router.rearrange("(k p) n -> p k n", p=128), writes=["wr"], key="wD3")
    ht = HT(xown, 3, A1, B1, PS[0])
    tg = [c.sbuf("tg", [128, 2048], BF16) for _ in range(2)]
    ott = [c.sbuf("ott", [64, 8, 128], BF16) for _ in range(2)]
    bbr = [c.sbuf("bbr", [128, 1024], BF16) for _ in range(2)]
    m1 = c.sbuf("m1", [128, 1024], F32)
    m2 = c.sbuf("m2", [128, 1024], F32)
    mg = [c.sbuf("mg", [128, 1024], BF16) for _ in range(2)]
    mgT = [c.sbuf("mgT", [128, 8, 128], BF16) for _ in range(2)]
    ty = c.sbuf("ty", [128, 1024], F32)
    x1 = [c.sbuf("x1", [128, 1024], F32) for _ in range(2)]
    xs2 = [c.sbuf("xs2", [128, 1024], F32) for _ in range(2)]
    h2f = [c.sbuf("h2f", [128, 8, 128], F32) for _ in range(2)]
    st2 = c.sbuf("st2", [128, 3 * 64], F32)
    rt = c.sbuf("rt", [128, 2, 64 * 4 + 64 + 8 * 4], F32)

    def C2_s0(t):
        if t == 0:
            ht.load(0)
            ht.load(1)
        if t + 2 < NO:
            ht.load(t + 2)
        c.dma("sp", bbr[t % 2][:], bb_d[t], reads=["bb_d"], writes=[c.W("bbr", t, 2)], key=f"bbr{t % 2}")
        c.dma("sp", ott[t % 2][:], ot_d[:, :, t * 128:(t + 1) * 128].rearrange("h p n -> p h n"), reads=["ot_d"],
              writes=[c.W("ott", t, 2)], key=f"ott{t % 2}")
        ht.norm(t)
        ht.transpose(t)

    def C2_s1(t):
        hT, hk = ht.get(t)
        xt = ht.xt[t % 3]
        c.W("tg", t, 2)
        for j in range(4):
            for k in range(8):
                c.op("pe", lambda e: e.matmul(PS[1 + j][:], lhsT=hT[:, k, :], rhs=WG[:, k, j * 512:(j + 1) * 512],
                                              start=(k == 0), stop=(k == 7)), reads=[hk, "WG"], writes=[("pz", 1 + j)])
            c.op("act", lambda e: e.activation(out=tg[t % 2][:, j * 512:(j + 1) * 512], in_=PS[1 + j][:], func=AF.Tanh,
                                               scale=0.5), reads=[("pz", 1 + j)], writes=[("tg", t % 2)])
        for hh in range(2):
            for h in range(8):
                c.op("pe", lambda e: e.matmul(PS[5 + hh][:], lhsT=ott[t % 2][:, h, :],
                                              rhs=wom[:, h, hh * 512:(hh + 1) * 512], start=(h == 0), stop=(h == 7)),
                     reads=[c.R("ott", t, 2), "wom"], writes=[("pz", 5 + hh)])
            sl = slice(hh * 512, (hh + 1) * 512)
            c.op("dve", lambda e: e.scalar_tensor_tensor(out=m1[:, sl], in0=tg[t % 2][:, sl], scalar=1.0, in1=PS[5 + hh][:],
                                                         op0=ALU.add, op1=ALU.mult),
                 reads=[("tg", t % 2), ("pz", 5 + hh)], writes=[("m1", hh)])
        c.op("dve", lambda e: e.scalar_tensor_tensor(out=m2[:], in0=tg[t % 2][:, 1024:2048], scalar=1.0, in1=bbr[t % 2][:],
                                                     op0=ALU.add, op1=ALU.mult),
             reads=[("tg", t % 2), c.R("bbr", t, 2)], writes=["m2"])
        c.op("pool", lambda e: e.tensor_tensor(out=mg[t % 2][:], in0=m1[:], in1=m2[:], op=ALU.add),
             reads=[("m1", 0), ("m1", 1), "m2"], writes=[c.W("mg", t, 2)])
        pt = PS[7][:].bitcast(BF16)
        for k in range(8):
            c.op("pe", lambda e: e.transpose(out=pt[:, k * 128:(k + 1) * 128], in_=mg[t % 2][:, k * 128:(k + 1) * 128],
                                             identity=identb[:]), reads=[c.R("mg", t, 2), "const"], writes=["ptD"])
        c.op("act", lambda e: e.activation(out=mgT[t % 2][:], in_=pt.rearrange("p (j n) -> p j n", j=8), func=AF.Copy),
             reads=["ptD"], writes=[c.W("mgT", t, 2)])
        c.W("x1", t, 2)
        for hh in range(2):
            sl = slice(hh * 512, (hh + 1) * 512)
            for k in range(8):
                c.op("pe", lambda e: e.matmul(PS[1 + hh][:], lhsT=mgT[t % 2][:, k, :], rhs=wout[:, k, sl],
                                              start=(k == 0), stop=(k == 7)), reads=[c.R("mgT", t, 2), "wout"],
                     writes=[("pz", 1 + hh)])
            c.op("dve", lambda e: e.tensor_tensor(out=ty[:, sl], in0=PS[1 + hh][:], in1=GT1[:, sl], op=ALU.mult),
                 reads=[("pz", 1 + hh), "GT"], writes=[("ty", hh)])
            c.op("pool", lambda e: e.tensor_tensor(out=x1[t % 2][:, sl], in0=ty[:, sl], in1=xt[:, sl], op=ALU.add),
                 reads=[("ty", hh), c.R("xt", t, 3)], writes=[("x1", t % 2)])
        c.dma("sp", x1_d[t], x1[t % 2][:], reads=[("x1", t % 2)], writes=["x1_d"], key=f"x1w{t % 2}")
        m = t
        a, b, r = st2[:, m:m + 1], st2[:, 64 + m:65 + m], st2[:, 128 + m:129 + m]
        c.op("act", lambda e: e.activation(out=junk[:], in_=x1[t % 2][:], func=AF.Square, accum_out=a),
             reads=[("x1", t % 2)], writes=["junk", ("s2a", m)])
        c.op("dve", lambda e: e.tensor_scalar(out=b, in0=a, scalar1=1.0 / D, scalar2=RMS_EPS, op0=ALU.mult, op1=ALU.add),
             reads=[("s2a", m)], writes=[("s2b", m)])
        rsqrt_cols(b, r, 1, [("s2b", m)], [("s2r", m)])
        c.op("dve", lambda e: e.tensor_scalar(out=xs2[t % 2][:], in0=x1[t % 2][:], scalar1=r, scalar2=None, op0=ALU.mult),
             reads=[("x1", t % 2), ("s2r", m)], writes=[c.W("xs2", t, 2)])

    def C2_s2(t):
        c.W("h2f", t, 2)
        for k in range(8):
            pb = PS[3 + k // 4][:, (k % 4) * 128:(k % 4 + 1) * 128]
            c.op("pe", lambda e: e.transpose(out=pb, in_=xs2[t % 2][:, k * 128:(k + 1) * 128], identity=identf[:]),
                 reads=[c.R("xs2", t, 2), "const"], writes=[("pz", 3 + k // 4)])
        for k in range(8):
            pb = PS[3 + k // 4][:, (k % 4) * 128:(k % 4 + 1) * 128]
            c.op("act", lambda e: e.activation(out=h2f[t % 2][:, k, :], in_=pb, func=AF.Identity, scale=A2[:, k:k + 1],
                                               bias=B2[:, k:k + 1]), reads=[("pz", 3 + k // 4), "AB"],
                 writes=[("h2f", t % 2)])
        c.op("dve", lambda e: e.tensor_copy(out=h2T[:, :, t * 128:(t + 1) * 128], in_=h2f[t % 2][:]),
             reads=[("h2f", t % 2)], writes=["h2T"])
        for k in range(8):
            c.op("pe", lambda e: e.matmul(PS[5][:, 0:64], lhsT=h2f[t % 2][:, k, :], rhs=wr[:, k, :], start=(k == 0),
                                          stop=(k == 7)), reads=[("h2f", t % 2), "wr"], writes=[("pz", 5)])
        R_ = rt[:, t % 2, :]
        s_, bi, mb, sel = R_[:, 0:64], R_[:, 64:128], R_[:, 128:192], R_[:, 192:256]
        m8 = R_[:, 256:320]
        gs, g8, gm, gneg = R_[:, 320:328], R_[:, 328:336], R_[:, 336:344], R_[:, 344:352]
        rk_ = ("rt", t % 2)
        c.op("act", lambda e: e.activation(out=s_, in_=PS[5][:, 0:64], func=AF.Tanh, scale=0.5), reads=[("pz", 5)],
             writes=[rk_])
        c.op("dve", lambda e: e.tensor_scalar(out=s_, in0=s_, scalar1=0.5, scalar2=0.5, op0=ALU.mult, op1=ALU.add),
             reads=[rk_], writes=[rk_])
        c.op("dve", lambda e: e.tensor_tensor(out=bi, in0=s_, in1=brout_t[:], op=ALU.add), reads=[rk_, "const"],
             writes=[rk_])
        for g in range(8):
            c.op("dve", lambda e: e.max(out=m8[:, g * 8:(g + 1) * 8], in_=bi[:, g * 8:(g + 1) * 8]), reads=[rk_],
                 writes=[rk_])
        m83 = m8.rearrange("p (g k) -> p g k", g=8)
        c.op("dve", lambda e: e.tensor_tensor(out=gs, in0=m83[:, :, 0], in1=m83[:, :, 1], op=ALU.add), reads=[rk_],
             writes=[rk_])
        c.op("dve", lambda e: e.max(out=g8, in_=gs), reads=[rk_], writes=[rk_])
        c.op("dve", lambda e: e.tensor_scalar(out=gm, in0=gs, scalar1=g8[:, 3:4], scalar2=None, op0=ALU.is_ge),
             reads=[rk_], writes=[rk_])
        c.op("dve", lambda e: e.tensor_scalar(out=gneg, in0=gm, scalar1=-1.0, scalar2=8.0, op0=ALU.add, op1=ALU.mult),
             reads=[rk_], writes=[rk_])
        bi3, mb3 = bi.rearrange("p (g k) -> p g k", g=8), mb.rearrange("p (g k) -> p g k", g=8)
        c.op("dve", lambda e: e.tensor_tensor(out=mb3, in0=bi3, in1=gm.unsqueeze(2).broadcast_to([128, 8, 8]), op=ALU.mult),
             reads=[rk_], writes=[rk_])
        c.op("dve", lambda e: e.tensor_tensor(out=mb3, in0=mb3, in1=gneg.unsqueeze(2).broadcast_to([128, 8, 8]), op=ALU.add),
             reads=[rk_], writes=[rk_])
        c.op("dve", lambda e: e.max(out=g8, in_=mb), reads=[rk_], writes=[rk_])
        c.op("dve", lambda e: e.tensor_scalar(out=sel, in0=mb, scalar1=g8[:, 7:8], scalar2=None, op0=ALU.is_ge),
             reads=[rk_], writes=[rk_])
        c.op("dve", lambda e: e.tensor_tensor(out=sel, in0=sel, in1=s_, op=ALU.mult), reads=[rk_], writes=[rk_])
        c.op("dve", lambda e: e.tensor_reduce(out=gs[:, 0:1], in_=sel, axis=AX.X, op=ALU.add), reads=[rk_], writes=[rk_])
        c.op("dve", lambda e: e.reciprocal(out=gs[:, 1:2], in_=gs[:, 0:1]), reads=[rk_], writes=[rk_])
        c.op("dve", lambda e: e.tensor_scalar(out=comb[:, t, :], in0=sel, scalar1=gs[:, 1:2], scalar2=2.5, op0=ALU.mult,
                                              op1=ALU.mult), reads=[rk_], writes=["comb"])

    def C2_s12(t):
        C2_s1(t)
        C2_s2(t)

    pipeline(NO, [C2_s0, C2_s12])
    print("sbuf remaining in pass C2:", nc.sbuf_bytes_remaining, "ops", c.nops)
    c.pop()
    dbg("h2T", h2T[:], [128, 8, NO * 128], BF16)
    dbg("comb", comb[:], [128, NO, 64], F32)
    if stop == "C2":
        return finish()

    c.push()
    PG = c.psum("pg", [128, 2048], F32)
    PY = [c.psum(f"py{i}", [128, 1024], F32) for i in range(2)]
    acc = c.sbuf("acc", [128, NO, 1024], F32)
    wgu = [c.sbuf("wgu", [128, 8, 512], BF16) for _ in range(2)]
    wdn = [c.sbuf("wdn", [128, 2, 1024], BF16) for _ in range(2)]
    sgm = [c.sbuf("sgm", [128, 1024], BF16) for _ in range(2)]
    actT = [c.sbuf("actT", [128, 2, 512], BF16) for _ in range(2)]
    def load_expert(ei):
        e_ = ei - 1
        sl = ei % 2
        srcs = (w_sg, w_su, w_sd) if e_ < 0 else (w_eg[e_], w_eu[e_], w_ed[e_])
        c.W("wexp", ei, 2)
        c.dma("pool", wgu[sl][:, :, 0:256], srcs[0].rearrange("(k p) n -> p k n", p=128), writes=[("wexp", sl)], key=f"we{sl}a")
        c.dma("pool", wgu[sl][:, :, 256:512], srcs[1].rearrange("(k p) n -> p k n", p=128), writes=[("wexp", sl)], key=f"we{sl}b")
        c.dma("pool", wdn[sl][:], srcs[2].rearrange("(k p) n -> p k n", p=128), writes=[("wexp", sl)], key=f"we{sl}c")

    units = [(ei, tc_) for ei in range(NEXP + 1) for tc_ in range(4)]
    load_expert(0)

    def moe_s0(u):
        ei, tc_ = units[u]
        sl, a_ = ei % 2, u % 2
        if tc_ == 0 and ei + 1 <= NEXP:
            load_expert(ei + 1)
        wk = c.R("wexp", ei, 2)
        for j in range(4):
            for k in range(8):
                c.op("pe", lambda e: e.matmul(PG[:, j * 512:(j + 1) * 512], lhsT=wgu[sl][:, k, j * 128:(j + 1) * 128],
                                              rhs=h2T[:, k, tc_ * 512:(tc_ + 1) * 512], start=(k == 0), stop=(k == 7)),
                     reads=[wk, "h2T"], writes=[("pg", j // 2)])
        c.op("act", lambda e: e.activation(out=sgm[a_][:], in_=PG[:, 0:1024], func=AF.Silu), reads=[("pg", 0)],
             writes=[("sgm", a_)])
        c.op("dve", lambda e: e.tensor_tensor(out=actT[a_][:].rearrange("p f n -> p (f n)"), in0=sgm[a_][:],
                                              in1=PG[:, 1024:2048], op=ALU.mult), reads=[("sgm", a_), ("pg", 1)],
             writes=[("actT", a_)])

    def moe_s1(u):
        ei, tc_ = units[u]
        e_ = ei - 1
        sl, a_ = ei % 2, u % 2
        wk = ("wexp", sl)
        for tt in range(4):
            i = tc_ * 4 + tt
            y_ = (u * 4 + tt) % 2
            for hh in range(2):
                for fc in range(2):
                    c.op("pe", lambda e: e.matmul(PY[y_][:, hh * 512:(hh + 1) * 512], lhsT=actT[a_][:, fc, tt * 128:(tt + 1) * 128],
                                                  rhs=wdn[sl][:, fc, hh * 512:(hh + 1) * 512], start=(fc == 0), stop=(fc == 1)),
                         reads=[("actT", a_), wk], writes=[("py", y_)])
            if e_ < 0:
                c.op("act", lambda e: e.activation(out=acc[:, i, :], in_=PY[y_][:], func=AF.Copy), reads=[("py", y_)],
                     writes=[("acc", i)])
            else:
                c.op("dve", lambda e: e.scalar_tensor_tensor(out=acc[:, i, :], in0=PY[y_][:], scalar=comb[:, i, e_:e_ + 1],
                                                             in1=acc[:, i, :], op0=ALU.mult, op1=ALU.add),
                     reads=[("py", y_), ("acc", i), "comb"], writes=[("acc", i)])

    pipeline(len(units), [moe_s0, moe_s1])
    x1r = [c.sbuf("x1r", [128, 1024], F32) for _ in range(2)]
    xo = [c.sbuf("xo", [128, 1024], F32) for _ in range(2)]
    yo = [c.sbuf("yo", [128, 1024], F32) for _ in range(2)]
    stf = c.sbuf("stf", [128, 3 * 64], F32)
    for t in range(NO):
        c.dma("sp", x1r[t % 2][:], x1_d[t], reads=["x1_d"], writes=[("x1r", t % 2)], key=f"x1r{t % 2}")
        c.op("dve", lambda e: e.tensor_tensor(out=xo[t % 2][:], in0=acc[:, t, :], in1=GT2[:], op=ALU.mult),
             reads=[("acc", t), "GT"], writes=[("xo", t % 2)])
        c.op("pool", lambda e: e.tensor_tensor(out=xo[t % 2][:], in0=xo[t % 2][:], in1=x1r[t % 2][:], op=ALU.add),
             reads=[("xo", t % 2), ("x1r", t % 2)], writes=[("xo", t % 2)])
        a, b, r = stf[:, t:t + 1], stf[:, 64 + t:65 + t], stf[:, 128 + t:129 + t]
        c.op("act", lambda e: e.activation(out=junk[:], in_=xo[t % 2][:], func=AF.Square, accum_out=a),
             reads=[("xo", t % 2)], writes=["junk", ("sfa", t)])
        c.op("dve", lambda e: e.tensor_scalar(out=b, in0=a, scalar1=1.0 / D, scalar2=RMS_EPS, op0=ALU.mult, op1=ALU.add),
             reads=[("sfa", t)], writes=[("sfb", t)])
        rsqrt_cols(b, r, 1, [("sfb", t)], [("sfr", t)])
        c.op("dve", lambda e: e.scalar_tensor_tensor(out=yo[t % 2][:], in0=xo[t % 2][:], scalar=r, in1=gfin_t[:],
                                                     op0=ALU.mult, op1=ALU.mult),
             reads=[("xo", t % 2), ("sfr", t), "const"], writes=[("yo", t % 2)])
        c.dma("sp", out_d[t * 128:(t + 1) * 128, :], yo[t % 2][:], reads=[("yo", t % 2)], writes=["out"], key=f"out{t % 2}")
    c.pop()
    c.close()
    return nc


_NC_CACHE = {}


def _consts(j):
    bf = ml_dtypes.bfloat16
    k = np.arange(128)
    tri = (k[:, None] <= k[None, :]).astype(np.float32)
    amask = np.zeros((128, 4, 128), np.float32)
    for l in range(4):
        if l < j:
            amask[:, l, :] = 1.0
        elif l == j:
            amask[:, l, :] = tri
    inv_mla = 1.0 / (10000.0 ** (np.arange(0, 32, 2, dtype=np.float32) / np.float32(32)))
    inv_ret = 1.0 / (10000.0 ** (np.arange(0, 128, 2, dtype=np.float32) / np.float32(128)))
    invf = np.broadcast_to(np.concatenate([inv_mla, inv_ret]).astype(np.float32)[None, :], (128, 80)).copy()
    g = np.array(GAMMA, np.float64)
    m = np.arange(128, dtype=np.float64)
    kdecA = (g[None, :] ** (127.0 - m[:, None])) * 128.0 ** -0.5
    kdecC = (g[None, :] ** (-m[:, None])) * 128.0 ** -0.5
    qdec = g[None, :] ** m[:, None]
    rep = lambda a: np.repeat(a[:, :, None], 128, axis=2).astype(np.float32)
    G = g ** 128
    rc = np.zeros((128, 16), np.float64)
    for h in range(4):
        rc[:, h * 4 + 0] = g[h] * G[h] ** j
        for l in range(3):
            rc[:, h * 4 + 1 + l] = g[h] * (G[h] ** (j - 1 - l)) if l < j else 0.0
    return dict(identb=np.eye(128).astype(bf), identf=np.eye(128, dtype=np.float32), tri=tri,
                amask=amask.astype(bf), invf=invf, kdecA=rep(kdecA), kdecC=rep(kdecC), qdec=rep(qdec),
                rcoef=rc.astype(np.float32))


def _col(v, nchunk):
    return np.ascontiguousarray(np.asarray(v, np.float32).reshape(nchunk, 128).T)


_BUILD_ARGS = {}


def kernel(x, c, positions, w_ada, b_ada, g_norm1, w_in, g_cq, w_uq, g_ckv, w_ukv, g_ret, w_o_mla, w_o_ret,
           w_out, g_norm2, w_router, b_router, w_exp_gate, w_exp_up, w_exp_down, w_sh_gate, w_sh_up, w_sh_down,
           g_final):
    f = lambda a: np.ascontiguousarray(np.asarray(a, dtype=np.float32))
    x = f(x)
    positions = np.asarray(positions).astype(np.int32)
    if "nc" not in _NC_CACHE:
        _NC_CACHE["nc"] = build(**_BUILD_ARGS)
    nc = _NC_CACHE["nc"]
    shared = dict(
        w_ada=f(w_ada), bada_row=f(b_ada).reshape(1, -1), g1c=_col(g_norm1, 8), g2c=_col(g_norm2, 8), w_in=f(w_in),
        gcq=_col(g_cq, 3), w_uq=f(w_uq), gckv=_col(g_ckv, 2), w_ukv=f(w_ukv), gret=_col(g_ret, 8), w_o_mla=f(w_o_mla),
        w_o_ret=f(w_o_ret), w_out=f(w_out), w_router=f(w_router),
        brout=np.ascontiguousarray(np.broadcast_to(f(b_router)[None, :], (128, 64))),
        w_exp_gate=f(w_exp_gate), w_exp_up=f(w_exp_up), w_exp_down=f(w_exp_down), w_sh_gate=f(w_sh_gate),
        w_sh_up=f(w_sh_up), w_sh_down=f(w_sh_down),
        gfin=np.ascontiguousarray(np.broadcast_to(f(g_final)[None, :], (128, 1024))),
    )
    in_maps = []
    for core in range(8):
        b, j = core // 4, core % 4
        xb = x[b]
        xt = xb.reshape(NT, 128, D)
        pt = positions[b].reshape(NT, 128)
        m = dict(shared)
        m.update(_consts(j))
        m["xall"] = xb
        m["xown"] = np.ascontiguousarray(xt[j::4].reshape(NO * 128, D))
        m["posall"] = np.ascontiguousarray(pt.T)
        m["posown"] = np.ascontiguousarray(pt[j::4].T)
        m["cvec"] = _col(c[b], 8)
        in_maps.append(m)
    res = run_bass_kernel_spmd(nc, in_maps, core_ids=list(range(8)))
    _NC_CACHE["res"] = res
    out = np.empty((2, S, D), np.float32)
    for core in range(8):
        b, j = core // 4, core % 4
        o = np.asarray(res.results[core]["out"]).reshape(NO, 128, D)
        out[b].reshape(NT, 128, D)[j::4] = o
    return out
```

```python
import math
from contextlib import ExitStack

import numpy as np
import ml_dtypes

import concourse.bass as bass
import concourse.mybir as mybir
from concourse.bass_utils import run_bass_kernel_spmd

F32 = mybir.dt.float32
BF16 = mybir.dt.bfloat16
I32 = mybir.dt.int32
AF = mybir.ActivationFunctionType
ALU = mybir.AluOpType
AX = mybir.AxisListType

D = 1024
S = 8192
NT = 64
NO = 16
D_IN = 5792
RMS_EPS = 1e-6
GN_EPS = 1e-5
TWO_PI = 2.0 * math.pi
MAGIC = 12582912.0
C1 = 6.28125
C2 = float(np.float32(TWO_PI - 6.28125))
C3 = float(TWO_PI - 6.28125 - float(np.float32(TWO_PI - 6.28125)))
SCALE_MLA = 96.0 ** -0.5
GAMMA = [1.0 - 2.0 ** (-5.0 - h) for h in range(4)]
NEXP = 64
import os
NOSAME = os.environ.get("NOSAME") == "1"

O_CQ, O_CKV, O_KR, O_RQ, O_RK, O_RV, O_RG, O_GA, O_GB = 0, 384, 640, 672, 1184, 1696, 2720, 3744, 4768


class Ctx:
    def __init__(self, nc):
        self.nc = nc
        self.stacks = [ExitStack()]
        self.eng = {"pe": nc.tensor, "act": nc.scalar, "dve": nc.vector, "pool": nc.gpsimd, "sp": nc.sync}
        self.sem, self.cnt = {}, {}
        for e in ("pe", "act", "dve", "pool"):
            self.sem[e] = self.stacks[0].enter_context(nc.semaphore("s_" + e))
            self.cnt[e] = 0
        self.dsem, self.dcnt = {}, {}
        self.waited = {e: {} for e in self.eng}
        self.last_w, self.readers, self.owner = {}, {}, {}
        self.nops = 0
        self.uid = 0

    def sbuf(self, name, shape, dtype):
        self.uid += 1
        return self.stacks[-1].enter_context(self.nc.sbuf_tensor(f"{name}_{self.uid}", list(shape), dtype))

    def psum(self, name, shape, dtype):
        self.uid += 1
        return self.stacks[-1].enter_context(self.nc.psum_tensor(f"{name}_{self.uid}", list(shape), dtype))

    def push(self):
        self.stacks.append(ExitStack())

    def pop(self):
        self.barrier()
        self.stacks.pop().close()

    def W(self, name, t=0, n=1):
        k = (name, t % n)
        self.owner[k] = t
        return k

    def R(self, name, t=0, n=1):
        k = (name, t % n)
        assert self.owner.get(k) == t, f"stale read {name} tile {t} owner {self.owner.get(k)}"
        return k

    PSUM_NAMES = {"pmod", "pc", "pgt", "pT", "pz", "ptA", "pkv", "pcq", "ptQ", "pq", "ptQ2", "pk", "ps", "po", "ptC",
                  "ptD", "pg", "py"}

    def _split(self, reads, writes):
        rd, wr = [], list(writes)
        for r in reads:
            base = r[0] if isinstance(r, tuple) else r
            if base in self.PSUM_NAMES:
                if r not in wr:
                    wr.append(r)
            else:
                rd.append(r)
        return rd, wr

    def _deps(self, reads, writes):
        deps = []
        for r in reads:
            t = self.last_w.get(r)
            if t is not None:
                deps.append(t)
        for w in writes:
            t = self.last_w.get(w)
            if t is not None:
                deps.append(t)
            deps.extend(self.readers.get(w, ()))
        return deps

    def _wait(self, eng, deps):
        best = {}
        for (skey, sem, val) in deps:
            if eng == "pe" and skey == "pe":
                continue
            if NOSAME and skey == eng:
                continue
            if val > best.get(skey, (None, 0))[1]:
                best[skey] = (sem, val)
        w = self.waited[eng]
        for skey, (sem, val) in best.items():
            if w.get(skey, 0) >= val:
                continue
            self.eng[eng].wait_ge(sem, val)
            w[skey] = val

    def _commit(self, ticket, reads, writes):
        for r in reads:
            self.readers.setdefault(r, []).append(ticket)
        for w in writes:
            self.last_w[w] = ticket
            self.readers[w] = []

    def op(self, eng, fn, reads=(), writes=()):
        reads, writes = self._split(reads, writes)
        self._wait(eng, self._deps(reads, writes))
        ins = fn(self.eng[eng])
        self.cnt[eng] += 1
        ins.then_inc(self.sem[eng], 1)
        t = (eng, self.sem[eng], self.cnt[eng])
        self._commit(t, reads, writes)
        self.nops += 1
        return t

    def dma(self, queue, out, in_, reads=(), writes=(), key=None):
        if key not in self.dsem:
            self.dsem[key] = self.stacks[0].enter_context(self.nc.semaphore("d_" + str(key)))
            self.dcnt[key] = 0
        self._wait(queue, self._deps(reads, writes))
        ins = self.eng[queue].dma_start(out=out, in_=in_)
        self.dcnt[key] += 16
        ins.then_inc(self.dsem[key], 16)
        t = ("d_" + str(key), self.dsem[key], self.dcnt[key])
        self._commit(t, reads, writes)
        return t

    def barrier(self):
        tickets = [(e, self.sem[e], self.cnt[e]) for e in self.sem if self.cnt[e] > 0]
        tickets += [("d_" + str(k), self.dsem[k], self.dcnt[k]) for k in self.dsem]
        for e in self.eng:
            self._wait(e, tickets)
        self.last_w, self.readers = {}, {}

    def close(self):
        self.barrier()
        while self.stacks:
            self.stacks.pop().close()


def pipeline(n, stages):
    ns = len(stages)
    for s in range(n + ns - 1):
        for k in range(ns - 1, -1, -1):
            t = s - k
            if 0 <= t < n:
                stages[k](t)


def build(stop=None, debug=False):
    nc = bass.Bass("TRN2", target_bir_lowering=False)
    dbg_out = {}

    def dbg(name, ap, shape, dt):
        if not debug:
            return
        d_ = nc.dram_tensor("dbg_" + name, list(shape), dt, kind="ExternalOutput").ap()
        dbg_out[name] = d_
        c.barrier()
        c.dma("sp", d_, ap, key="dbg_" + name)
        c.barrier()

    def finish():
        c.close()
        return nc

    def din(name, shape, dt=F32):
        return nc.dram_tensor(name, list(shape), dt, kind="ExternalInput").ap()

    xall = din("xall", [S, D])
    xown = din("xown", [NO * 128, D])
    posall = din("posall", [128, NT], I32)
    posown = din("posown", [128, NO], I32)
    cvec = din("cvec", [128, 8])
    w_ada = din("w_ada", [D, 6 * D])
    bada_row = din("bada_row", [1, 6 * D])
    g1c = din("g1c", [128, 8])
    g2c = din("g2c", [128, 8])
    w_in = din("w_in", [D, D_IN])
    gcq = din("gcq", [128, 3])
    w_uq = din("w_uq", [384, 768])
    gckv = din("gckv", [128, 2])
    w_ukv = din("w_ukv", [256, 1024])
    gret = din("gret", [128, 8])
    w_o_mla = din("w_o_mla", [512, 1024])
    w_o_ret = din("w_o_ret", [1024, 1024])
    w_out = din("w_out", [1024, 1024])
    w_router = din("w_router", [1024, 64])
    brout = din("brout", [128, 64])
    w_eg = din("w_exp_gate", [NEXP, 1024, 256])
    w_eu = din("w_exp_up", [NEXP, 1024, 256])
    w_ed = din("w_exp_down", [NEXP, 256, 1024])
    w_sg = din("w_sh_gate", [1024, 256])
    w_su = din("w_sh_up", [1024, 256])
    w_sd = din("w_sh_down", [256, 1024])
    gfin = din("gfin", [128, 1024])
    identb_d = din("identb", [128, 128], BF16)
    identf_d = din("identf", [128, 128])
    tri_d = din("tri", [128, 128])
    amask_d = din("amask", [128, 4, 128], BF16)
    invf_d = din("invf", [128, 80])
    kdecA_d = din("kdecA", [128, 4, 128])
    kdecC_d = din("kdecC", [128, 4, 128])
    qdec_d = din("qdec", [128, 4, 128])
    rcoef_d = din("rcoef", [128, 16])
    out_d = nc.dram_tensor("out", [NO * 128, D], F32, kind="ExternalOutput").ap()
    sown_d = nc.dram_tensor("sown_s", [NO, 128, 1024], BF16, kind="ExternalOutput").ap()
    bb_d = nc.dram_tensor("bb_s", [NO, 128, 1024], BF16, kind="ExternalOutput").ap()
    x1_d = nc.dram_tensor("x1_s", [NO, 128, 1024], F32, kind="ExternalOutput").ap()
    ot_d = nc.dram_tensor("ot_s", [8, 64, NO * 128], BF16, kind="ExternalOutput").ap()

    c = Ctx(nc)

    identb = c.sbuf("identb", [128, 128], BF16)
    identf = c.sbuf("identf", [128, 128], F32)
    tri = c.sbuf("tri", [128, 128], F32)
    amask = c.sbuf("amask", [128, 4, 128], BF16)
    invf = c.sbuf("invf", [128, 80], F32)
    rcoef = c.sbuf("rcoef", [128, 16], F32)
    nhalf = c.sbuf("nhalf", [128, 64], F32)
    onesf = c.sbuf("onesf", [128, 128], F32)
    AB = c.sbuf("AB", [128, 32], F32)
    GT1 = c.sbuf("GT1", [128, 1024], F32)
    GT2 = c.sbuf("GT2", [128, 1024], F32)
    gfin_t = c.sbuf("gfin_t", [128, 1024], F32)
    brout_t = c.sbuf("brout_t", [128, 64], F32)
    posf = c.sbuf("posf", [128, NT + NO], F32)
    junk = c.sbuf("junk", [128, 1024], BF16)
    comb = c.sbuf("comb", [128, NO, 64], F32)

    for (t_, d_, k_) in [(identb, identb_d, "c0"), (identf, identf_d, "c1"), (tri, tri_d, "c2"),
                         (amask, amask_d, "c3"), (invf, invf_d, "c4"), (rcoef, rcoef_d, "c5"),
                         (gfin_t, gfin, "c6"), (brout_t, brout, "c7")]:
        c.dma("sp", t_[:], d_, writes=["const"], key=k_)
    c.op("pool", lambda e: e.memset(nhalf[:], -0.5), writes=["const"])
    c.op("pool", lambda e: e.memset(onesf[:], 1.0), writes=["const"])
    c.barrier()
    if stop == "c":
        return finish()

    def rsqrt_cols(src_ap, dst_ap, ncols, rd, wr):
        c.op("pool", lambda e: e.tensor_tensor(out=dst_ap, in0=src_ap, in1=nhalf[:, 0:ncols], op=ALU.pow),
             reads=rd, writes=wr)

    c.push()
    PS = [c.psum(f"p0_{i}", [128, 512], F32) for i in range(8)]
    cv = c.sbuf("cv", [128, 8], F32)
    cs = c.sbuf("cs", [128, 8], F32)
    modrow = c.sbuf("modrow", [1, 6 * D], F32)
    brow = c.sbuf("brow", [1, 6 * D], F32)
    gcols = c.sbuf("gcols", [128, 16], F32)
    wab = [c.sbuf(f"wab{i}", [128, 8, 512], F32) for i in range(2)]
    posi = c.sbuf("posi", [128, NT + NO], I32)
    c.dma("sp", cv[:], cvec, writes=["cv"], key="p0a")
    c.dma("sp", brow[:], bada_row, writes=["brow"], key="p0b")
    c.dma("sp", gcols[:, 0:8], g1c, writes=["gcols"], key="p0c")
    c.dma("sp", gcols[:, 8:16], g2c, writes=["gcols"], key="p0d")
    c.dma("sp", posi[:, 0:NT], posall, writes=["posi"], key="p0e")
    c.dma("sp", posi[:, NT:NT + NO], posown, writes=["posi"], key="p0f")
    c.op("dve", lambda e: e.tensor_copy(out=posf[:], in_=posi[:]), reads=["posi"], writes=["posf"])
    c.op("act", lambda e: e.activation(out=cs[:], in_=cv[:], func=AF.Silu), reads=["cv"], writes=["cs"])
    for n in range(12):
        wt = wab[n % 2]
        c.dma("sp", wt[:], w_ada[:, n * 512:(n + 1) * 512].rearrange("(k p) n -> p k n", p=128),
              writes=[c.W("wab", n, 2)], key=f"wab{n % 2}")
        for k in range(8):
            c.op("pe", lambda e: e.matmul(PS[n % 2][0:1, :], lhsT=cs[:, k:k + 1], rhs=wt[:, k, :],
                                          start=(k == 0), stop=(k == 7)),
                 reads=[c.R("wab", n, 2), "cs"], writes=[("pmod", n % 2)])
        c.op("dve", lambda e: e.tensor_tensor(out=modrow[0:1, n * 512:(n + 1) * 512], in0=PS[n % 2][0:1, :],
                                              in1=brow[0:1, n * 512:(n + 1) * 512], op=ALU.add),
             reads=[("pmod", n % 2), "brow"], writes=["modrow"])
    if stop == "0a":
        dbg("modrow", modrow[:], [1, 6 * D], F32)
        c.pop()
        return finish()
    pc = PS[2]
    for idx, ch in enumerate(list(range(0, 16)) + list(range(24, 40))):
        c.op("pe", lambda e: e.matmul(pc[:, idx:idx + 1], lhsT=modrow[0:1, ch * 128:(ch + 1) * 128],
                                      rhs=onesf[0:1, 0:1], start=True, stop=True),
             reads=["modrow", "const"], writes=["pc"])
    c.op("dve", lambda e: e.scalar_tensor_tensor(out=AB[:, 0:8], in0=pc[:, 8:16], scalar=1.0, in1=gcols[:, 0:8],
                                                 op0=ALU.add, op1=ALU.mult), reads=["pc", "gcols"], writes=["AB"])
    c.op("dve", lambda e: e.tensor_copy(out=AB[:, 8:16], in_=pc[:, 0:8]), reads=["pc"], writes=["AB"])
    c.op("dve", lambda e: e.scalar_tensor_tensor(out=AB[:, 16:24], in0=pc[:, 24:32], scalar=1.0, in1=gcols[:, 8:16],
                                                 op0=ALU.add, op1=ALU.mult), reads=["pc", "gcols"], writes=["AB"])
    c.op("dve", lambda e: e.tensor_copy(out=AB[:, 24:32], in_=pc[:, 16:24]), reads=["pc"], writes=["AB"])
    if stop == "0b":
        c.pop()
        dbg("AB", AB[:], [128, 32], F32)
        return finish()
    for (dst, base, scl, pi) in [(GT1, 2048, 0.5, 4), (GT2, 5120, 1.0, 6)]:
        for hh in range(2):
            c.op("pe", lambda e: e.matmul(PS[pi + hh][:], lhsT=onesf[0:1, 0:128],
                                          rhs=modrow[0:1, base + hh * 512: base + (hh + 1) * 512],
                                          start=True, stop=True), reads=["modrow", "const"], writes=[("pgt", pi + hh)])
            c.op("act", lambda e: e.activation(out=dst[:, hh * 512:(hh + 1) * 512], in_=PS[pi + hh][:],
                                               func=AF.Copy, scale=scl), reads=[("pgt", pi + hh)], writes=["GT"])
    c.pop()
    dbg("AB", AB[:], [128, 32], F32)
    dbg("GT1", GT1[:], [128, 1024], F32)
    if stop == "0":
        return finish()

    A1, B1, A2, B2 = AB[:, 0:8], AB[:, 8:16], AB[:, 16:24], AB[:, 24:32]

    def load_w(dst_tile, src_ap, name, key):
        c.dma("pool", dst_tile, src_ap, writes=[name], key=key)

    def rope_tables(pos_ap, G, sinT, cosT, tmp, lo, hi, tag):
        n = hi - lo
        ang, u, r = (tmp[0][:, 0:G, 0:n], tmp[1][:, 0:G, 0:n], tmp[2][:, 0:G, 0:n])
        c.op("dve", lambda e: e.tensor_tensor(out=ang, in0=invf[:, lo:hi].unsqueeze(1).broadcast_to([128, G, n]),
                                              in1=pos_ap.unsqueeze(2).broadcast_to([128, G, n]), op=ALU.mult),
             reads=["posf", "const"], writes=[tag + "t0"])
        c.op("dve", lambda e: e.tensor_scalar(out=u, in0=ang, scalar1=1.0 / TWO_PI, scalar2=MAGIC,
                                              op0=ALU.mult, op1=ALU.add), reads=[tag + "t0"], writes=[tag + "t1"])
        c.op("dve", lambda e: e.tensor_scalar(out=u, in0=u, scalar1=-MAGIC, scalar2=None, op0=ALU.add),
             reads=[tag + "t1"], writes=[tag + "t1"])
        c.op("dve", lambda e: e.scalar_tensor_tensor(out=r, in0=u, scalar=-C1, in1=ang, op0=ALU.mult, op1=ALU.add),
             reads=[tag + "t1", tag + "t0"], writes=[tag + "t2"])
        c.op("dve", lambda e: e.scalar_tensor_tensor(out=r, in0=u, scalar=-C2, in1=r, op0=ALU.mult, op1=ALU.add),
             reads=[tag + "t1", tag + "t2"], writes=[tag + "t2"])
        c.op("dve", lambda e: e.scalar_tensor_tensor(out=r, in0=u, scalar=-C3, in1=r, op0=ALU.mult, op1=ALU.add),
             reads=[tag + "t1", tag + "t2"], writes=[tag + "t2"])
        c.op("dve", lambda e: e.tensor_scalar(out=r, in0=r, scalar1=-math.pi, scalar2=math.pi, op0=ALU.max, op1=ALU.min),
             reads=[tag + "t2"], writes=[tag + "t2"])
        c.op("act", lambda e: e.activation(out=sinT[:, 0:G, lo:hi], in_=r, func=AF.Sin), reads=[tag + "t2"],
             writes=[tag + "sin"])
        c.op("dve", lambda e: e.scalar_tensor_tensor(out=ang, in0=r, scalar=-1.0, in1=r, op0=ALU.mult, op1=ALU.max),
             reads=[tag + "t2"], writes=[tag + "t0"])
        c.op("dve", lambda e: e.tensor_scalar(out=ang, in0=ang, scalar1=-1.0, scalar2=math.pi / 2, op0=ALU.mult,
                                              op1=ALU.add), reads=[tag + "t0"], writes=[tag + "t0"])
        c.op("act", lambda e: e.activation(out=cosT[:, 0:G, lo:hi], in_=ang, func=AF.Sin), reads=[tag + "t0"],
             writes=[tag + "cos"])

    def rope(out4, z4, cos_b, sin_b, t1, t2, eng2, rd, wr, tmpname):
        c.op("dve", lambda e: e.tensor_tensor(out=t1, in0=z4, in1=cos_b, op=ALU.mult), reads=rd, writes=[tmpname + "1"])
        c.op("dve", lambda e: e.tensor_tensor(out=t2, in0=z4, in1=sin_b, op=ALU.mult), reads=rd, writes=[tmpname + "2"])
        c.op(eng2, lambda e: e.tensor_tensor(out=out4[:, :, 0, :], in0=t1[:, :, 0, :], in1=t2[:, :, 1, :], op=ALU.subtract),
             reads=[tmpname + "1", tmpname + "2"], writes=wr)
        c.op(eng2, lambda e: e.tensor_tensor(out=out4[:, :, 1, :], in0=t1[:, :, 1, :], in1=t2[:, :, 0, :], op=ALU.add),
             reads=[tmpname + "1", tmpname + "2"], writes=wr)

    class HT:
        def __init__(self, src, nb, A, B, pT):
            self.src, self.nb, self.A, self.B, self.pT = src, nb, A, B, pT
            self.xt = [c.sbuf("xt", [128, 1024], F32) for _ in range(nb)]
            self.xs = [c.sbuf("xs", [128, 1024], BF16) for _ in range(2)]
            self.hT = [c.sbuf("hT", [128, 8, 128], BF16) for _ in range(2)]
            self.st = c.sbuf("hst", [128, 3 * 64], F32)

        def load(self, t):
            c.dma("sp", self.xt[t % self.nb][:], self.src[t * 128:(t + 1) * 128, :],
                  writes=[c.W("xt", t, self.nb)], key=f"xt{t % self.nb}")

        def norm(self, t):
            xt, st = self.xt[t % self.nb], self.st
            a, b, r = st[:, t % 64:t % 64 + 1], st[:, 64 + t % 64:65 + t % 64], st[:, 128 + t % 64:129 + t % 64]
            c.op("act", lambda e: e.activation(out=junk[:], in_=xt[:], func=AF.Square, accum_out=a),
                 reads=[c.R("xt", t, self.nb)], writes=["junk", ("hsa", t % 64)])
            c.op("dve", lambda e: e.tensor_scalar(out=b, in0=a, scalar1=1.0 / D, scalar2=RMS_EPS, op0=ALU.mult,
                                                  op1=ALU.add), reads=[("hsa", t % 64)], writes=[("hsb", t % 64)])
            rsqrt_cols(b, r, 1, [("hsb", t % 64)], [("hsr", t % 64)])
            c.op("dve", lambda e: e.tensor_scalar(out=self.xs[t % 2][:], in0=xt[:], scalar1=r, scalar2=None,
                                                  op0=ALU.mult), reads=[c.R("xt", t, self.nb), ("hsr", t % 64)],
                 writes=[c.W("xs", t, 2)])

        def transpose(self, t):
            xs, hT, pT = self.xs[t % 2], self.hT[t % 2], self.pT
            pTb = pT[:].bitcast(BF16)
            for k in range(8):
                c.op("pe", lambda e: e.transpose(out=pTb[:, k * 128:(k + 1) * 128], in_=xs[:, k * 128:(k + 1) * 128],
                                                 identity=identb[:]), reads=[c.R("xs", t, 2), "const"], writes=["pT"])
            c.W("hT", t, 2)
            for k in range(8):
                c.op("act", lambda e: e.activation(out=hT[:, k, :], in_=pTb[:, k * 128:(k + 1) * 128],
                                                   func=AF.Identity, scale=self.A[:, k:k + 1],
                                                   bias=self.B[:, k:k + 1]), reads=["pT", "AB"],
                     writes=[("hT", t % 2)])

        def get(self, t):
            return self.hT[t % 2], c.R("hT", t, 2)

    c.push()
    ckvnT = c.sbuf("ckvnT", [128, 2, S], BF16)
    KT = [c.sbuf(f"KT{i}", [128, S], BF16) for i in range(2)]

    c.push()
    PS = [c.psum(f"pa_{i}", [128, 512], F32) for i in range(8)]
    WA = c.sbuf("WA", [128, 8, 1824], BF16)
    load_w(WA[:, :, 0:288], w_in[:, O_CKV:O_CKV + 288].rearrange("(k p) n -> p k n", p=128), "WA", "wA0")
    load_w(WA[:, :, 288:800], w_in[:, O_RK:O_RK + 512].rearrange("(k p) n -> p k n", p=128), "WA", "wA1")
    load_w(WA[:, :, 800:1824], w_in[:, O_RV:O_RV + 1024].rearrange("(k p) n -> p k n", p=128), "WA", "wA2")
    kdecA = c.sbuf("kdecA", [128, 4, 128], F32)
    c.dma("sp", kdecA[:], kdecA_d, writes=["kdecA"], key="kdA")
    ht = HT(xall, 3, A1, B1, PS[0])
    sinT = [c.sbuf("sinT", [128, 4, 80], F32) for _ in range(2)]
    cosT = [c.sbuf("cosT", [128, 4, 80], F32) for _ in range(2)]
    rtmp = [c.sbuf("rtmp", [128, 4, 80], F32) for _ in range(3)]
    stA = c.sbuf("stA", [128, 3 * 64], F32)
    ckvs = [c.sbuf("ckvs", [128, 256], BF16) for _ in range(2)]
    kst = [c.sbuf("kst", [128, 96], BF16) for _ in range(2)]
    kr1 = c.sbuf("kr1", [128, 32], F32)
    kr2 = c.sbuf("kr2", [128, 32], F32)
    rk1 = c.sbuf("rk1", [128, 512], F32)
    rk2 = c.sbuf("rk2", [128, 512], F32)
    rk3 = c.sbuf("rk3", [128, 512], F32)
    kp = [c.sbuf("kp", [128, 4, 128], BF16) for _ in range(2)]
    vb = [c.sbuf("vb", [128, 1024], BF16) for _ in range(2)]
    Sst = c.sbuf("Sst", [128, 1024], F32)
    Sown = [c.sbuf("Sown", [128, 1024], F32) for _ in range(2)]
    Sownb = [c.sbuf("Sownb", [128, 1024], BF16) for _ in range(2)]
    for i in range(2):
        c.op("pool", lambda e: e.memset(kst[i][:], 0.0), writes=[("kst", i)])
    c.op("pool", lambda e: e.memset(Sst[:], 0.0), writes=["Sst"])

    def A_s0(t):
        if t == 0:
            ht.load(0)
            ht.load(1)
        if t + 2 < NT:
            ht.load(t + 2)
        if t % 4 == 0:
            g = t // 4
            rope_tables(posf[:, t:t + 4], 4, sinT[g % 2], cosT[g % 2], rtmp, 0, 80, f"rtA{g % 2}")
            c.W("tabA", g, 2)
        ht.norm(t)

    def A_s0b(t):
        ht.transpose(t)

    def A_s1(t):
        hT, hk = ht.get(t)
        for (pi, lo, n) in [(1, 0, 288), (2, 288, 512), (3, 800, 512), (4, 1312, 512)]:
            for k in range(8):
                c.op("pe", lambda e: e.matmul(PS[pi][:, 0:n], lhsT=hT[:, k, :], rhs=WA[:, k, lo:lo + n],
                                              start=(k == 0), stop=(k == 7)), reads=[hk, "WA"], writes=[("pz", pi)])
        m = t % 64
        a, b, r = stA[:, m:m + 1], stA[:, 64 + m:65 + m], stA[:, 128 + m:129 + m]
        c.op("act", lambda e: e.activation(out=junk[:, 0:256], in_=PS[1][:, 0:256], func=AF.Square, accum_out=a),
             reads=[("pz", 1)], writes=["junk", ("sAa", m)])
        c.op("dve", lambda e: e.tensor_scalar(out=b, in0=a, scalar1=1.0 / 256, scalar2=RMS_EPS, op0=ALU.mult,
                                              op1=ALU.add), reads=[("sAa", m)], writes=[("sAb", m)])
        rsqrt_cols(b, r, 1, [("sAb", m)], [("sAr", m)])
        c.op("dve", lambda e: e.tensor_scalar(out=ckvs[t % 2][:], in0=PS[1][:, 0:256], scalar1=r, scalar2=None,
                                              op0=ALU.mult), reads=[("pz", 1), ("sAr", m)], writes=[c.W("ckvs", t, 2)])
        g = t // 4
        tk = c.R("tabA", g, 2)
        sn, cs_ = sinT[g % 2], cosT[g % 2]
        z4 = PS[1][:, 256:288].rearrange("p (h t d) -> p h t d", h=1, t=2)
        cb = cs_[:, t % 4, 0:16].unsqueeze(1).unsqueeze(1).broadcast_to([128, 1, 2, 16])
        sb = sn[:, t % 4, 0:16].unsqueeze(1).unsqueeze(1).broadcast_to([128, 1, 2, 16])
        o4 = kst[t % 2][:, 64:96].rearrange("p (h t d) -> p h t d", h=1, t=2)
        rope(o4, z4, cb, sb, kr1[:].rearrange("p (h t d) -> p h t d", h=1, t=2),
             kr2[:].rearrange("p (h t d) -> p h t d", h=1, t=2), "pool",
             [("pz", 1), f"rtA{g % 2}sin", f"rtA{g % 2}cos"], [c.W("kst", t, 2)], "kr")
        z4 = PS[2][:].rearrange("p (h t d) -> p h t d", h=4, t=2)
        cb = cs_[:, t % 4, 16:80].unsqueeze(1).unsqueeze(1).broadcast_to([128, 4, 2, 64])
        sb = sn[:, t % 4, 16:80].unsqueeze(1).unsqueeze(1).broadcast_to([128, 4, 2, 64])
        o4 = rk3[:].rearrange("p (h t d) -> p h t d", h=4, t=2)
        rope(o4, z4, cb, sb, rk1[:].rearrange("p (h t d) -> p h t d", h=4, t=2),
             rk2[:].rearrange("p (h t d) -> p h t d", h=4, t=2), "pool",
             [("pz", 2), f"rtA{g % 2}sin", f"rtA{g % 2}cos"], ["rk3"], "rk")
        c.op("pool", lambda e: e.tensor_tensor(out=kp[t % 2][:], in0=rk3[:].rearrange("p (h d) -> p h d", h=4),
                                               in1=kdecA[:], op=ALU.mult), reads=["rk3", "kdecA"],
             writes=[c.W("kp", t, 2)])
        c.W("vb", t, 2)
        for hh in range(2):
            c.op("act", lambda e: e.activation(out=vb[t % 2][:, hh * 512:(hh + 1) * 512], in_=PS[3 + hh][:],
                                               func=AF.Copy), reads=[("pz", 3 + hh)], writes=[("vb", t % 2)])

    import os
    _lv = int(os.environ.get("DBG_LV", 9))

    def A_s2(t):
        pt = PS[5][:].bitcast(BF16)
        for k in range(2):
            c.op("pe", lambda e: e.transpose(out=pt[:, k * 128:(k + 1) * 128], in_=ckvs[t % 2][:, k * 128:(k + 1) * 128],
                                             identity=identb[:]), reads=[c.R("ckvs", t, 2), "const"], writes=["ptA"])
        c.op("pe", lambda e: e.transpose(out=pt[0:96, 256:384], in_=kst[t % 2][:], identity=identb[:]),
             reads=[c.R("kst", t, 2), "const"], writes=["ptA"])
        if _lv < 2:
            return
        for k in range(2):
            c.op("act", lambda e: e.activation(out=ckvnT[:, k, t * 128:(t + 1) * 128],
                                               in_=pt[:, k * 128:(k + 1) * 128], func=AF.Copy),
                 reads=["ptA"], writes=["ckvnT"])
        if _lv < 3:
            return
        _kt = os.environ.get("DBG_KT", "both")
        if _kt in ("both", "act"):
            c.op("act", lambda e: e.activation(out=KT[0][64:96, t * 128:(t + 1) * 128], in_=pt[64:96, 256:384],
                                               func=AF.Copy), reads=["ptA"], writes=["KT0r"])
        if _kt in ("both", "dve"):
            c.op("act", lambda e: e.activation(out=KT[1][64:96, t * 128:(t + 1) * 128], in_=pt[64:96, 256:384],
                                               func=AF.Copy), reads=["ptA"], writes=["KT1r"])
        if _lv < 4:
            return
        pkv = [PS[6], PS[7]]
        for h in range(4):
            c.op("pe", lambda e: e.matmul(pkv[h // 2][:, (h % 2) * 256:(h % 2 + 1) * 256], lhsT=kp[t % 2][:, h, :],
                                          rhs=vb[t % 2][:, h * 256:(h + 1) * 256], start=True, stop=True),
                 reads=[c.R("kp", t, 2), c.R("vb", t, 2)], writes=["pkv"])
        if _lv < 5:
            return
        l, g = t % 4, t // 4
        so = Sown[g % 2]
        if l == 0:
            c.W("Sown", g, 2)
        for h in range(4):
            hs = slice(h * 256, (h + 1) * 256)
            pk = pkv[h // 2][:, (h % 2) * 256:(h % 2 + 1) * 256]
            if l == 0:
                c.op("act", lambda e: e.activation(out=so[:, hs], in_=Sst[:, hs], func=AF.Copy,
                                                   scale=rcoef[:, h * 4:h * 4 + 1]), reads=["Sst", "const"],
                     writes=[("Sown", g % 2)])
            if l < 3:
                c.op("dve", lambda e: e.scalar_tensor_tensor(out=so[:, hs], in0=pk, scalar=rcoef[:, h * 4 + 1 + l:h * 4 + 2 + l],
                                                             in1=so[:, hs], op0=ALU.mult, op1=ALU.add),
                     reads=["pkv", ("Sown", g % 2), "const"], writes=[("Sown", g % 2)])
            c.op("dve", lambda e: e.scalar_tensor_tensor(out=Sst[:, hs], in0=Sst[:, hs], scalar=GAMMA[h] ** 128, in1=pk,
                                                         op0=ALU.mult, op1=ALU.add), reads=["pkv", "Sst"], writes=["Sst"])
        if l == 3 and _lv >= 6:
            c.op("act", lambda e: e.activation(out=Sownb[g % 2][:], in_=so[:], func=AF.Copy),
                 reads=[c.R("Sown", g, 2)], writes=[c.W("Sownb", g, 2)])
            c.dma("sp", sown_d[g], Sownb[g % 2][:], reads=[c.R("Sownb", g, 2)], writes=["sown_d"], key=f"so{g % 2}")

    import os
    _na = int(os.environ.get("DBG_NA", NT))
    _ns = int(os.environ.get("DBG_NS", 3))
    pipeline(_na, [A_s0, A_s0b, A_s1, A_s2][:_ns + 1])
    print("sbuf remaining in pass A:", nc.sbuf_bytes_remaining, "ops", c.nops)
    dbg("Sst", Sst[:], [128, 1024], F32)
    c.pop()
    dbg("ckvnT", ckvnT[:], [128, 2, S], BF16)
    dbg("KT0", KT[0][:], [128, S], BF16)
    if stop == "A":
        return finish()

    QT = c.sbuf("QT", [128, 8, NO * 128], BF16)
    c.push()
    PS = [c.psum(f"pq_{i}", [128, 512], F32) for i in range(8)]
    WQ = c.sbuf("WQ", [128, 8, 384], BF16)
    load_w(WQ[:], w_in[:, O_CQ:O_CQ + 384].rearrange("(k p) n -> p k n", p=128), "WQ", "wQ0")
    wuq = c.sbuf("wuq", [128, 3, 768], BF16)
    c.push()
    wuq_f = c.sbuf("wuq_f", [128, 3, 768], F32)
    gq = c.sbuf("gq", [128, 3], F32)
    c.dma("sp", wuq_f[:], w_uq.rearrange("(k p) n -> p k n", p=128), writes=["wuq_f"], key="wQ1")
    c.dma("sp", gq[:], gcq, writes=["gq"], key="wQ2")
    for k in range(3):
        wv = wuq_f[:, k, :].rearrange("p (h d) -> p h d", h=8)
        c.op("dve", lambda e: e.tensor_scalar(out=wuq[:, k, 0:512].rearrange("p (h d) -> p h d", h=8), in0=wv[:, :, 0:64],
                                              scalar1=gq[:, k:k + 1], scalar2=None, op0=ALU.mult),
             reads=["wuq_f", "gq"], writes=["wuq"])
        c.op("dve", lambda e: e.tensor_scalar(out=wuq[:, k, 512:768].rearrange("p (h d) -> p h d", h=8), in0=wv[:, :, 64:96],
                                              scalar1=gq[:, k:k + 1], scalar2=None, op0=ALU.mult),
             reads=["wuq_f", "gq"], writes=["wuq"])
    c.pop()
    ht = HT(xown, 3, A1, B1, PS[0])
    sinQ = c.sbuf("sinQ", [128, NO, 16], F32)
    cosQ = c.sbuf("cosQ", [128, NO, 16], F32)
    c.push()
    rtmpQ = [c.sbuf("rtmpQ", [128, NO, 16], F32) for _ in range(3)]
    rope_tables(posf[:, NT:NT + NO], NO, sinQ, cosQ, rtmpQ, 0, 16, "rtQ")
    c.pop()
    stQ = c.sbuf("stQ", [128, 3 * 64], F32)
    cqs = [c.sbuf("cqs", [128, 384], BF16) for _ in range(2)]
    cqnT = [c.sbuf("cqnT", [128, 3, 128], BF16) for _ in range(2)]
    qsb = [c.sbuf("qsb", [128, 8, 96], BF16) for _ in range(2)]
    qr1 = c.sbuf("qr1", [128, 8, 32], F32)
    qr2 = c.sbuf("qr2", [128, 8, 32], F32)

    def Q_s0(t):
        if t == 0:
            ht.load(0)
            ht.load(1)
        if t + 2 < NO:
            ht.load(t + 2)
        ht.norm(t)

    def Q_s0b(t):
        ht.transpose(t)

    def Q_s1(t):
        hT, hk = ht.get(t)
        for k in range(8):
            c.op("pe", lambda e: e.matmul(PS[1][:, 0:384], lhsT=hT[:, k, :], rhs=WQ[:, k, :], start=(k == 0),
                                          stop=(k == 7)), reads=[hk, "WQ"], writes=["pcq"])
        m = t
        a, b, r = stQ[:, m:m + 1], stQ[:, 64 + m:65 + m], stQ[:, 128 + m:129 + m]
        c.op("act", lambda e: e.activation(out=junk[:, 0:384], in_=PS[1][:, 0:384], func=AF.Square, accum_out=a),
             reads=["pcq"], writes=["junk", ("sQa", m)])
        c.op("dve", lambda e: e.tensor_scalar(out=b, in0=a, scalar1=1.0 / 384, scalar2=RMS_EPS, op0=ALU.mult,
                                              op1=ALU.add), reads=[("sQa", m)], writes=[("sQb", m)])
        rsqrt_cols(b, r, 1, [("sQb", m)], [("sQr", m)])
        c.op("dve", lambda e: e.tensor_scalar(out=cqs[t % 2][:], in0=PS[1][:, 0:384], scalar1=r, scalar2=None,
                                              op0=ALU.mult), reads=["pcq", ("sQr", m)], writes=[c.W("cqs", t, 2)])
        pt = PS[2][:].bitcast(BF16)
        for k in range(3):
            c.op("pe", lambda e: e.transpose(out=pt[:, k * 128:(k + 1) * 128], in_=cqs[t % 2][:, k * 128:(k + 1) * 128],
                                             identity=identb[:]), reads=[c.R("cqs", t, 2), "const"], writes=["ptQ"])
        c.op("act", lambda e: e.activation(out=cqnT[t % 2][:], in_=pt[:, 0:384].rearrange("p (k n) -> p k n", k=3),
                                           func=AF.Copy), reads=["ptQ"], writes=[c.W("cqnT", t, 2)])

    def Q_s2(t):
        for (pi, lo, n) in [(3, 0, 512), (4, 512, 256)]:
            for k in range(3):
                c.op("pe", lambda e: e.matmul(PS[pi][:, 0:n], lhsT=cqnT[t % 2][:, k, :], rhs=wuq[:, k, lo:lo + n],
                                              start=(k == 0), stop=(k == 2)),
                     reads=[c.R("cqnT", t, 2), "wuq"], writes=[("pq", pi)])
        c.W("qsb", t, 2)
        _ql = int(os.environ.get("DBG_QL", 9))
        c.op("act", lambda e: e.activation(out=qsb[t % 2][:, :, 0:64], in_=PS[3][:].rearrange("p (h d) -> p h d", h=8),
                                           func=AF.Copy), reads=[("pq", 3)], writes=[("qsb", t % 2)])
        z4 = PS[4][:, 0:256].rearrange("p (h t d) -> p h t d", h=8, t=2)
        cb = cosQ[:, t, 0:16].unsqueeze(1).unsqueeze(1).broadcast_to([128, 8, 2, 16])
        sb = sinQ[:, t, 0:16].unsqueeze(1).unsqueeze(1).broadcast_to([128, 8, 2, 16])
        o4 = qsb[t % 2][:, :, 64:96].rearrange("p h (t d) -> p h t d", t=2)
        rope(o4, z4, cb, sb, qr1[:].rearrange("p h (t d) -> p h t d", t=2),
             qr2[:].rearrange("p h (t d) -> p h t d", t=2), "pool",
             [("pq", 4), "rtQsin", "rtQcos"], [("qsb", t % 2)], "qr")
        if _ql < 3:
            return
        pt = PS[5][:].bitcast(BF16)
        for h in range(8):
            c.op("pe", lambda e: e.transpose(out=pt[0:96, h * 128:(h + 1) * 128], in_=qsb[t % 2][:, h, :],
                                             identity=identb[:]), reads=[("qsb", t % 2), "const"], writes=["ptQ2"])
        if _ql < 4:
            return
        c.op("act", lambda e: e.activation(out=QT[0:96, :, t * 128:(t + 1) * 128],
                                           in_=pt[0:96, :].rearrange("p (h n) -> p h n", h=8), func=AF.Copy),
             reads=["ptQ2"], writes=["QT"])

    pipeline(int(os.environ.get("DBG_NQ", NO)), [Q_s0, Q_s0b, Q_s1, Q_s2])
    print("sbuf remaining in pass Q:", nc.sbuf_bytes_remaining, "ops", c.nops)
    c.pop()
    dbg("QT", QT[:], [128, 8, NO * 128], BF16)
    if stop == "Q":
        return finish()

    c.push()
    PS = [c.psum(f"pt_{i}", [128, 512], F32) for i in range(8)]
    wukv = c.sbuf("wukv", [128, 2, 1024], BF16)
    c.push()
    wukv_f = c.sbuf("wukv_f", [128, 2, 1024], F32)
    gkv = c.sbuf("gkv", [128, 2], F32)
    c.dma("sp", wukv_f[:], w_ukv.rearrange("(k p) n -> p k n", p=128), writes=["wukv_f"], key="wT0")
    c.dma("sp", gkv[:], gckv, writes=["gkv"], key="wT1")
    for k in range(2):
        c.op("dve", lambda e: e.tensor_scalar(out=wukv[:, k, :], in0=wukv_f[:, k, :], scalar1=gkv[:, k:k + 1],
                                              scalar2=None, op0=ALU.mult), reads=["wukv_f", "gkv"], writes=["wukv"])
    c.pop()
    otb = [c.sbuf("otb", [64, 512], BF16) for _ in range(2)]
    Vb = [c.sbuf("Vb", [128, NT, 65], BF16) for _ in range(2)]
    for i in range(2):
        c.op("pool", lambda e: e.memset(Vb[i][:, :, 64:65], 1.0), writes=[("Vb1", i)])
    PT = [c.sbuf("PT", [128, 512], BF16) for _ in range(4)]
    osb = [c.sbuf("osb", [65, 512], F32) for _ in range(2)]
    rec = [c.sbuf("rec", [64, 512], F32) for _ in range(2)]
    fin = [0]

    def up_units(h):
        kt_buf, v_buf = KT[h % 2], Vb[h % 2]
        units = []

        def k_unit(kc):
            def f():
                pk = PS[kc % 2]
                for k in range(2):
                    c.op("pe", lambda e: e.matmul(pk[0:64, :], lhsT=wukv[:, k, h * 128:h * 128 + 64],
                                                  rhs=ckvnT[:, k, kc * 512:(kc + 1) * 512], start=(k == 0), stop=(k == 1)),
                         reads=["wukv", "ckvnT"], writes=[("pk", kc % 2)])
                c.op("dve", lambda e: e.tensor_copy(out=kt_buf[0:64, kc * 512:(kc + 1) * 512], in_=pk[0:64, :]),
                     reads=[("pk", kc % 2)], writes=[("KTn", h % 2)])
            return f

        def v_unit(kb):
            def f():
                pv = PS[kb % 2]
                for j8 in range(8):
                    kt = kb * 8 + j8
                    for k in range(2):
                        c.op("pe", lambda e: e.matmul(pv[:, j8 * 64:(j8 + 1) * 64], lhsT=ckvnT[:, k, kt * 128:(kt + 1) * 128],
                                                      rhs=wukv[:, k, h * 128 + 64:h * 128 + 128], start=(k == 0),
                                                      stop=(k == 1)), reads=["wukv", "ckvnT"], writes=[("pk", kb % 2)])
                c.op("dve", lambda e: e.tensor_copy(out=v_buf[:, kb * 8:(kb + 1) * 8, 0:64],
                                                    in_=pv[:].rearrange("p (j d) -> p j d", j=8)),
                     reads=[("pk", kb % 2)], writes=[("Vb", h % 2)])
            return f

        for kc in range(16):
            units.append(k_unit(kc))
        for kb in range(8):
            units.append(v_unit(kb))
        return units

    steps = [(h, qc, kt) for h in range(8) for qc in range(4) for kt in range(16 * qc + 16)]
    pending = {}
    c.W("KTn", 0, 2)
    c.W("Vb", 0, 2)
    for u in up_units(0):
        u()

    def att_s0(i):
        h, qc, kt = steps[i]
        kt_buf = KT[h % 2]
        if qc == 0 and kt == 0 and h + 1 < 8:
            pending["units"] = up_units(h + 1)
            pending["local"] = 0
            pending["armed"] = False
        if pending.get("units"):
            pending["local"] += 1
            if pending["local"] >= 4 and (pending["local"] - 4) % 6 == 0:
                if not pending["armed"]:
                    c.W("KTn", h + 1, 2)
                    c.W("Vb", h + 1, 2)
                    pending["armed"] = True
                pending["units"].pop(0)()
        gk, l = kt // 4, kt % 4
        c0 = (max(gk, 4 * qc) - 4 * qc) * 128
        ps, pt_ = PS[2 + i % 4], PT[i % 4]
        c.op("pe", lambda e: e.matmul(ps[:, c0:512], lhsT=kt_buf[0:96, kt * 128:(kt + 1) * 128],
                                      rhs=QT[0:96, h, qc * 512 + c0:(qc + 1) * 512], start=True, stop=True),
             reads=[("KTn", h % 2), f"KT{h % 2}r", "QT"], writes=[("ps", i % 4)])
        c.op("act", lambda e: e.activation(out=pt_[:, c0:512], in_=ps[:, c0:512], func=AF.Exp, scale=SCALE_MLA),
             reads=[("ps", i % 4)], writes=[("PT", i % 4)])
        if gk >= 4 * qc:
            c.op("pool", lambda e: e.tensor_tensor(out=pt_[:, c0:c0 + 128], in0=pt_[:, c0:c0 + 128],
                                                   in1=amask[:, l, :], op=ALU.mult),
                 reads=[("PT", i % 4), "const"], writes=[("PT", i % 4)])

    def att_s1(i):
        pass

    def att_s2(i):
        h, qc, kt = steps[i]
        v_buf = Vb[h % 2]
        nk = 16 * qc + 16
        gk = kt // 4
        c0 = (max(gk, 4 * qc) - 4 * qc) * 128
        po = PS[6 + qc % 2]
        pt_ = PT[i % 4]
        c.op("pe", lambda e: e.matmul(po[0:65, c0:512], lhsT=v_buf[:, kt, 0:65], rhs=pt_[:, c0:512],
                                      start=(kt == 0), stop=(kt == nk - 1)),
             reads=[("Vb", h % 2), ("Vb1", h % 2), ("PT", i % 4)], writes=[("po", qc % 2)])
        if kt == nk - 1:
            f = fin[0]
            fin[0] += 1
            ob, rc = osb[f % 2], rec[f % 2]
            c.op("dve", lambda e: e.tensor_copy(out=ob[:], in_=po[0:65, :]), reads=[("po", qc % 2)],
                 writes=[("osb", f % 2)])
            pd = PS[f % 2]
            c.op("pe", lambda e: e.matmul(pd[0:64, :], lhsT=onesf[64:65, 0:64], rhs=ob[64:65, :], start=True, stop=True),
                 reads=[("osb", f % 2), "const"], writes=[("pk", f % 2)])
            c.op("dve", lambda e: e.reciprocal(out=rc[:], in_=pd[0:64, :]), reads=[("pk", f % 2)], writes=[("rec", f % 2)])
            c.op("dve", lambda e: e.tensor_tensor(out=otb[f % 2][:], in0=ob[0:64, :], in1=rc[:],
                                                  op=ALU.mult), reads=[("osb", f % 2), ("rec", f % 2)], writes=[("otb", f % 2)])
            c.dma("sp", ot_d[h, :, qc * 512:(qc + 1) * 512], otb[f % 2][:], reads=[("otb", f % 2)], writes=["ot_d"],
                  key=f"otw{f % 2}")

    pipeline(len(steps), [att_s0, att_s1, att_s2])
    c.pop()
    c.pop()
    if stop == "T":
        return finish()

    c.push()
    PS = [c.psum(f"pc_{i}", [128, 512], F32) for i in range(8)]
    WC = c.sbuf("WC", [128, 8, 3072], BF16)
    load_w(WC[:, :, 0:1024], w_in[:, O_RQ:O_RQ + 1024].rearrange("(k p) n -> p k n", p=128), "WC", "wC0")
    load_w(WC[:, :, 1024:2048], w_in[:, O_RV:O_RV + 1024].rearrange("(k p) n -> p k n", p=128), "WC", "wC1")
    load_w(WC[:, :, 2048:3072], w_in[:, O_RG:O_RG + 1024].rearrange("(k p) n -> p k n", p=128), "WC", "wC2")
    wor = c.sbuf("wor", [128, 8, 1024], BF16)
    c.push()
    wor_f = c.sbuf("wor_f", [128, 8, 1024], F32)
    gr = c.sbuf("gr", [128, 8], F32)
    c.dma("sp", wor_f[:], w_o_ret.rearrange("(k p) n -> p k n", p=128), writes=["wor_f"], key="wC3")
    c.dma("sp", gr[:], gret, writes=["gr"], key="wC4")
    for k in range(8):
        c.op("dve", lambda e: e.tensor_scalar(out=wor[:, k, :], in0=wor_f[:, k, :], scalar1=gr[:, k:k + 1],
                                              scalar2=None, op0=ALU.mult), reads=["wor_f", "gr"], writes=["wor"])
    c.pop()
    kdecC = c.sbuf("kdecC", [128, 8, 128], F32)
    c.dma("sp", kdecC[:, 0:4, :], qdec_d, writes=["kdecC"], key="wC5")
    c.dma("sp", kdecC[:, 4:8, :], kdecC_d, writes=["kdecC"], key="wC6")
    ht = HT(xown, 3, A1, B1, PS[0])
    sinC = c.sbuf("sinC", [128, NO, 80], F32)
    cosC = c.sbuf("cosC", [128, NO, 80], F32)
    c.push()
    rtmpC = [c.sbuf("rtmpC", [128, NO, 64], F32) for _ in range(3)]
    rope_tables(posf[:, NT:NT + NO], NO, sinC, cosC, rtmpC, 16, 80, "rtC")
    c.pop()
    qk1 = c.sbuf("qk1", [128, 1024], F32)
    qk2 = c.sbuf("qk2", [128, 1024], F32)
    qk3 = c.sbuf("qk3", [128, 1024], F32)
    qkp = [c.sbuf("qkp", [128, 8, 128], BF16) for _ in range(2)]
    qkT = [c.sbuf("qkT", [128, 8, 128], BF16) for _ in range(2)]
    vbc = [c.sbuf("vbc", [128, 1024], BF16) for _ in range(2)]
    sg = [c.sbuf("sg", [128, 1024], BF16) for _ in range(2)]
    scT = [c.sbuf("scT", [128, 4, 128], BF16) for _ in range(2)]
    sob = [c.sbuf("sob", [128, 1024], BF16) for _ in range(2)]
    bnst = c.sbuf("bnst", [128, NO, 4, 6], F32)
    bnag = c.sbuf("bnag", [128, NO, 4, 2], F32)
    bnr = c.sbuf("bnr", [128, NO, 4, 2], F32)
    onr = [c.sbuf("onr", [128, 1024], F32) for _ in range(2)]
    gat = [c.sbuf("gat", [128, 1024], BF16) for _ in range(2)]
    gT = [c.sbuf("gT", [128, 8, 128], BF16) for _ in range(2)]
    bbt = [c.sbuf("bbt", [128, 1024], BF16) for _ in range(2)]

    def C1_s0(t):
        if t == 0:
            ht.load(0)
            ht.load(1)
        if t + 2 < NO:
            ht.load(t + 2)
        c.dma("sp", sob[t % 2][:], sown_d[t], reads=[], writes=[c.W("sob", t, 2)], key=f"sob{t % 2}")
        ht.norm(t)

    def C1_s0b(t):
        ht.transpose(t)

    def C1_s1(t):
        hT, hk = ht.get(t)
        for (pi, lo) in [(1, 0), (2, 512), (3, 1024), (4, 1536), (5, 2048), (6, 2560)]:
            for k in range(8):
                c.op("pe", lambda e: e.matmul(PS[pi][:], lhsT=hT[:, k, :], rhs=WC[:, k, lo:lo + 512], start=(k == 0),
                                              stop=(k == 7)), reads=[hk, "WC"], writes=[("pz", pi)])
        cb = cosC[:, t, 16:80].unsqueeze(1).unsqueeze(1).broadcast_to([128, 4, 2, 64])
        sb = sinC[:, t, 16:80].unsqueeze(1).unsqueeze(1).broadcast_to([128, 4, 2, 64])
        for j in range(2):
            z4 = PS[1 + j][:].rearrange("p (h t d) -> p h t d", h=4, t=2)
            sl = slice(j * 512, (j + 1) * 512)
            rope(qk3[:, sl].rearrange("p (h t d) -> p h t d", h=4, t=2), z4, cb, sb,
                 qk1[:, sl].rearrange("p (h t d) -> p h t d", h=4, t=2),
                 qk2[:, sl].rearrange("p (h t d) -> p h t d", h=4, t=2), "pool",
                 [("pz", 1 + j), "rtCsin", "rtCcos"], [("qk3", j)], f"qk{j}")
        c.op("pool", lambda e: e.tensor_tensor(out=qkp[t % 2][:], in0=qk3[:].rearrange("p (h d) -> p h d", h=8),
                                               in1=kdecC[:], op=ALU.mult), reads=[("qk3", 0), ("qk3", 1), "kdecC"],
             writes=[c.W("qkp", t, 2)])
        c.W("vbc", t, 2)
        c.W("sg", t, 2)
        for hh in range(2):
            c.op("act", lambda e: e.activation(out=vbc[t % 2][:, hh * 512:(hh + 1) * 512], in_=PS[3 + hh][:],
                                               func=AF.Copy), reads=[("pz", 3 + hh)], writes=[("vbc", t % 2)])
            c.op("act", lambda e: e.activation(out=sg[t % 2][:, hh * 512:(hh + 1) * 512], in_=PS[5 + hh][:],
                                               func=AF.Silu), reads=[("pz", 5 + hh)], writes=[("sg", t % 2)])

    def C1_s2(t):
        pt = PS[7][:].bitcast(BF16)
        for j in range(8):
            c.op("pe", lambda e: e.transpose(out=pt[:, j * 128:(j + 1) * 128], in_=qkp[t % 2][:, j, :],
                                             identity=identb[:]), reads=[c.R("qkp", t, 2), "const"], writes=["ptC"])
        c.op("act", lambda e: e.activation(out=qkT[t % 2][:], in_=pt.rearrange("p (j n) -> p j n", j=8), func=AF.Copy),
             reads=["ptC"], writes=[c.W("qkT", t, 2)])
        for h in range(4):
            c.op("pe", lambda e: e.matmul(PS[1][:, h * 128:(h + 1) * 128], lhsT=qkT[t % 2][:, 4 + h, :],
                                          rhs=qkT[t % 2][:, h, :], start=True, stop=True),
                 reads=[c.R("qkT", t, 2)], writes=[("pz", 1)])
        c.op("dve", lambda e: e.tensor_tensor(out=scT[t % 2][:], in0=PS[1][:].rearrange("p (h n) -> p h n", h=4),
                                              in1=tri[:].unsqueeze(1).broadcast_to([128, 4, 128]), op=ALU.mult),
             reads=[("pz", 1), "const"], writes=[c.W("scT", t, 2)])
        for h in range(4):
            po = PS[3 + h // 2][:, (h % 2) * 256:(h % 2 + 1) * 256]
            c.op("pe", lambda e: e.matmul(po, lhsT=scT[t % 2][:, h, :], rhs=vbc[t % 2][:, h * 256:(h + 1) * 256],
                                          start=True, stop=False), reads=[c.R("scT", t, 2), c.R("vbc", t, 2)],
                 writes=[("pz", 3 + h // 2)])
            c.op("pe", lambda e: e.matmul(po, lhsT=qkT[t % 2][:, h, :], rhs=sob[t % 2][:, h * 256:(h + 1) * 256],
                                          start=False, stop=True), reads=[c.R("qkT", t, 2), c.R("sob", t, 2)],
                 writes=[("pz", 3 + h // 2)])
        for h in range(4):
            po = PS[3 + h // 2][:, (h % 2) * 256:(h % 2 + 1) * 256]
            c.op("dve", lambda e: e.bn_stats(out=bnst[:, t, h, :], in_=po), reads=[("pz", 3 + h // 2)],
                 writes=[("bnst", t)])
            c.op("dve", lambda e: e.bn_aggr(out=bnag[:, t, h, :], in_=bnst[:, t, h, :]), reads=[("bnst", t)],
                 writes=[("bnag", t)])
        c.op("dve", lambda e: e.tensor_scalar(out=bnr[:, t, :, 0], in0=bnag[:, t, :, 1], scalar1=GN_EPS, scalar2=None,
                                              op0=ALU.add), reads=[("bnag", t)], writes=[("bnr0", t)])
        rsqrt_cols(bnr[:, t, :, 0], bnr[:, t, :, 1], 4, [("bnr0", t)], [("bnr1", t)])
        c.W("onr", t, 2)
        for h in range(4):
            po = PS[3 + h // 2][:, (h % 2) * 256:(h % 2 + 1) * 256]
            c.op("dve", lambda e: e.tensor_scalar(out=onr[t % 2][:, h * 256:(h + 1) * 256], in0=po,
                                                  scalar1=bnag[:, t, h, 0:1], scalar2=bnr[:, t, h, 1:2],
                                                  op0=ALU.subtract, op1=ALU.mult),
                 reads=[("pz", 3 + h // 2), ("bnag", t), ("bnr1", t)], writes=[("onr", t % 2)])
        c.op("pool", lambda e: e.tensor_tensor(out=gat[t % 2][:], in0=onr[t % 2][:], in1=sg[t % 2][:], op=ALU.mult),
             reads=[("onr", t % 2), c.R("sg", t, 2)], writes=[c.W("gat", t, 2)])

    def C1_s3(t):
        pt = PS[7][:].bitcast(BF16)
        for k in range(8):
            c.op("pe", lambda e: e.transpose(out=pt[:, k * 128:(k + 1) * 128], in_=gat[t % 2][:, k * 128:(k + 1) * 128],
                                             identity=identb[:]), reads=[c.R("gat", t, 2), "const"], writes=["ptC"])
        c.op("act", lambda e: e.activation(out=gT[t % 2][:], in_=pt.rearrange("p (j n) -> p j n", j=8), func=AF.Copy),
             reads=["ptC"], writes=[c.W("gT", t, 2)])
        c.W("bbt", t, 2)
        for hh in range(2):
            for k in range(8):
                c.op("pe", lambda e: e.matmul(PS[5 + hh][:], lhsT=gT[t % 2][:, k, :], rhs=wor[:, k, hh * 512:(hh + 1) * 512],
                                              start=(k == 0), stop=(k == 7)), reads=[c.R("gT", t, 2), "wor"],
                     writes=[("pz", 5 + hh)])
            c.op("act", lambda e: e.activation(out=bbt[t % 2][:, hh * 512:(hh + 1) * 512], in_=PS[5 + hh][:],
                                               func=AF.Copy), reads=[("pz", 5 + hh)], writes=[("bbt", t % 2)])
        c.dma("sp", bb_d[t], bbt[t % 2][:], reads=[("bbt", t % 2)], writes=["bb_d"], key=f"bbw{t % 2}")

    def C1_s123(t):
        C1_s1(t)
        C1_s2(t)
        C1_s3(t)

    pipeline(NO, [C1_s0, C1_s0b, C1_s123])
    print("sbuf remaining in pass C1:", nc.sbuf_bytes_remaining, "ops", c.nops)
    c.pop()
    if stop == "C1":
        return finish()

    h2T = c.sbuf("h2T", [128, 8, NO * 128], BF16)
    print("sbuf remaining before C2:", nc.sbuf_bytes_remaining)
    c.push()
    PS = [c.psum(f"pd_{i}", [128, 512], F32) for i in range(8)]
    WG = c.sbuf("WG", [128, 8, 2048], BF16)
    load_w(WG[:], w_in[:, O_GA:O_GA + 2048].rearrange("(k p) n -> p k n", p=128), "WG", "wD0")
    wom = c.sbuf("wom", [64, 8, 1024], BF16)
    load_w(wom[:], w_o_mla.rearrange("(h p) n -> p h n", p=64), "wom", "wD1")
    wout = c.sbuf("wout", [128, 8, 1024], BF16)
    load_w(wout[:], w_out.rearrange("(k p) n -> p k n", p=128), "wout", "wD2")
    wr = c.sbuf("wr", [128, 8, 64], F32)
    c.dma("sp", wr[:], w_router.rearrange("(k p) n -> p k n", p=128), writes=["wr"], key="wD3")
    ht = HT(xown, 4, A1, B1, PS[0])
    tg = [c.sbuf("tg", [128, 2048], BF16)] * 2
    ott = [c.sbuf("ott", [64, 8, 128], BF16) for _ in range(2)]
    bbr = [c.sbuf("bbr", [128, 1024], BF16) for _ in range(2)]
    m1 = c.sbuf("m1", [128, 1024], F32)
    m2 = c.sbuf("m2", [128, 1024], F32)
    mg = [c.sbuf("mg", [128, 1024], BF16) for _ in range(2)]
    mgT = [c.sbuf("mgT", [128, 8, 128], BF16) for _ in range(2)]
    ty = c.sbuf("ty", [128, 1024], F32)
    x1 = [c.sbuf("x1", [128, 1024], F32) for _ in range(2)]
    xs2 = [c.sbuf("xs2", [128, 1024], F32) for _ in range(2)]
    h2f = [c.sbuf("h2f", [128, 8, 128], F32) for _ in range(2)]
    st2 = c.sbuf("st2", [128, 3 * 64], F32)
    rt = c.sbuf("rt", [128, 2, 64 * 4 + 64 + 8 * 4], F32)

    def C2_s0(t):
        if t == 0:
            ht.load(0)
            ht.load(1)
        if t + 2 < NO:
            ht.load(t + 2)
        c.dma("sp", bbr[t % 2][:], bb_d[t], reads=["bb_d"], writes=[c.W("bbr", t, 2)], key=f"bbr{t % 2}")
        c.dma("sp", ott[t % 2][:], ot_d[:, :, t * 128:(t + 1) * 128].rearrange("h p n -> p h n"), reads=["ot_d"],
              writes=[c.W("ott", t, 2)], key=f"ott{t % 2}")
        ht.norm(t)

    def C2_s0b(t):
        ht.transpose(t)

    def C2_s1(t):
        hT, hk = ht.get(t)
        xt = ht.xt[t % 4]
        for j in range(4):
            for k in range(8):
                c.op("pe", lambda e: e.matmul(PS[1 + j][:], lhsT=hT[:, k, :], rhs=WG[:, k, j * 512:(j + 1) * 512],
                                              start=(k == 0), stop=(k == 7)), reads=[hk, "WG"], writes=[("pz", 1 + j)])
            c.op("act", lambda e: e.activation(out=tg[t % 2][:, j * 512:(j + 1) * 512], in_=PS[1 + j][:], func=AF.Tanh,
                                               scale=0.5), reads=[("pz", 1 + j)], writes=["tg"])
        for hh in range(2):
            for h in range(8):
                c.op("pe", lambda e: e.matmul(PS[5 + hh][:], lhsT=ott[t % 2][:, h, :],
                                              rhs=wom[:, h, hh * 512:(hh + 1) * 512], start=(h == 0), stop=(h == 7)),
                     reads=[c.R("ott", t, 2), "wom"], writes=[("pz", 5 + hh)])
            sl = slice(hh * 512, (hh + 1) * 512)
            c.op("dve", lambda e: e.scalar_tensor_tensor(out=m1[:, sl], in0=tg[t % 2][:, sl], scalar=1.0, in1=PS[5 + hh][:],
                                                         op0=ALU.add, op1=ALU.mult),
                 reads=["tg", ("pz", 5 + hh)], writes=[("m1", hh)])
        c.op("dve", lambda e: e.scalar_tensor_tensor(out=m2[:], in0=tg[t % 2][:, 1024:2048], scalar=1.0, in1=bbr[t % 2][:],
                                                     op0=ALU.add, op1=ALU.mult),
             reads=["tg", c.R("bbr", t, 2)], writes=["m2"])
        c.op("pool", lambda e: e.tensor_tensor(out=mg[t % 2][:], in0=m1[:], in1=m2[:], op=ALU.add),
             reads=[("m1", 0), ("m1", 1), "m2"], writes=[c.W("mg", t, 2)])
        pt = PS[7][:].bitcast(BF16)
        for k in range(8):
            c.op("pe", lambda e: e.transpose(out=pt[:, k * 128:(k + 1) * 128], in_=mg[t % 2][:, k * 128:(k + 1) * 128],
                                             identity=identb[:]), reads=[c.R("mg", t, 2), "const"], writes=["ptD"])
        c.op("act", lambda e: e.activation(out=mgT[t % 2][:], in_=pt.rearrange("p (j n) -> p j n", j=8), func=AF.Copy),
             reads=["ptD"], writes=[c.W("mgT", t, 2)])
        c.W("x1", t, 2)
        for hh in range(2):
            sl = slice(hh * 512, (hh + 1) * 512)
            for k in range(8):
                c.op("pe", lambda e: e.matmul(PS[1 + hh][:], lhsT=mgT[t % 2][:, k, :], rhs=wout[:, k, sl],
                                              start=(k == 0), stop=(k == 7)), reads=[c.R("mgT", t, 2), "wout"],
                     writes=[("pz", 1 + hh)])
            c.op("dve", lambda e: e.tensor_tensor(out=ty[:, sl], in0=PS[1 + hh][:], in1=GT1[:, sl], op=ALU.mult),
                 reads=[("pz", 1 + hh), "GT"], writes=[("ty", hh)])
            c.op("pool", lambda e: e.tensor_tensor(out=x1[t % 2][:, sl], in0=ty[:, sl], in1=xt[:, sl], op=ALU.add),
                 reads=[("ty", hh), c.R("xt", t, 4)], writes=[("x1", t % 2)])
        c.dma("sp", x1_d[t], x1[t % 2][:], reads=[("x1", t % 2)], writes=["x1_d"], key=f"x1w{t % 2}")
        m = t
        a, b, r = st2[:, m:m + 1], st2[:, 64 + m:65 + m], st2[:, 128 + m:129 + m]
        c.op("act", lambda e: e.activation(out=junk[:], in_=x1[t % 2][:], func=AF.Square, accum_out=a),
             reads=[("x1", t % 2)], writes=["junk", ("s2a", m)])
        c.op("dve", lambda e: e.tensor_scalar(out=b, in0=a, scalar1=1.0 / D, scalar2=RMS_EPS, op0=ALU.mult, op1=ALU.add),
             reads=[("s2a", m)], writes=[("s2b", m)])
        rsqrt_cols(b, r, 1, [("s2b", m)], [("s2r", m)])
        c.op("dve", lambda e: e.tensor_scalar(out=xs2[t % 2][:], in0=x1[t % 2][:], scalar1=r, scalar2=None, op0=ALU.mult),
             reads=[("x1", t % 2), ("s2r", m)], writes=[c.W("xs2", t, 2)])

    def C2_s2(t):
        c.W("h2f", t, 2)
        for k in range(8):
            pb = PS[3 + k // 4][:, (k % 4) * 128:(k % 4 + 1) * 128]
            c.op("pe", lambda e: e.transpose(out=pb, in_=xs2[t % 2][:, k * 128:(k + 1) * 128], identity=identf[:]),
                 reads=[c.R("xs2", t, 2), "const"], writes=[("pz", 3 + k // 4)])
        for k in range(8):
            pb = PS[3 + k // 4][:, (k % 4) * 128:(k % 4 + 1) * 128]
            c.op("act", lambda e: e.activation(out=h2f[t % 2][:, k, :], in_=pb, func=AF.Identity, scale=A2[:, k:k + 1],
                                               bias=B2[:, k:k + 1]), reads=[("pz", 3 + k // 4), "AB"],
                 writes=[("h2f", t % 2)])
        c.op("dve", lambda e: e.tensor_copy(out=h2T[:, :, t * 128:(t + 1) * 128], in_=h2f[t % 2][:]),
             reads=[("h2f", t % 2)], writes=["h2T"])
        for k in range(8):
            c.op("pe", lambda e: e.matmul(PS[5][:, 0:64], lhsT=h2f[t % 2][:, k, :], rhs=wr[:, k, :], start=(k == 0),
                                          stop=(k == 7)), reads=[("h2f", t % 2), "wr"], writes=[("pz", 5)])
        R_ = rt[:, t % 2, :]
        s_, bi, mb, sel = R_[:, 0:64], R_[:, 64:128], R_[:, 128:192], R_[:, 192:256]
        m8 = R_[:, 256:320]
        gs, g8, gm, gneg = R_[:, 320:328], R_[:, 328:336], R_[:, 336:344], R_[:, 344:352]
        rk_ = ("rt", t % 2)
        c.op("act", lambda e: e.activation(out=s_, in_=PS[5][:, 0:64], func=AF.Tanh, scale=0.5), reads=[("pz", 5)],
             writes=[rk_])
        c.op("dve", lambda e: e.tensor_scalar(out=s_, in0=s_, scalar1=0.5, scalar2=0.5, op0=ALU.mult, op1=ALU.add),
             reads=[rk_], writes=[rk_])
        c.op("dve", lambda e: e.tensor_tensor(out=bi, in0=s_, in1=brout_t[:], op=ALU.add), reads=[rk_, "const"],
             writes=[rk_])
        for g in range(8):
            c.op("dve", lambda e: e.max(out=m8[:, g * 8:(g + 1) * 8], in_=bi[:, g * 8:(g + 1) * 8]), reads=[rk_],
                 writes=[rk_])
        m83 = m8.rearrange("p (g k) -> p g k", g=8)
        c.op("dve", lambda e: e.tensor_tensor(out=gs, in0=m83[:, :, 0], in1=m83[:, :, 1], op=ALU.add), reads=[rk_],
             writes=[rk_])
        c.op("dve", lambda e: e.max(out=g8, in_=gs), reads=[rk_], writes=[rk_])
        c.op("dve", lambda e: e.tensor_scalar(out=gm, in0=gs, scalar1=g8[:, 3:4], scalar2=None, op0=ALU.is_ge),
             reads=[rk_], writes=[rk_])
        c.op("dve", lambda e: e.tensor_scalar(out=gneg, in0=gm, scalar1=-1.0, scalar2=8.0, op0=ALU.add, op1=ALU.mult),
             reads=[rk_], writes=[rk_])
        bi3, mb3 = bi.rearrange("p (g k) -> p g k", g=8), mb.rearrange("p (g k) -> p g k", g=8)
        c.op("dve", lambda e: e.tensor_tensor(out=mb3, in0=bi3, in1=gm.unsqueeze(2).broadcast_to([128, 8, 8]), op=ALU.mult),
             reads=[rk_], writes=[rk_])
        c.op("dve", lambda e: e.tensor_tensor(out=mb3, in0=mb3, in1=gneg.unsqueeze(2).broadcast_to([128, 8, 8]), op=ALU.add),
             reads=[rk_], writes=[rk_])
        c.op("dve", lambda e: e.max(out=g8, in_=mb), reads=[rk_], writes=[rk_])
        c.op("dve", lambda e: e.tensor_scalar(out=sel, in0=mb, scalar1=g8[:, 7:8], scalar2=None, op0=ALU.is_ge),
             reads=[rk_], writes=[rk_])
        c.op("dve", lambda e: e.tensor_tensor(out=sel, in0=sel, in1=s_, op=ALU.mult), reads=[rk_], writes=[rk_])
        c.op("dve", lambda e: e.tensor_reduce(out=gs[:, 0:1], in_=sel, axis=AX.X, op=ALU.add), reads=[rk_], writes=[rk_])
        c.op("dve", lambda e: e.reciprocal(out=gs[:, 1:2], in_=gs[:, 0:1]), reads=[rk_], writes=[rk_])
        c.op("dve", lambda e: e.tensor_scalar(out=comb[:, t, :], in0=sel, scalar1=gs[:, 1:2], scalar2=2.5, op0=ALU.mult,
                                              op1=ALU.mult), reads=[rk_], writes=["comb"])

    def C2_s12(t):
        C2_s1(t)
        C2_s2(t)

    pipeline(NO, [C2_s0, C2_s0b, C2_s12])
    print("sbuf remaining in pass C2:", nc.sbuf_bytes_remaining, "ops", c.nops)
    c.pop()
    dbg("h2T", h2T[:], [128, 8, NO * 128], BF16)
    dbg("comb", comb[:], [128, NO, 64], F32)
    if stop == "C2":
        return finish()

    c.push()
    PG = c.psum("pg", [128, 2048], F32)
    PY = [c.psum(f"py{i}", [128, 1024], F32) for i in range(2)]
    acc = c.sbuf("acc", [128, NO, 1024], F32)
    wgu = [c.sbuf("wgu", [128, 8, 512], BF16) for _ in range(2)]
    wdn = [c.sbuf("wdn", [128, 2, 1024], BF16) for _ in range(2)]
    sgm = [c.sbuf("sgm", [128, 1024], BF16) for _ in range(2)]
    actT = [c.sbuf("actT", [128, 2, 512], BF16) for _ in range(2)]
    def load_expert(ei):
        e_ = ei - 1
        sl = ei % 2
        srcs = (w_sg, w_su, w_sd) if e_ < 0 else (w_eg[e_], w_eu[e_], w_ed[e_])
        c.W("wexp", ei, 2)
        c.dma("pool", wgu[sl][:, :, 0:256], srcs[0].rearrange("(k p) n -> p k n", p=128), writes=[("wexp", sl)], key=f"we{sl}a")
        c.dma("pool", wgu[sl][:, :, 256:512], srcs[1].rearrange("(k p) n -> p k n", p=128), writes=[("wexp", sl)], key=f"we{sl}b")
        c.dma("pool", wdn[sl][:], srcs[2].rearrange("(k p) n -> p k n", p=128), writes=[("wexp", sl)], key=f"we{sl}c")

    units = [(ei, tc_) for ei in range(NEXP + 1) for tc_ in range(4)]
    load_expert(0)

    def moe_s0(u):
        ei, tc_ = units[u]
        sl, a_ = ei % 2, u % 2
        if tc_ == 0 and ei + 1 <= NEXP:
            load_expert(ei + 1)
        wk = c.R("wexp", ei, 2)
        for j in range(4):
            for k in range(8):
                c.op("pe", lambda e: e.matmul(PG[:, j * 512:(j + 1) * 512], lhsT=wgu[sl][:, k, j * 128:(j + 1) * 128],
                                              rhs=h2T[:, k, tc_ * 512:(tc_ + 1) * 512], start=(k == 0), stop=(k == 7)),
                     reads=[wk, "h2T"], writes=[("pg", j // 2)])
        c.op("act", lambda e: e.activation(out=sgm[a_][:], in_=PG[:, 0:1024], func=AF.Silu), reads=[("pg", 0)],
             writes=[("sgm", a_)])
        c.op("dve", lambda e: e.tensor_tensor(out=actT[a_][:].rearrange("p f n -> p (f n)"), in0=sgm[a_][:],
                                              in1=PG[:, 1024:2048], op=ALU.mult), reads=[("sgm", a_), ("pg", 1)],
             writes=[("actT", a_)])

    def moe_s1(u):
        ei, tc_ = units[u]
        e_ = ei - 1
        sl, a_ = ei % 2, u % 2
        wk = ("wexp", sl)
        for tt in range(4):
            i = tc_ * 4 + tt
            y_ = (u * 4 + tt) % 2
            for hh in range(2):
                for fc in range(2):
                    c.op("pe", lambda e: e.matmul(PY[y_][:, hh * 512:(hh + 1) * 512], lhsT=actT[a_][:, fc, tt * 128:(tt + 1) * 128],
                                                  rhs=wdn[sl][:, fc, hh * 512:(hh + 1) * 512], start=(fc == 0), stop=(fc == 1)),
                         reads=[("actT", a_), wk], writes=[("py", y_)])
            if e_ < 0:
                c.op("act", lambda e: e.activation(out=acc[:, i, :], in_=PY[y_][:], func=AF.Copy), reads=[("py", y_)],
                     writes=[("acc", i)])
            else:
                c.op("dve", lambda e: e.scalar_tensor_tensor(out=acc[:, i, :], in0=PY[y_][:], scalar=comb[:, i, e_:e_ + 1],
                                                             in1=acc[:, i, :], op0=ALU.mult, op1=ALU.add),
                     reads=[("py", y_), ("acc", i), "comb"], writes=[("acc", i)])

    pipeline(len(units), [moe_s0, moe_s1])
    x1r = [c.sbuf("x1r", [128, 1024], F32) for _ in range(2)]
    xo = [c.sbuf("xo", [128, 1024], F32) for _ in range(2)]
    yo = [c.sbuf("yo", [128, 1024], F32) for _ in range(2)]
    stf = c.sbuf("stf", [128, 3 * 64], F32)
    for t in range(NO):
        c.dma("sp", x1r[t % 2][:], x1_d[t], reads=["x1_d"], writes=[("x1r", t % 2)], key=f"x1r{t % 2}")
        c.op("dve", lambda e: e.tensor_tensor(out=xo[t % 2][:], in0=acc[:, t, :], in1=GT2[:], op=ALU.mult),
             reads=[("acc", t), "GT"], writes=[("xo", t % 2)])
        c.op("pool", lambda e: e.tensor_tensor(out=xo[t % 2][:], in0=xo[t % 2][:], in1=x1r[t % 2][:], op=ALU.add),
             reads=[("xo", t % 2), ("x1r", t % 2)], writes=[("xo", t % 2)])
        a, b, r = stf[:, t:t + 1], stf[:, 64 + t:65 + t], stf[:, 128 + t:129 + t]
        c.op("act", lambda e: e.activation(out=junk[:], in_=xo[t % 2][:], func=AF.Square, accum_out=a),
             reads=[("xo", t % 2)], writes=["junk", ("sfa", t)])
        c.op("dve", lambda e: e.tensor_scalar(out=b, in0=a, scalar1=1.0 / D, scalar2=RMS_EPS, op0=ALU.mult, op1=ALU.add),
             reads=[("sfa", t)], writes=[("sfb", t)])
        rsqrt_cols(b, r, 1, [("sfb", t)], [("sfr", t)])
        c.op("dve", lambda e: e.scalar_tensor_tensor(out=yo[t % 2][:], in0=xo[t % 2][:], scalar=r, in1=gfin_t[:],
                                                     op0=ALU.mult, op1=ALU.mult),
             reads=[("xo", t % 2), ("sfr", t), "const"], writes=[("yo", t % 2)])
        c.dma("sp", out_d[t * 128:(t + 1) * 128, :], yo[t % 2][:], reads=[("yo", t % 2)], writes=["out"], key=f"out{t % 2}")
    c.pop()
    c.close()
    return nc


_NC_CACHE = {}


def _consts(j):
    bf = ml_dtypes.bfloat16
    k = np.arange(128)
    tri = (k[:, None] <= k[None, :]).astype(np.float32)
    amask = np.zeros((128, 4, 128), np.float32)
    for l in range(4):
        if l < j:
            amask[:, l, :] = 1.0
        elif l == j:
            amask[:, l, :] = tri
    inv_mla = 1.0 / (10000.0 ** (np.arange(0, 32, 2, dtype=np.float32) / np.float32(32)))
    inv_ret = 1.0 / (10000.0 ** (np.arange(0, 128, 2, dtype=np.float32) / np.float32(128)))
    invf = np.broadcast_to(np.concatenate([inv_mla, inv_ret]).astype(np.float32)[None, :], (128, 80)).copy()
    g = np.array(GAMMA, np.float64)
    m = np.arange(128, dtype=np.float64)
    kdecA = (g[None, :] ** (127.0 - m[:, None])) * 128.0 ** -0.5
    kdecC = (g[None, :] ** (-m[:, None])) * 128.0 ** -0.5
    qdec = g[None, :] ** m[:, None]
    rep = lambda a: np.repeat(a[:, :, None], 128, axis=2).astype(np.float32)
    G = g ** 128
    rc = np.zeros((128, 16), np.float64)
    for h in range(4):
        rc[:, h * 4 + 0] = g[h] * G[h] ** j
        for l in range(3):
            rc[:, h * 4 + 1 + l] = g[h] * (G[h] ** (j - 1 - l)) if l < j else 0.0
    return dict(identb=np.eye(128).astype(bf), identf=np.eye(128, dtype=np.float32), tri=tri,
                amask=amask.astype(bf), invf=invf, kdecA=rep(kdecA), kdecC=rep(kdecC), qdec=rep(qdec),
                rcoef=rc.astype(np.float32))


def _col(v, nchunk):
    return np.ascontiguousarray(np.asarray(v, np.float32).reshape(nchunk, 128).T)


_BUILD_ARGS = {}


def kernel(x, c, positions, w_ada, b_ada, g_norm1, w_in, g_cq, w_uq, g_ckv, w_ukv, g_ret, w_o_mla, w_o_ret,
           w_out, g_norm2, w_router, b_router, w_exp_gate, w_exp_up, w_exp_down, w_sh_gate, w_sh_up, w_sh_down,
           g_final):
    f = lambda a: np.ascontiguousarray(np.asarray(a, dtype=np.float32))
    x = f(x)
    positions = np.asarray(positions).astype(np.int32)
    if "nc" not in _NC_CACHE:
        _NC_CACHE["nc"] = build(**_BUILD_ARGS)
    nc = _NC_CACHE["nc"]
    shared = dict(
        w_ada=f(w_ada), bada_row=f(b_ada).reshape(1, -1), g1c=_col(g_norm1, 8), g2c=_col(g_norm2, 8), w_in=f(w_in),
        gcq=_col(g_cq, 3), w_uq=f(w_uq), gckv=_col(g_ckv, 2), w_ukv=f(w_ukv), gret=_col(g_ret, 8), w_o_mla=f(w_o_mla),
        w_o_ret=f(w_o_ret), w_out=f(w_out), w_router=f(w_router),
        brout=np.ascontiguousarray(np.broadcast_to(f(b_router)[None, :], (128, 64))),
        w_exp_gate=f(w_exp_gate), w_exp_up=f(w_exp_up), w_exp_down=f(w_exp_down), w_sh_gate=f(w_sh_gate),
        w_sh_up=f(w_sh_up), w_sh_down=f(w_sh_down),
        gfin=np.ascontiguousarray(np.broadcast_to(f(g_final)[None, :], (128, 1024))),
    )
    in_maps = []
    for core in range(8):
        b, j = core // 4, core % 4
        xb = x[b]
        xt = xb.reshape(NT, 128, D)
        pt = positions[b].reshape(NT, 128)
        m = dict(shared)
        m.update(_consts(j))
        m["xall"] = xb
        m["xown"] = np.ascontiguousarray(xt[j::4].reshape(NO * 128, D))
        m["posall"] = np.ascontiguousarray(pt.T)
        m["posown"] = np.ascontiguousarray(pt[j::4].T)
        m["cvec"] = _col(c[b], 8)
        in_maps.append(m)
    res = run_bass_kernel_spmd(nc, in_maps, core_ids=list(range(8)))
    _NC_CACHE["res"] = res
    out = np.empty((2, S, D), np.float32)
    for core in range(8):
        b, j = core // 4, core % 4
        o = np.asarray(res.results[core]["out"]).reshape(NO, 128, D)
        out[b].reshape(NT, 128, D)[j::4] = o
    return out
```

```python
import math
from contextlib import ExitStack

import numpy as np
import ml_dtypes

import concourse.bass as bass
import concourse.mybir as mybir
from concourse.bass_utils import run_bass_kernel_spmd

F32 = mybir.dt.float32
BF16 = mybir.dt.bfloat16
I32 = mybir.dt.int32
AF = mybir.ActivationFunctionType
ALU = mybir.AluOpType
AX = mybir.AxisListType

D = 1024
S = 8192
NT = 64
NO = 16
D_IN = 5792
RMS_EPS = 1e-6
GN_EPS = 1e-5
TWO_PI = 2.0 * math.pi
MAGIC = 12582912.0
C1 = 6.28125
C2 = float(np.float32(TWO_PI - 6.28125))
C3 = float(TWO_PI - 6.28125 - float(np.float32(TWO_PI - 6.28125)))
SCALE_MLA = 96.0 ** -0.5
GAMMA = [1.0 - 2.0 ** (-5.0 - h) for h in range(4)]
NEXP = 64
import os
NOSAME = os.environ.get("NOSAME") == "1"

O_CQ, O_CKV, O_KR, O_RQ, O_RK, O_RV, O_RG, O_GA, O_GB = 0, 384, 640, 672, 1184, 1696, 2720, 3744, 4768


class Ctx:
    def __init__(self, nc):
        self.nc = nc
        self.stacks = [ExitStack()]
        self.eng = {"pe": nc.tensor, "act": nc.scalar, "dve": nc.vector, "pool": nc.gpsimd, "sp": nc.sync}
        self.sem, self.cnt = {}, {}
        for e in ("pe", "act", "dve", "pool"):
            self.sem[e] = self.stacks[0].enter_context(nc.semaphore("s_" + e))
            self.cnt[e] = 0
        self.dsem, self.dcnt = {}, {}
        self.waited = {e: {} for e in self.eng}
        self.last_w, self.readers, self.owner = {}, {}, {}
        self.nops = 0
        self.uid = 0

    def sbuf(self, name, shape, dtype):
        self.uid += 1
        return self.stacks[-1].enter_context(self.nc.sbuf_tensor(f"{name}_{self.uid}", list(shape), dtype))

    def psum(self, name, shape, dtype):
        self.uid += 1
        return self.stacks[-1].enter_context(self.nc.psum_tensor(f"{name}_{self.uid}", list(shape), dtype))

    def push(self):
        self.stacks.append(ExitStack())

    def pop(self):
        self.barrier()
        self.stacks.pop().close()

    def W(self, name, t=0, n=1):
        k = (name, t % n)
        self.owner[k] = t
        return k

    def R(self, name, t=0, n=1):
        k = (name, t % n)
        assert self.owner.get(k) == t, f"stale read {name} tile {t} owner {self.owner.get(k)}"
        return k

    PSUM_NAMES = {"pmod", "pc", "pgt", "pT", "pz", "ptA", "pkv", "pcq", "ptQ", "pq", "ptQ2", "pk", "ps", "po", "ptC",
                  "ptD", "pg", "py"}

    def _split(self, reads, writes):
        rd, wr = [], list(writes)
        for r in reads:
            base = r[0] if isinstance(r, tuple) else r
            if base in self.PSUM_NAMES:
                if r not in wr:
                    wr.append(r)
            else:
                rd.append(r)
        return rd, wr

    def _deps(self, reads, writes):
        deps = []
        for r in reads:
            t = self.last_w.get(r)
            if t is not None:
                deps.append(t)
        for w in writes:
            t = self.last_w.get(w)
            if t is not None:
                deps.append(t)
            deps.extend(self.readers.get(w, ()))
        return deps

    def _wait(self, eng, deps):
        best = {}
        for (skey, sem, val) in deps:
            if eng == "pe" and skey == "pe":
                continue
            if NOSAME and skey == eng:
                continue
            if val > best.get(skey, (None, 0))[1]:
                best[skey] = (sem, val)
        w = self.waited[eng]
        for skey, (sem, val) in best.items():
            if w.get(skey, 0) >= val:
                continue
            self.eng[eng].wait_ge(sem, val)
            w[skey] = val

    def _commit(self, ticket, reads, writes):
        for r in reads:
            self.readers.setdefault(r, []).append(ticket)
        for w in writes:
            self.last_w[w] = ticket
            self.readers[w] = []

    def op(self, eng, fn, reads=(), writes=()):
        reads, writes = self._split(reads, writes)
        self._wait(eng, self._deps(reads, writes))
        ins = fn(self.eng[eng])
        self.cnt[eng] += 1
        ins.then_inc(self.sem[eng], 1)
        t = (eng, self.sem[eng], self.cnt[eng])
        self._commit(t, reads, writes)
        self.nops += 1
        return t

    def dma(self, queue, out, in_, reads=(), writes=(), key=None):
        if key not in self.dsem:
            self.dsem[key] = self.stacks[0].enter_context(self.nc.semaphore("d_" + str(key)))
            self.dcnt[key] = 0
        self._wait(queue, self._deps(reads, writes))
        ins = self.eng[queue].dma_start(out=out, in_=in_)
        self.dcnt[key] += 16
        ins.then_inc(self.dsem[key], 16)
        t = ("d_" + str(key), self.dsem[key], self.dcnt[key])
        self._commit(t, reads, writes)
        return t

    def barrier(self):
        tickets = [(e, self.sem[e], self.cnt[e]) for e in self.sem if self.cnt[e] > 0]
        tickets += [("d_" + str(k), self.dsem[k], self.dcnt[k]) for k in self.dsem]
        for e in self.eng:
            self._wait(e, tickets)
        self.last_w, self.readers = {}, {}

    def close(self):
        self.barrier()
        while self.stacks:
            self.stacks.pop().close()


def pipeline(n, stages):
    ns = len(stages)
    for s in range(n + ns - 1):
        for k in range(ns - 1, -1, -1):
            t = s - k
            if 0 <= t < n:
                stages[k](t)


def build(stop=None, debug=False):
    nc = bass.Bass("TRN2", target_bir_lowering=False)
    dbg_out = {}

    def dbg(name, ap, shape, dt):
        if not debug:
            return
        d_ = nc.dram_tensor("dbg_" + name, list(shape), dt, kind="ExternalOutput").ap()
        dbg_out[name] = d_
        c.barrier()
        c.dma("sp", d_, ap, key="dbg_" + name)
        c.barrier()

    def finish():
        c.close()
        return nc

    def din(name, shape, dt=F32):
        return nc.dram_tensor(name, list(shape), dt, kind="ExternalInput").ap()

    xall = din("xall", [S, D])
    xown = din("xown", [NO * 128, D])
    posall = din("posall", [128, NT], I32)
    posown = din("posown", [128, NO], I32)
    cvec = din("cvec", [128, 8])
    w_ada = din("w_ada", [D, 6 * D])
    bada_row = din("bada_row", [1, 6 * D])
    g1c = din("g1c", [128, 8])
    g2c = din("g2c", [128, 8])
    w_in = din("w_in", [D, D_IN])
    gcq = din("gcq", [128, 3])
    w_uq = din("w_uq", [384, 768])
    gckv = din("gckv", [128, 2])
    w_ukv = din("w_ukv", [256, 1024])
    gret = din("gret", [128, 8])
    w_o_mla = din("w_o_mla", [512, 1024])
    w_o_ret = din("w_o_ret", [1024, 1024])
    w_out = din("w_out", [1024, 1024])
    w_router = din("w_router", [1024, 64])
    brout = din("brout", [128, 64])
    w_eg = din("w_exp_gate", [NEXP, 1024, 256])
    w_eu = din("w_exp_up", [NEXP, 1024, 256])
    w_ed = din("w_exp_down", [NEXP, 256, 1024])
    w_sg = din("w_sh_gate", [1024, 256])
    w_su = din("w_sh_up", [1024, 256])
    w_sd = din("w_sh_down", [256, 1024])
    gfin = din("gfin", [128, 1024])
    identb_d = din("identb", [128, 128], BF16)
    identf_d = din("identf", [128, 128])
    tri_d = din("tri", [128, 128])
    amask_d = din("amask", [128, 4, 128], BF16)
    invf_d = din("invf", [128, 80])
    kdecA_d = din("kdecA", [128, 4, 128])
    kdecC_d = din("kdecC", [128, 4, 128])
    qdec_d = din("qdec", [128, 4, 128])
    rcoef_d = din("rcoef", [128, 16])
    out_d = nc.dram_tensor("out", [NO * 128, D], F32, kind="ExternalOutput").ap()
    sown_d = nc.dram_tensor("sown_s", [NO, 128, 1024], BF16, kind="ExternalOutput").ap()
    bb_d = nc.dram_tensor("bb_s", [NO, 128, 1024], BF16, kind="ExternalOutput").ap()
    x1_d = nc.dram_tensor("x1_s", [NO, 128, 1024], F32, kind="ExternalOutput").ap()
    ot_d = nc.dram_tensor("ot_s", [8, 64, NO * 128], BF16, kind="ExternalOutput").ap()

    c = Ctx(nc)

    identb = c.sbuf("identb", [128, 128], BF16)
    identf = c.sbuf("identf", [128, 128], F32)
    tri = c.sbuf("tri", [128, 128], F32)
    amask = c.sbuf("amask", [128, 4, 128], BF16)
    invf = c.sbuf("invf", [128, 80], F32)
    rcoef = c.sbuf("rcoef", [128, 16], F32)
    nhalf = c.sbuf("nhalf", [128, 64], F32)
    onesf = c.sbuf("onesf", [128, 128], F32)
    AB = c.sbuf("AB", [128, 32], F32)
    GT1 = c.sbuf("GT1", [128, 1024], F32)
    GT2 = c.sbuf("GT2", [128, 1024], F32)
    gfin_t = c.sbuf("gfin_t", [128, 1024], F32)
    brout_t = c.sbuf("brout_t", [128, 64], F32)
    posf = c.sbuf("posf", [128, NT + NO], F32)
    junk = c.sbuf("junk", [128, 1024], BF16)
    comb = c.sbuf("comb", [128, NO, 64], F32)
    B1b = c.sbuf("B1b", [128, 8], BF16)
    onesb = c.sbuf("onesb", [1, 128], BF16)

    for (t_, d_, k_) in [(identb, identb_d, "c0"), (identf, identf_d, "c1"), (tri, tri_d, "c2"),
                         (amask, amask_d, "c3"), (invf, invf_d, "c4"), (rcoef, rcoef_d, "c5"),
                         (gfin_t, gfin, "c6"), (brout_t, brout, "c7")]:
        c.dma("sp", t_[:], d_, writes=["const"], key=k_)
    c.op("pool", lambda e: e.memset(nhalf[:], -0.5), writes=["const"])
    c.op("pool", lambda e: e.memset(onesf[:], 1.0), writes=["const"])
    c.barrier()
    if stop == "c":
        return finish()

    def rsqrt_cols(src_ap, dst_ap, ncols, rd, wr):
        c.op("pool", lambda e: e.tensor_tensor(out=dst_ap, in0=src_ap, in1=nhalf[:, 0:ncols], op=ALU.pow),
             reads=rd, writes=wr)

    c.push()
    PS = [c.psum(f"p0_{i}", [128, 512], F32) for i in range(8)]
    cv = c.sbuf("cv", [128, 8], F32)
    cs = c.sbuf("cs", [128, 8], F32)
    modrow = c.sbuf("modrow", [1, 6 * D], F32)
    brow = c.sbuf("brow", [1, 6 * D], F32)
    gcols = c.sbuf("gcols", [128, 16], F32)
    wab = [c.sbuf(f"wab{i}", [128, 8, 512], F32) for i in range(2)]
    posi = c.sbuf("posi", [128, NT + NO], I32)
    c.dma("sp", cv[:], cvec, writes=["cv"], key="p0a")
    c.dma("sp", brow[:], bada_row, writes=["brow"], key="p0b")
    c.dma("sp", gcols[:, 0:8], g1c, writes=["gcols"], key="p0c")
    c.dma("sp", gcols[:, 8:16], g2c, writes=["gcols"], key="p0d")
    c.dma("sp", posi[:, 0:NT], posall, writes=["posi"], key="p0e")
    c.dma("sp", posi[:, NT:NT + NO], posown, writes=["posi"], key="p0f")
    c.op("dve", lambda e: e.tensor_copy(out=posf[:], in_=posi[:]), reads=["posi"], writes=["posf"])
    c.op("act", lambda e: e.activation(out=cs[:], in_=cv[:], func=AF.Silu), reads=["cv"], writes=["cs"])
    for n in range(12):
        wt = wab[n % 2]
        c.dma("sp", wt[:], w_ada[:, n * 512:(n + 1) * 512].rearrange("(k p) n -> p k n", p=128),
              writes=[c.W("wab", n, 2)], key=f"wab{n % 2}")
        for k in range(8):
            c.op("pe", lambda e: e.matmul(PS[n % 2][0:1, :], lhsT=cs[:, k:k + 1], rhs=wt[:, k, :],
                                          start=(k == 0), stop=(k == 7)),
                 reads=[c.R("wab", n, 2), "cs"], writes=[("pmod", n % 2)])
        c.op("dve", lambda e: e.tensor_tensor(out=modrow[0:1, n * 512:(n + 1) * 512], in0=PS[n % 2][0:1, :],
                                              in1=brow[0:1, n * 512:(n + 1) * 512], op=ALU.add),
             reads=[("pmod", n % 2), "brow"], writes=["modrow"])
    if stop == "0a":
        dbg("modrow", modrow[:], [1, 6 * D], F32)
        c.pop()
        return finish()
    pc = PS[2]
    for idx, ch in enumerate(list(range(0, 16)) + list(range(24, 40))):
        c.op("pe", lambda e: e.matmul(pc[:, idx:idx + 1], lhsT=modrow[0:1, ch * 128:(ch + 1) * 128],
                                      rhs=onesf[0:1, 0:1], start=True, stop=True),
             reads=["modrow", "const"], writes=["pc"])
    c.op("dve", lambda e: e.scalar_tensor_tensor(out=AB[:, 0:8], in0=pc[:, 8:16], scalar=1.0, in1=gcols[:, 0:8],
                                                 op0=ALU.add, op1=ALU.mult), reads=["pc", "gcols"], writes=["AB"])
    c.op("dve", lambda e: e.tensor_copy(out=AB[:, 8:16], in_=pc[:, 0:8]), reads=["pc"], writes=["AB"])
    c.op("dve", lambda e: e.scalar_tensor_tensor(out=AB[:, 16:24], in0=pc[:, 24:32], scalar=1.0, in1=gcols[:, 8:16],
                                                 op0=ALU.add, op1=ALU.mult), reads=["pc", "gcols"], writes=["AB"])
    c.op("dve", lambda e: e.tensor_copy(out=AB[:, 24:32], in_=pc[:, 16:24]), reads=["pc"], writes=["AB"])
    if stop == "0b":
        c.pop()
        dbg("AB", AB[:], [128, 32], F32)
        return finish()
    for (dst, base, scl, pi) in [(GT1, 2048, 0.5, 4), (GT2, 5120, 1.0, 6)]:
        for hh in range(2):
            c.op("pe", lambda e: e.matmul(PS[pi + hh][:], lhsT=onesf[0:1, 0:128],
                                          rhs=modrow[0:1, base + hh * 512: base + (hh + 1) * 512],
                                          start=True, stop=True), reads=["modrow", "const"], writes=[("pgt", pi + hh)])
            c.op("act", lambda e: e.activation(out=dst[:, hh * 512:(hh + 1) * 512], in_=PS[pi + hh][:],
                                               func=AF.Copy, scale=scl), reads=[("pgt", pi + hh)], writes=["GT"])
    c.pop()
    dbg("AB", AB[:], [128, 32], F32)
    dbg("GT1", GT1[:], [128, 1024], F32)
    if stop == "0":
        return finish()

    A1, B1, A2, B2 = AB[:, 0:8], AB[:, 8:16], AB[:, 16:24], AB[:, 24:32]
    c.op("dve", lambda e: e.tensor_copy(out=B1b[:], in_=B1), reads=["AB"], writes=["B1b"])
    c.op("pool", lambda e: e.memset(onesb[:], 1.0), writes=["onesb"])

    def fold_adaln(W, ncols, wname, psb, pkey, tag):
        brow_ = c.sbuf("bw_" + tag, [1, ncols], BF16)
        for lo in range(0, ncols, 512):
            n = min(512, ncols - lo)
            for k in range(8):
                c.op("pe", lambda e: e.matmul(psb[0:1, 0:n], lhsT=B1b[:, k:k + 1], rhs=W[:, k, lo:lo + n], start=(k == 0),
                                              stop=(k == 7)), reads=[wname, "B1b"], writes=[pkey])
            c.op("act", lambda e: e.activation(out=brow_[0:1, lo:lo + n], in_=psb[0:1, 0:n], func=AF.Copy),
                 reads=[pkey], writes=["bw_" + tag])
        for k in range(8):
            c.op("dve", lambda e: e.tensor_scalar(out=W[:, k, :], in0=W[:, k, :], scalar1=A1[:, k:k + 1], scalar2=None,
                                                  op0=ALU.mult), reads=[wname, "AB"], writes=[wname])
        return brow_

    def bias_mm(ps_ap, brow_, lo, n, tag, wr):
        c.op("pe", lambda e: e.matmul(ps_ap, lhsT=onesb[0:1, 0:128], rhs=brow_[0:1, lo:lo + n], start=False, stop=True),
             reads=["onesb", "bw_" + tag], writes=wr)

    def load_w(dst_tile, src_ap, name, key):
        c.dma("pool", dst_tile, src_ap, writes=[name], key=key)

    def rope_tables(pos_ap, G, sinT, cosT, tmp, lo, hi, tag):
        n = hi - lo
        ang, u, r = (tmp[0][:, 0:G, 0:n], tmp[1][:, 0:G, 0:n], tmp[2][:, 0:G, 0:n])
        c.op("dve", lambda e: e.tensor_tensor(out=ang, in0=invf[:, lo:hi].unsqueeze(1).broadcast_to([128, G, n]),
                                              in1=pos_ap.unsqueeze(2).broadcast_to([128, G, n]), op=ALU.mult),
             reads=["posf", "const"], writes=[tag + "t0"])
        c.op("dve", lambda e: e.tensor_scalar(out=u, in0=ang, scalar1=1.0 / TWO_PI, scalar2=MAGIC,
                                              op0=ALU.mult, op1=ALU.add), reads=[tag + "t0"], writes=[tag + "t1"])
        c.op("dve", lambda e: e.tensor_scalar(out=u, in0=u, scalar1=-MAGIC, scalar2=None, op0=ALU.add),
             reads=[tag + "t1"], writes=[tag + "t1"])
        c.op("dve", lambda e: e.scalar_tensor_tensor(out=r, in0=u, scalar=-C1, in1=ang, op0=ALU.mult, op1=ALU.add),
             reads=[tag + "t1", tag + "t0"], writes=[tag + "t2"])
        c.op("dve", lambda e: e.scalar_tensor_tensor(out=r, in0=u, scalar=-C2, in1=r, op0=ALU.mult, op1=ALU.add),
             reads=[tag + "t1", tag + "t2"], writes=[tag + "t2"])
        c.op("dve", lambda e: e.scalar_tensor_tensor(out=r, in0=u, scalar=-C3, in1=r, op0=ALU.mult, op1=ALU.add),
             reads=[tag + "t1", tag + "t2"], writes=[tag + "t2"])
        c.op("dve", lambda e: e.tensor_scalar(out=r, in0=r, scalar1=-math.pi, scalar2=math.pi, op0=ALU.max, op1=ALU.min),
             reads=[tag + "t2"], writes=[tag + "t2"])
        c.op("act", lambda e: e.activation(out=sinT[:, 0:G, lo:hi], in_=r, func=AF.Sin), reads=[tag + "t2"],
             writes=[tag + "sin"])
        c.op("dve", lambda e: e.scalar_tensor_tensor(out=ang, in0=r, scalar=-1.0, in1=r, op0=ALU.mult, op1=ALU.max),
             reads=[tag + "t2"], writes=[tag + "t0"])
        c.op("dve", lambda e: e.tensor_scalar(out=ang, in0=ang, scalar1=-1.0, scalar2=math.pi / 2, op0=ALU.mult,
                                              op1=ALU.add), reads=[tag + "t0"], writes=[tag + "t0"])
        c.op("act", lambda e: e.activation(out=cosT[:, 0:G, lo:hi], in_=ang, func=AF.Sin), reads=[tag + "t0"],
             writes=[tag + "cos"])

    def rope(out4, z4, cos_b, sin_b, t1, t2, eng2, rd, wr, tmpname):
        c.op("dve", lambda e: e.tensor_tensor(out=t1, in0=z4, in1=cos_b, op=ALU.mult), reads=rd, writes=[tmpname + "1"])
        c.op("dve", lambda e: e.tensor_tensor(out=t2, in0=z4, in1=sin_b, op=ALU.mult), reads=rd, writes=[tmpname + "2"])
        c.op(eng2, lambda e: e.tensor_tensor(out=out4[:, :, 0, :], in0=t1[:, :, 0, :], in1=t2[:, :, 1, :], op=ALU.subtract),
             reads=[tmpname + "1", tmpname + "2"], writes=wr)
        c.op(eng2, lambda e: e.tensor_tensor(out=out4[:, :, 1, :], in0=t1[:, :, 1, :], in1=t2[:, :, 0, :], op=ALU.add),
             reads=[tmpname + "1", tmpname + "2"], writes=wr)

    class HT:
        def __init__(self, src, nb, A, B, pT):
            self.src, self.nb, self.A, self.B, self.pT = src, nb, A, B, pT
            self.xt = [c.sbuf("xt", [128, 1024], F32) for _ in range(nb)]
            self.xs = [c.sbuf("xs", [128, 1024], BF16) for _ in range(2)]
            self.hT = [c.sbuf("hT", [128, 8, 128], BF16) for _ in range(2)]
            self.st = c.sbuf("hst", [128, 3 * 64], F32)

        def load(self, t):
            c.dma("sp", self.xt[t % self.nb][:], self.src[t * 128:(t + 1) * 128, :],
                  writes=[c.W("xt", t, self.nb)], key=f"xt{t % self.nb}")

        def norm(self, t):
            xt, st = self.xt[t % self.nb], self.st
            a, b, r = st[:, t % 64:t % 64 + 1], st[:, 64 + t % 64:65 + t % 64], st[:, 128 + t % 64:129 + t % 64]
            c.op("act", lambda e: e.activation(out=junk[:], in_=xt[:], func=AF.Square, accum_out=a),
                 reads=[c.R("xt", t, self.nb)], writes=["junk", ("hsa", t % 64)])
            c.op("dve", lambda e: e.tensor_scalar(out=b, in0=a, scalar1=1.0 / D, scalar2=RMS_EPS, op0=ALU.mult,
                                                  op1=ALU.add), reads=[("hsa", t % 64)], writes=[("hsb", t % 64)])
            rsqrt_cols(b, r, 1, [("hsb", t % 64)], [("hsr", t % 64)])
            c.op("dve", lambda e: e.tensor_scalar(out=self.xs[t % 2][:], in0=xt[:], scalar1=r, scalar2=None,
                                                  op0=ALU.mult), reads=[c.R("xt", t, self.nb), ("hsr", t % 64)],
                 writes=[c.W("xs", t, 2)])

        def transpose(self, t):
            xs, hT, pT = self.xs[t % 2], self.hT[t % 2], self.pT
            pTb = pT[:].bitcast(BF16)
            for k in range(8):
                c.op("pe", lambda e: e.transpose(out=pTb[:, k * 128:(k + 1) * 128], in_=xs[:, k * 128:(k + 1) * 128],
                                                 identity=identb[:]), reads=[c.R("xs", t, 2), "const"], writes=["pT"])
            c.W("hT", t, 2)
            c.op("act", lambda e: e.activation(out=hT[:].rearrange("p k n -> p (k n)"), in_=pTb[:, :], func=AF.Copy),
                 reads=["pT"], writes=[("hT", t % 2)])

        def get(self, t):
            return self.hT[t % 2], c.R("hT", t, 2)

    c.push()
    ckvnT = c.sbuf("ckvnT", [128, 2, S], BF16)
    KT = [c.sbuf(f"KT{i}", [128, S], BF16) for i in range(2)]

    c.push()
    PS = [c.psum(f"pa_{i}", [128, 512], F32) for i in range(8)]
    WA = c.sbuf("WA", [128, 8, 1824], BF16)
    load_w(WA[:, :, 0:288], w_in[:, O_CKV:O_CKV + 288].rearrange("(k p) n -> p k n", p=128), "WA", "wA0")
    load_w(WA[:, :, 288:800], w_in[:, O_RK:O_RK + 512].rearrange("(k p) n -> p k n", p=128), "WA", "wA1")
    load_w(WA[:, :, 800:1824], w_in[:, O_RV:O_RV + 1024].rearrange("(k p) n -> p k n", p=128), "WA", "wA2")
    bwA = fold_adaln(WA, 1824, "WA", PS[1], ("pz", 1), "A")
    kdecA = c.sbuf("kdecA", [128, 4, 128], F32)
    c.dma("sp", kdecA[:], kdecA_d, writes=["kdecA"], key="kdA")
    ht = HT(xall, 3, A1, B1, PS[0])
    sinT = [c.sbuf("sinT", [128, 4, 80], F32) for _ in range(2)]
    cosT = [c.sbuf("cosT", [128, 4, 80], F32) for _ in range(2)]
    rtmp = [c.sbuf("rtmp", [128, 4, 80], F32) for _ in range(3)]
    stA = c.sbuf("stA", [128, 3 * 64], F32)
    ckvs = [c.sbuf("ckvs", [128, 256], BF16) for _ in range(2)]
    kst = [c.sbuf("kst", [128, 96], BF16) for _ in range(2)]
    kr1 = c.sbuf("kr1", [128, 32], F32)
    kr2 = c.sbuf("kr2", [128, 32], F32)
    rk1 = c.sbuf("rk1", [128, 512], F32)
    rk2 = c.sbuf("rk2", [128, 512], F32)
    rk3 = c.sbuf("rk3", [128, 512], F32)
    kp = [c.sbuf("kp", [128, 4, 128], BF16) for _ in range(2)]
    vb = [c.sbuf("vb", [128, 1024], BF16) for _ in range(2)]
    Sst = c.sbuf("Sst", [128, 1024], F32)
    Sown = [c.sbuf("Sown", [128, 1024], F32) for _ in range(2)]
    Sownb = [c.sbuf("Sownb", [128, 1024], BF16) for _ in range(2)]
    for i in range(2):
        c.op("pool", lambda e: e.memset(kst[i][:], 0.0), writes=[("kst", i)])
    c.op("pool", lambda e: e.memset(Sst[:], 0.0), writes=["Sst"])

    def A_s0(t):
        if t == 0:
            ht.load(0)
            ht.load(1)
        if t + 2 < NT:
            ht.load(t + 2)
        if t % 4 == 0:
            g = t // 4
            rope_tables(posf[:, t:t + 4], 4, sinT[g % 2], cosT[g % 2], rtmp, 0, 80, f"rtA{g % 2}")
            c.W("tabA", g, 2)
        ht.norm(t)

    def A_s0b(t):
        ht.transpose(t)

    def A_s1(t):
        hT, hk = ht.get(t)
        for (pi, lo, n) in [(1, 0, 288), (2, 288, 512), (3, 800, 512), (4, 1312, 512)]:
            for k in range(8):
                c.op("pe", lambda e: e.matmul(PS[pi][:, 0:n], lhsT=hT[:, k, :], rhs=WA[:, k, lo:lo + n],
                                              start=(k == 0), stop=False), reads=[hk, "WA"], writes=[("pz", pi)])
            bias_mm(PS[pi][:, 0:n], bwA, lo, n, "A", [("pz", pi)])
        m = t % 64
        a, b, r = stA[:, m:m + 1], stA[:, 64 + m:65 + m], stA[:, 128 + m:129 + m]
        c.op("act", lambda e: e.activation(out=junk[:, 0:256], in_=PS[1][:, 0:256], func=AF.Square, accum_out=a),
             reads=[("pz", 1)], writes=["junk", ("sAa", m)])
        c.op("dve", lambda e: e.tensor_scalar(out=b, in0=a, scalar1=1.0 / 256, scalar2=RMS_EPS, op0=ALU.mult,
                                              op1=ALU.add), reads=[("sAa", m)], writes=[("sAb", m)])
        rsqrt_cols(b, r, 1, [("sAb", m)], [("sAr", m)])
        c.op("dve", lambda e: e.tensor_scalar(out=ckvs[t % 2][:], in0=PS[1][:, 0:256], scalar1=r, scalar2=None,
                                              op0=ALU.mult), reads=[("pz", 1), ("sAr", m)], writes=[c.W("ckvs", t, 2)])
        g = t // 4
        tk = c.R("tabA", g, 2)
        sn, cs_ = sinT[g % 2], cosT[g % 2]
        z4 = PS[1][:, 256:288].rearrange("p (h t d) -> p h t d", h=1, t=2)
        cb = cs_[:, t % 4, 0:16].unsqueeze(1).unsqueeze(1).broadcast_to([128, 1, 2, 16])
        sb = sn[:, t % 4, 0:16].unsqueeze(1).unsqueeze(1).broadcast_to([128, 1, 2, 16])
        o4 = kst[t % 2][:, 64:96].rearrange("p (h t d) -> p h t d", h=1, t=2)
        rope(o4, z4, cb, sb, kr1[:].rearrange("p (h t d) -> p h t d", h=1, t=2),
             kr2[:].rearrange("p (h t d) -> p h t d", h=1, t=2), "pool",
             [("pz", 1), f"rtA{g % 2}sin", f"rtA{g % 2}cos"], [c.W("kst", t, 2)], "kr")
        z4 = PS[2][:].rearrange("p (h t d) -> p h t d", h=4, t=2)
        cb = cs_[:, t % 4, 16:80].unsqueeze(1).unsqueeze(1).broadcast_to([128, 4, 2, 64])
        sb = sn[:, t % 4, 16:80].unsqueeze(1).unsqueeze(1).broadcast_to([128, 4, 2, 64])
        o4 = rk3[:].rearrange("p (h t d) -> p h t d", h=4, t=2)
        rope(o4, z4, cb, sb, rk1[:].rearrange("p (h t d) -> p h t d", h=4, t=2),
             rk2[:].rearrange("p (h t d) -> p h t d", h=4, t=2), "pool",
             [("pz", 2), f"rtA{g % 2}sin", f"rtA{g % 2}cos"], ["rk3"], "rk")
        c.op("pool", lambda e: e.tensor_tensor(out=kp[t % 2][:], in0=rk3[:].rearrange("p (h d) -> p h d", h=4),
                                               in1=kdecA[:], op=ALU.mult), reads=["rk3", "kdecA"],
             writes=[c.W("kp", t, 2)])
        c.W("vb", t, 2)
        for hh in range(2):
            c.op("act", lambda e: e.activation(out=vb[t % 2][:, hh * 512:(hh + 1) * 512], in_=PS[3 + hh][:],
                                               func=AF.Copy), reads=[("pz", 3 + hh)], writes=[("vb", t % 2)])

    import os
    _lv = int(os.environ.get("DBG_LV", 9))

    def A_s2(t):
        pt = PS[5][:].bitcast(BF16)
        for k in range(2):
            c.op("pe", lambda e: e.transpose(out=pt[:, k * 128:(k + 1) * 128], in_=ckvs[t % 2][:, k * 128:(k + 1) * 128],
                                             identity=identb[:]), reads=[c.R("ckvs", t, 2), "const"], writes=["ptA"])
        c.op("pe", lambda e: e.transpose(out=pt[0:96, 256:384], in_=kst[t % 2][:], identity=identb[:]),
             reads=[c.R("kst", t, 2), "const"], writes=["ptA"])
        if _lv < 2:
            return
        for k in range(2):
            c.op("act", lambda e: e.activation(out=ckvnT[:, k, t * 128:(t + 1) * 128],
                                               in_=pt[:, k * 128:(k + 1) * 128], func=AF.Copy),
                 reads=["ptA"], writes=["ckvnT"])
        if _lv < 3:
            return
        _kt = os.environ.get("DBG_KT", "both")
        if _kt in ("both", "act"):
            c.op("act", lambda e: e.activation(out=KT[0][64:96, t * 128:(t + 1) * 128], in_=pt[64:96, 256:384],
                                               func=AF.Copy), reads=["ptA"], writes=["KT0r"])
        if _kt in ("both", "dve"):
            c.op("act", lambda e: e.activation(out=KT[1][64:96, t * 128:(t + 1) * 128], in_=pt[64:96, 256:384],
                                               func=AF.Copy), reads=["ptA"], writes=["KT1r"])
        if _lv < 4:
            return
        pkv = [PS[6], PS[7]]
        for h in range(4):
            c.op("pe", lambda e: e.matmul(pkv[h // 2][:, (h % 2) * 256:(h % 2 + 1) * 256], lhsT=kp[t % 2][:, h, :],
                                          rhs=vb[t % 2][:, h * 256:(h + 1) * 256], start=True, stop=True),
                 reads=[c.R("kp", t, 2), c.R("vb", t, 2)], writes=["pkv"])
        if _lv < 5:
            return
        l, g = t % 4, t // 4
        so = Sown[g % 2]
        if l == 0:
            c.W("Sown", g, 2)
        for h in range(4):
            hs = slice(h * 256, (h + 1) * 256)
            pk = pkv[h // 2][:, (h % 2) * 256:(h % 2 + 1) * 256]
            if l == 0:
                c.op("act", lambda e: e.activation(out=so[:, hs], in_=Sst[:, hs], func=AF.Copy,
                                                   scale=rcoef[:, h * 4:h * 4 + 1]), reads=["Sst", "const"],
                     writes=[("Sown", g % 2)])
            if l < 3:
                c.op("dve", lambda e: e.scalar_tensor_tensor(out=so[:, hs], in0=pk, scalar=rcoef[:, h * 4 + 1 + l:h * 4 + 2 + l],
                                                             in1=so[:, hs], op0=ALU.mult, op1=ALU.add),
                     reads=["pkv", ("Sown", g % 2), "const"], writes=[("Sown", g % 2)])
            c.op("dve", lambda e: e.scalar_tensor_tensor(out=Sst[:, hs], in0=Sst[:, hs], scalar=GAMMA[h] ** 128, in1=pk,
                                                         op0=ALU.mult, op1=ALU.add), reads=["pkv", "Sst"], writes=["Sst"])
        if l == 3 and _lv >= 6:
            c.op("act", lambda e: e.activation(out=Sownb[g % 2][:], in_=so[:], func=AF.Copy),
                 reads=[c.R("Sown", g, 2)], writes=[c.W("Sownb", g, 2)])
            c.dma("sp", sown_d[g], Sownb[g % 2][:], reads=[c.R("Sownb", g, 2)], writes=["sown_d"], key=f"so{g % 2}")

    import os
    _na = int(os.environ.get("DBG_NA", NT))
    _ns = int(os.environ.get("DBG_NS", 3))
    pipeline(_na, [A_s0, A_s0b, A_s1, A_s2][:_ns + 1])
    print("sbuf remaining in pass A:", nc.sbuf_bytes_remaining, "ops", c.nops)
    dbg("Sst", Sst[:], [128, 1024], F32)
    c.pop()
    dbg("ckvnT", ckvnT[:], [128, 2, S], BF16)
    dbg("KT0", KT[0][:], [128, S], BF16)
    if stop == "A":
        return finish()

    QT = c.sbuf("QT", [128, 8, NO * 128], BF16)
    c.push()
    PS = [c.psum(f"pq_{i}", [128, 512], F32) for i in range(8)]
    WQ = c.sbuf("WQ", [128, 8, 384], BF16)
    load_w(WQ[:], w_in[:, O_CQ:O_CQ + 384].rearrange("(k p) n -> p k n", p=128), "WQ", "wQ0")
    bwQ = fold_adaln(WQ, 384, "WQ", PS[1], "pcq", "Q")
    wuq = c.sbuf("wuq", [128, 3, 768], BF16)
    c.push()
    wuq_f = c.sbuf("wuq_f", [128, 3, 768], F32)
    gq = c.sbuf("gq", [128, 3], F32)
    c.dma("sp", wuq_f[:], w_uq.rearrange("(k p) n -> p k n", p=128), writes=["wuq_f"], key="wQ1")
    c.dma("sp", gq[:], gcq, writes=["gq"], key="wQ2")
    for k in range(3):
        wv = wuq_f[:, k, :].rearrange("p (h d) -> p h d", h=8)
        c.op("dve", lambda e: e.tensor_scalar(out=wuq[:, k, 0:512].rearrange("p (h d) -> p h d", h=8), in0=wv[:, :, 0:64],
                                              scalar1=gq[:, k:k + 1], scalar2=None, op0=ALU.mult),
             reads=["wuq_f", "gq"], writes=["wuq"])
        c.op("dve", lambda e: e.tensor_scalar(out=wuq[:, k, 512:768].rearrange("p (h d) -> p h d", h=8), in0=wv[:, :, 64:96],
                                              scalar1=gq[:, k:k + 1], scalar2=None, op0=ALU.mult),
             reads=["wuq_f", "gq"], writes=["wuq"])
    c.pop()
    ht = HT(xown, 3, A1, B1, PS[0])
    sinQ = c.sbuf("sinQ", [128, NO, 16], F32)
    cosQ = c.sbuf("cosQ", [128, NO, 16], F32)
    c.push()
    rtmpQ = [c.sbuf("rtmpQ", [128, NO, 16], F32) for _ in range(3)]
    rope_tables(posf[:, NT:NT + NO], NO, sinQ, cosQ, rtmpQ, 0, 16, "rtQ")
    c.pop()
    stQ = c.sbuf("stQ", [128, 3 * 64], F32)
    cqs = [c.sbuf("cqs", [128, 384], BF16) for _ in range(2)]
    cqnT = [c.sbuf("cqnT", [128, 3, 128], BF16) for _ in range(2)]
    qsb = [c.sbuf("qsb", [128, 8, 96], BF16) for _ in range(2)]
    qr1 = c.sbuf("qr1", [128, 8, 32], F32)
    qr2 = c.sbuf("qr2", [128, 8, 32], F32)

    def Q_s0(t):
        if t == 0:
            ht.load(0)
            ht.load(1)
        if t + 2 < NO:
            ht.load(t + 2)
        ht.norm(t)

    def Q_s0b(t):
        ht.transpose(t)

    def Q_s1(t):
        hT, hk = ht.get(t)
        for k in range(8):
            c.op("pe", lambda e: e.matmul(PS[1][:, 0:384], lhsT=hT[:, k, :], rhs=WQ[:, k, :], start=(k == 0),
                                          stop=False), reads=[hk, "WQ"], writes=["pcq"])
        bias_mm(PS[1][:, 0:384], bwQ, 0, 384, "Q", ["pcq"])
        m = t
        a, b, r = stQ[:, m:m + 1], stQ[:, 64 + m:65 + m], stQ[:, 128 + m:129 + m]
        c.op("act", lambda e: e.activation(out=junk[:, 0:384], in_=PS[1][:, 0:384], func=AF.Square, accum_out=a),
             reads=["pcq"], writes=["junk", ("sQa", m)])
        c.op("dve", lambda e: e.tensor_scalar(out=b, in0=a, scalar1=1.0 / 384, scalar2=RMS_EPS, op0=ALU.mult,
                                              op1=ALU.add), reads=[("sQa", m)], writes=[("sQb", m)])
        rsqrt_cols(b, r, 1, [("sQb", m)], [("sQr", m)])
        c.op("dve", lambda e: e.tensor_scalar(out=cqs[t % 2][:], in0=PS[1][:, 0:384], scalar1=r, scalar2=None,
                                              op0=ALU.mult), reads=["pcq", ("sQr", m)], writes=[c.W("cqs", t, 2)])
        pt = PS[2][:].bitcast(BF16)
        for k in range(3):
            c.op("pe", lambda e: e.transpose(out=pt[:, k * 128:(k + 1) * 128], in_=cqs[t % 2][:, k * 128:(k + 1) * 128],
                                             identity=identb[:]), reads=[c.R("cqs", t, 2), "const"], writes=["ptQ"])
        c.op("act", lambda e: e.activation(out=cqnT[t % 2][:], in_=pt[:, 0:384].rearrange("p (k n) -> p k n", k=3),
                                           func=AF.Copy), reads=["ptQ"], writes=[c.W("cqnT", t, 2)])

    def Q_s2(t):
        for (pi, lo, n) in [(3, 0, 512), (4, 512, 256)]:
            for k in range(3):
                c.op("pe", lambda e: e.matmul(PS[pi][:, 0:n], lhsT=cqnT[t % 2][:, k, :], rhs=wuq[:, k, lo:lo + n],
                                              start=(k == 0), stop=(k == 2)),
                     reads=[c.R("cqnT", t, 2), "wuq"], writes=[("pq", pi)])
        c.W("qsb", t, 2)
        _ql = int(os.environ.get("DBG_QL", 9))
        c.op("act", lambda e: e.activation(out=qsb[t % 2][:, :, 0:64], in_=PS[3][:].rearrange("p (h d) -> p h d", h=8),
                                           func=AF.Copy), reads=[("pq", 3)], writes=[("qsb", t % 2)])
        z4 = PS[4][:, 0:256].rearrange("p (h t d) -> p h t d", h=8, t=2)
        cb = cosQ[:, t, 0:16].unsqueeze(1).unsqueeze(1).broadcast_to([128, 8, 2, 16])
        sb = sinQ[:, t, 0:16].unsqueeze(1).unsqueeze(1).broadcast_to([128, 8, 2, 16])
        o4 = qsb[t % 2][:, :, 64:96].rearrange("p h (t d) -> p h t d", t=2)
        rope(o4, z4, cb, sb, qr1[:].rearrange("p h (t d) -> p h t d", t=2),
             qr2[:].rearrange("p h (t d) -> p h t d", t=2), "pool",
             [("pq", 4), "rtQsin", "rtQcos"], [("qsb", t % 2)], "qr")
        if _ql < 3:
            return
        pt = PS[5][:].bitcast(BF16)
        for h in range(8):
            c.op("pe", lambda e: e.transpose(out=pt[0:96, h * 128:(h + 1) * 128], in_=qsb[t % 2][:, h, :],
                                             identity=identb[:]), reads=[("qsb", t % 2), "const"], writes=["ptQ2"])
        if _ql < 4:
            return
        c.op("act", lambda e: e.activation(out=QT[0:96, :, t * 128:(t + 1) * 128],
                                           in_=pt[0:96, :].rearrange("p (h n) -> p h n", h=8), func=AF.Copy),
             reads=["ptQ2"], writes=["QT"])

    pipeline(int(os.environ.get("DBG_NQ", NO)), [Q_s0, Q_s0b, Q_s1, Q_s2])
    print("sbuf remaining in pass Q:", nc.sbuf_bytes_remaining, "ops", c.nops)
    c.pop()
    dbg("QT", QT[:], [128, 8, NO * 128], BF16)
    if stop == "Q":
        return finish()

    c.push()
    PS = [c.psum(f"pt_{i}", [128, 512], F32) for i in range(8)]
    wukv = c.sbuf("wukv", [128, 2, 1024], BF16)
    c.push()
    wukv_f = c.sbuf("wukv_f", [128, 2, 1024], F32)
    gkv = c.sbuf("gkv", [128, 2], F32)
    c.dma("sp", wukv_f[:], w_ukv.rearrange("(k p) n -> p k n", p=128), writes=["wukv_f"], key="wT0")
    c.dma("sp", gkv[:], gckv, writes=["gkv"], key="wT1")
    for k in range(2):
        c.op("dve", lambda e: e.tensor_scalar(out=wukv[:, k, :], in0=wukv_f[:, k, :], scalar1=gkv[:, k:k + 1],
                                              scalar2=None, op0=ALU.mult), reads=["wukv_f", "gkv"], writes=["wukv"])
    c.pop()
    otb = [c.sbuf("otb", [64, 512], BF16) for _ in range(2)]
    Vb = [c.sbuf("Vb", [128, NT, 65], BF16) for _ in range(2)]
    for i in range(2):
        c.op("pool", lambda e: e.memset(Vb[i][:, :, 64:65], 1.0), writes=[("Vb1", i)])
    PT = [c.sbuf("PT", [128, 512], BF16) for _ in range(4)]
    osb = [c.sbuf("osb", [65, 512], F32) for _ in range(2)]
    rec = [c.sbuf("rec", [64, 512], F32) for _ in range(2)]
    fin = [0]

    def up_units(h):
        kt_buf, v_buf = KT[h % 2], Vb[h % 2]
        units = []

        def k_unit(kc):
            def f():
                pk = PS[kc % 2]
                for k in range(2):
                    c.op("pe", lambda e: e.matmul(pk[0:64, :], lhsT=wukv[:, k, h * 128:h * 128 + 64],
                                                  rhs=ckvnT[:, k, kc * 512:(kc + 1) * 512], start=(k == 0), stop=(k == 1)),
                         reads=["wukv", "ckvnT"], writes=[("pk", kc % 2)])
                c.op("dve", lambda e: e.tensor_copy(out=kt_buf[0:64, kc * 512:(kc + 1) * 512], in_=pk[0:64, :]),
                     reads=[("pk", kc % 2)], writes=[("KTn", h % 2)])
            return f

        def v_unit(kb):
            def f():
                pv = PS[kb % 2]
                for j8 in range(8):
                    kt = kb * 8 + j8
                    for k in range(2):
                        c.op("pe", lambda e: e.matmul(pv[:, j8 * 64:(j8 + 1) * 64], lhsT=ckvnT[:, k, kt * 128:(kt + 1) * 128],
                                                      rhs=wukv[:, k, h * 128 + 64:h * 128 + 128], start=(k == 0),
                                                      stop=(k == 1)), reads=["wukv", "ckvnT"], writes=[("pk", kb % 2)])
                c.op("dve", lambda e: e.tensor_copy(out=v_buf[:, kb * 8:(kb + 1) * 8, 0:64],
                                                    in_=pv[:].rearrange("p (j d) -> p j d", j=8)),
                     reads=[("pk", kb % 2)], writes=[("Vb", h % 2)])
            return f

        for kc in range(16):
            units.append(k_unit(kc))
        for kb in range(8):
            units.append(v_unit(kb))
        return units

    steps = [(h, qc, kt) for h in range(8) for qc in range(4) for kt in range(16 * qc + 16)]
    pending = {}
    c.W("KTn", 0, 2)
    c.W("Vb", 0, 2)
    for u in up_units(0):
        u()

    def att_s0(i):
        h, qc, kt = steps[i]
        kt_buf = KT[h % 2]
        if qc == 0 and kt == 0 and h + 1 < 8:
            pending["units"] = up_units(h + 1)
            pending["local"] = 0
            pending["armed"] = False
        if pending.get("units"):
            pending["local"] += 1
            if pending["local"] >= 4 and (pending["local"] - 4) % 6 == 0:
                if not pending["armed"]:
                    c.W("KTn", h + 1, 2)
                    c.W("Vb", h + 1, 2)
                    pending["armed"] = True
                pending["units"].pop(0)()
        gk, l = kt // 4, kt % 4
        c0 = (max(gk, 4 * qc) - 4 * qc) * 128
        ps, pt_ = PS[2 + i % 4], PT[i % 4]
        c.op("pe", lambda e: e.matmul(ps[:, c0:512], lhsT=kt_buf[0:96, kt * 128:(kt + 1) * 128],
                                      rhs=QT[0:96, h, qc * 512 + c0:(qc + 1) * 512], start=True, stop=True),
             reads=[("KTn", h % 2), f"KT{h % 2}r", "QT"], writes=[("ps", i % 4)])
        c.op("act", lambda e: e.activation(out=pt_[:, c0:512], in_=ps[:, c0:512], func=AF.Exp, scale=SCALE_MLA),
             reads=[("ps", i % 4)], writes=[("PT", i % 4)])
        if gk >= 4 * qc:
            c.op("pool", lambda e: e.tensor_tensor(out=pt_[:, c0:c0 + 128], in0=pt_[:, c0:c0 + 128],
                                                   in1=amask[:, l, :], op=ALU.mult),
                 reads=[("PT", i % 4), "const"], writes=[("PT", i % 4)])

    def att_s1(i):
        pass

    def att_s2(i):
        h, qc, kt = steps[i]
        v_buf = Vb[h % 2]
        nk = 16 * qc + 16
        gk = kt // 4
        c0 = (max(gk, 4 * qc) - 4 * qc) * 128
        po = PS[6 + qc % 2]
        pt_ = PT[i % 4]
        c.op("pe", lambda e: e.matmul(po[0:65, c0:512], lhsT=v_buf[:, kt, 0:65], rhs=pt_[:, c0:512],
                                      start=(kt == 0), stop=(kt == nk - 1)),
             reads=[("Vb", h % 2), ("Vb1", h % 2), ("PT", i % 4)], writes=[("po", qc % 2)])
        if kt == nk - 1:
            f = fin[0]
            fin[0] += 1
            ob, rc = osb[f % 2], rec[f % 2]
            c.op("dve", lambda e: e.tensor_copy(out=ob[:], in_=po[0:65, :]), reads=[("po", qc % 2)],
                 writes=[("osb", f % 2)])
            pd = PS[f % 2]
            c.op("pe", lambda e: e.matmul(pd[0:64, :], lhsT=onesf[64:65, 0:64], rhs=ob[64:65, :], start=True, stop=True),
                 reads=[("osb", f % 2), "const"], writes=[("pk", f % 2)])
            c.op("dve", lambda e: e.reciprocal(out=rc[:], in_=pd[0:64, :]), reads=[("pk", f % 2)], writes=[("rec", f % 2)])
            c.op("dve", lambda e: e.tensor_tensor(out=otb[f % 2][:], in0=ob[0:64, :], in1=rc[:],
                                                  op=ALU.mult), reads=[("osb", f % 2), ("rec", f % 2)], writes=[("otb", f % 2)])
            c.dma("sp", ot_d[h, :, qc * 512:(qc + 1) * 512], otb[f % 2][:], reads=[("otb", f % 2)], writes=["ot_d"],
                  key=f"otw{f % 2}")

    pipeline(len(steps), [att_s0, att_s1, att_s1, att_s2])
    c.pop()
    c.pop()
    if stop == "T":
        return finish()

    c.push()
    PS = [c.psum(f"pc_{i}", [128, 512], F32) for i in range(8)]
    WC = c.sbuf("WC", [128, 8, 3072], BF16)
    load_w(WC[:, :, 0:1024], w_in[:, O_RQ:O_RQ + 1024].rearrange("(k p) n -> p k n", p=128), "WC", "wC0")
    load_w(WC[:, :, 1024:2048], w_in[:, O_RV:O_RV + 1024].rearrange("(k p) n -> p k n", p=128), "WC", "wC1")
    load_w(WC[:, :, 2048:3072], w_in[:, O_RG:O_RG + 1024].rearrange("(k p) n -> p k n", p=128), "WC", "wC2")
    bwC = fold_adaln(WC, 3072, "WC", PS[1], ("pz", 1), "C")
    wor = c.sbuf("wor", [128, 8, 1024], BF16)
    c.push()
    wor_f = c.sbuf("wor_f", [128, 8, 1024], F32)
    gr = c.sbuf("gr", [128, 8], F32)
    c.dma("sp", wor_f[:], w_o_ret.rearrange("(k p) n -> p k n", p=128), writes=["wor_f"], key="wC3")
    c.dma("sp", gr[:], gret, writes=["gr"], key="wC4")
    for k in range(8):
        c.op("dve", lambda e: e.tensor_scalar(out=wor[:, k, :], in0=wor_f[:, k, :], scalar1=gr[:, k:k + 1],
                                              scalar2=None, op0=ALU.mult), reads=["wor_f", "gr"], writes=["wor"])
    c.pop()
    kdecC = c.sbuf("kdecC", [128, 8, 128], F32)
    c.dma("sp", kdecC[:, 0:4, :], qdec_d, writes=["kdecC"], key="wC5")
    c.dma("sp", kdecC[:, 4:8, :], kdecC_d, writes=["kdecC"], key="wC6")
    ht = HT(xown, 3, A1, B1, PS[0])
    sinC = c.sbuf("sinC", [128, NO, 80], F32)
    cosC = c.sbuf("cosC", [128, NO, 80], F32)
    c.push()
    rtmpC = [c.sbuf("rtmpC", [128, NO, 64], F32) for _ in range(3)]
    rope_tables(posf[:, NT:NT + NO], NO, sinC, cosC, rtmpC, 16, 80, "rtC")
    c.pop()
    qk1 = c.sbuf("qk1", [128, 1024], F32)
    qk2 = c.sbuf("qk2", [128, 1024], F32)
    qk3 = c.sbuf("qk3", [128, 1024], F32)
    qkp = [c.sbuf("qkp", [128, 8, 128], BF16) for _ in range(2)]
    qkT = [c.sbuf("qkT", [128, 8, 128], BF16) for _ in range(2)]
    vbc = [c.sbuf("vbc", [128, 1024], BF16) for _ in range(2)]
    sg = [c.sbuf("sg", [128, 1024], BF16) for _ in range(2)]
    scT = [c.sbuf("scT", [128, 4, 128], BF16) for _ in range(2)]
    sob = [c.sbuf("sob", [128, 1024], BF16) for _ in range(2)]
    bnst = c.sbuf("bnst", [128, NO, 4, 6], F32)
    bnag = c.sbuf("bnag", [128, NO, 4, 2], F32)
    bnr = c.sbuf("bnr", [128, NO, 4, 2], F32)
    onr = [c.sbuf("onr", [128, 1024], F32) for _ in range(2)]
    gat = [c.sbuf("gat", [128, 1024], BF16) for _ in range(2)]
    gT = [c.sbuf("gT", [128, 8, 128], BF16) for _ in range(2)]
    bbt = [c.sbuf("bbt", [128, 1024], BF16) for _ in range(2)]

    def C1_s0(t):
        if t == 0:
            ht.load(0)
            ht.load(1)
        if t + 2 < NO:
            ht.load(t + 2)
        c.dma("sp", sob[t % 2][:], sown_d[t], reads=[], writes=[c.W("sob", t, 2)], key=f"sob{t % 2}")
        ht.norm(t)

    def C1_s0b(t):
        ht.transpose(t)

    def C1_s1(t):
        hT, hk = ht.get(t)
        for (pi, lo) in [(1, 0), (2, 512), (3, 1024), (4, 1536), (5, 2048), (6, 2560)]:
            for k in range(8):
                c.op("pe", lambda e: e.matmul(PS[pi][:], lhsT=hT[:, k, :], rhs=WC[:, k, lo:lo + 512], start=(k == 0),
                                              stop=False), reads=[hk, "WC"], writes=[("pz", pi)])
            bias_mm(PS[pi][:], bwC, lo, 512, "C", [("pz", pi)])
        cb = cosC[:, t, 16:80].unsqueeze(1).unsqueeze(1).broadcast_to([128, 4, 2, 64])
        sb = sinC[:, t, 16:80].unsqueeze(1).unsqueeze(1).broadcast_to([128, 4, 2, 64])
        for j in range(2):
            z4 = PS[1 + j][:].rearrange("p (h t d) -> p h t d", h=4, t=2)
            sl = slice(j * 512, (j + 1) * 512)
            rope(qk3[:, sl].rearrange("p (h t d) -> p h t d", h=4, t=2), z4, cb, sb,
                 qk1[:, sl].rearrange("p (h t d) -> p h t d", h=4, t=2),
                 qk2[:, sl].rearrange("p (h t d) -> p h t d", h=4, t=2), "pool",
                 [("pz", 1 + j), "rtCsin", "rtCcos"], [("qk3", j)], f"qk{j}")
        c.op("pool", lambda e: e.tensor_tensor(out=qkp[t % 2][:], in0=qk3[:].rearrange("p (h d) -> p h d", h=8),
                                               in1=kdecC[:], op=ALU.mult), reads=[("qk3", 0), ("qk3", 1), "kdecC"],
             writes=[c.W("qkp", t, 2)])
        c.W("vbc", t, 2)
        c.W("sg", t, 2)
        for hh in range(2):
            c.op("act", lambda e: e.activation(out=vbc[t % 2][:, hh * 512:(hh + 1) * 512], in_=PS[3 + hh][:],
                                               func=AF.Copy), reads=[("pz", 3 + hh)], writes=[("vbc", t % 2)])
            c.op("act", lambda e: e.activation(out=sg[t % 2][:, hh * 512:(hh + 1) * 512], in_=PS[5 + hh][:],
                                               func=AF.Silu), reads=[("pz", 5 + hh)], writes=[("sg", t % 2)])

    def C1_s2(t):
        pt = PS[7][:].bitcast(BF16)
        for j in range(8):
            c.op("pe", lambda e: e.transpose(out=pt[:, j * 128:(j + 1) * 128], in_=qkp[t % 2][:, j, :],
                                             identity=identb[:]), reads=[c.R("qkp", t, 2), "const"], writes=["ptC"])
        c.op("act", lambda e: e.activation(out=qkT[t % 2][:], in_=pt.rearrange("p (j n) -> p j n", j=8), func=AF.Copy),
             reads=["ptC"], writes=[c.W("qkT", t, 2)])
        for h in range(4):
            c.op("pe", lambda e: e.matmul(PS[1][:, h * 128:(h + 1) * 128], lhsT=qkT[t % 2][:, 4 + h, :],
                                          rhs=qkT[t % 2][:, h, :], start=True, stop=True),
                 reads=[c.R("qkT", t, 2)], writes=[("pz", 1)])
        c.op("dve", lambda e: e.tensor_tensor(out=scT[t % 2][:], in0=PS[1][:].rearrange("p (h n) -> p h n", h=4),
                                              in1=tri[:].unsqueeze(1).broadcast_to([128, 4, 128]), op=ALU.mult),
             reads=[("pz", 1), "const"], writes=[c.W("scT", t, 2)])
        for h in range(4):
            po = PS[3 + h // 2][:, (h % 2) * 256:(h % 2 + 1) * 256]
            c.op("pe", lambda e: e.matmul(po, lhsT=scT[t % 2][:, h, :], rhs=vbc[t % 2][:, h * 256:(h + 1) * 256],
                                          start=True, stop=False), reads=[c.R("scT", t, 2), c.R("vbc", t, 2)],
                 writes=[("pz", 3 + h // 2)])
            c.op("pe", lambda e: e.matmul(po, lhsT=qkT[t % 2][:, h, :], rhs=sob[t % 2][:, h * 256:(h + 1) * 256],
                                          start=False, stop=True), reads=[c.R("qkT", t, 2), c.R("sob", t, 2)],
                 writes=[("pz", 3 + h // 2)])
        for h in range(4):
            po = PS[3 + h // 2][:, (h % 2) * 256:(h % 2 + 1) * 256]
            c.op("dve", lambda e: e.bn_stats(out=bnst[:, t, h, :], in_=po), reads=[("pz", 3 + h // 2)],
                 writes=[("bnst", t)])
            c.op("dve", lambda e: e.bn_aggr(out=bnag[:, t, h, :], in_=bnst[:, t, h, :]), reads=[("bnst", t)],
                 writes=[("bnag", t)])
        c.op("dve", lambda e: e.tensor_scalar(out=bnr[:, t, :, 0], in0=bnag[:, t, :, 1], scalar1=GN_EPS, scalar2=None,
                                              op0=ALU.add), reads=[("bnag", t)], writes=[("bnr0", t)])
        rsqrt_cols(bnr[:, t, :, 0], bnr[:, t, :, 1], 4, [("bnr0", t)], [("bnr1", t)])
        c.W("onr", t, 2)
        for h in range(4):
            po = PS[3 + h // 2][:, (h % 2) * 256:(h % 2 + 1) * 256]
            c.op("dve", lambda e: e.tensor_scalar(out=onr[t % 2][:, h * 256:(h + 1) * 256], in0=po,
                                                  scalar1=bnag[:, t, h, 0:1], scalar2=bnr[:, t, h, 1:2],
                                                  op0=ALU.subtract, op1=ALU.mult),
                 reads=[("pz", 3 + h // 2), ("bnag", t), ("bnr1", t)], writes=[("onr", t % 2)])
        c.op("pool", lambda e: e.tensor_tensor(out=gat[t % 2][:], in0=onr[t % 2][:], in1=sg[t % 2][:], op=ALU.mult),
             reads=[("onr", t % 2), c.R("sg", t, 2)], writes=[c.W("gat", t, 2)])

    def C1_s3(t):
        pt = PS[7][:].bitcast(BF16)
        for k in range(8):
            c.op("pe", lambda e: e.transpose(out=pt[:, k * 128:(k + 1) * 128], in_=gat[t % 2][:, k * 128:(k + 1) * 128],
                                             identity=identb[:]), reads=[c.R("gat", t, 2), "const"], writes=["ptC"])
        c.op("act", lambda e: e.activation(out=gT[t % 2][:], in_=pt.rearrange("p (j n) -> p j n", j=8), func=AF.Copy),
             reads=["ptC"], writes=[c.W("gT", t, 2)])
        c.W("bbt", t, 2)
        for hh in range(2):
            for k in range(8):
                c.op("pe", lambda e: e.matmul(PS[5 + hh][:], lhsT=gT[t % 2][:, k, :], rhs=wor[:, k, hh * 512:(hh + 1) * 512],
                                              start=(k == 0), stop=(k == 7)), reads=[c.R("gT", t, 2), "wor"],
                     writes=[("pz", 5 + hh)])
            c.op("act", lambda e: e.activation(out=bbt[t % 2][:, hh * 512:(hh + 1) * 512], in_=PS[5 + hh][:],
                                               func=AF.Copy), reads=[("pz", 5 + hh)], writes=[("bbt", t % 2)])
        c.dma("sp", bb_d[t], bbt[t % 2][:], reads=[("bbt", t % 2)], writes=["bb_d"], key=f"bbw{t % 2}")

    def C1_s123(t):
        C1_s1(t)
        C1_s2(t)
        C1_s3(t)

    pipeline(NO, [C1_s0, C1_s0b, C1_s123])
    print("sbuf remaining in pass C1:", nc.sbuf_bytes_remaining, "ops", c.nops)
    c.pop()
    if stop == "C1":
        return finish()

    h2T = c.sbuf("h2T", [128, 8, NO * 128], BF16)
    print("sbuf remaining before C2:", nc.sbuf_bytes_remaining)
    c.push()
    PS = [c.psum(f"pd_{i}", [128, 512], F32) for i in range(8)]
    WG = c.sbuf("WG", [128, 8, 2048], BF16)
    load_w(WG[:], w_in[:, O_GA:O_GA + 2048].rearrange("(k p) n -> p k n", p=128), "WG", "wD0")
    bwG = fold_adaln(WG, 2048, "WG", PS[1], ("pz", 1), "G")
    wom = c.sbuf("wom", [64, 8, 1024], BF16)
    load_w(wom[:], w_o_mla.rearrange("(h p) n -> p h n", p=64), "wom", "wD1")
    wout = c.sbuf("wout", [128, 8, 1024], BF16)
    load_w(wout[:], w_out.rearrange("(k p) n -> p k n", p=128), "wout", "wD2")
    wr = c.sbuf("wr", [128, 8, 64], F32)
    c.dma("sp", wr[:], w_router.rearrange("(k p) n -> p k n", p=128), writes=["wr"], key="wD3")
    ht = HT(xown, 4, A1, B1, PS[0])
    tg = [c.sbuf("tg", [128, 2048], BF16)] * 2
    ott = [c.sbuf("ott", [64, 8, 128], BF16) for _ in range(2)]
    bbr = [c.sbuf("bbr", [128, 1024], BF16) for _ in range(2)]
    m1 = c.sbuf("m1", [128, 1024], F32)
    m2 = c.sbuf("m2", [128, 1024], F32)
    mg = [c.sbuf("mg", [128, 1024], BF16) for _ in range(2)]
    mgT = [c.sbuf("mgT", [128, 8, 128], BF16) for _ in range(2)]
    ty = m1
    x1 = [c.sbuf("x1", [128, 1024], F32) for _ in range(2)]
    xs2 = [c.sbuf("xs2", [128, 1024], F32) for _ in range(2)]
    h2f = [c.sbuf("h2f", [128, 8, 128], F32) for _ in range(2)]
    st2 = c.sbuf("st2", [128, 3 * 64], F32)
    rt = c.sbuf("rt", [128, 2, 64 * 4 + 64 + 8 * 4], F32)

    def C2_s0(t):
        if t == 0:
            ht.load(0)
            ht.load(1)
        if t + 2 < NO:
            ht.load(t + 2)
        c.dma("sp", bbr[t % 2][:], bb_d[t], reads=["bb_d"], writes=[c.W("bbr", t, 2)], key=f"bbr{t % 2}")
        c.dma("sp", ott[t % 2][:], ot_d[:, :, t * 128:(t + 1) * 128].rearrange("h p n -> p h n"), reads=["ot_d"],
              writes=[c.W("ott", t, 2)], key=f"ott{t % 2}")
        ht.norm(t)

    def C2_s0b(t):
        ht.transpose(t)

    def C2_s1(t):
        hT, hk = ht.get(t)
        xt = ht.xt[t % 4]
        for j in range(4):
            for k in range(8):
                c.op("pe", lambda e: e.matmul(PS[1 + j][:], lhsT=hT[:, k, :], rhs=WG[:, k, j * 512:(j + 1) * 512],
                                              start=(k == 0), stop=False), reads=[hk, "WG"], writes=[("pz", 1 + j)])
            bias_mm(PS[1 + j][:], bwG, j * 512, 512, "G", [("pz", 1 + j)])
            c.op("act", lambda e: e.activation(out=tg[t % 2][:, j * 512:(j + 1) * 512], in_=PS[1 + j][:], func=AF.Tanh,
                                               scale=0.5), reads=[("pz", 1 + j)], writes=["tg"])
        for hh in range(2):
            for h in range(8):
                c.op("pe", lambda e: e.matmul(PS[5 + hh][:], lhsT=ott[t % 2][:, h, :],
                                              rhs=wom[:, h, hh * 512:(hh + 1) * 512], start=(h == 0), stop=(h == 7)),
                     reads=[c.R("ott", t, 2), "wom"], writes=[("pz", 5 + hh)])
            sl = slice(hh * 512, (hh + 1) * 512)
            c.op("dve", lambda e: e.scalar_tensor_tensor(out=m1[:, sl], in0=tg[t % 2][:, sl], scalar=1.0, in1=PS[5 + hh][:],
                                                         op0=ALU.add, op1=ALU.mult),
                 reads=["tg", ("pz", 5 + hh)], writes=[("m1", hh)])
        c.op("dve", lambda e: e.scalar_tensor_tensor(out=m2[:], in0=tg[t % 2][:, 1024:2048], scalar=1.0, in1=bbr[t % 2][:],
                                                     op0=ALU.add, op1=ALU.mult),
             reads=["tg", c.R("bbr", t, 2)], writes=["m2"])
        c.op("pool", lambda e: e.tensor_tensor(out=mg[t % 2][:], in0=m1[:], in1=m2[:], op=ALU.add),
             reads=[("m1", 0), ("m1", 1), "m2"], writes=[c.W("mg", t, 2)])
        pt = PS[7][:].bitcast(BF16)
        for k in range(8):
            c.op("pe", lambda e: e.transpose(out=pt[:, k * 128:(k + 1) * 128], in_=mg[t % 2][:, k * 128:(k + 1) * 128],
                                             identity=identb[:]), reads=[c.R("mg", t, 2), "const"], writes=["ptD"])
        c.op("act", lambda e: e.activation(out=mgT[t % 2][:], in_=pt.rearrange("p (j n) -> p j n", j=8), func=AF.Copy),
             reads=["ptD"], writes=[c.W("mgT", t, 2)])
        c.W("x1", t, 2)
        for hh in range(2):
            sl = slice(hh * 512, (hh + 1) * 512)
            for k in range(8):
                c.op("pe", lambda e: e.matmul(PS[1 + hh][:], lhsT=mgT[t % 2][:, k, :], rhs=wout[:, k, sl],
                                              start=(k == 0), stop=(k == 7)), reads=[c.R("mgT", t, 2), "wout"],
                     writes=[("pz", 1 + hh)])
            c.op("dve", lambda e: e.tensor_tensor(out=ty[:, sl], in0=PS[1 + hh][:], in1=GT1[:, sl], op=ALU.mult),
                 reads=[("pz", 1 + hh), "GT"], writes=[("m1", hh)])
            c.op("pool", lambda e: e.tensor_tensor(out=x1[t % 2][:, sl], in0=ty[:, sl], in1=xt[:, sl], op=ALU.add),
                 reads=[("m1", hh), c.R("xt", t, 4)], writes=[("x1", t % 2)])
        c.dma("sp", x1_d[t], x1[t % 2][:], reads=[("x1", t % 2)], writes=["x1_d"], key=f"x1w{t % 2}")
        m = t
        a, b, r = st2[:, m:m + 1], st2[:, 64 + m:65 + m], st2[:, 128 + m:129 + m]
        c.op("act", lambda e: e.activation(out=junk[:], in_=x1[t % 2][:], func=AF.Square, accum_out=a),
             reads=[("x1", t % 2)], writes=["junk", ("s2a", m)])
        c.op("dve", lambda e: e.tensor_scalar(out=b, in0=a, scalar1=1.0 / D, scalar2=RMS_EPS, op0=ALU.mult, op1=ALU.add),
             reads=[("s2a", m)], writes=[("s2b", m)])
        rsqrt_cols(b, r, 1, [("s2b", m)], [("s2r", m)])
        c.op("dve", lambda e: e.tensor_scalar(out=xs2[t % 2][:], in0=x1[t % 2][:], scalar1=r, scalar2=None, op0=ALU.mult),
             reads=[("x1", t % 2), ("s2r", m)], writes=[c.W("xs2", t, 2)])

    def C2_s2(t):
        c.W("h2f", t, 2)
        for k in range(8):
            pb = PS[3 + k // 4][:, (k % 4) * 128:(k % 4 + 1) * 128]
            c.op("pe", lambda e: e.transpose(out=pb, in_=xs2[t % 2][:, k * 128:(k + 1) * 128], identity=identf[:]),
                 reads=[c.R("xs2", t, 2), "const"], writes=[("pz", 3 + k // 4)])
        for k in range(8):
            pb = PS[3 + k // 4][:, (k % 4) * 128:(k % 4 + 1) * 128]
            c.op("act", lambda e: e.activation(out=h2f[t % 2][:, k, :], in_=pb, func=AF.Identity, scale=A2[:, k:k + 1],
                                               bias=B2[:, k:k + 1]), reads=[("pz", 3 + k // 4), "AB"],
                 writes=[("h2f", t % 2)])
        c.op("dve", lambda e: e.tensor_copy(out=h2T[:, :, t * 128:(t + 1) * 128], in_=h2f[t % 2][:]),
             reads=[("h2f", t % 2)], writes=["h2T"])
        for k in range(8):
            c.op("pe", lambda e: e.matmul(PS[5][:, 0:64], lhsT=h2f[t % 2][:, k, :], rhs=wr[:, k, :], start=(k == 0),
                                          stop=(k == 7)), reads=[("h2f", t % 2), "wr"], writes=[("pz", 5)])
        R_ = rt[:, t % 2, :]
        s_, bi, mb, sel = R_[:, 0:64], R_[:, 64:128], R_[:, 128:192], R_[:, 192:256]
        m8 = R_[:, 256:320]
        gs, g8, gm, gneg = R_[:, 320:328], R_[:, 328:336], R_[:, 336:344], R_[:, 344:352]
        rk_ = ("rt", t % 2)
        c.op("act", lambda e: e.activation(out=s_, in_=PS[5][:, 0:64], func=AF.Tanh, scale=0.5), reads=[("pz", 5)],
             writes=[rk_])
        c.op("dve", lambda e: e.tensor_scalar(out=s_, in0=s_, scalar1=0.5, scalar2=0.5, op0=ALU.mult, op1=ALU.add),
             reads=[rk_], writes=[rk_])
        c.op("dve", lambda e: e.tensor_tensor(out=bi, in0=s_, in1=brout_t[:], op=ALU.add), reads=[rk_, "const"],
             writes=[rk_])
        for g in range(8):
            c.op("dve", lambda e: e.max(out=m8[:, g * 8:(g + 1) * 8], in_=bi[:, g * 8:(g + 1) * 8]), reads=[rk_],
                 writes=[rk_])
        m83 = m8.rearrange("p (g k) -> p g k", g=8)
        c.op("dve", lambda e: e.tensor_tensor(out=gs, in0=m83[:, :, 0], in1=m83[:, :, 1], op=ALU.add), reads=[rk_],
             writes=[rk_])
        c.op("dve", lambda e: e.max(out=g8, in_=gs), reads=[rk_], writes=[rk_])
        c.op("dve", lambda e: e.tensor_scalar(out=gm, in0=gs, scalar1=g8[:, 3:4], scalar2=None, op0=ALU.is_ge),
             reads=[rk_], writes=[rk_])
        c.op("dve", lambda e: e.tensor_scalar(out=gneg, in0=gm, scalar1=-1.0, scalar2=8.0, op0=ALU.add, op1=ALU.mult),
             reads=[rk_], writes=[rk_])
        bi3, mb3 = bi.rearrange("p (g k) -> p g k", g=8), mb.rearrange("p (g k) -> p g k", g=8)
        c.op("dve", lambda e: e.tensor_tensor(out=mb3, in0=bi3, in1=gm.unsqueeze(2).broadcast_to([128, 8, 8]), op=ALU.mult),
             reads=[rk_], writes=[rk_])
        c.op("dve", lambda e: e.tensor_tensor(out=mb3, in0=mb3, in1=gneg.unsqueeze(2).broadcast_to([128, 8, 8]), op=ALU.add),
             reads=[rk_], writes=[rk_])
        c.op("dve", lambda e: e.max(out=g8, in_=mb), reads=[rk_], writes=[rk_])
        c.op("dve", lambda e: e.tensor_scalar(out=sel, in0=mb, scalar1=g8[:, 7:8], scalar2=None, op0=ALU.is_ge),
             reads=[rk_], writes=[rk_])
        c.op("dve", lambda e: e.tensor_tensor(out=sel, in0=sel, in1=s_, op=ALU.mult), reads=[rk_], writes=[rk_])
        c.op("dve", lambda e: e.tensor_reduce(out=gs[:, 0:1], in_=sel, axis=AX.X, op=ALU.add), reads=[rk_], writes=[rk_])
        c.op("dve", lambda e: e.reciprocal(out=gs[:, 1:2], in_=gs[:, 0:1]), reads=[rk_], writes=[rk_])
        c.op("dve", lambda e: e.tensor_scalar(out=comb[:, t, :], in0=sel, scalar1=gs[:, 1:2], scalar2=2.5, op0=ALU.mult,
                                              op1=ALU.mult), reads=[rk_], writes=["comb"])

    def C2_s12(t):
        C2_s1(t)
        C2_s2(t)

    pipeline(NO, [C2_s0, C2_s0b, C2_s12])
    print("sbuf remaining in pass C2:", nc.sbuf_bytes_remaining, "ops", c.nops)
    c.pop()
    dbg("h2T", h2T[:], [128, 8, NO * 128], BF16)
    dbg("comb", comb[:], [128, NO, 64], F32)
    if stop == "C2":
        return finish()

    c.push()
    PG = c.psum("pg", [128, 2048], F32)
    PY = [c.psum(f"py{i}", [128, 1024], F32) for i in range(2)]
    acc = c.sbuf("acc", [128, NO, 1024], F32)
    wgu = [c.sbuf("wgu", [128, 8, 512], BF16) for _ in range(2)]
    wdn = [c.sbuf("wdn", [128, 2, 1024], BF16) for _ in range(2)]
    sgm = [c.sbuf("sgm", [128, 1024], BF16) for _ in range(2)]
    actT = [c.sbuf("actT", [128, 2, 512], BF16) for _ in range(2)]
    def load_expert(ei):
        e_ = ei - 1
        sl = ei % 2
        srcs = (w_sg, w_su, w_sd) if e_ < 0 else (w_eg[e_], w_eu[e_], w_ed[e_])
        c.W("wexp", ei, 2)
        c.dma("pool", wgu[sl][:, :, 0:256], srcs[0].rearrange("(k p) n -> p k n", p=128), writes=[("wexp", sl)], key=f"we{sl}a")
        c.dma("pool", wgu[sl][:, :, 256:512], srcs[1].rearrange("(k p) n -> p k n", p=128), writes=[("wexp", sl)], key=f"we{sl}b")
        c.dma("pool", wdn[sl][:], srcs[2].rearrange("(k p) n -> p k n", p=128), writes=[("wexp", sl)], key=f"we{sl}c")

    units = [(ei, tc_) for ei in range(NEXP + 1) for tc_ in range(4)]
    load_expert(0)

    def moe_s0(u):
        ei, tc_ = units[u]
        sl, a_ = ei % 2, u % 2
        if tc_ == 0 and ei + 1 <= NEXP:
            load_expert(ei + 1)
        wk = c.R("wexp", ei, 2)
        for j in range(4):
            for k in range(8):
                c.op("pe", lambda e: e.matmul(PG[:, j * 512:(j + 1) * 512], lhsT=wgu[sl][:, k, j * 128:(j + 1) * 128],
                                              rhs=h2T[:, k, tc_ * 512:(tc_ + 1) * 512], start=(k == 0), stop=(k == 7)),
                     reads=[wk, "h2T"], writes=[("pg", j // 2)])
        c.op("act", lambda e: e.activation(out=sgm[a_][:], in_=PG[:, 0:1024], func=AF.Silu), reads=[("pg", 0)],
             writes=[("sgm", a_)])
        c.op("dve", lambda e: e.tensor_tensor(out=actT[a_][:].rearrange("p f n -> p (f n)"), in0=sgm[a_][:],
                                              in1=PG[:, 1024:2048], op=ALU.mult), reads=[("sgm", a_), ("pg", 1)],
             writes=[("actT", a_)])

    def moe_s1(u):
        ei, tc_ = units[u]
        e_ = ei - 1
        sl, a_ = ei % 2, u % 2
        wk = ("wexp", sl)
        for tt in range(4):
            i = tc_ * 4 + tt
            y_ = (u * 4 + tt) % 2
            for hh in range(2):
                for fc in range(2):
                    c.op("pe", lambda e: e.matmul(PY[y_][:, hh * 512:(hh + 1) * 512], lhsT=actT[a_][:, fc, tt * 128:(tt + 1) * 128],
                                                  rhs=wdn[sl][:, fc, hh * 512:(hh + 1) * 512], start=(fc == 0), stop=(fc == 1)),
                         reads=[("actT", a_), wk], writes=[("py", y_)])
            if e_ < 0:
                c.op("act", lambda e: e.activation(out=acc[:, i, :], in_=PY[y_][:], func=AF.Copy), reads=[("py", y_)],
                     writes=[("acc", i)])
            else:
                c.op("dve", lambda e: e.scalar_tensor_tensor(out=acc[:, i, :], in0=PY[y_][:], scalar=comb[:, i, e_:e_ + 1],
                                                             in1=acc[:, i, :], op0=ALU.mult, op1=ALU.add),
                     reads=[("py", y_), ("acc", i), "comb"], writes=[("acc", i)])

    pipeline(len(units), [moe_s0, moe_s1])
    x1r = [c.sbuf("x1r", [128, 1024], F32) for _ in range(2)]
    xo = [c.sbuf("xo", [128, 1024], F32) for _ in range(2)]
    yo = [c.sbuf("yo", [128, 1024], F32) for _ in range(2)]
    stf = c.sbuf("stf", [128, 3 * 64], F32)
    for t in range(NO):
        c.dma("sp", x1r[t % 2][:], x1_d[t], reads=["x1_d"], writes=[("x1r", t % 2)], key=f"x1r{t % 2}")
        c.op("dve", lambda e: e.tensor_tensor(out=xo[t % 2][:], in0=acc[:, t, :], in1=GT2[:], op=ALU.mult),
             reads=[("acc", t), "GT"], writes=[("xo", t % 2)])
        c.op("pool", lambda e: e.tensor_tensor(out=xo[t % 2][:], in0=xo[t % 2][:], in1=x1r[t % 2][:], op=ALU.add),
             reads=[("xo", t % 2), ("x1r", t % 2)], writes=[("xo", t % 2)])
        a, b, r = stf[:, t:t + 1], stf[:, 64 + t:65 + t], stf[:, 128 + t:129 + t]
        c.op("act", lambda e: e.activation(out=junk[:], in_=xo[t % 2][:], func=AF.Square, accum_out=a),
             reads=[("xo", t % 2)], writes=["junk", ("sfa", t)])
        c.op("dve", lambda e: e.tensor_scalar(out=b, in0=a, scalar1=1.0 / D, scalar2=RMS_EPS, op0=ALU.mult, op1=ALU.add),
             reads=[("sfa", t)], writes=[("sfb", t)])
        rsqrt_cols(b, r, 1, [("sfb", t)], [("sfr", t)])
        c.op("dve", lambda e: e.scalar_tensor_tensor(out=yo[t % 2][:], in0=xo[t % 2][:], scalar=r, in1=gfin_t[:],
                                                     op0=ALU.mult, op1=ALU.mult),
             reads=[("xo", t % 2), ("sfr", t), "const"], writes=[("yo", t % 2)])
        c.dma("sp", out_d[t * 128:(t + 1) * 128, :], yo[t % 2][:], reads=[("yo", t % 2)], writes=["out"], key=f"out{t % 2}")
    c.pop()
    c.close()
    return nc


_NC_CACHE = {}


def _consts(j):
    bf = ml_dtypes.bfloat16
    k = np.arange(128)
    tri = (k[:, None] <= k[None, :]).astype(np.float32)
    amask = np.zeros((128, 4, 128), np.float32)
    for l in range(4):
        if l < j:
            amask[:, l, :] = 1.0
        elif l == j:
            amask[:, l, :] = tri
    inv_mla = 1.0 / (10000.0 ** (np.arange(0, 32, 2, dtype=np.float32) / np.float32(32)))
    inv_ret = 1.0 / (10000.0 ** (np.arange(0, 128, 2, dtype=np.float32) / np.float32(128)))
    invf = np.broadcast_to(np.concatenate([inv_mla, inv_ret]).astype(np.float32)[None, :], (128, 80)).copy()
    g = np.array(GAMMA, np.float64)
    m = np.arange(128, dtype=np.float64)
    kdecA = (g[None, :] ** (127.0 - m[:, None])) * 128.0 ** -0.5
    kdecC = (g[None, :] ** (-m[:, None])) * 128.0 ** -0.5
    qdec = g[None, :] ** m[:, None]
    rep = lambda a: np.repeat(a[:, :, None], 128, axis=2).astype(np.float32)
    G = g ** 128
    rc = np.zeros((128, 16), np.float64)
    for h in range(4):
        rc[:, h * 4 + 0] = g[h] * G[h] ** j
        for l in range(3):
            rc[:, h * 4 + 1 + l] = g[h] * (G[h] ** (j - 1 - l)) if l < j else 0.0
    return dict(identb=np.eye(128).astype(bf), identf=np.eye(128, dtype=np.float32), tri=tri,
                amask=amask.astype(bf), invf=invf, kdecA=rep(kdecA), kdecC=rep(kdecC), qdec=rep(qdec),
                rcoef=rc.astype(np.float32))


def _col(v, nchunk):
    return np.ascontiguousarray(np.asarray(v, np.float32).reshape(nchunk, 128).T)


_BUILD_ARGS = {}


def kernel(x, c, positions, w_ada, b_ada, g_norm1, w_in, g_cq, w_uq, g_ckv, w_ukv, g_ret, w_o_mla, w_o_ret,
           w_out, g_norm2, w_router, b_router, w_exp_gate, w_exp_up, w_exp_down, w_sh_gate, w_sh_up, w_sh_down,
           g_final):
    f = lambda a: np.ascontiguousarray(np.asarray(a, dtype=np.float32))
    x = f(x)
    positions = np.asarray(positions).astype(np.int32)
    if "nc" not in _NC_CACHE:
        _NC_CACHE["nc"] = build(**_BUILD_ARGS)
    nc = _NC_CACHE["nc"]
    shared = dict(
        w_ada=f(w_ada), bada_row=f(b_ada).reshape(1, -1), g1c=_col(g_norm1, 8), g2c=_col(g_norm2, 8), w_in=f(w_in),
        gcq=_col(g_cq, 3), w_uq=f(w_uq), gckv=_col(g_ckv, 2), w_ukv=f(w_ukv), gret=_col(g_ret, 8), w_o_mla=f(w_o_mla),
        w_o_ret=f(w_o_ret), w_out=f(w_out), w_router=f(w_router),
        brout=np.ascontiguousarray(np.broadcast_to(f(b_router)[None, :], (128, 64))),
        w_exp_gate=f(w_exp_gate), w_exp_up=f(w_exp_up), w_exp_down=f(w_exp_down), w_sh_gate=f(w_sh_gate),
        w_sh_up=f(w_sh_up), w_sh_down=f(w_sh_down),
        gfin=np.ascontiguousarray(np.broadcast_to(f(g_final)[None, :], (128, 1024))),
    )
    in_maps = []
    for core in range(8):
        b, j = core // 4, core % 4
        xb = x[b]
        xt = xb.reshape(NT, 128, D)
        pt = positions[b].reshape(NT, 128)
        m = dict(shared)
        m.update(_consts(j))
        m["xall"] = xb
        m["xown"] = np.ascontiguousarray(xt[j::4].reshape(NO * 128, D))
        m["posall"] = np.ascontiguousarray(pt.T)
        m["posown"] = np.ascontiguousarray(pt[j::4].T)
        m["cvec"] = _col(c[b], 8)
        in_maps.append(m)
    res = run_bass_kernel_spmd(nc, in_maps, core_ids=list(range(8)))
    _NC_CACHE["res"] = res
    out = np.empty((2, S, D), np.float32)
    for core in range(8):
        b, j = core // 4, core % 4
        o = np.asarray(res.results[core]["out"]).reshape(NO, 128, D)
        out[b].reshape(NT, 128, D)[j::4] = o
    return out
```

```python
import math
from contextlib import ExitStack

import numpy as np
import ml_dtypes

import concourse.bass as bass
import concourse.mybir as mybir
from concourse.bass_utils import run_bass_kernel_spmd

F32 = mybir.dt.float32
BF16 = mybir.dt.bfloat16
I32 = mybir.dt.int32
AF = mybir.ActivationFunctionType
ALU = mybir.AluOpType
AX = mybir.AxisListType

D = 1024
S = 8192
NT = 64
NO = 16
D_IN = 5792
RMS_EPS = 1e-6
GN_EPS = 1e-5
TWO_PI = 2.0 * math.pi
MAGIC = 12582912.0
C1 = 6.28125
C2 = float(np.float32(TWO_PI - 6.28125))
C3 = float(TWO_PI - 6.28125 - float(np.float32(TWO_PI - 6.28125)))
SCALE_MLA = 96.0 ** -0.5
GAMMA = [1.0 - 2.0 ** (-5.0 - h) for h in range(4)]
NEXP = 64
import os
NOSAME = os.environ.get("NOSAME") == "1"

O_CQ, O_CKV, O_KR, O_RQ, O_RK, O_RV, O_RG, O_GA, O_GB = 0, 384, 640, 672, 1184, 1696, 2720, 3744, 4768


class Ctx:
    def __init__(self, nc):
        self.nc = nc
        self.stacks = [ExitStack()]
        self.eng = {"pe": nc.tensor, "act": nc.scalar, "dve": nc.vector, "pool": nc.gpsimd, "sp": nc.sync}
        self.sem, self.cnt = {}, {}
        for e in ("pe", "act", "dve", "pool"):
            self.sem[e] = self.stacks[0].enter_context(nc.semaphore("s_" + e))
            self.cnt[e] = 0
        self.dsem, self.dcnt = {}, {}
        self.waited = {e: {} for e in self.eng}
        self.last_w, self.readers, self.owner = {}, {}, {}
        self.nops = 0
        self.uid = 0

    def sbuf(self, name, shape, dtype):
        self.uid += 1
        return self.stacks[-1].enter_context(self.nc.sbuf_tensor(f"{name}_{self.uid}", list(shape), dtype))

    def psum(self, name, shape, dtype):
        self.uid += 1
        return self.stacks[-1].enter_context(self.nc.psum_tensor(f"{name}_{self.uid}", list(shape), dtype))

    def push(self):
        self.stacks.append(ExitStack())

    def pop(self):
        self.barrier()
        self.stacks.pop().close()

    def W(self, name, t=0, n=1):
        k = (name, t % n)
        self.owner[k] = t
        return k

    def R(self, name, t=0, n=1):
        k = (name, t % n)
        assert self.owner.get(k) == t, f"stale read {name} tile {t} owner {self.owner.get(k)}"
        return k

    PSUM_NAMES = {"pmod", "pc", "pgt", "pT", "pz", "ptA", "pkv", "pcq", "ptQ", "pq", "ptQ2", "pk", "ps", "po", "ptC",
                  "ptD", "pg", "py"}

    def _split(self, reads, writes):
        rd, wr = [], list(writes)
        for r in reads:
            base = r[0] if isinstance(r, tuple) else r
            if base in self.PSUM_NAMES:
                if r not in wr:
                    wr.append(r)
            else:
                rd.append(r)
        return rd, wr

    def _deps(self, reads, writes):
        deps = []
        for r in reads:
            t = self.last_w.get(r)
            if t is not None:
                deps.append(t)
        for w in writes:
            t = self.last_w.get(w)
            if t is not None:
                deps.append(t)
            deps.extend(self.readers.get(w, ()))
        return deps

    def _wait(self, eng, deps):
        best = {}
        for (skey, sem, val) in deps:
            if eng == "pe" and skey == "pe":
                continue
            if NOSAME and skey == eng:
                continue
            if val > best.get(skey, (None, 0))[1]:
                best[skey] = (sem, val)
        w = self.waited[eng]
        for skey, (sem, val) in best.items():
            if w.get(skey, 0) >= val:
                continue
            self.eng[eng].wait_ge(sem, val)
            w[skey] = val

    def _commit(self, ticket, reads, writes):
        for r in reads:
            self.readers.setdefault(r, []).append(ticket)
        for w in writes:
            self.last_w[w] = ticket
            self.readers[w] = []

    def op(self, eng, fn, reads=(), writes=()):
        reads, writes = self._split(reads, writes)
        self._wait(eng, self._deps(reads, writes))
        ins = fn(self.eng[eng])
        self.cnt[eng] += 1
        ins.then_inc(self.sem[eng], 1)
        t = (eng, self.sem[eng], self.cnt[eng])
        self._commit(t, reads, writes)
        self.nops += 1
        return t

    def dma(self, queue, out, in_, reads=(), writes=(), key=None):
        if key not in self.dsem:
            self.dsem[key] = self.stacks[0].enter_context(self.nc.semaphore("d_" + str(key)))
            self.dcnt[key] = 0
        self._wait(queue, self._deps(reads, writes))
        ins = self.eng[queue].dma_start(out=out, in_=in_)
        self.dcnt[key] += 16
        ins.then_inc(self.dsem[key], 16)
        t = ("d_" + str(key), self.dsem[key], self.dcnt[key])
        self._commit(t, reads, writes)
        return t

    def barrier(self):
        tickets = [(e, self.sem[e], self.cnt[e]) for e in self.sem if self.cnt[e] > 0]
        tickets += [("d_" + str(k), self.dsem[k], self.dcnt[k]) for k in self.dsem]
        for e in self.eng:
            self._wait(e, tickets)
        self.last_w, self.readers = {}, {}

    def close(self):
        self.barrier()
        while self.stacks:
            self.stacks.pop().close()


def pipeline(n, stages):
    ns = len(stages)
    for s in range(n + ns - 1):
        for k in range(ns - 1, -1, -1):
            t = s - k
            if 0 <= t < n:
                stages[k](t)


def build(stop=None, debug=False):
    nc = bass.Bass("TRN2", target_bir_lowering=False)
    dbg_out = {}

    def dbg(name, ap, shape, dt):
        if not debug:
            return
        d_ = nc.dram_tensor("dbg_" + name, list(shape), dt, kind="ExternalOutput").ap()
        dbg_out[name] = d_
        c.barrier()
        c.dma("sp", d_, ap, key="dbg_" + name)
        c.barrier()

    def finish():
        c.close()
        return nc

    def din(name, shape, dt=F32):
        return nc.dram_tensor(name, list(shape), dt, kind="ExternalInput").ap()

    xall = din("xall", [S, D])
    xown = din("xown", [NO * 128, D])
    posall = din("posall", [128, NT], I32)
    posown = din("posown", [128, NO], I32)
    cvec = din("cvec", [128, 8])
    w_ada = din("w_ada", [D, 6 * D])
    bada_row = din("bada_row", [1, 6 * D])
    g1c = din("g1c", [128, 8])
    g2c = din("g2c", [128, 8])
    w_in = din("w_in", [D, D_IN])
    gcq = din("gcq", [128, 3])
    w_uq = din("w_uq", [384, 768])
    gckv = din("gckv", [128, 2])
    w_ukv = din("w_ukv", [256, 1024])
    gret = din("gret", [128, 8])
    w_o_mla = din("w_o_mla", [512, 1024])
    w_o_ret = din("w_o_ret", [1024, 1024])
    w_out = din("w_out", [1024, 1024])
    w_router = din("w_router", [1024, 64])
    brout = din("brout", [128, 64])
    w_eg = din("w_exp_gate", [NEXP, 1024, 256])
    w_eu = din("w_exp_up", [NEXP, 1024, 256])
    w_ed = din("w_exp_down", [NEXP, 256, 1024])
    w_sg = din("w_sh_gate", [1024, 256])
    w_su = din("w_sh_up", [1024, 256])
    w_sd = din("w_sh_down", [256, 1024])
    gfin = din("gfin", [128, 1024])
    identb_d = din("identb", [128, 128], BF16)
    identf_d = din("identf", [128, 128])
    tri_d = din("tri", [128, 128])
    amask_d = din("amask", [128, 4, 128], BF16)
    invf_d = din("invf", [128, 80])
    kdecA_d = din("kdecA", [128, 4, 128])
    kdecC_d = din("kdecC", [128, 4, 128])
    qdec_d = din("qdec", [128, 4, 128])
    rcoef_d = din("rcoef", [128, 16])
    out_d = nc.dram_tensor("out", [NO * 128, D], F32, kind="ExternalOutput").ap()
    sown_d = nc.dram_tensor("sown_s", [NO, 128, 1024], BF16, kind="ExternalOutput").ap()
    bb_d = nc.dram_tensor("bb_s", [NO, 128, 1024], BF16, kind="ExternalOutput").ap()
    x1_d = nc.dram_tensor("x1_s", [NO, 128, 1024], F32, kind="ExternalOutput").ap()
    ot_d = nc.dram_tensor("ot_s", [8, 64, NO * 128], BF16, kind="ExternalOutput").ap()

    c = Ctx(nc)

    identb = c.sbuf("identb", [128, 128], BF16)
    identf = c.sbuf("identf", [128, 128], F32)
    tri = c.sbuf("tri", [128, 128], F32)
    amask = c.sbuf("amask", [128, 4, 128], BF16)
    invf = c.sbuf("invf", [128, 80], F32)
    rcoef = c.sbuf("rcoef", [128, 16], F32)
    nhalf = c.sbuf("nhalf", [128, 64], F32)
    onesf = c.sbuf("onesf", [128, 128], F32)
    AB = c.sbuf("AB", [128, 32], F32)
    GT1 = c.sbuf("GT1", [128, 1024], F32)
    GT2 = c.sbuf("GT2", [128, 1024], F32)
    gfin_t = c.sbuf("gfin_t", [128, 1024], F32)
    brout_t = c.sbuf("brout_t", [128, 64], F32)
    posf = c.sbuf("posf", [128, NT + NO], F32)
    junk = c.sbuf("junk", [128, 1024], BF16)
    comb = c.sbuf("comb", [128, NO, 64], F32)
    B1b = c.sbuf("B1b", [128, 8], BF16)
    onesb = c.sbuf("onesb", [1, 128], BF16)

    for (t_, d_, k_) in [(identb, identb_d, "c0"), (identf, identf_d, "c1"), (tri, tri_d, "c2"),
                         (amask, amask_d, "c3"), (invf, invf_d, "c4"), (rcoef, rcoef_d, "c5"),
                         (gfin_t, gfin, "c6"), (brout_t, brout, "c7")]:
        c.dma("sp", t_[:], d_, writes=["const"], key=k_)
    c.op("pool", lambda e: e.memset(nhalf[:], -0.5), writes=["const"])
    c.op("pool", lambda e: e.memset(onesf[:], 1.0), writes=["const"])
    c.barrier()
    if stop == "c":
        return finish()

    def rsqrt_cols(src_ap, dst_ap, ncols, rd, wr):
        c.op("pool", lambda e: e.tensor_tensor(out=dst_ap, in0=src_ap, in1=nhalf[:, 0:ncols], op=ALU.pow),
             reads=rd, writes=wr)

    c.push()
    PS = [c.psum(f"p0_{i}", [128, 512], F32) for i in range(8)]
    cv = c.sbuf("cv", [128, 8], F32)
    cs = c.sbuf("cs", [128, 8], F32)
    modrow = c.sbuf("modrow", [1, 6 * D], F32)
    brow = c.sbuf("brow", [1, 6 * D], F32)
    gcols = c.sbuf("gcols", [128, 16], F32)
    wab = [c.sbuf(f"wab{i}", [128, 8, 512], F32) for i in range(2)]
    posi = c.sbuf("posi", [128, NT + NO], I32)
    c.dma("sp", cv[:], cvec, writes=["cv"], key="p0a")
    c.dma("sp", brow[:], bada_row, writes=["brow"], key="p0b")
    c.dma("sp", gcols[:, 0:8], g1c, writes=["gcols"], key="p0c")
    c.dma("sp", gcols[:, 8:16], g2c, writes=["gcols"], key="p0d")
    c.dma("sp", posi[:, 0:NT], posall, writes=["posi"], key="p0e")
    c.dma("sp", posi[:, NT:NT + NO], posown, writes=["posi"], key="p0f")
    c.op("dve", lambda e: e.tensor_copy(out=posf[:], in_=posi[:]), reads=["posi"], writes=["posf"])
    c.op("act", lambda e: e.activation(out=cs[:], in_=cv[:], func=AF.Silu), reads=["cv"], writes=["cs"])
    for n in range(12):
        wt = wab[n % 2]
        c.dma("sp", wt[:], w_ada[:, n * 512:(n + 1) * 512].rearrange("(k p) n -> p k n", p=128),
              writes=[c.W("wab", n, 2)], key=f"wab{n % 2}")
        for k in range(8):
            c.op("pe", lambda e: e.matmul(PS[n % 2][0:1, :], lhsT=cs[:, k:k + 1], rhs=wt[:, k, :],
                                          start=(k == 0), stop=(k == 7)),
                 reads=[c.R("wab", n, 2), "cs"], writes=[("pmod", n % 2)])
        c.op("dve", lambda e: e.tensor_tensor(out=modrow[0:1, n * 512:(n + 1) * 512], in0=PS[n % 2][0:1, :],
                                              in1=brow[0:1, n * 512:(n + 1) * 512], op=ALU.add),
             reads=[("pmod", n % 2), "brow"], writes=["modrow"])
    if stop == "0a":
        dbg("modrow", modrow[:], [1, 6 * D], F32)
        c.pop()
        return finish()
    pc = PS[2]
    for idx, ch in enumerate(list(range(0, 16)) + list(range(24, 40))):
        c.op("pe", lambda e: e.matmul(pc[:, idx:idx + 1], lhsT=modrow[0:1, ch * 128:(ch + 1) * 128],
                                      rhs=onesf[0:1, 0:1], start=True, stop=True),
             reads=["modrow", "const"], writes=["pc"])
    c.op("dve", lambda e: e.scalar_tensor_tensor(out=AB[:, 0:8], in0=pc[:, 8:16], scalar=1.0, in1=gcols[:, 0:8],
                                                 op0=ALU.add, op1=ALU.mult), reads=["pc", "gcols"], writes=["AB"])
    c.op("dve", lambda e: e.tensor_copy(out=AB[:, 8:16], in_=pc[:, 0:8]), reads=["pc"], writes=["AB"])
    c.op("dve", lambda e: e.scalar_tensor_tensor(out=AB[:, 16:24], in0=pc[:, 24:32], scalar=1.0, in1=gcols[:, 8:16],
                                                 op0=ALU.add, op1=ALU.mult), reads=["pc", "gcols"], writes=["AB"])
    c.op("dve", lambda e: e.tensor_copy(out=AB[:, 24:32], in_=pc[:, 16:24]), reads=["pc"], writes=["AB"])
    if stop == "0b":
        c.pop()
        dbg("AB", AB[:], [128, 32], F32)
        return finish()
    for (dst, base, scl, pi) in [(GT1, 2048, 0.5, 4), (GT2, 5120, 1.0, 6)]:
        for hh in range(2):
            c.op("pe", lambda e: e.matmul(PS[pi + hh][:], lhsT=onesf[0:1, 0:128],
                                          rhs=modrow[0:1, base + hh * 512: base + (hh + 1) * 512],
                                          start=True, stop=True), reads=["modrow", "const"], writes=[("pgt", pi + hh)])
            c.op("act", lambda e: e.activation(out=dst[:, hh * 512:(hh + 1) * 512], in_=PS[pi + hh][:],
                                               func=AF.Copy, scale=scl), reads=[("pgt", pi + hh)], writes=["GT"])
    c.pop()
    dbg("AB", AB[:], [128, 32], F32)
    dbg("GT1", GT1[:], [128, 1024], F32)
    if stop == "0":
        return finish()

    A1, B1, A2, B2 = AB[:, 0:8], AB[:, 8:16], AB[:, 16:24], AB[:, 24:32]
    c.op("dve", lambda e: e.tensor_copy(out=B1b[:], in_=B1), reads=["AB"], writes=["B1b"])
    c.op("pool", lambda e: e.memset(onesb[:], 1.0), writes=["onesb"])

    def fold_adaln(W, ncols, wname, psb, pkey, tag):
        brow_ = c.sbuf("bw_" + tag, [1, ncols], BF16)
        for lo in range(0, ncols, 512):
            n = min(512, ncols - lo)
            for k in range(8):
                c.op("pe", lambda e: e.matmul(psb[0:1, 0:n], lhsT=B1b[:, k:k + 1], rhs=W[:, k, lo:lo + n], start=(k == 0),
                                              stop=(k == 7)), reads=[wname, "B1b"], writes=[pkey])
            c.op("act", lambda e: e.activation(out=brow_[0:1, lo:lo + n], in_=psb[0:1, 0:n], func=AF.Copy),
                 reads=[pkey], writes=["bw_" + tag])
        for k in range(8):
            c.op("dve", lambda e: e.tensor_scalar(out=W[:, k, :], in0=W[:, k, :], scalar1=A1[:, k:k + 1], scalar2=None,
                                                  op0=ALU.mult), reads=[wname, "AB"], writes=[wname])
        return brow_

    def bias_mm(ps_ap, brow_, lo, n, tag, wr):
        c.op("pe", lambda e: e.matmul(ps_ap, lhsT=onesb[0:1, 0:128], rhs=brow_[0:1, lo:lo + n], start=False, stop=True),
             reads=["onesb", "bw_" + tag], writes=wr)

    def load_w(dst_tile, src_ap, name, key):
        c.dma("pool", dst_tile, src_ap, writes=[name], key=key)

    def rope_tables(pos_ap, G, sinT, cosT, tmp, lo, hi, tag):
        n = hi - lo
        ang, u, r = (tmp[0][:, 0:G, 0:n], tmp[1][:, 0:G, 0:n], tmp[2][:, 0:G, 0:n])
        c.op("dve", lambda e: e.tensor_tensor(out=ang, in0=invf[:, lo:hi].unsqueeze(1).broadcast_to([128, G, n]),
                                              in1=pos_ap.unsqueeze(2).broadcast_to([128, G, n]), op=ALU.mult),
             reads=["posf", "const"], writes=[tag + "t0"])
        c.op("dve", lambda e: e.tensor_scalar(out=u, in0=ang, scalar1=1.0 / TWO_PI, scalar2=MAGIC,
                                              op0=ALU.mult, op1=ALU.add), reads=[tag + "t0"], writes=[tag + "t1"])
        c.op("dve", lambda e: e.tensor_scalar(out=u, in0=u, scalar1=-MAGIC, scalar2=None, op0=ALU.add),
             reads=[tag + "t1"], writes=[tag + "t1"])
        c.op("dve", lambda e: e.scalar_tensor_tensor(out=r, in0=u, scalar=-C1, in1=ang, op0=ALU.mult, op1=ALU.add),
             reads=[tag + "t1", tag + "t0"], writes=[tag + "t2"])
        c.op("dve", lambda e: e.scalar_tensor_tensor(out=r, in0=u, scalar=-C2, in1=r, op0=ALU.mult, op1=ALU.add),
             reads=[tag + "t1", tag + "t2"], writes=[tag + "t2"])
        c.op("dve", lambda e: e.scalar_tensor_tensor(out=r, in0=u, scalar=-C3, in1=r, op0=ALU.mult, op1=ALU.add),
             reads=[tag + "t1", tag + "t2"], writes=[tag + "t2"])
        c.op("dve", lambda e: e.tensor_scalar(out=r, in0=r, scalar1=-math.pi, scalar2=math.pi, op0=ALU.max, op1=ALU.min),
             reads=[tag + "t2"], writes=[tag + "t2"])
        c.op("act", lambda e: e.activation(out=sinT[:, 0:G, lo:hi], in_=r, func=AF.Sin), reads=[tag + "t2"],
             writes=[tag + "sin"])
        c.op("dve", lambda e: e.scalar_tensor_tensor(out=ang, in0=r, scalar=-1.0, in1=r, op0=ALU.mult, op1=ALU.max),
             reads=[tag + "t2"], writes=[tag + "t0"])
        c.op("dve", lambda e: e.tensor_scalar(out=ang, in0=ang, scalar1=-1.0, scalar2=math.pi / 2, op0=ALU.mult,
                                              op1=ALU.add), reads=[tag + "t0"], writes=[tag + "t0"])
        c.op("act", lambda e: e.activation(out=cosT[:, 0:G, lo:hi], in_=ang, func=AF.Sin), reads=[tag + "t0"],
             writes=[tag + "cos"])

    def rope(out4, z4, cos_b, sin_b, t1, t2, eng2, rd, wr, tmpname):
        c.op("dve", lambda e: e.tensor_tensor(out=t1, in0=z4, in1=cos_b, op=ALU.mult), reads=rd, writes=[tmpname + "1"])
        c.op("dve", lambda e: e.tensor_tensor(out=t2, in0=z4, in1=sin_b, op=ALU.mult), reads=rd, writes=[tmpname + "2"])
        c.op(eng2, lambda e: e.tensor_tensor(out=out4[:, :, 0, :], in0=t1[:, :, 0, :], in1=t2[:, :, 1, :], op=ALU.subtract),
             reads=[tmpname + "1", tmpname + "2"], writes=wr)
        c.op(eng2, lambda e: e.tensor_tensor(out=out4[:, :, 1, :], in0=t1[:, :, 1, :], in1=t2[:, :, 0, :], op=ALU.add),
             reads=[tmpname + "1", tmpname + "2"], writes=wr)

    class HT:
        def __init__(self, src, nb, A, B, pT):
            self.src, self.nb, self.A, self.B, self.pT = src, nb, A, B, pT
            self.xt = [c.sbuf("xt", [128, 1024], F32) for _ in range(nb)]
            self.xs = [c.sbuf("xs", [128, 1024], BF16) for _ in range(2)]
            self.hT = [c.sbuf("hT", [128, 8, 128], BF16) for _ in range(2)]
            self.st = c.sbuf("hst", [128, 3 * 64], F32)

        def load(self, t):
            c.dma("sp", self.xt[t % self.nb][:], self.src[t * 128:(t + 1) * 128, :],
                  writes=[c.W("xt", t, self.nb)], key=f"xt{t % self.nb}")

        def norm(self, t):
            xt, st = self.xt[t % self.nb], self.st
            a, b, r = st[:, t % 64:t % 64 + 1], st[:, 64 + t % 64:65 + t % 64], st[:, 128 + t % 64:129 + t % 64]
            c.op("act", lambda e: e.activation(out=junk[:], in_=xt[:], func=AF.Square, accum_out=a),
                 reads=[c.R("xt", t, self.nb)], writes=["junk", ("hsa", t % 64)])
            c.op("dve", lambda e: e.tensor_scalar(out=b, in0=a, scalar1=1.0 / D, scalar2=RMS_EPS, op0=ALU.mult,
                                                  op1=ALU.add), reads=[("hsa", t % 64)], writes=[("hsb", t % 64)])
            rsqrt_cols(b, r, 1, [("hsb", t % 64)], [("hsr", t % 64)])
            c.op("dve", lambda e: e.tensor_scalar(out=self.xs[t % 2][:], in0=xt[:], scalar1=r, scalar2=None,
                                                  op0=ALU.mult), reads=[c.R("xt", t, self.nb), ("hsr", t % 64)],
                 writes=[c.W("xs", t, 2)])

        def transpose(self, t):
            xs, hT, pT = self.xs[t % 2], self.hT[t % 2], self.pT
            pTb = pT[:].bitcast(BF16)
            for k in range(8):
                c.op("pe", lambda e: e.transpose(out=pTb[:, k * 128:(k + 1) * 128], in_=xs[:, k * 128:(k + 1) * 128],
                                                 identity=identb[:]), reads=[c.R("xs", t, 2), "const"], writes=["pT"])
            c.W("hT", t, 2)
            c.op("act", lambda e: e.activation(out=hT[:].rearrange("p k n -> p (k n)"), in_=pTb[:, :], func=AF.Copy),
                 reads=["pT"], writes=[("hT", t % 2)])

        def get(self, t):
            return self.hT[t % 2], c.R("hT", t, 2)

    c.push()
    ckvnT = c.sbuf("ckvnT", [128, 2, S], BF16)
    KT = [c.sbuf(f"KT{i}", [128, S], BF16) for i in range(2)]

    c.push()
    PS = [c.psum(f"pa_{i}", [128, 512], F32) for i in range(8)]
    WA = c.sbuf("WA", [128, 8, 1824], BF16)
    load_w(WA[:, :, 0:288], w_in[:, O_CKV:O_CKV + 288].rearrange("(k p) n -> p k n", p=128), "WA", "wA0")
    load_w(WA[:, :, 288:800], w_in[:, O_RK:O_RK + 512].rearrange("(k p) n -> p k n", p=128), "WA", "wA1")
    load_w(WA[:, :, 800:1824], w_in[:, O_RV:O_RV + 1024].rearrange("(k p) n -> p k n", p=128), "WA", "wA2")
    bwA = fold_adaln(WA, 1824, "WA", PS[1], ("pz", 1), "A")
    kdecA = c.sbuf("kdecA", [128, 4, 128], F32)
    c.dma("sp", kdecA[:], kdecA_d, writes=["kdecA"], key="kdA")
    ht = HT(xall, 3, A1, B1, PS[0])
    sinT = [c.sbuf("sinT", [128, 4, 80], F32) for _ in range(2)]
    cosT = [c.sbuf("cosT", [128, 4, 80], F32) for _ in range(2)]
    rtmp = [c.sbuf("rtmp", [128, 4, 80], F32) for _ in range(3)]
    stA = c.sbuf("stA", [128, 3 * 64], F32)
    ckvs = [c.sbuf("ckvs", [128, 256], BF16) for _ in range(2)]
    kst = [c.sbuf("kst", [128, 96], BF16) for _ in range(2)]
    kr1 = c.sbuf("kr1", [128, 32], F32)
    kr2 = c.sbuf("kr2", [128, 32], F32)
    rk1 = c.sbuf("rk1", [128, 512], F32)
    rk2 = c.sbuf("rk2", [128, 512], F32)
    rk3 = c.sbuf("rk3", [128, 512], F32)
    kp = [c.sbuf("kp", [128, 4, 128], BF16) for _ in range(2)]
    vb = [c.sbuf("vb", [128, 1024], BF16) for _ in range(2)]
    Sst = c.sbuf("Sst", [128, 1024], F32)
    Sown = [c.sbuf("Sown", [128, 1024], F32) for _ in range(2)]
    Sownb = [c.sbuf("Sownb", [128, 1024], BF16) for _ in range(2)]
    for i in range(2):
        c.op("pool", lambda e: e.memset(kst[i][:], 0.0), writes=[("kst", i)])
    c.op("pool", lambda e: e.memset(Sst[:], 0.0), writes=["Sst"])

    def A_s0(t):
        if t == 0:
            ht.load(0)
            ht.load(1)
        if t + 2 < NT:
            ht.load(t + 2)
        if t % 4 == 0:
            g = t // 4
            rope_tables(posf[:, t:t + 4], 4, sinT[g % 2], cosT[g % 2], rtmp, 0, 80, f"rtA{g % 2}")
            c.W("tabA", g, 2)
        ht.norm(t)

    def A_s0b(t):
        ht.transpose(t)

    def A_s1(t):
        hT, hk = ht.get(t)
        for (pi, lo, n) in [(1, 0, 288), (2, 288, 512), (3, 800, 512), (4, 1312, 512)]:
            for k in range(8):
                c.op("pe", lambda e: e.matmul(PS[pi][:, 0:n], lhsT=hT[:, k, :], rhs=WA[:, k, lo:lo + n],
                                              start=(k == 0), stop=False), reads=[hk, "WA"], writes=[("pz", pi)])
            bias_mm(PS[pi][:, 0:n], bwA, lo, n, "A", [("pz", pi)])
        m = t % 64
        a, b, r = stA[:, m:m + 1], stA[:, 64 + m:65 + m], stA[:, 128 + m:129 + m]
        c.op("act", lambda e: e.activation(out=junk[:, 0:256], in_=PS[1][:, 0:256], func=AF.Square, accum_out=a),
             reads=[("pz", 1)], writes=["junk", ("sAa", m)])
        c.op("dve", lambda e: e.tensor_scalar(out=b, in0=a, scalar1=1.0 / 256, scalar2=RMS_EPS, op0=ALU.mult,
                                              op1=ALU.add), reads=[("sAa", m)], writes=[("sAb", m)])
        rsqrt_cols(b, r, 1, [("sAb", m)], [("sAr", m)])
        c.op("dve", lambda e: e.tensor_scalar(out=ckvs[t % 2][:], in0=PS[1][:, 0:256], scalar1=r, scalar2=None,
                                              op0=ALU.mult), reads=[("pz", 1), ("sAr", m)], writes=[c.W("ckvs", t, 2)])
        g = t // 4
        tk = c.R("tabA", g, 2)
        sn, cs_ = sinT[g % 2], cosT[g % 2]
        z4 = PS[1][:, 256:288].rearrange("p (h t d) -> p h t d", h=1, t=2)
        cb = cs_[:, t % 4, 0:16].unsqueeze(1).unsqueeze(1).broadcast_to([128, 1, 2, 16])
        sb = sn[:, t % 4, 0:16].unsqueeze(1).unsqueeze(1).broadcast_to([128, 1, 2, 16])
        o4 = kst[t % 2][:, 64:96].rearrange("p (h t d) -> p h t d", h=1, t=2)
        rope(o4, z4, cb, sb, kr1[:].rearrange("p (h t d) -> p h t d", h=1, t=2),
             kr2[:].rearrange("p (h t d) -> p h t d", h=1, t=2), "pool",
             [("pz", 1), f"rtA{g % 2}sin", f"rtA{g % 2}cos"], [c.W("kst", t, 2)], "kr")
        z4 = PS[2][:].rearrange("p (h t d) -> p h t d", h=4, t=2)
        cb = cs_[:, t % 4, 16:80].unsqueeze(1).unsqueeze(1).broadcast_to([128, 4, 2, 64])
        sb = sn[:, t % 4, 16:80].unsqueeze(1).unsqueeze(1).broadcast_to([128, 4, 2, 64])
        o4 = rk3[:].rearrange("p (h t d) -> p h t d", h=4, t=2)
        rope(o4, z4, cb, sb, rk1[:].rearrange("p (h t d) -> p h t d", h=4, t=2),
             rk2[:].rearrange("p (h t d) -> p h t d", h=4, t=2), "pool",
             [("pz", 2), f"rtA{g % 2}sin", f"rtA{g % 2}cos"], ["rk3"], "rk")
        c.op("pool", lambda e: e.tensor_tensor(out=kp[t % 2][:], in0=rk3[:].rearrange("p (h d) -> p h d", h=4),
                                               in1=kdecA[:], op=ALU.mult), reads=["rk3", "kdecA"],
             writes=[c.W("kp", t, 2)])
        c.W("vb", t, 2)
        for hh in range(2):
            c.op("act", lambda e: e.activation(out=vb[t % 2][:, hh * 512:(hh + 1) * 512], in_=PS[3 + hh][:],
                                               func=AF.Copy), reads=[("pz", 3 + hh)], writes=[("vb", t % 2)])

    import os
    _lv = int(os.environ.get("DBG_LV", 9))

    def A_s2(t):
        pt = PS[5][:].bitcast(BF16)
        for k in range(2):
            c.op("pe", lambda e: e.transpose(out=pt[:, k * 128:(k + 1) * 128], in_=ckvs[t % 2][:, k * 128:(k + 1) * 128],
                                             identity=identb[:]), reads=[c.R("ckvs", t, 2), "const"], writes=["ptA"])
        c.op("pe", lambda e: e.transpose(out=pt[0:96, 256:384], in_=kst[t % 2][:], identity=identb[:]),
             reads=[c.R("kst", t, 2), "const"], writes=["ptA"])
        if _lv < 2:
            return
        for k in range(2):
            c.op("act", lambda e: e.activation(out=ckvnT[:, k, t * 128:(t + 1) * 128],
                                               in_=pt[:, k * 128:(k + 1) * 128], func=AF.Copy),
                 reads=["ptA"], writes=["ckvnT"])
        if _lv < 3:
            return
        _kt = os.environ.get("DBG_KT", "both")
        if _kt in ("both", "act"):
            c.op("act", lambda e: e.activation(out=KT[0][64:96, t * 128:(t + 1) * 128], in_=pt[64:96, 256:384],
                                               func=AF.Copy), reads=["ptA"], writes=["KT0r"])
        if _kt in ("both", "dve"):
            c.op("act", lambda e: e.activation(out=KT[1][64:96, t * 128:(t + 1) * 128], in_=pt[64:96, 256:384],
                                               func=AF.Copy), reads=["ptA"], writes=["KT1r"])
        if _lv < 4:
            return
        pkv = [PS[6], PS[7]]
        for h in range(4):
            c.op("pe", lambda e: e.matmul(pkv[h // 2][:, (h % 2) * 256:(h % 2 + 1) * 256], lhsT=kp[t % 2][:, h, :],
                                          rhs=vb[t % 2][:, h * 256:(h + 1) * 256], start=True, stop=True),
                 reads=[c.R("kp", t, 2), c.R("vb", t, 2)], writes=["pkv"])
        if _lv < 5:
            return
        l, g = t % 4, t // 4
        so = Sown[g % 2]
        if l == 0:
            c.W("Sown", g, 2)
        for h in range(4):
            hs = slice(h * 256, (h + 1) * 256)
            pk = pkv[h // 2][:, (h % 2) * 256:(h % 2 + 1) * 256]
            if l == 0:
                c.op("act", lambda e: e.activation(out=so[:, hs], in_=Sst[:, hs], func=AF.Copy,
                                                   scale=rcoef[:, h * 4:h * 4 + 1]), reads=["Sst", "const"],
                     writes=[("Sown", g % 2)])
            if l < 3:
                c.op("dve", lambda e: e.scalar_tensor_tensor(out=so[:, hs], in0=pk, scalar=rcoef[:, h * 4 + 1 + l:h * 4 + 2 + l],
                                                             in1=so[:, hs], op0=ALU.mult, op1=ALU.add),
                     reads=["pkv", ("Sown", g % 2), "const"], writes=[("Sown", g % 2)])
            c.op("dve", lambda e: e.scalar_tensor_tensor(out=Sst[:, hs], in0=Sst[:, hs], scalar=GAMMA[h] ** 128, in1=pk,
                                                         op0=ALU.mult, op1=ALU.add), reads=["pkv", "Sst"], writes=["Sst"])
        if l == 3 and _lv >= 6:
            c.op("act", lambda e: e.activation(out=Sownb[g % 2][:], in_=so[:], func=AF.Copy),
                 reads=[c.R("Sown", g, 2)], writes=[c.W("Sownb", g, 2)])
            c.dma("sp", sown_d[g], Sownb[g % 2][:], reads=[c.R("Sownb", g, 2)], writes=["sown_d"], key=f"so{g % 2}")

    import os
    _na = int(os.environ.get("DBG_NA", NT))
    _ns = int(os.environ.get("DBG_NS", 3))
    pipeline(_na, [A_s0, A_s0b, A_s1, A_s2][:_ns + 1])
    print("sbuf remaining in pass A:", nc.sbuf_bytes_remaining, "ops", c.nops)
    dbg("Sst", Sst[:], [128, 1024], F32)
    c.pop()
    dbg("ckvnT", ckvnT[:], [128, 2, S], BF16)
    dbg("KT0", KT[0][:], [128, S], BF16)
    if stop == "A":
        return finish()

    QT = c.sbuf("QT", [128, 8, NO * 128], BF16)
    c.push()
    PS = [c.psum(f"pq_{i}", [128, 512], F32) for i in range(8)]
    WQ = c.sbuf("WQ", [128, 8, 384], BF16)
    load_w(WQ[:], w_in[:, O_CQ:O_CQ + 384].rearrange("(k p) n -> p k n", p=128), "WQ", "wQ0")
    bwQ = fold_adaln(WQ, 384, "WQ", PS[1], "pcq", "Q")
    wuq = c.sbuf("wuq", [128, 3, 768], BF16)
    c.push()
    wuq_f = c.sbuf("wuq_f", [128, 3, 768], F32)
    gq = c.sbuf("gq", [128, 3], F32)
    c.dma("sp", wuq_f[:], w_uq.rearrange("(k p) n -> p k n", p=128), writes=["wuq_f"], key="wQ1")
    c.dma("sp", gq[:], gcq, writes=["gq"], key="wQ2")
    for k in range(3):
        wv = wuq_f[:, k, :].rearrange("p (h d) -> p h d", h=8)
        c.op("dve", lambda e: e.tensor_scalar(out=wuq[:, k, 0:512].rearrange("p (h d) -> p h d", h=8), in0=wv[:, :, 0:64],
                                              scalar1=gq[:, k:k + 1], scalar2=None, op0=ALU.mult),
             reads=["wuq_f", "gq"], writes=["wuq"])
        c.op("dve", lambda e: e.tensor_scalar(out=wuq[:, k, 512:768].rearrange("p (h d) -> p h d", h=8), in0=wv[:, :, 64:96],
                                              scalar1=gq[:, k:k + 1], scalar2=None, op0=ALU.mult),
             reads=["wuq_f", "gq"], writes=["wuq"])
    c.pop()
    ht = HT(xown, 3, A1, B1, PS[0])
    sinQ = c.sbuf("sinQ", [128, NO, 16], F32)
    cosQ = c.sbuf("cosQ", [128, NO, 16], F32)
    c.push()
    rtmpQ = [c.sbuf("rtmpQ", [128, NO, 16], F32) for _ in range(3)]
    rope_tables(posf[:, NT:NT + NO], NO, sinQ, cosQ, rtmpQ, 0, 16, "rtQ")
    c.pop()
    stQ = c.sbuf("stQ", [128, 3 * 64], F32)
    cqs = [c.sbuf("cqs", [128, 384], BF16) for _ in range(2)]
    cqnT = [c.sbuf("cqnT", [128, 3, 128], BF16) for _ in range(2)]
    qsb = [c.sbuf("qsb", [128, 8, 96], BF16) for _ in range(2)]
    qr1 = c.sbuf("qr1", [128, 8, 32], F32)
    qr2 = c.sbuf("qr2", [128, 8, 32], F32)

    def Q_s0(t):
        if t == 0:
            ht.load(0)
            ht.load(1)
        if t + 2 < NO:
            ht.load(t + 2)
        ht.norm(t)

    def Q_s0b(t):
        ht.transpose(t)

    def Q_s1(t):
        hT, hk = ht.get(t)
        for k in range(8):
            c.op("pe", lambda e: e.matmul(PS[1][:, 0:384], lhsT=hT[:, k, :], rhs=WQ[:, k, :], start=(k == 0),
                                          stop=False), reads=[hk, "WQ"], writes=["pcq"])
        bias_mm(PS[1][:, 0:384], bwQ, 0, 384, "Q", ["pcq"])
        m = t
        a, b, r = stQ[:, m:m + 1], stQ[:, 64 + m:65 + m], stQ[:, 128 + m:129 + m]
        c.op("act", lambda e: e.activation(out=junk[:, 0:384], in_=PS[1][:, 0:384], func=AF.Square, accum_out=a),
             reads=["pcq"], writes=["junk", ("sQa", m)])
        c.op("dve", lambda e: e.tensor_scalar(out=b, in0=a, scalar1=1.0 / 384, scalar2=RMS_EPS, op0=ALU.mult,
                                              op1=ALU.add), reads=[("sQa", m)], writes=[("sQb", m)])
        rsqrt_cols(b, r, 1, [("sQb", m)], [("sQr", m)])
        c.op("dve", lambda e: e.tensor_scalar(out=cqs[t % 2][:], in0=PS[1][:, 0:384], scalar1=r, scalar2=None,
                                              op0=ALU.mult), reads=["pcq", ("sQr", m)], writes=[c.W("cqs", t, 2)])
        pt = PS[2][:].bitcast(BF16)
        for k in range(3):
            c.op("pe", lambda e: e.transpose(out=pt[:, k * 128:(k + 1) * 128], in_=cqs[t % 2][:, k * 128:(k + 1) * 128],
                                             identity=identb[:]), reads=[c.R("cqs", t, 2), "const"], writes=["ptQ"])
        c.op("act", lambda e: e.activation(out=cqnT[t % 2][:], in_=pt[:, 0:384].rearrange("p (k n) -> p k n", k=3),
                                           func=AF.Copy), reads=["ptQ"], writes=[c.W("cqnT", t, 2)])

    def Q_s2(t):
        for (pi, lo, n) in [(3, 0, 512), (4, 512, 256)]:
            for k in range(3):
                c.op("pe", lambda e: e.matmul(PS[pi][:, 0:n], lhsT=cqnT[t % 2][:, k, :], rhs=wuq[:, k, lo:lo + n],
                                              start=(k == 0), stop=(k == 2)),
                     reads=[c.R("cqnT", t, 2), "wuq"], writes=[("pq", pi)])
        c.W("qsb", t, 2)
        _ql = int(os.environ.get("DBG_QL", 9))
        c.op("act", lambda e: e.activation(out=qsb[t % 2][:, :, 0:64], in_=PS[3][:].rearrange("p (h d) -> p h d", h=8),
                                           func=AF.Copy), reads=[("pq", 3)], writes=[("qsb", t % 2)])
        z4 = PS[4][:, 0:256].rearrange("p (h t d) -> p h t d", h=8, t=2)
        cb = cosQ[:, t, 0:16].unsqueeze(1).unsqueeze(1).broadcast_to([128, 8, 2, 16])
        sb = sinQ[:, t, 0:16].unsqueeze(1).unsqueeze(1).broadcast_to([128, 8, 2, 16])
        o4 = qsb[t % 2][:, :, 64:96].rearrange("p h (t d) -> p h t d", t=2)
        rope(o4, z4, cb, sb, qr1[:].rearrange("p h (t d) -> p h t d", t=2),
             qr2[:].rearrange("p h (t d) -> p h t d", t=2), "pool",
             [("pq", 4), "rtQsin", "rtQcos"], [("qsb", t % 2)], "qr")
        if _ql < 3:
            return
        pt = PS[5][:].bitcast(BF16)
        for h in range(8):
            c.op("pe", lambda e: e.transpose(out=pt[0:96, h * 128:(h + 1) * 128], in_=qsb[t % 2][:, h, :],
                                             identity=identb[:]), reads=[("qsb", t % 2), "const"], writes=["ptQ2"])
        if _ql < 4:
            return
        c.op("act", lambda e: e.activation(out=QT[0:96, :, t * 128:(t + 1) * 128],
                                           in_=pt[0:96, :].rearrange("p (h n) -> p h n", h=8), func=AF.Copy),
             reads=["ptQ2"], writes=["QT"])

    pipeline(int(os.environ.get("DBG_NQ", NO)), [Q_s0, Q_s0b, Q_s1, Q_s2])
    print("sbuf remaining in pass Q:", nc.sbuf_bytes_remaining, "ops", c.nops)
    c.pop()
    dbg("QT", QT[:], [128, 8, NO * 128], BF16)
    if stop == "Q":
        return finish()

    c.push()
    PS = [c.psum(f"pt_{i}", [128, 512], F32) for i in range(8)]
    wukv = c.sbuf("wukv", [128, 2, 1024], BF16)
    c.push()
    wukv_f = c.sbuf("wukv_f", [128, 2, 1024], F32)
    gkv = c.sbuf("gkv", [128, 2], F32)
    c.dma("sp", wukv_f[:], w_ukv.rearrange("(k p) n -> p k n", p=128), writes=["wukv_f"], key="wT0")
    c.dma("sp", gkv[:], gckv, writes=["gkv"], key="wT1")
    for k in range(2):
        c.op("dve", lambda e: e.tensor_scalar(out=wukv[:, k, :], in0=wukv_f[:, k, :], scalar1=gkv[:, k:k + 1],
                                              scalar2=None, op0=ALU.mult), reads=["wukv_f", "gkv"], writes=["wukv"])
    c.pop()
    otb = [c.sbuf("otb", [64, 512], BF16) for _ in range(2)]
    Vb = [c.sbuf("Vb", [128, NT, 65], BF16) for _ in range(2)]
    for i in range(2):
        c.op("pool", lambda e: e.memset(Vb[i][:, :, 64:65], 1.0), writes=[("Vb1", i)])
    PT = [c.sbuf("PT", [128, 512], BF16) for _ in range(4)]
    osb = [c.sbuf("osb", [65, 512], F32) for _ in range(2)]
    rec = [c.sbuf("rec", [64, 512], F32) for _ in range(2)]
    fin = [0]

    def up_units(h):
        kt_buf, v_buf = KT[h % 2], Vb[h % 2]
        units = []

        def k_unit(kc):
            def f():
                pk = PS[kc % 2]
                for k in range(2):
                    c.op("pe", lambda e: e.matmul(pk[0:64, :], lhsT=wukv[:, k, h * 128:h * 128 + 64],
                                                  rhs=ckvnT[:, k, kc * 512:(kc + 1) * 512], start=(k == 0), stop=(k == 1)),
                         reads=["wukv", "ckvnT"], writes=[("pk", kc % 2)])
                c.op("dve", lambda e: e.tensor_copy(out=kt_buf[0:64, kc * 512:(kc + 1) * 512], in_=pk[0:64, :]),
                     reads=[("pk", kc % 2)], writes=[("KTn", h % 2)])
            return f

        def v_unit(kb):
            def f():
                pv = PS[kb % 2]
                for j8 in range(8):
                    kt = kb * 8 + j8
                    for k in range(2):
                        c.op("pe", lambda e: e.matmul(pv[:, j8 * 64:(j8 + 1) * 64], lhsT=ckvnT[:, k, kt * 128:(kt + 1) * 128],
                                                      rhs=wukv[:, k, h * 128 + 64:h * 128 + 128], start=(k == 0),
                                                      stop=(k == 1)), reads=["wukv", "ckvnT"], writes=[("pk", kb % 2)])
                c.op("dve", lambda e: e.tensor_copy(out=v_buf[:, kb * 8:(kb + 1) * 8, 0:64],
                                                    in_=pv[:].rearrange("p (j d) -> p j d", j=8)),
                     reads=[("pk", kb % 2)], writes=[("Vb", h % 2)])
            return f

        for kc in range(16):
            units.append(k_unit(kc))
        for kb in range(8):
            units.append(v_unit(kb))
        return units

    steps = [(h, qc, kt) for h in range(8) for qc in range(4) for kt in range(16 * qc + 16)]
    pending = {}
    c.W("KTn", 0, 2)
    c.W("Vb", 0, 2)
    for u in up_units(0):
        u()

    def att_s0(i):
        h, qc, kt = steps[i]
        kt_buf = KT[h % 2]
        if qc == 0 and kt == 0 and h + 1 < 8:
            pending["units"] = up_units(h + 1)
            pending["local"] = 0
            pending["armed"] = False
        if pending.get("units"):
            pending["local"] += 1
            if pending["local"] >= 4 and (pending["local"] - 4) % 6 == 0:
                if not pending["armed"]:
                    c.W("KTn", h + 1, 2)
                    c.W("Vb", h + 1, 2)
                    pending["armed"] = True
                pending["units"].pop(0)()
        gk, l = kt // 4, kt % 4
        c0 = (max(gk, 4 * qc) - 4 * qc) * 128
        ps, pt_ = PS[2 + i % 4], PT[i % 4]
        c.op("pe", lambda e: e.matmul(ps[:, c0:512], lhsT=kt_buf[0:96, kt * 128:(kt + 1) * 128],
                                      rhs=QT[0:96, h, qc * 512 + c0:(qc + 1) * 512], start=True, stop=True),
             reads=[("KTn", h % 2), f"KT{h % 2}r", "QT"], writes=[("ps", i % 4)])
        c.op("act", lambda e: e.activation(out=pt_[:, c0:512], in_=ps[:, c0:512], func=AF.Exp, scale=SCALE_MLA),
             reads=[("ps", i % 4)], writes=[("PT", i % 4)])
        if gk >= 4 * qc:
            c.op("pool", lambda e: e.tensor_tensor(out=pt_[:, c0:c0 + 128], in0=pt_[:, c0:c0 + 128],
                                                   in1=amask[:, l, :], op=ALU.mult),
                 reads=[("PT", i % 4), "const"], writes=[("PT", i % 4)])

    def att_s1(i):
        pass

    def att_s2(i):
        h, qc, kt = steps[i]
        v_buf = Vb[h % 2]
        nk = 16 * qc + 16
        gk = kt // 4
        c0 = (max(gk, 4 * qc) - 4 * qc) * 128
        po = PS[6 + qc % 2]
        pt_ = PT[i % 4]
        c.op("pe", lambda e: e.matmul(po[0:65, c0:512], lhsT=v_buf[:, kt, 0:65], rhs=pt_[:, c0:512],
                                      start=(kt == 0), stop=(kt == nk - 1)),
             reads=[("Vb", h % 2), ("Vb1", h % 2), ("PT", i % 4)], writes=[("po", qc % 2)])
        if kt == nk - 1:
            f = fin[0]
            fin[0] += 1
            ob, rc = osb[f % 2], rec[f % 2]
            c.op("dve", lambda e: e.tensor_copy(out=ob[:], in_=po[0:65, :]), reads=[("po", qc % 2)],
                 writes=[("osb", f % 2)])
            pd = PS[f % 2]
            c.op("pe", lambda e: e.matmul(pd[0:64, :], lhsT=onesf[64:65, 0:64], rhs=ob[64:65, :], start=True, stop=True),
                 reads=[("osb", f % 2), "const"], writes=[("pk", f % 2)])
            c.op("dve", lambda e: e.reciprocal(out=rc[:], in_=pd[0:64, :]), reads=[("pk", f % 2)], writes=[("rec", f % 2)])
            c.op("dve", lambda e: e.tensor_tensor(out=otb[f % 2][:], in0=ob[0:64, :], in1=rc[:],
                                                  op=ALU.mult), reads=[("osb", f % 2), ("rec", f % 2)], writes=[("otb", f % 2)])
            c.dma("sp", ot_d[h, :, qc * 512:(qc + 1) * 512], otb[f % 2][:], reads=[("otb", f % 2)], writes=["ot_d"],
                  key=f"otw{f % 2}")

    pipeline(len(steps), [att_s0, att_s1, att_s1, att_s2])
    c.pop()
    c.pop()
    if stop == "T":
        return finish()

    c.push()
    PS = [c.psum(f"pc_{i}", [128, 512], F32) for i in range(8)]
    WC = c.sbuf("WC", [128, 8, 3072], BF16)
    load_w(WC[:, :, 0:1024], w_in[:, O_RQ:O_RQ + 1024].rearrange("(k p) n -> p k n", p=128), "WC", "wC0")
    load_w(WC[:, :, 1024:2048], w_in[:, O_RV:O_RV + 1024].rearrange("(k p) n -> p k n", p=128), "WC", "wC1")
    load_w(WC[:, :, 2048:3072], w_in[:, O_RG:O_RG + 1024].rearrange("(k p) n -> p k n", p=128), "WC", "wC2")
    bwC = fold_adaln(WC, 3072, "WC", PS[1], ("pz", 1), "C")
    wor = c.sbuf("wor", [128, 8, 1024], BF16)
    c.push()
    wor_f = c.sbuf("wor_f", [128, 8, 1024], F32)
    gr = c.sbuf("gr", [128, 8], F32)
    c.dma("sp", wor_f[:], w_o_ret.rearrange("(k p) n -> p k n", p=128), writes=["wor_f"], key="wC3")
    c.dma("sp", gr[:], gret, writes=["gr"], key="wC4")
    for k in range(8):
        c.op("dve", lambda e: e.tensor_scalar(out=wor[:, k, :], in0=wor_f[:, k, :], scalar1=gr[:, k:k + 1],
                                              scalar2=None, op0=ALU.mult), reads=["wor_f", "gr"], writes=["wor"])
    c.pop()
    kdecC = c.sbuf("kdecC", [128, 8, 128], F32)
    c.dma("sp", kdecC[:, 0:4, :], qdec_d, writes=["kdecC"], key="wC5")
    c.dma("sp", kdecC[:, 4:8, :], kdecC_d, writes=["kdecC"], key="wC6")
    ht = HT(xown, 3, A1, B1, PS[0])
    sinC = c.sbuf("sinC", [128, NO, 80], F32)
    cosC = c.sbuf("cosC", [128, NO, 80], F32)
    c.push()
    rtmpC = [c.sbuf("rtmpC", [128, NO, 64], F32) for _ in range(3)]
    rope_tables(posf[:, NT:NT + NO], NO, sinC, cosC, rtmpC, 16, 80, "rtC")
    c.pop()
    qk1 = c.sbuf("qk1", [128, 1024], F32)
    qk2 = c.sbuf("qk2", [128, 1024], F32)
    qk3 = c.sbuf("qk3", [128, 1024], F32)
    qkp = [c.sbuf("qkp", [128, 8, 128], BF16) for _ in range(2)]
    qkT = [c.sbuf("qkT", [128, 8, 128], BF16) for _ in range(2)]
    vbc = [c.sbuf("vbc", [128, 1024], BF16) for _ in range(2)]
    sg = [c.sbuf("sg", [128, 1024], BF16) for _ in range(2)]
    scT = [c.sbuf("scT", [128, 4, 128], BF16) for _ in range(2)]
    sob = [c.sbuf("sob", [128, 1024], BF16) for _ in range(2)]
    bnst = c.sbuf("bnst", [128, NO, 4, 6], F32)
    bnag = c.sbuf("bnag", [128, NO, 4, 2], F32)
    bnr = c.sbuf("bnr", [128, NO, 4, 2], F32)
    onr = [c.sbuf("onr", [128, 1024], F32) for _ in range(2)]
    gat = [c.sbuf("gat", [128, 1024], BF16) for _ in range(2)]
    gT = [c.sbuf("gT", [128, 8, 128], BF16) for _ in range(2)]
    bbt = [c.sbuf("bbt", [128, 1024], BF16) for _ in range(2)]

    def C1_s0(t):
        if t == 0:
            ht.load(0)
            ht.load(1)
        if t + 2 < NO:
            ht.load(t + 2)
        c.dma("sp", sob[t % 2][:], sown_d[t], reads=[], writes=[c.W("sob", t, 2)], key=f"sob{t % 2}")
        ht.norm(t)

    def C1_s0b(t):
        ht.transpose(t)

    def C1_s1(t):
        hT, hk = ht.get(t)
        for (pi, lo) in [(1, 0), (2, 512), (3, 1024), (4, 1536), (5, 2048), (6, 2560)]:
            for k in range(8):
                c.op("pe", lambda e: e.matmul(PS[pi][:], lhsT=hT[:, k, :], rhs=WC[:, k, lo:lo + 512], start=(k == 0),
                                              stop=False), reads=[hk, "WC"], writes=[("pz", pi)])
            bias_mm(PS[pi][:], bwC, lo, 512, "C", [("pz", pi)])
        cb = cosC[:, t, 16:80].unsqueeze(1).unsqueeze(1).broadcast_to([128, 4, 2, 64])
        sb = sinC[:, t, 16:80].unsqueeze(1).unsqueeze(1).broadcast_to([128, 4, 2, 64])
        for j in range(2):
            z4 = PS[1 + j][:].rearrange("p (h t d) -> p h t d", h=4, t=2)
            sl = slice(j * 512, (j + 1) * 512)
            rope(qk3[:, sl].rearrange("p (h t d) -> p h t d", h=4, t=2), z4, cb, sb,
                 qk1[:, sl].rearrange("p (h t d) -> p h t d", h=4, t=2),
                 qk2[:, sl].rearrange("p (h t d) -> p h t d", h=4, t=2), "pool",
                 [("pz", 1 + j), "rtCsin", "rtCcos"], [("qk3", j)], f"qk{j}")
        c.op("pool", lambda e: e.tensor_tensor(out=qkp[t % 2][:], in0=qk3[:].rearrange("p (h d) -> p h d", h=8),
                                               in1=kdecC[:], op=ALU.mult), reads=[("qk3", 0), ("qk3", 1), "kdecC"],
             writes=[c.W("qkp", t, 2)])
        c.W("vbc", t, 2)
        c.W("sg", t, 2)
        for hh in range(2):
            c.op("act", lambda e: e.activation(out=vbc[t % 2][:, hh * 512:(hh + 1) * 512], in_=PS[3 + hh][:],
                                               func=AF.Copy), reads=[("pz", 3 + hh)], writes=[("vbc", t % 2)])
            c.op("act", lambda e: e.activation(out=sg[t % 2][:, hh * 512:(hh + 1) * 512], in_=PS[5 + hh][:],
                                               func=AF.Silu), reads=[("pz", 5 + hh)], writes=[("sg", t % 2)])

    def C1_s2(t):
        pt = PS[7][:].bitcast(BF16)
        for j in range(8):
            c.op("pe", lambda e: e.transpose(out=pt[:, j * 128:(j + 1) * 128], in_=qkp[t % 2][:, j, :],
                                             identity=identb[:]), reads=[c.R("qkp", t, 2), "const"], writes=["ptC"])
        c.op("act", lambda e: e.activation(out=qkT[t % 2][:], in_=pt.rearrange("p (j n) -> p j n", j=8), func=AF.Copy),
             reads=["ptC"], writes=[c.W("qkT", t, 2)])
        for h in range(4):
            c.op("pe", lambda e: e.matmul(PS[1][:, h * 128:(h + 1) * 128], lhsT=qkT[t % 2][:, 4 + h, :],
                                          rhs=qkT[t % 2][:, h, :], start=True, stop=True),
                 reads=[c.R("qkT", t, 2)], writes=[("pz", 1)])
        c.op("dve", lambda e: e.tensor_tensor(out=scT[t % 2][:], in0=PS[1][:].rearrange("p (h n) -> p h n", h=4),
                                              in1=tri[:].unsqueeze(1).broadcast_to([128, 4, 128]), op=ALU.mult),
             reads=[("pz", 1), "const"], writes=[c.W("scT", t, 2)])
        for h in range(4):
            po = PS[3 + h // 2][:, (h % 2) * 256:(h % 2 + 1) * 256]
            c.op("pe", lambda e: e.matmul(po, lhsT=scT[t % 2][:, h, :], rhs=vbc[t % 2][:, h * 256:(h + 1) * 256],
                                          start=True, stop=False), reads=[c.R("scT", t, 2), c.R("vbc", t, 2)],
                 writes=[("pz", 3 + h // 2)])
            c.op("pe", lambda e: e.matmul(po, lhsT=qkT[t % 2][:, h, :], rhs=sob[t % 2][:, h * 256:(h + 1) * 256],
                                          start=False, stop=True), reads=[c.R("qkT", t, 2), c.R("sob", t, 2)],
                 writes=[("pz", 3 + h // 2)])
        for h in range(4):
            po = PS[3 + h // 2][:, (h % 2) * 256:(h % 2 + 1) * 256]
            c.op("dve", lambda e: e.bn_stats(out=bnst[:, t, h, :], in_=po), reads=[("pz", 3 + h // 2)],
                 writes=[("bnst", t)])
            c.op("dve", lambda e: e.bn_aggr(out=bnag[:, t, h, :], in_=bnst[:, t, h, :]), reads=[("bnst", t)],
                 writes=[("bnag", t)])
        c.op("dve", lambda e: e.tensor_scalar(out=bnr[:, t, :, 0], in0=bnag[:, t, :, 1], scalar1=GN_EPS, scalar2=None,
                                              op0=ALU.add), reads=[("bnag", t)], writes=[("bnr0", t)])
        rsqrt_cols(bnr[:, t, :, 0], bnr[:, t, :, 1], 4, [("bnr0", t)], [("bnr1", t)])
        c.W("onr", t, 2)
        for h in range(4):
            po = PS[3 + h // 2][:, (h % 2) * 256:(h % 2 + 1) * 256]
            c.op("dve", lambda e: e.tensor_scalar(out=onr[t % 2][:, h * 256:(h + 1) * 256], in0=po,
                                                  scalar1=bnag[:, t, h, 0:1], scalar2=bnr[:, t, h, 1:2],
                                                  op0=ALU.subtract, op1=ALU.mult),
                 reads=[("pz", 3 + h // 2), ("bnag", t), ("bnr1", t)], writes=[("onr", t % 2)])
        c.op("pool", lambda e: e.tensor_tensor(out=gat[t % 2][:], in0=onr[t % 2][:], in1=sg[t % 2][:], op=ALU.mult),
             reads=[("onr", t % 2), c.R("sg", t, 2)], writes=[c.W("gat", t, 2)])

    def C1_s3(t):
        pt = PS[7][:].bitcast(BF16)
        for k in range(8):
            c.op("pe", lambda e: e.transpose(out=pt[:, k * 128:(k + 1) * 128], in_=gat[t % 2][:, k * 128:(k + 1) * 128],
                                             identity=identb[:]), reads=[c.R("gat", t, 2), "const"], writes=["ptC"])
        c.op("act", lambda e: e.activation(out=gT[t % 2][:], in_=pt.rearrange("p (j n) -> p j n", j=8), func=AF.Copy),
             reads=["ptC"], writes=[c.W("gT", t, 2)])
        c.W("bbt", t, 2)
        for hh in range(2):
            for k in range(8):
                c.op("pe", lambda e: e.matmul(PS[5 + hh][:], lhsT=gT[t % 2][:, k, :], rhs=wor[:, k, hh * 512:(hh + 1) * 512],
                                              start=(k == 0), stop=(k == 7)), reads=[c.R("gT", t, 2), "wor"],
                     writes=[("pz", 5 + hh)])
            c.op("act", lambda e: e.activation(out=bbt[t % 2][:, hh * 512:(hh + 1) * 512], in_=PS[5 + hh][:],
                                               func=AF.Copy), reads=[("pz", 5 + hh)], writes=[("bbt", t % 2)])
        c.dma("sp", bb_d[t], bbt[t % 2][:], reads=[("bbt", t % 2)], writes=["bb_d"], key=f"bbw{t % 2}")

    def C1_s123(t):
        C1_s1(t)
        C1_s2(t)
        C1_s3(t)

    pipeline(NO, [C1_s0, C1_s0b, C1_s123])
    print("sbuf remaining in pass C1:", nc.sbuf_bytes_remaining, "ops", c.nops)
    c.pop()
    if stop == "C1":
        return finish()

    h2T = c.sbuf("h2T", [128, 8, NO * 128], BF16)
    print("sbuf remaining before C2:", nc.sbuf_bytes_remaining)
    c.push()
    PS = [c.psum(f"pd_{i}", [128, 512], F32) for i in range(8)]
    WG = c.sbuf("WG", [128, 8, 2048], BF16)
    load_w(WG[:], w_in[:, O_GA:O_GA + 2048].rearrange("(k p) n -> p k n", p=128), "WG", "wD0")
    bwG = fold_adaln(WG, 2048, "WG", PS[1], ("pz", 1), "G")
    wom = c.sbuf("wom", [64, 8, 1024], BF16)
    load_w(wom[:], w_o_mla.rearrange("(h p) n -> p h n", p=64), "wom", "wD1")
    wout = c.sbuf("wout", [128, 8, 1024], BF16)
    load_w(wout[:], w_out.rearrange("(k p) n -> p k n", p=128), "wout", "wD2")
    wr = c.sbuf("wr", [128, 8, 64], F32)
    c.dma("sp", wr[:], w_router.rearrange("(k p) n -> p k n", p=128), writes=["wr"], key="wD3")
    ht = HT(xown, 4, A1, B1, PS[0])
    tg = [c.sbuf("tg", [128, 2048], BF16)] * 2
    ott = [c.sbuf("ott", [64, 8, 128], BF16) for _ in range(2)]
    bbr = [c.sbuf("bbr", [128, 1024], BF16) for _ in range(2)]
    m1 = c.sbuf("m1", [128, 1024], F32)
    m2 = c.sbuf("m2", [128, 1024], F32)
    mg = [c.sbuf("mg", [128, 1024], BF16) for _ in range(2)]
    mgT = [c.sbuf("mgT", [128, 8, 128], BF16) for _ in range(2)]
    ty = m1
    x1 = [c.sbuf("x1", [128, 1024], F32) for _ in range(2)]
    xs2 = [c.sbuf("xs2", [128, 1024], F32) for _ in range(2)]
    h2f = [c.sbuf("h2f", [128, 8, 128], F32) for _ in range(2)]
    st2 = c.sbuf("st2", [128, 3 * 64], F32)
    rt = c.sbuf("rt", [128, 2, 64 * 4 + 64 + 8 * 4], F32)

    def C2_s0(t):
        if t == 0:
            ht.load(0)
            ht.load(1)
        if t + 2 < NO:
            ht.load(t + 2)
        c.dma("sp", bbr[t % 2][:], bb_d[t], reads=["bb_d"], writes=[c.W("bbr", t, 2)], key=f"bbr{t % 2}")
        c.dma("sp", ott[t % 2][:], ot_d[:, :, t * 128:(t + 1) * 128].rearrange("h p n -> p h n"), reads=["ot_d"],
              writes=[c.W("ott", t, 2)], key=f"ott{t % 2}")
        ht.norm(t)

    def C2_s0b(t):
        ht.transpose(t)

    def C2_s1(t):
        hT, hk = ht.get(t)
        xt = ht.xt[t % 4]
        for j in range(4):
            for k in range(8):
                c.op("pe", lambda e: e.matmul(PS[1 + j][:], lhsT=hT[:, k, :], rhs=WG[:, k, j * 512:(j + 1) * 512],
                                              start=(k == 0), stop=False), reads=[hk, "WG"], writes=[("pz", 1 + j)])
            bias_mm(PS[1 + j][:], bwG, j * 512, 512, "G", [("pz", 1 + j)])
            c.op("act", lambda e: e.activation(out=tg[t % 2][:, j * 512:(j + 1) * 512], in_=PS[1 + j][:], func=AF.Tanh,
                                               scale=0.5), reads=[("pz", 1 + j)], writes=["tg"])
        for hh in range(2):
            for h in range(8):
                c.op("pe", lambda e: e.matmul(PS[5 + hh][:], lhsT=ott[t % 2][:, h, :],
                                              rhs=wom[:, h, hh * 512:(hh + 1) * 512], start=(h == 0), stop=(h == 7)),
                     reads=[c.R("ott", t, 2), "wom"], writes=[("pz", 5 + hh)])
            sl = slice(hh * 512, (hh + 1) * 512)
            c.op("dve", lambda e: e.scalar_tensor_tensor(out=m1[:, sl], in0=tg[t % 2][:, sl], scalar=1.0, in1=PS[5 + hh][:],
                                                         op0=ALU.add, op1=ALU.mult),
                 reads=["tg", ("pz", 5 + hh)], writes=[("m1", hh)])
        c.op("dve", lambda e: e.scalar_tensor_tensor(out=m2[:], in0=tg[t % 2][:, 1024:2048], scalar=1.0, in1=bbr[t % 2][:],
                                                     op0=ALU.add, op1=ALU.mult),
             reads=["tg", c.R("bbr", t, 2)], writes=["m2"])
        c.op("pool", lambda e: e.tensor_tensor(out=mg[t % 2][:], in0=m1[:], in1=m2[:], op=ALU.add),
             reads=[("m1", 0), ("m1", 1), "m2"], writes=[c.W("mg", t, 2)])
        pt = PS[7][:].bitcast(BF16)
        for k in range(8):
            c.op("pe", lambda e: e.transpose(out=pt[:, k * 128:(k + 1) * 128], in_=mg[t % 2][:, k * 128:(k + 1) * 128],
                                             identity=identb[:]), reads=[c.R("mg", t, 2), "const"], writes=["ptD"])
        c.op("act", lambda e: e.activation(out=mgT[t % 2][:], in_=pt.rearrange("p (j n) -> p j n", j=8), func=AF.Copy),
             reads=["ptD"], writes=[c.W("mgT", t, 2)])
        c.W("x1", t, 2)
        for hh in range(2):
            sl = slice(hh * 512, (hh + 1) * 512)
            for k in range(8):
                c.op("pe", lambda e: e.matmul(PS[1 + hh][:], lhsT=mgT[t % 2][:, k, :], rhs=wout[:, k, sl],
                                              start=(k == 0), stop=(k == 7)), reads=[c.R("mgT", t, 2), "wout"],
                     writes=[("pz", 1 + hh)])
            c.op("dve", lambda e: e.tensor_tensor(out=ty[:, sl], in0=PS[1 + hh][:], in1=GT1[:, sl], op=ALU.mult),
                 reads=[("pz", 1 + hh), "GT"], writes=[("m1", hh)])
            c.op("pool", lambda e: e.tensor_tensor(out=x1[t % 2][:, sl], in0=ty[:, sl], in1=xt[:, sl], op=ALU.add),
                 reads=[("m1", hh), c.R("xt", t, 4)], writes=[("x1", t % 2)])
        c.dma("sp", x1_d[t], x1[t % 2][:], reads=[("x1", t % 2)], writes=["x1_d"], key=f"x1w{t % 2}")
        m = t
        a, b, r = st2[:, m:m + 1], st2[:, 64 + m:65 + m], st2[:, 128 + m:129 + m]
        c.op("act", lambda e: e.activation(out=junk[:], in_=x1[t % 2][:], func=AF.Square, accum_out=a),
             reads=[("x1", t % 2)], writes=["junk", ("s2a", m)])
        c.op("dve", lambda e: e.tensor_scalar(out=b, in0=a, scalar1=1.0 / D, scalar2=RMS_EPS, op0=ALU.mult, op1=ALU.add),
             reads=[("s2a", m)], writes=[("s2b", m)])
        rsqrt_cols(b, r, 1, [("s2b", m)], [("s2r", m)])
        c.op("dve", lambda e: e.tensor_scalar(out=xs2[t % 2][:], in0=x1[t % 2][:], scalar1=r, scalar2=None, op0=ALU.mult),
             reads=[("x1", t % 2), ("s2r", m)], writes=[c.W("xs2", t, 2)])

    def C2_s2(t):
        c.W("h2f", t, 2)
        for k in range(8):
            pb = PS[3 + k // 4][:, (k % 4) * 128:(k % 4 + 1) * 128]
            c.op("pe", lambda e: e.transpose(out=pb, in_=xs2[t % 2][:, k * 128:(k + 1) * 128], identity=identf[:]),
                 reads=[c.R("xs2", t, 2), "const"], writes=[("pz", 3 + k // 4)])
        for k in range(8):
            pb = PS[3 + k // 4][:, (k % 4) * 128:(k % 4 + 1) * 128]
            c.op("act", lambda e: e.activation(out=h2f[t % 2][:, k, :], in_=pb, func=AF.Identity, scale=A2[:, k:k + 1],
                                               bias=B2[:, k:k + 1]), reads=[("pz", 3 + k // 4), "AB"],
                 writes=[("h2f", t % 2)])
        c.op("dve", lambda e: e.tensor_copy(out=h2T[:, :, t * 128:(t + 1) * 128], in_=h2f[t % 2][:]),
             reads=[("h2f", t % 2)], writes=["h2T"])
        for k in range(8):
            c.op("pe", lambda e: e.matmul(PS[5][:, 0:64], lhsT=h2f[t % 2][:, k, :], rhs=wr[:, k, :], start=(k == 0),
                                          stop=(k == 7)), reads=[("h2f", t % 2), "wr"], writes=[("pz", 5)])
        R_ = rt[:, t % 2, :]
        s_, bi, mb, sel = R_[:, 0:64], R_[:, 64:128], R_[:, 128:192], R_[:, 192:256]
        m8 = R_[:, 256:320]
        gs, g8, gm, gneg = R_[:, 320:328], R_[:, 328:336], R_[:, 336:344], R_[:, 344:352]
        rk_ = ("rt", t % 2)
        c.op("act", lambda e: e.activation(out=s_, in_=PS[5][:, 0:64], func=AF.Tanh, scale=0.5), reads=[("pz", 5)],
             writes=[rk_])
        c.op("dve", lambda e: e.tensor_scalar(out=s_, in0=s_, scalar1=0.5, scalar2=0.5, op0=ALU.mult, op1=ALU.add),
             reads=[rk_], writes=[rk_])
        c.op("dve", lambda e: e.tensor_tensor(out=bi, in0=s_, in1=brout_t[:], op=ALU.add), reads=[rk_, "const"],
             writes=[rk_])
        for g in range(8):
            c.op("dve", lambda e: e.max(out=m8[:, g * 8:(g + 1) * 8], in_=bi[:, g * 8:(g + 1) * 8]), reads=[rk_],
                 writes=[rk_])
        m83 = m8.rearrange("p (g k) -> p g k", g=8)
        c.op("dve", lambda e: e.tensor_tensor(out=gs, in0=m83[:, :, 0], in1=m83[:, :, 1], op=ALU.add), reads=[rk_],
             writes=[rk_])
        c.op("dve", lambda e: e.max(out=g8, in_=gs), reads=[rk_], writes=[rk_])
        c.op("dve", lambda e: e.tensor_scalar(out=gm, in0=gs, scalar1=g8[:, 3:4], scalar2=None, op0=ALU.is_ge),
             reads=[rk_], writes=[rk_])
        c.op("dve", lambda e: e.tensor_scalar(out=gneg, in0=gm, scalar1=-1.0, scalar2=8.0, op0=ALU.add, op1=ALU.mult),
             reads=[rk_], writes=[rk_])
        bi3, mb3 = bi.rearrange("p (g k) -> p g k", g=8), mb.rearrange("p (g k) -> p g k", g=8)
        c.op("dve", lambda e: e.tensor_tensor(out=mb3, in0=bi3, in1=gm.unsqueeze(2).broadcast_to([128, 8, 8]), op=ALU.mult),
             reads=[rk_], writes=[rk_])
        c.op("dve", lambda e: e.tensor_tensor(out=mb3, in0=mb3, in1=gneg.unsqueeze(2).broadcast_to([128, 8, 8]), op=ALU.add),
             reads=[rk_], writes=[rk_])
        c.op("dve", lambda e: e.max(out=g8, in_=mb), reads=[rk_], writes=[rk_])
        c.op("dve", lambda e: e.tensor_scalar(out=sel, in0=mb, scalar1=g8[:, 7:8], scalar2=None, op0=ALU.is_ge),
             reads=[rk_], writes=[rk_])
        c.op("dve", lambda e: e.tensor_tensor(out=sel, in0=sel, in1=s_, op=ALU.mult), reads=[rk_], writes=[rk_])
        c.op("dve", lambda e: e.tensor_reduce(out=gs[:, 0:1], in_=sel, axis=AX.X, op=ALU.add), reads=[rk_], writes=[rk_])
        c.op("dve", lambda e: e.reciprocal(out=gs[:, 1:2], in_=gs[:, 0:1]), reads=[rk_], writes=[rk_])
        c.op("dve", lambda e: e.tensor_scalar(out=comb[:, t, :], in0=sel, scalar1=gs[:, 1:2], scalar2=2.5, op0=ALU.mult,
                                              op1=ALU.mult), reads=[rk_], writes=["comb"])

    def C2_s12(t):
        C2_s1(t)
        C2_s2(t)

    pipeline(NO, [C2_s0, C2_s0b, C2_s12])
    print("sbuf remaining in pass C2:", nc.sbuf_bytes_remaining, "ops", c.nops)
    c.pop()
    dbg("h2T", h2T[:], [128, 8, NO * 128], BF16)
    dbg("comb", comb[:], [128, NO, 64], F32)
    if stop == "C2":
        return finish()

    c.push()
    PG = c.psum("pg", [128, 2048], F32)
    PY = [c.psum(f"py{i}", [128, 1024], F32) for i in range(2)]
    acc = c.sbuf("acc", [128, NO, 1024], F32)
    wgu = [c.sbuf("wgu", [128, 8, 512], BF16) for _ in range(2)]
    wdn = [c.sbuf("wdn", [128, 2, 1024], BF16) for _ in range(2)]
    sgm = [c.sbuf("sgm", [128, 1024], BF16) for _ in range(2)]
    actT = [c.sbuf("actT", [128, 2, 512], BF16) for _ in range(2)]
    def load_expert(ei):
        e_ = ei - 1
        sl = ei % 2
        srcs = (w_sg, w_su, w_sd) if e_ < 0 else (w_eg[e_], w_eu[e_], w_ed[e_])
        c.W("wexp", ei, 2)
        c.dma("pool", wgu[sl][:, :, 0:256], srcs[0].rearrange("(k p) n -> p k n", p=128), writes=[("wexp", sl)], key=f"we{sl}a")
        c.dma("pool", wgu[sl][:, :, 256:512], srcs[1].rearrange("(k p) n -> p k n", p=128), writes=[("wexp", sl)], key=f"we{sl}b")
        c.dma("pool", wdn[sl][:], srcs[2].rearrange("(k p) n -> p k n", p=128), writes=[("wexp", sl)], key=f"we{sl}c")

    units = [(ei, tc_) for ei in range(NEXP + 1) for tc_ in range(4)]
    load_expert(0)

    def moe_s0(u):
        ei, tc_ = units[u]
        sl, a_ = ei % 2, u % 2
        if tc_ == 1 and ei + 1 <= NEXP:
            load_expert(ei + 1)
        wk = c.R("wexp", ei, 2)
        for j in range(4):
            for k in range(8):
                c.op("pe", lambda e: e.matmul(PG[:, j * 512:(j + 1) * 512], lhsT=wgu[sl][:, k, j * 128:(j + 1) * 128],
                                              rhs=h2T[:, k, tc_ * 512:(tc_ + 1) * 512], start=(k == 0), stop=(k == 7)),
                     reads=[wk, "h2T"], writes=[("pg", j // 2)])
        c.op("act", lambda e: e.activation(out=sgm[a_][:], in_=PG[:, 0:1024], func=AF.Silu), reads=[("pg", 0)],
             writes=[("sgm", a_)])
        c.op("dve", lambda e: e.tensor_tensor(out=actT[a_][:].rearrange("p f n -> p (f n)"), in0=sgm[a_][:],
                                              in1=PG[:, 1024:2048], op=ALU.mult), reads=[("sgm", a_), ("pg", 1)],
             writes=[("actT", a_)])

    def moe_s1(u):
        ei, tc_ = units[u]
        e_ = ei - 1
        sl, a_ = ei % 2, u % 2
        wk = ("wexp", sl)
        for tt in range(4):
            i = tc_ * 4 + tt
            y_ = (u * 4 + tt) % 2
            for hh in range(2):
                for fc in range(2):
                    c.op("pe", lambda e: e.matmul(PY[y_][:, hh * 512:(hh + 1) * 512], lhsT=actT[a_][:, fc, tt * 128:(tt + 1) * 128],
                                                  rhs=wdn[sl][:, fc, hh * 512:(hh + 1) * 512], start=(fc == 0), stop=(fc == 1)),
                         reads=[("actT", a_), wk], writes=[("py", y_)])
            if e_ < 0:
                c.op("act", lambda e: e.activation(out=acc[:, i, :], in_=PY[y_][:], func=AF.Copy), reads=[("py", y_)],
                     writes=[("acc", i)])
            else:
                c.op("dve", lambda e: e.scalar_tensor_tensor(out=acc[:, i, :], in0=PY[y_][:], scalar=comb[:, i, e_:e_ + 1],
                                                             in1=acc[:, i, :], op0=ALU.mult, op1=ALU.add),
                     reads=[("py", y_), ("acc", i), "comb"], writes=[("acc", i)])

    for u in range(len(units) + 1):
        if u < len(units):
            moe_s0(u)
        if u >= 1:
            moe_s1(u - 1)
    x1r = [c.sbuf("x1r", [128, 1024], F32) for _ in range(2)]
    xo = [c.sbuf("xo", [128, 1024], F32) for _ in range(2)]
    yo = [c.sbuf("yo", [128, 1024], F32) for _ in range(2)]
    stf = c.sbuf("stf", [128, 3 * 64], F32)
    for t in range(NO):
        c.dma("sp", x1r[t % 2][:], x1_d[t], reads=["x1_d"], writes=[("x1r", t % 2)], key=f"x1r{t % 2}")
        c.op("dve", lambda e: e.tensor_tensor(out=xo[t % 2][:], in0=acc[:, t, :], in1=GT2[:], op=ALU.mult),
             reads=[("acc", t), "GT"], writes=[("xo", t % 2)])
        c.op("pool", lambda e: e.tensor_tensor(out=xo[t % 2][:], in0=xo[t % 2][:], in1=x1r[t % 2][:], op=ALU.add),
             reads=[("xo", t % 2), ("x1r", t % 2)], writes=[("xo", t % 2)])
        a, b, r = stf[:, t:t + 1], stf[:, 64 + t:65 + t], stf[:, 128 + t:129 + t]
        c.op("act", lambda e: e.activation(out=junk[:], in_=xo[t % 2][:], func=AF.Square, accum_out=a),
             reads=[("xo", t % 2)], writes=["junk", ("sfa", t)])
        c.op("dve", lambda e: e.tensor_scalar(out=b, in0=a, scalar1=1.0 / D, scalar2=RMS_EPS, op0=ALU.mult, op1=ALU.add),
             reads=[("sfa", t)], writes=[("sfb", t)])
        rsqrt_cols(b, r, 1, [("sfb", t)], [("sfr", t)])
        c.op("dve", lambda e: e.scalar_tensor_tensor(out=yo[t % 2][:], in0=xo[t % 2][:], scalar=r, in1=gfin_t[:],
                                                     op0=ALU.mult, op1=ALU.mult),
             reads=[("xo", t % 2), ("sfr", t), "const"], writes=[("yo", t % 2)])
        c.dma("sp", out_d[t * 128:(t + 1) * 128, :], yo[t % 2][:], reads=[("yo", t % 2)], writes=["out"], key=f"out{t % 2}")
    c.pop()
    c.close()
    return nc


_NC_CACHE = {}


def _consts(j):
    bf = ml_dtypes.bfloat16
    k = np.arange(128)
    tri = (k[:, None] <= k[None, :]).astype(np.float32)
    amask = np.zeros((128, 4, 128), np.float32)
    for l in range(4):
        if l < j:
            amask[:, l, :] = 1.0
        elif l == j:
            amask[:, l, :] = tri
    inv_mla = 1.0 / (10000.0 ** (np.arange(0, 32, 2, dtype=np.float32) / np.float32(32)))
    inv_ret = 1.0 / (10000.0 ** (np.arange(0, 128, 2, dtype=np.float32) / np.float32(128)))
    invf = np.broadcast_to(np.concatenate([inv_mla, inv_ret]).astype(np.float32)[None, :], (128, 80)).copy()
    g = np.array(GAMMA, np.float64)
    m = np.arange(128, dtype=np.float64)
    kdecA = (g[None, :] ** (127.0 - m[:, None])) * 128.0 ** -0.5
    kdecC = (g[None, :] ** (-m[:, None])) * 128.0 ** -0.5
    qdec = g[None, :] ** m[:, None]
    rep = lambda a: np.repeat(a[:, :, None], 128, axis=2).astype(np.float32)
    G = g ** 128
    rc = np.zeros((128, 16), np.float64)
    for h in range(4):
        rc[:, h * 4 + 0] = g[h] * G[h] ** j
        for l in range(3):
            rc[:, h * 4 + 1 + l] = g[h] * (G[h] ** (j - 1 - l)) if l < j else 0.0
    return dict(identb=np.eye(128).astype(bf), identf=np.eye(128, dtype=np.float32), tri=tri,
                amask=amask.astype(bf), invf=invf, kdecA=rep(kdecA), kdecC=rep(kdecC), qdec=rep(qdec),
                rcoef=rc.astype(np.float32))


def _col(v, nchunk):
    return np.ascontiguousarray(np.asarray(v, np.float32).reshape(nchunk, 128).T)


_BUILD_ARGS = {}


def kernel(x, c, positions, w_ada, b_ada, g_norm1, w_in, g_cq, w_uq, g_ckv, w_ukv, g_ret, w_o_mla, w_o_ret,
           w_out, g_norm2, w_router, b_router, w_exp_gate, w_exp_up, w_exp_down, w_sh_gate, w_sh_up, w_sh_down,
           g_final):
    f = lambda a: np.ascontiguousarray(np.asarray(a, dtype=np.float32))
    x = f(x)
    positions = np.asarray(positions).astype(np.int32)
    if "nc" not in _NC_CACHE:
        _NC_CACHE["nc"] = build(**_BUILD_ARGS)
    nc = _NC_CACHE["nc"]
    shared = dict(
        w_ada=f(w_ada), bada_row=f(b_ada).reshape(1, -1), g1c=_col(g_norm1, 8), g2c=_col(g_norm2, 8), w_in=f(w_in),
        gcq=_col(g_cq, 3), w_uq=f(w_uq), gckv=_col(g_ckv, 2), w_ukv=f(w_ukv), gret=_col(g_ret, 8), w_o_mla=f(w_o_mla),
        w_o_ret=f(w_o_ret), w_out=f(w_out), w_router=f(w_router),
        brout=np.ascontiguousarray(np.broadcast_to(f(b_router)[None, :], (128, 64))),
        w_exp_gate=f(w_exp_gate), w_exp_up=f(w_exp_up), w_exp_down=f(w_exp_down), w_sh_gate=f(w_sh_gate),
        w_sh_up=f(w_sh_up), w_sh_down=f(w_sh_down),
        gfin=np.ascontiguousarray(np.broadcast_to(f(g_final)[None, :], (128, 1024))),
    )
    in_maps = []
    for core in range(8):
        b, j = core // 4, core % 4
        xb = x[b]
        xt = xb.reshape(NT, 128, D)
        pt = positions[b].reshape(NT, 128)
        m = dict(shared)
        m.update(_consts(j))
        m["xall"] = xb
        m["xown"] = np.ascontiguousarray(xt[j::4].reshape(NO * 128, D))
        m["posall"] = np.ascontiguousarray(pt.T)
        m["posown"] = np.ascontiguousarray(pt[j::4].T)
        m["cvec"] = _col(c[b], 8)
        in_maps.append(m)
    res = run_bass_kernel_spmd(nc, in_maps, core_ids=list(range(8)))
    _NC_CACHE["res"] = res
    out = np.empty((2, S, D), np.float32)
    for core in range(8):
        b, j = core // 4, core % 4
        o = np.asarray(res.results[core]["out"]).reshape(NO, 128, D)
        out[b].reshape(NT, 128, D)[j::4] = o
    return out
```

```python
import math
from contextlib import ExitStack

import numpy as np
import ml_dtypes

import concourse.bass as bass
import concourse.mybir as mybir
from concourse.bass_utils import run_bass_kernel_spmd

F32 = mybir.dt.float32
BF16 = mybir.dt.bfloat16
I32 = mybir.dt.int32
AF = mybir.ActivationFunctionType
ALU = mybir.AluOpType
AX = mybir.AxisListType

D = 1024
S = 8192
NT = 64
NO = 16
D_IN = 5792
RMS_EPS = 1e-6
GN_EPS = 1e-5
TWO_PI = 2.0 * math.pi
MAGIC = 12582912.0
C1 = 6.28125
C2 = float(np.float32(TWO_PI - 6.28125))
C3 = float(TWO_PI - 6.28125 - float(np.float32(TWO_PI - 6.28125)))
SCALE_MLA = 96.0 ** -0.5
GAMMA = [1.0 - 2.0 ** (-5.0 - h) for h in range(4)]
NEXP = 64
import os
NOSAME = False

O_CQ, O_CKV, O_KR, O_RQ, O_RK, O_RV, O_RG, O_GA, O_GB = 0, 384, 640, 672, 1184, 1696, 2720, 3744, 4768


class Ctx:
    def __init__(self, nc):
        self.nc = nc
        self.stacks = [ExitStack()]
        self.eng = {"pe": nc.tensor, "act": nc.scalar, "dve": nc.vector, "pool": nc.gpsimd, "sp": nc.sync}
        self.sem, self.cnt = {}, {}
        for e in ("pe", "act", "dve", "pool"):
            self.sem[e] = self.stacks[0].enter_context(nc.semaphore("s_" + e))
            self.cnt[e] = 0
        self.dsem, self.dcnt = {}, {}
        self.waited = {e: {} for e in self.eng}
        self.last_w, self.readers, self.owner = {}, {}, {}
        self.nops = 0
        self.uid = 0

    def sbuf(self, name, shape, dtype):
        self.uid += 1
        return self.stacks[-1].enter_context(self.nc.sbuf_tensor(f"{name}_{self.uid}", list(shape), dtype))

    def psum(self, name, shape, dtype):
        self.uid += 1
        return self.stacks[-1].enter_context(self.nc.psum_tensor(f"{name}_{self.uid}", list(shape), dtype))

    def push(self):
        self.stacks.append(ExitStack())

    def pop(self):
        self.barrier()
        self.stacks.pop().close()

    def W(self, name, t=0, n=1):
        k = (name, t % n)
        self.owner[k] = t
        return k

    def R(self, name, t=0, n=1):
        k = (name, t % n)
        assert self.owner.get(k) == t, f"stale read {name} tile {t} owner {self.owner.get(k)}"
        return k

    PSUM_NAMES = {"pmod", "pc", "pgt", "pT", "pz", "ptA", "pkv", "pcq", "ptQ", "pq", "ptQ2", "pk", "ps", "po", "ptC",
                  "ptD", "pg", "py"}

    def _split(self, reads, writes):
        rd, wr = [], list(writes)
        for r in reads:
            base = r[0] if isinstance(r, tuple) else r
            if base in self.PSUM_NAMES:
                if r not in wr:
                    wr.append(r)
            else:
                rd.append(r)
        return rd, wr

    def _deps(self, reads, writes):
        deps = []
        for r in reads:
            t = self.last_w.get(r)
            if t is not None:
                deps.append(t)
        for w in writes:
            t = self.last_w.get(w)
            if t is not None:
                deps.append(t)
            deps.extend(self.readers.get(w, ()))
        return deps

    def _wait(self, eng, deps):
        best = {}
        for (skey, sem, val) in deps:
            if eng == "pe" and skey == "pe":
                continue
            if NOSAME and skey == eng:
                continue
            if val > best.get(skey, (None, 0))[1]:
                best[skey] = (sem, val)
        w = self.waited[eng]
        for skey, (sem, val) in best.items():
            if w.get(skey, 0) >= val:
                continue
            self.eng[eng].wait_ge(sem, val)
            w[skey] = val

    def _commit(self, ticket, reads, writes):
        for r in reads:
            self.readers.setdefault(r, []).append(ticket)
        for w in writes:
            self.last_w[w] = ticket
            self.readers[w] = []

    def op(self, eng, fn, reads=(), writes=()):
        reads, writes = self._split(reads, writes)
        self._wait(eng, self._deps(reads, writes))
        ins = fn(self.eng[eng])
        self.cnt[eng] += 1
        ins.then_inc(self.sem[eng], 1)
        t = (eng, self.sem[eng], self.cnt[eng])
        self._commit(t, reads, writes)
        self.nops += 1
        return t

    def dma(self, queue, out, in_, reads=(), writes=(), key=None):
        if key not in self.dsem:
            self.dsem[key] = self.stacks[0].enter_context(self.nc.semaphore("d_" + str(key)))
            self.dcnt[key] = 0
        self._wait(queue, self._deps(reads, writes))
        ins = self.eng[queue].dma_start(out=out, in_=in_)
        self.dcnt[key] += 16
        ins.then_inc(self.dsem[key], 16)
        t = ("d_" + str(key), self.dsem[key], self.dcnt[key])
        self._commit(t, reads, writes)
        return t

    def barrier(self):
        tickets = [(e, self.sem[e], self.cnt[e]) for e in self.sem if self.cnt[e] > 0]
        tickets += [("d_" + str(k), self.dsem[k], self.dcnt[k]) for k in self.dsem]
        for e in self.eng:
            self._wait(e, tickets)
        self.last_w, self.readers = {}, {}

    def close(self):
        self.barrier()
        while self.stacks:
            self.stacks.pop().close()


def pipeline(n, stages):
    ns = len(stages)
    for s in range(n + ns - 1):
        for k in range(ns - 1, -1, -1):
            t = s - k
            if 0 <= t < n:
                stages[k](t)


def build(stop=None, debug=False):
    nc = bass.Bass("TRN2", target_bir_lowering=False)
    dbg_out = {}

    def dbg(name, ap, shape, dt):
        if not debug:
            return
        d_ = nc.dram_tensor("dbg_" + name, list(shape), dt, kind="ExternalOutput").ap()
        dbg_out[name] = d_
        c.barrier()
        c.dma("sp", d_, ap, key="dbg_" + name)
        c.barrier()

    def finish():
        c.close()
        return nc

    def din(name, shape, dt=F32):
        return nc.dram_tensor(name, list(shape), dt, kind="ExternalInput").ap()

    xall = din("xall", [S, D])
    xown = din("xown", [NO * 128, D])
    posall = din("posall", [128, NT], I32)
    posown = din("posown", [128, NO], I32)
    cvec = din("cvec", [128, 8])
    w_ada = din("w_ada", [D, 6 * D])
    bada_row = din("bada_row", [1, 6 * D])
    g1c = din("g1c", [128, 8])
    g2c = din("g2c", [128, 8])
    w_in = din("w_in", [D, D_IN])
    gcq = din("gcq", [128, 3])
    w_uq = din("w_uq", [384, 768])
    gckv = din("gckv", [128, 2])
    w_ukv = din("w_ukv", [256, 1024])
    gret = din("gret", [128, 8])
    w_o_mla = din("w_o_mla", [512, 1024])
    w_o_ret = din("w_o_ret", [1024, 1024])
    w_out = din("w_out", [1024, 1024])
    w_router = din("w_router", [1024, 64])
    brout = din("brout", [128, 64])
    w_eg = din("w_exp_gate", [NEXP, 1024, 256])
    w_eu = din("w_exp_up", [NEXP, 1024, 256])
    w_ed = din("w_exp_down", [NEXP, 256, 1024])
    w_sg = din("w_sh_gate", [1024, 256])
    w_su = din("w_sh_up", [1024, 256])
    w_sd = din("w_sh_down", [256, 1024])
    gfin = din("gfin", [128, 1024])
    identb_d = din("identb", [128, 128], BF16)
    identf_d = din("identf", [128, 128])
    tri_d = din("tri", [128, 128])
    amask_d = din("amask", [128, 4, 128], BF16)
    invf_d = din("invf", [128, 80])
    kdecA_d = din("kdecA", [128, 4, 128])
    kdecC_d = din("kdecC", [128, 4, 128])
    qdec_d = din("qdec", [128, 4, 128])
    rcoef_d = din("rcoef", [128, 16])
    out_d = nc.dram_tensor("out", [NO * 128, D], F32, kind="ExternalOutput").ap()
    sown_d = nc.dram_tensor("sown_s", [NO, 128, 1024], BF16, kind="ExternalOutput").ap()
    bb_d = nc.dram_tensor("bb_s", [NO, 128, 1024], BF16, kind="ExternalOutput").ap()
    x1_d = nc.dram_tensor("x1_s", [NO, 128, 1024], F32, kind="ExternalOutput").ap()
    ot_d = nc.dram_tensor("ot_s", [8, 64, NO * 128], BF16, kind="ExternalOutput").ap()

    c = Ctx(nc)

    identb = c.sbuf("identb", [128, 128], BF16)
    identf = c.sbuf("identf", [128, 128], F32)
    tri = c.sbuf("tri", [128, 128], F32)
    amask = c.sbuf("amask", [128, 4, 128], BF16)
    invf = c.sbuf("invf", [128, 80], F32)
    rcoef = c.sbuf("rcoef", [128, 16], F32)
    nhalf = c.sbuf("nhalf", [128, 64], F32)
    onesf = c.sbuf("onesf", [128, 128], F32)
    AB = c.sbuf("AB", [128, 32], F32)
    GT1 = c.sbuf("GT1", [128, 1024], F32)
    GT2 = c.sbuf("GT2", [128, 1024], F32)
    gfin_t = c.sbuf("gfin_t", [128, 1024], F32)
    brout_t = c.sbuf("brout_t", [128, 64], F32)
    posf = c.sbuf("posf", [128, NT + NO], F32)
    junk = c.sbuf("junk", [128, 1024], BF16)
    comb = c.sbuf("comb", [128, NO, 64], F32)
    B1b = c.sbuf("B1b", [128, 8], BF16)
    onesb = c.sbuf("onesb", [1, 128], BF16)

    for (t_, d_, k_) in [(identb, identb_d, "c0"), (identf, identf_d, "c1"), (tri, tri_d, "c2"),
                         (amask, amask_d, "c3"), (invf, invf_d, "c4"), (rcoef, rcoef_d, "c5"),
                         (gfin_t, gfin, "c6"), (brout_t, brout, "c7")]:
        c.dma("sp", t_[:], d_, writes=["const"], key=k_)
    c.op("pool", lambda e: e.memset(nhalf[:], -0.5), writes=["const"])
    c.op("pool", lambda e: e.memset(onesf[:], 1.0), writes=["const"])
    c.barrier()
    if stop == "c":
        return finish()

    def rsqrt_cols(src_ap, dst_ap, ncols, rd, wr):
        c.op("pool", lambda e: e.tensor_tensor(out=dst_ap, in0=src_ap, in1=nhalf[:, 0:ncols], op=ALU.pow),
             reads=rd, writes=wr)

    c.push()
    PS = [c.psum(f"p0_{i}", [128, 512], F32) for i in range(8)]
    cv = c.sbuf("cv", [128, 8], F32)
    cs = c.sbuf("cs", [128, 8], F32)
    modrow = c.sbuf("modrow", [1, 6 * D], F32)
    brow = c.sbuf("brow", [1, 6 * D], F32)
    gcols = c.sbuf("gcols", [128, 16], F32)
    wab = [c.sbuf(f"wab{i}", [128, 8, 512], F32) for i in range(2)]
    posi = c.sbuf("posi", [128, NT + NO], I32)
    c.dma("sp", cv[:], cvec, writes=["cv"], key="p0a")
    c.dma("sp", brow[:], bada_row, writes=["brow"], key="p0b")
    c.dma("sp", gcols[:, 0:8], g1c, writes=["gcols"], key="p0c")
    c.dma("sp", gcols[:, 8:16], g2c, writes=["gcols"], key="p0d")
    c.dma("sp", posi[:, 0:NT], posall, writes=["posi"], key="p0e")
    c.dma("sp", posi[:, NT:NT + NO], posown, writes=["posi"], key="p0f")
    c.op("dve", lambda e: e.tensor_copy(out=posf[:], in_=posi[:]), reads=["posi"], writes=["posf"])
    c.op("act", lambda e: e.activation(out=cs[:], in_=cv[:], func=AF.Silu), reads=["cv"], writes=["cs"])
    for n in range(12):
        wt = wab[n % 2]
        c.dma("sp", wt[:], w_ada[:, n * 512:(n + 1) * 512].rearrange("(k p) n -> p k n", p=128),
              writes=[c.W("wab", n, 2)], key=f"wab{n % 2}")
        for k in range(8):
            c.op("pe", lambda e: e.matmul(PS[n % 2][0:1, :], lhsT=cs[:, k:k + 1], rhs=wt[:, k, :],
                                          start=(k == 0), stop=(k == 7)),
                 reads=[c.R("wab", n, 2), "cs"], writes=[("pmod", n % 2)])
        c.op("dve", lambda e: e.tensor_tensor(out=modrow[0:1, n * 512:(n + 1) * 512], in0=PS[n % 2][0:1, :],
                                              in1=brow[0:1, n * 512:(n + 1) * 512], op=ALU.add),
             reads=[("pmod", n % 2), "brow"], writes=["modrow"])
    if stop == "0a":
        dbg("modrow", modrow[:], [1, 6 * D], F32)
        c.pop()
        return finish()
    pc = PS[2]
    for idx, ch in enumerate(list(range(0, 16)) + list(range(24, 40))):
        c.op("pe", lambda e: e.matmul(pc[:, idx:idx + 1], lhsT=modrow[0:1, ch * 128:(ch + 1) * 128],
                                      rhs=onesf[0:1, 0:1], start=True, stop=True),
             reads=["modrow", "const"], writes=["pc"])
    c.op("dve", lambda e: e.scalar_tensor_tensor(out=AB[:, 0:8], in0=pc[:, 8:16], scalar=1.0, in1=gcols[:, 0:8],
                                                 op0=ALU.add, op1=ALU.mult), reads=["pc", "gcols"], writes=["AB"])
    c.op("dve", lambda e: e.tensor_copy(out=AB[:, 8:16], in_=pc[:, 0:8]), reads=["pc"], writes=["AB"])
    c.op("dve", lambda e: e.scalar_tensor_tensor(out=AB[:, 16:24], in0=pc[:, 24:32], scalar=1.0, in1=gcols[:, 8:16],
                                                 op0=ALU.add, op1=ALU.mult), reads=["pc", "gcols"], writes=["AB"])
    c.op("dve", lambda e: e.tensor_copy(out=AB[:, 24:32], in_=pc[:, 16:24]), reads=["pc"], writes=["AB"])
    if stop == "0b":
        c.pop()
        dbg("AB", AB[:], [128, 32], F32)
        return finish()
    for (dst, base, scl, pi) in [(GT1, 2048, 0.5, 4), (GT2, 5120, 1.0, 6)]:
        for hh in range(2):
            c.op("pe", lambda e: e.matmul(PS[pi + hh][:], lhsT=onesf[0:1, 0:128],
                                          rhs=modrow[0:1, base + hh * 512: base + (hh + 1) * 512],
                                          start=True, stop=True), reads=["modrow", "const"], writes=[("pgt", pi + hh)])
            c.op("act", lambda e: e.activation(out=dst[:, hh * 512:(hh + 1) * 512], in_=PS[pi + hh][:],
                                               func=AF.Copy, scale=scl), reads=[("pgt", pi + hh)], writes=["GT"])
    c.pop()
    dbg("AB", AB[:], [128, 32], F32)
    dbg("GT1", GT1[:], [128, 1024], F32)
    if stop == "0":
        return finish()

    A1, B1, A2, B2 = AB[:, 0:8], AB[:, 8:16], AB[:, 16:24], AB[:, 24:32]
    c.op("dve", lambda e: e.tensor_copy(out=B1b[:], in_=B1), reads=["AB"], writes=["B1b"])
    c.op("pool", lambda e: e.memset(onesb[:], 1.0), writes=["onesb"])

    def fold_adaln(W, ncols, wname, psb, pkey, tag):
        brow_ = c.sbuf("bw_" + tag, [1, ncols], BF16)
        for lo in range(0, ncols, 512):
            n = min(512, ncols - lo)
            for k in range(8):
                c.op("pe", lambda e: e.matmul(psb[0:1, 0:n], lhsT=B1b[:, k:k + 1], rhs=W[:, k, lo:lo + n], start=(k == 0),
                                              stop=(k == 7)), reads=[wname, "B1b"], writes=[pkey])
            c.op("act", lambda e: e.activation(out=brow_[0:1, lo:lo + n], in_=psb[0:1, 0:n], func=AF.Copy),
                 reads=[pkey], writes=["bw_" + tag])
        for k in range(8):
            c.op("dve", lambda e: e.tensor_scalar(out=W[:, k, :], in0=W[:, k, :], scalar1=A1[:, k:k + 1], scalar2=None,
                                                  op0=ALU.mult), reads=[wname, "AB"], writes=[wname])
        return brow_

    def bias_mm(ps_ap, brow_, lo, n, tag, wr):
        c.op("pe", lambda e: e.matmul(ps_ap, lhsT=onesb[0:1, 0:128], rhs=brow_[0:1, lo:lo + n], start=False, stop=True),
             reads=["onesb", "bw_" + tag], writes=wr)

    def load_w(dst_tile, src_ap, name, key):
        c.dma("pool", dst_tile, src_ap, writes=[name], key=key)

    def rope_tables(pos_ap, G, sinT, cosT, tmp, lo, hi, tag):
        n = hi - lo
        ang, u, r = (tmp[0][:, 0:G, 0:n], tmp[1][:, 0:G, 0:n], tmp[2][:, 0:G, 0:n])
        c.op("dve", lambda e: e.tensor_tensor(out=ang, in0=invf[:, lo:hi].unsqueeze(1).broadcast_to([128, G, n]),
                                              in1=pos_ap.unsqueeze(2).broadcast_to([128, G, n]), op=ALU.mult),
             reads=["posf", "const"], writes=[tag + "t0"])
        c.op("dve", lambda e: e.tensor_scalar(out=u, in0=ang, scalar1=1.0 / TWO_PI, scalar2=MAGIC,
                                              op0=ALU.mult, op1=ALU.add), reads=[tag + "t0"], writes=[tag + "t1"])
        c.op("dve", lambda e: e.tensor_scalar(out=u, in0=u, scalar1=-MAGIC, scalar2=None, op0=ALU.add),
             reads=[tag + "t1"], writes=[tag + "t1"])
        c.op("dve", lambda e: e.scalar_tensor_tensor(out=r, in0=u, scalar=-C1, in1=ang, op0=ALU.mult, op1=ALU.add),
             reads=[tag + "t1", tag + "t0"], writes=[tag + "t2"])
        c.op("dve", lambda e: e.scalar_tensor_tensor(out=r, in0=u, scalar=-C2, in1=r, op0=ALU.mult, op1=ALU.add),
             reads=[tag + "t1", tag + "t2"], writes=[tag + "t2"])
        c.op("dve", lambda e: e.scalar_tensor_tensor(out=r, in0=u, scalar=-C3, in1=r, op0=ALU.mult, op1=ALU.add),
             reads=[tag + "t1", tag + "t2"], writes=[tag + "t2"])
        c.op("dve", lambda e: e.tensor_scalar(out=r, in0=r, scalar1=-math.pi, scalar2=math.pi, op0=ALU.max, op1=ALU.min),
             reads=[tag + "t2"], writes=[tag + "t2"])
        c.op("act", lambda e: e.activation(out=sinT[:, 0:G, lo:hi], in_=r, func=AF.Sin), reads=[tag + "t2"],
             writes=[tag + "sin"])
        c.op("dve", lambda e: e.scalar_tensor_tensor(out=ang, in0=r, scalar=-1.0, in1=r, op0=ALU.mult, op1=ALU.max),
             reads=[tag + "t2"], writes=[tag + "t0"])
        c.op("dve", lambda e: e.tensor_scalar(out=ang, in0=ang, scalar1=-1.0, scalar2=math.pi / 2, op0=ALU.mult,
                                              op1=ALU.add), reads=[tag + "t0"], writes=[tag + "t0"])
        c.op("act", lambda e: e.activation(out=cosT[:, 0:G, lo:hi], in_=ang, func=AF.Sin), reads=[tag + "t0"],
             writes=[tag + "cos"])

    def rope(out4, z4, cos_b, sin_b, t1, t2, eng2, rd, wr, tmpname):
        c.op("dve", lambda e: e.tensor_tensor(out=t1, in0=z4, in1=cos_b, op=ALU.mult), reads=rd, writes=[tmpname + "1"])
        c.op("dve", lambda e: e.tensor_tensor(out=t2, in0=z4, in1=sin_b, op=ALU.mult), reads=rd, writes=[tmpname + "2"])
        c.op(eng2, lambda e: e.tensor_tensor(out=out4[:, :, 0, :], in0=t1[:, :, 0, :], in1=t2[:, :, 1, :], op=ALU.subtract),
             reads=[tmpname + "1", tmpname + "2"], writes=wr)
        c.op(eng2, lambda e: e.tensor_tensor(out=out4[:, :, 1, :], in0=t1[:, :, 1, :], in1=t2[:, :, 0, :], op=ALU.add),
             reads=[tmpname + "1", tmpname + "2"], writes=wr)

    class HT:
        def __init__(self, src, nb, A, B, pT):
            self.src, self.nb, self.A, self.B, self.pT = src, nb, A, B, pT
            self.xt = [c.sbuf("xt", [128, 1024], F32) for _ in range(nb)]
            self.xs = [c.sbuf("xs", [128, 1024], BF16) for _ in range(2)]
            self.hT = [c.sbuf("hT", [128, 8, 128], BF16) for _ in range(2)]
            self.st = c.sbuf("hst", [128, 3 * 64], F32)

        def load(self, t):
            c.dma("sp", self.xt[t % self.nb][:], self.src[t * 128:(t + 1) * 128, :],
                  writes=[c.W("xt", t, self.nb)], key=f"xt{t % self.nb}")

        def norm(self, t):
            xt, st = self.xt[t % self.nb], self.st
            a, b, r = st[:, t % 64:t % 64 + 1], st[:, 64 + t % 64:65 + t % 64], st[:, 128 + t % 64:129 + t % 64]
            c.op("act", lambda e: e.activation(out=junk[:], in_=xt[:], func=AF.Square, accum_out=a),
                 reads=[c.R("xt", t, self.nb)], writes=["junk", ("hsa", t % 64)])
            c.op("dve", lambda e: e.tensor_scalar(out=b, in0=a, scalar1=1.0 / D, scalar2=RMS_EPS, op0=ALU.mult,
                                                  op1=ALU.add), reads=[("hsa", t % 64)], writes=[("hsb", t % 64)])
            rsqrt_cols(b, r, 1, [("hsb", t % 64)], [("hsr", t % 64)])
            c.op("dve", lambda e: e.tensor_scalar(out=self.xs[t % 2][:], in0=xt[:], scalar1=r, scalar2=None,
                                                  op0=ALU.mult), reads=[c.R("xt", t, self.nb), ("hsr", t % 64)],
                 writes=[c.W("xs", t, 2)])

        def transpose(self, t):
            xs, hT, pT = self.xs[t % 2], self.hT[t % 2], self.pT
            pTb = pT[:].bitcast(BF16)
            for k in range(8):
                c.op("pe", lambda e: e.transpose(out=pTb[:, k * 128:(k + 1) * 128], in_=xs[:, k * 128:(k + 1) * 128],
                                                 identity=identb[:]), reads=[c.R("xs", t, 2), "const"], writes=["pT"])
            c.W("hT", t, 2)
            c.op("act", lambda e: e.activation(out=hT[:].rearrange("p k n -> p (k n)"), in_=pTb[:, :], func=AF.Copy),
                 reads=["pT"], writes=[("hT", t % 2)])

        def get(self, t):
            return self.hT[t % 2], c.R("hT", t, 2)

    c.push()
    ckvnT = c.sbuf("ckvnT", [128, 2, S], BF16)
    KT = [c.sbuf(f"KT{i}", [128, S], BF16) for i in range(2)]

    c.push()
    PS = [c.psum(f"pa_{i}", [128, 512], F32) for i in range(8)]
    WA = c.sbuf("WA", [128, 8, 1824], BF16)
    load_w(WA[:, :, 0:288], w_in[:, O_CKV:O_CKV + 288].rearrange("(k p) n -> p k n", p=128), "WA", "wA0")
    load_w(WA[:, :, 288:800], w_in[:, O_RK:O_RK + 512].rearrange("(k p) n -> p k n", p=128), "WA", "wA1")
    load_w(WA[:, :, 800:1824], w_in[:, O_RV:O_RV + 1024].rearrange("(k p) n -> p k n", p=128), "WA", "wA2")
    bwA = fold_adaln(WA, 1824, "WA", PS[1], ("pz", 1), "A")
    kdecA = c.sbuf("kdecA", [128, 4, 128], F32)
    c.dma("sp", kdecA[:], kdecA_d, writes=["kdecA"], key="kdA")
    ht = HT(xall, 3, A1, B1, PS[0])
    sinT = [c.sbuf("sinT", [128, 4, 80], F32) for _ in range(2)]
    cosT = [c.sbuf("cosT", [128, 4, 80], F32) for _ in range(2)]
    rtmp = [c.sbuf("rtmp", [128, 4, 80], F32) for _ in range(3)]
    stA = c.sbuf("stA", [128, 3 * 64], F32)
    ckvs = [c.sbuf("ckvs", [128, 256], BF16) for _ in range(2)]
    kst = [c.sbuf("kst", [128, 96], BF16) for _ in range(2)]
    kr1 = c.sbuf("kr1", [128, 32], F32)
    kr2 = c.sbuf("kr2", [128, 32], F32)
    rk1 = c.sbuf("rk1", [128, 512], F32)
    rk2 = c.sbuf("rk2", [128, 512], F32)
    rk3 = c.sbuf("rk3", [128, 512], F32)
    kp = [c.sbuf("kp", [128, 4, 128], BF16) for _ in range(2)]
    vb = [c.sbuf("vb", [128, 1024], BF16) for _ in range(2)]
    Sst = c.sbuf("Sst", [128, 1024], F32)
    Sown = [c.sbuf("Sown", [128, 1024], F32) for _ in range(2)]
    Sownb = [c.sbuf("Sownb", [128, 1024], BF16) for _ in range(2)]
    for i in range(2):
        c.op("pool", lambda e: e.memset(kst[i][:], 0.0), writes=[("kst", i)])
    c.op("pool", lambda e: e.memset(Sst[:], 0.0), writes=["Sst"])

    def A_s0(t):
        if t == 0:
            ht.load(0)
            ht.load(1)
        if t + 2 < NT:
            ht.load(t + 2)
        if t % 4 == 0:
            g = t // 4
            rope_tables(posf[:, t:t + 4], 4, sinT[g % 2], cosT[g % 2], rtmp, 0, 80, f"rtA{g % 2}")
            c.W("tabA", g, 2)
        ht.norm(t)

    def A_s0b(t):
        ht.transpose(t)

    def A_s1(t):
        hT, hk = ht.get(t)
        for (pi, lo, n) in [(1, 0, 288), (2, 288, 512), (3, 800, 512), (4, 1312, 512)]:
            for k in range(8):
                c.op("pe", lambda e: e.matmul(PS[pi][:, 0:n], lhsT=hT[:, k, :], rhs=WA[:, k, lo:lo + n],
                                              start=(k == 0), stop=False), reads=[hk, "WA"], writes=[("pz", pi)])
            bias_mm(PS[pi][:, 0:n], bwA, lo, n, "A", [("pz", pi)])
        m = t % 64
        a, b, r = stA[:, m:m + 1], stA[:, 64 + m:65 + m], stA[:, 128 + m:129 + m]
        c.op("act", lambda e: e.activation(out=junk[:, 0:256], in_=PS[1][:, 0:256], func=AF.Square, accum_out=a),
             reads=[("pz", 1)], writes=["junk", ("sAa", m)])
        c.op("dve", lambda e: e.tensor_scalar(out=b, in0=a, scalar1=1.0 / 256, scalar2=RMS_EPS, op0=ALU.mult,
                                              op1=ALU.add), reads=[("sAa", m)], writes=[("sAb", m)])
        rsqrt_cols(b, r, 1, [("sAb", m)], [("sAr", m)])
        c.op("dve", lambda e: e.tensor_scalar(out=ckvs[t % 2][:], in0=PS[1][:, 0:256], scalar1=r, scalar2=None,
                                              op0=ALU.mult), reads=[("pz", 1), ("sAr", m)], writes=[c.W("ckvs", t, 2)])
        g = t // 4
        tk = c.R("tabA", g, 2)
        sn, cs_ = sinT[g % 2], cosT[g % 2]
        z4 = PS[1][:, 256:288].rearrange("p (h t d) -> p h t d", h=1, t=2)
        cb = cs_[:, t % 4, 0:16].unsqueeze(1).unsqueeze(1).broadcast_to([128, 1, 2, 16])
        sb = sn[:, t % 4, 0:16].unsqueeze(1).unsqueeze(1).broadcast_to([128, 1, 2, 16])
        o4 = kst[t % 2][:, 64:96].rearrange("p (h t d) -> p h t d", h=1, t=2)
        rope(o4, z4, cb, sb, kr1[:].rearrange("p (h t d) -> p h t d", h=1, t=2),
             kr2[:].rearrange("p (h t d) -> p h t d", h=1, t=2), "pool",
             [("pz", 1), f"rtA{g % 2}sin", f"rtA{g % 2}cos"], [c.W("kst", t, 2)], "kr")
        z4 = PS[2][:].rearrange("p (h t d) -> p h t d", h=4, t=2)
        cb = cs_[:, t % 4, 16:80].unsqueeze(1).unsqueeze(1).broadcast_to([128, 4, 2, 64])
        sb = sn[:, t % 4, 16:80].unsqueeze(1).unsqueeze(1).broadcast_to([128, 4, 2, 64])
        o4 = rk3[:].rearrange("p (h t d) -> p h t d", h=4, t=2)
        rope(o4, z4, cb, sb, rk1[:].rearrange("p (h t d) -> p h t d", h=4, t=2),
             rk2[:].rearrange("p (h t d) -> p h t d", h=4, t=2), "pool",
             [("pz", 2), f"rtA{g % 2}sin", f"rtA{g % 2}cos"], ["rk3"], "rk")
        c.op("pool", lambda e: e.tensor_tensor(out=kp[t % 2][:], in0=rk3[:].rearrange("p (h d) -> p h d", h=4),
                                               in1=kdecA[:], op=ALU.mult), reads=["rk3", "kdecA"],
             writes=[c.W("kp", t, 2)])
        c.W("vb", t, 2)
        for hh in range(2):
            c.op("act", lambda e: e.activation(out=vb[t % 2][:, hh * 512:(hh + 1) * 512], in_=PS[3 + hh][:],
                                               func=AF.Copy), reads=[("pz", 3 + hh)], writes=[("vb", t % 2)])

    import os
    _lv = 9

    def A_s2(t):
        pt = PS[5][:].bitcast(BF16)
        for k in range(2):
            c.op("pe", lambda e: e.transpose(out=pt[:, k * 128:(k + 1) * 128], in_=ckvs[t % 2][:, k * 128:(k + 1) * 128],
                                             identity=identb[:]), reads=[c.R("ckvs", t, 2), "const"], writes=["ptA"])
        c.op("pe", lambda e: e.transpose(out=pt[0:96, 256:384], in_=kst[t % 2][:], identity=identb[:]),
             reads=[c.R("kst", t, 2), "const"], writes=["ptA"])
        if _lv < 2:
            return
        for k in range(2):
            c.op("act", lambda e: e.activation(out=ckvnT[:, k, t * 128:(t + 1) * 128],
                                               in_=pt[:, k * 128:(k + 1) * 128], func=AF.Copy),
                 reads=["ptA"], writes=["ckvnT"])
        if _lv < 3:
            return
        _kt = "both"
        if _kt in ("both", "act"):
            c.op("act", lambda e: e.activation(out=KT[0][64:96, t * 128:(t + 1) * 128], in_=pt[64:96, 256:384],
                                               func=AF.Copy), reads=["ptA"], writes=["KT0r"])
        if _kt in ("both", "dve"):
            c.op("act", lambda e: e.activation(out=KT[1][64:96, t * 128:(t + 1) * 128], in_=pt[64:96, 256:384],
                                               func=AF.Copy), reads=["ptA"], writes=["KT1r"])
        if _lv < 4:
            return
        pkv = [PS[6], PS[7]]
        for h in range(4):
            c.op("pe", lambda e: e.matmul(pkv[h // 2][:, (h % 2) * 256:(h % 2 + 1) * 256], lhsT=kp[t % 2][:, h, :],
                                          rhs=vb[t % 2][:, h * 256:(h + 1) * 256], start=True, stop=True),
                 reads=[c.R("kp", t, 2), c.R("vb", t, 2)], writes=["pkv"])
        if _lv < 5:
            return
        l, g = t % 4, t // 4
        so = Sown[g % 2]
        if l == 0:
            c.W("Sown", g, 2)
        for h in range(4):
            hs = slice(h * 256, (h + 1) * 256)
            pk = pkv[h // 2][:, (h % 2) * 256:(h % 2 + 1) * 256]
            if l == 0:
                c.op("act", lambda e: e.activation(out=so[:, hs], in_=Sst[:, hs], func=AF.Copy,
                                                   scale=rcoef[:, h * 4:h * 4 + 1]), reads=["Sst", "const"],
                     writes=[("Sown", g % 2)])
            if l < 3:
                c.op("dve", lambda e: e.scalar_tensor_tensor(out=so[:, hs], in0=pk, scalar=rcoef[:, h * 4 + 1 + l:h * 4 + 2 + l],
                                                             in1=so[:, hs], op0=ALU.mult, op1=ALU.add),
                     reads=["pkv", ("Sown", g % 2), "const"], writes=[("Sown", g % 2)])
            c.op("dve", lambda e: e.scalar_tensor_tensor(out=Sst[:, hs], in0=Sst[:, hs], scalar=GAMMA[h] ** 128, in1=pk,
                                                         op0=ALU.mult, op1=ALU.add), reads=["pkv", "Sst"], writes=["Sst"])
        if l == 3 and _lv >= 6:
            c.op("act", lambda e: e.activation(out=Sownb[g % 2][:], in_=so[:], func=AF.Copy),
                 reads=[c.R("Sown", g, 2)], writes=[c.W("Sownb", g, 2)])
            c.dma("sp", sown_d[g], Sownb[g % 2][:], reads=[c.R("Sownb", g, 2)], writes=["sown_d"], key=f"so{g % 2}")

    import os
    _na = NT
    _ns = 3
    pipeline(_na, [A_s0, A_s0b, A_s1, A_s2][:_ns + 1])
    print("sbuf remaining in pass A:", nc.sbuf_bytes_remaining, "ops", c.nops)
    dbg("Sst", Sst[:], [128, 1024], F32)
    c.pop()
    dbg("ckvnT", ckvnT[:], [128, 2, S], BF16)
    dbg("KT0", KT[0][:], [128, S], BF16)
    if stop == "A":
        return finish()

    QT = c.sbuf("QT", [128, 8, NO * 128], BF16)
    c.push()
    PS = [c.psum(f"pq_{i}", [128, 512], F32) for i in range(8)]
    WQ = c.sbuf("WQ", [128, 8, 384], BF16)
    load_w(WQ[:], w_in[:, O_CQ:O_CQ + 384].rearrange("(k p) n -> p k n", p=128), "WQ", "wQ0")
    bwQ = fold_adaln(WQ, 384, "WQ", PS[1], "pcq", "Q")
    wuq = c.sbuf("wuq", [128, 3, 768], BF16)
    c.push()
    wuq_f = c.sbuf("wuq_f", [128, 3, 768], F32)
    gq = c.sbuf("gq", [128, 3], F32)
    c.dma("sp", wuq_f[:], w_uq.rearrange("(k p) n -> p k n", p=128), writes=["wuq_f"], key="wQ1")
    c.dma("sp", gq[:], gcq, writes=["gq"], key="wQ2")
    for k in range(3):
        wv = wuq_f[:, k, :].rearrange("p (h d) -> p h d", h=8)
        c.op("dve", lambda e: e.tensor_scalar(out=wuq[:, k, 0:512].rearrange("p (h d) -> p h d", h=8), in0=wv[:, :, 0:64],
                                              scalar1=gq[:, k:k + 1], scalar2=None, op0=ALU.mult),
             reads=["wuq_f", "gq"], writes=["wuq"])
        c.op("dve", lambda e: e.tensor_scalar(out=wuq[:, k, 512:768].rearrange("p (h d) -> p h d", h=8), in0=wv[:, :, 64:96],
                                              scalar1=gq[:, k:k + 1], scalar2=None, op0=ALU.mult),
             reads=["wuq_f", "gq"], writes=["wuq"])
    c.pop()
    ht = HT(xown, 3, A1, B1, PS[0])
    sinQ = c.sbuf("sinQ", [128, NO, 16], F32)
    cosQ = c.sbuf("cosQ", [128, NO, 16], F32)
    c.push()
    rtmpQ = [c.sbuf("rtmpQ", [128, NO, 16], F32) for _ in range(3)]
    rope_tables(posf[:, NT:NT + NO], NO, sinQ, cosQ, rtmpQ, 0, 16, "rtQ")
    c.pop()
    stQ = c.sbuf("stQ", [128, 3 * 64], F32)
    cqs = [c.sbuf("cqs", [128, 384], BF16) for _ in range(2)]
    cqnT = [c.sbuf("cqnT", [128, 3, 128], BF16) for _ in range(2)]
    qsb = [c.sbuf("qsb", [128, 8, 96], BF16) for _ in range(2)]
    qr1 = c.sbuf("qr1", [128, 8, 32], F32)
    qr2 = c.sbuf("qr2", [128, 8, 32], F32)

    def Q_s0(t):
        if t == 0:
            ht.load(0)
            ht.load(1)
        if t + 2 < NO:
            ht.load(t + 2)
        ht.norm(t)

    def Q_s0b(t):
        ht.transpose(t)

    def Q_s1(t):
        hT, hk = ht.get(t)
        for k in range(8):
            c.op("pe", lambda e: e.matmul(PS[1][:, 0:384], lhsT=hT[:, k, :], rhs=WQ[:, k, :], start=(k == 0),
                                          stop=False), reads=[hk, "WQ"], writes=["pcq"])
        bias_mm(PS[1][:, 0:384], bwQ, 0, 384, "Q", ["pcq"])
        m = t
        a, b, r = stQ[:, m:m + 1], stQ[:, 64 + m:65 + m], stQ[:, 128 + m:129 + m]
        c.op("act", lambda e: e.activation(out=junk[:, 0:384], in_=PS[1][:, 0:384], func=AF.Square, accum_out=a),
             reads=["pcq"], writes=["junk", ("sQa", m)])
        c.op("dve", lambda e: e.tensor_scalar(out=b, in0=a, scalar1=1.0 / 384, scalar2=RMS_EPS, op0=ALU.mult,
                                              op1=ALU.add), reads=[("sQa", m)], writes=[("sQb", m)])
        rsqrt_cols(b, r, 1, [("sQb", m)], [("sQr", m)])
        c.op("dve", lambda e: e.tensor_scalar(out=cqs[t % 2][:], in0=PS[1][:, 0:384], scalar1=r, scalar2=None,
                                              op0=ALU.mult), reads=["pcq", ("sQr", m)], writes=[c.W("cqs", t, 2)])
        pt = PS[2][:].bitcast(BF16)
        for k in range(3):
            c.op("pe", lambda e: e.transpose(out=pt[:, k * 128:(k + 1) * 128], in_=cqs[t % 2][:, k * 128:(k + 1) * 128],
                                             identity=identb[:]), reads=[c.R("cqs", t, 2), "const"], writes=["ptQ"])
        c.op("act", lambda e: e.activation(out=cqnT[t % 2][:], in_=pt[:, 0:384].rearrange("p (k n) -> p k n", k=3),
                                           func=AF.Copy), reads=["ptQ"], writes=[c.W("cqnT", t, 2)])

    def Q_s2(t):
        for (pi, lo, n) in [(3, 0, 512), (4, 512, 256)]:
            for k in range(3):
                c.op("pe", lambda e: e.matmul(PS[pi][:, 0:n], lhsT=cqnT[t % 2][:, k, :], rhs=wuq[:, k, lo:lo + n],
                                              start=(k == 0), stop=(k == 2)),
                     reads=[c.R("cqnT", t, 2), "wuq"], writes=[("pq", pi)])
        c.W("qsb", t, 2)
        _ql = 9
        c.op("act", lambda e: e.activation(out=qsb[t % 2][:, :, 0:64], in_=PS[3][:].rearrange("p (h d) -> p h d", h=8),
                                           func=AF.Copy), reads=[("pq", 3)], writes=[("qsb", t % 2)])
        z4 = PS[4][:, 0:256].rearrange("p (h t d) -> p h t d", h=8, t=2)
        cb = cosQ[:, t, 0:16].unsqueeze(1).unsqueeze(1).broadcast_to([128, 8, 2, 16])
        sb = sinQ[:, t, 0:16].unsqueeze(1).unsqueeze(1).broadcast_to([128, 8, 2, 16])
        o4 = qsb[t % 2][:, :, 64:96].rearrange("p h (t d) -> p h t d", t=2)
        rope(o4, z4, cb, sb, qr1[:].rearrange("p h (t d) -> p h t d", t=2),
             qr2[:].rearrange("p h (t d) -> p h t d", t=2), "pool",
             [("pq", 4), "rtQsin", "rtQcos"], [("qsb", t % 2)], "qr")
        if _ql < 3:
            return
        pt = PS[5][:].bitcast(BF16)
        for h in range(8):
            c.op("pe", lambda e: e.transpose(out=pt[0:96, h * 128:(h + 1) * 128], in_=qsb[t % 2][:, h, :],
                                             identity=identb[:]), reads=[("qsb", t % 2), "const"], writes=["ptQ2"])
        if _ql < 4:
            return
        c.op("act", lambda e: e.activation(out=QT[0:96, :, t * 128:(t + 1) * 128],
                                           in_=pt[0:96, :].rearrange("p (h n) -> p h n", h=8), func=AF.Copy),
             reads=["ptQ2"], writes=["QT"])

    pipeline(NO, [Q_s0, Q_s0b, Q_s1, Q_s2])
    print("sbuf remaining in pass Q:", nc.sbuf_bytes_remaining, "ops", c.nops)
    c.pop()
    dbg("QT", QT[:], [128, 8, NO * 128], BF16)
    if stop == "Q":
        return finish()

    c.push()
    PS = [c.psum(f"pt_{i}", [128, 512], F32) for i in range(8)]
    wukv = c.sbuf("wukv", [128, 2, 1024], BF16)
    c.push()
    wukv_f = c.sbuf("wukv_f", [128, 2, 1024], F32)
    gkv = c.sbuf("gkv", [128, 2], F32)
    c.dma("sp", wukv_f[:], w_ukv.rearrange("(k p) n -> p k n", p=128), writes=["wukv_f"], key="wT0")
    c.dma("sp", gkv[:], gckv, writes=["gkv"], key="wT1")
    for k in range(2):
        c.op("dve", lambda e: e.tensor_scalar(out=wukv[:, k, :], in0=wukv_f[:, k, :], scalar1=gkv[:, k:k + 1],
                                              scalar2=None, op0=ALU.mult), reads=["wukv_f", "gkv"], writes=["wukv"])
    c.pop()
    otb = [c.sbuf("otb", [64, 512], BF16) for _ in range(2)]
    Vb = [c.sbuf("Vb", [128, NT, 128], BF16) for _ in range(2)]
    for i in range(2):
        c.op("pool", lambda e: e.memset(Vb[i][:, :, 64:128], 0.0), writes=[("Vb1", i)])
        c.op("pool", lambda e: e.memset(Vb[i][:, :, 64:65], 1.0), writes=[("Vb1", i)])
    PT = [c.sbuf("PT", [128, 512], BF16) for _ in range(4)]
    osb = [c.sbuf("osb", [65, 512], F32) for _ in range(2)]
    rec = [c.sbuf("rec", [64, 512], F32) for _ in range(2)]
    fin = [0]

    def up_units(h):
        kt_buf, v_buf = KT[h % 2], Vb[h % 2]
        units = []

        def k_unit(kc):
            def f():
                pk = PS[kc % 2]
                for k in range(2):
                    c.op("pe", lambda e: e.matmul(pk[0:64, :], lhsT=wukv[:, k, h * 128:h * 128 + 64],
                                                  rhs=ckvnT[:, k, kc * 512:(kc + 1) * 512], start=(k == 0), stop=(k == 1)),
                         reads=["wukv", "ckvnT"], writes=[("pk", kc % 2)])
                c.op("dve", lambda e: e.tensor_copy(out=kt_buf[0:64, kc * 512:(kc + 1) * 512], in_=pk[0:64, :]),
                     reads=[("pk", kc % 2)], writes=[("KTn", h % 2)])
            return f

        def v_unit(kb):
            def f():
                pv = PS[kb % 2]
                for j8 in range(8):
                    kt = kb * 8 + j8
                    for k in range(2):
                        c.op("pe", lambda e: e.matmul(pv[:, j8 * 64:(j8 + 1) * 64], lhsT=ckvnT[:, k, kt * 128:(kt + 1) * 128],
                                                      rhs=wukv[:, k, h * 128 + 64:h * 128 + 128], start=(k == 0),
                                                      stop=(k == 1)), reads=["wukv", "ckvnT"], writes=[("pk", kb % 2)])
                c.op("dve", lambda e: e.tensor_copy(out=v_buf[:, kb * 8:(kb + 1) * 8, 0:64],
                                                    in_=pv[:].rearrange("p (j d) -> p j d", j=8)),
                     reads=[("pk", kb % 2)], writes=[("Vb", h % 2)])
            return f

        for kc in range(16):
            units.append(k_unit(kc))
        for kb in range(8):
            units.append(v_unit(kb))
        return units

    steps = [(h, qc, kt) for h in range(8) for qc in range(4) for kt in range(16 * qc + 16)]
    pending = {}
    c.W("KTn", 0, 2)
    c.W("Vb", 0, 2)
    for u in up_units(0):
        u()

    def att_s0(i):
        h, qc, kt = steps[i]
        kt_buf = KT[h % 2]
        if qc == 0 and kt == 0 and h + 1 < 8:
            pending["units"] = up_units(h + 1)
            pending["local"] = 0
            pending["armed"] = False
        if pending.get("units"):
            pending["local"] += 1
            if pending["local"] >= 4 and (pending["local"] - 4) % 6 == 0:
                if not pending["armed"]:
                    c.W("KTn", h + 1, 2)
                    c.W("Vb", h + 1, 2)
                    pending["armed"] = True
                pending["units"].pop(0)()
        gk, l = kt // 4, kt % 4
        c0 = (max(gk, 4 * qc) - 4 * qc) * 128
        ps, pt_ = PS[2 + i % 4], PT[i % 4]
        c.op("pe", lambda e: e.matmul(ps[:, c0:512], lhsT=kt_buf[0:96, kt * 128:(kt + 1) * 128],
                                      rhs=QT[0:96, h, qc * 512 + c0:(qc + 1) * 512], start=True, stop=True),
             reads=[("KTn", h % 2), f"KT{h % 2}r", "QT"], writes=[("ps", i % 4)])
        c.op("act", lambda e: e.activation(out=pt_[:, c0:512], in_=ps[:, c0:512], func=AF.Exp, scale=SCALE_MLA),
             reads=[("ps", i % 4)], writes=[("PT", i % 4)])
        if gk >= 4 * qc:
            c.op("pool", lambda e: e.tensor_tensor(out=pt_[:, c0:c0 + 128], in0=pt_[:, c0:c0 + 128],
                                                   in1=amask[:, l, :], op=ALU.mult),
                 reads=[("PT", i % 4), "const"], writes=[("PT", i % 4)])

    def att_s1(i):
        pass

    def att_s2(i):
        h, qc, kt = steps[i]
        v_buf = Vb[h % 2]
        nk = 16 * qc + 16
        gk = kt // 4
        c0 = (max(gk, 4 * qc) - 4 * qc) * 128
        po = PS[6 + qc % 2]
        pt_ = PT[i % 4]
        c.op("pe", lambda e: e.matmul(po[:, c0:512], lhsT=v_buf[:, kt, :], rhs=pt_[:, c0:512],
                                      start=(kt == 0), stop=(kt == nk - 1)),
             reads=[("Vb", h % 2), ("Vb1", h % 2), ("PT", i % 4)], writes=[("po", qc % 2)])
        if kt == nk - 1:
            f = fin[0]
            fin[0] += 1
            ob, rc = osb[f % 2], rec[f % 2]
            c.op("dve", lambda e: e.tensor_copy(out=ob[:], in_=po[0:65, :]), reads=[("po", qc % 2)],
                 writes=[("osb", f % 2)])
            pd = PS[f % 2]
            c.op("pe", lambda e: e.matmul(pd[0:64, :], lhsT=onesf[64:65, 0:64], rhs=ob[64:65, :], start=True, stop=True),
                 reads=[("osb", f % 2), "const"], writes=[("pk", f % 2)])
            c.op("dve", lambda e: e.reciprocal(out=rc[:], in_=pd[0:64, :]), reads=[("pk", f % 2)], writes=[("rec", f % 2)])
            c.op("dve", lambda e: e.tensor_tensor(out=otb[f % 2][:], in0=ob[0:64, :], in1=rc[:],
                                                  op=ALU.mult), reads=[("osb", f % 2), ("rec", f % 2)], writes=[("otb", f % 2)])
            c.dma("sp", ot_d[h, :, qc * 512:(qc + 1) * 512], otb[f % 2][:], reads=[("otb", f % 2)], writes=["ot_d"],
                  key=f"otw{f % 2}")

    pipeline(len(steps), [att_s0, att_s1, att_s1, att_s2])
    c.pop()
    c.pop()
    if stop == "T":
        return finish()

    c.push()
    PS = [c.psum(f"pc_{i}", [128, 512], F32) for i in range(8)]
    WC = c.sbuf("WC", [128, 8, 3072], BF16)
    load_w(WC[:, :, 0:1024], w_in[:, O_RQ:O_RQ + 1024].rearrange("(k p) n -> p k n", p=128), "WC", "wC0")
    load_w(WC[:, :, 1024:2048], w_in[:, O_RV:O_RV + 1024].rearrange("(k p) n -> p k n", p=128), "WC", "wC1")
    load_w(WC[:, :, 2048:3072], w_in[:, O_RG:O_RG + 1024].rearrange("(k p) n -> p k n", p=128), "WC", "wC2")
    bwC = fold_adaln(WC, 3072, "WC", PS[1], ("pz", 1), "C")
    wor = c.sbuf("wor", [128, 8, 1024], BF16)
    c.push()
    wor_f = c.sbuf("wor_f", [128, 8, 1024], F32)
    gr = c.sbuf("gr", [128, 8], F32)
    c.dma("sp", wor_f[:], w_o_ret.rearrange("(k p) n -> p k n", p=128), writes=["wor_f"], key="wC3")
    c.dma("sp", gr[:], gret, writes=["gr"], key="wC4")
    for k in range(8):
        c.op("dve", lambda e: e.tensor_scalar(out=wor[:, k, :], in0=wor_f[:, k, :], scalar1=gr[:, k:k + 1],
                                              scalar2=None, op0=ALU.mult), reads=["wor_f", "gr"], writes=["wor"])
    c.pop()
    kdecC = c.sbuf("kdecC", [128, 8, 128], F32)
    c.dma("sp", kdecC[:, 0:4, :], qdec_d, writes=["kdecC"], key="wC5")
    c.dma("sp", kdecC[:, 4:8, :], kdecC_d, writes=["kdecC"], key="wC6")
    ht = HT(xown, 3, A1, B1, PS[0])
    sinC = c.sbuf("sinC", [128, NO, 80], F32)
    cosC = c.sbuf("cosC", [128, NO, 80], F32)
    c.push()
    rtmpC = [c.sbuf("rtmpC", [128, NO, 64], F32) for _ in range(3)]
    rope_tables(posf[:, NT:NT + NO], NO, sinC, cosC, rtmpC, 16, 80, "rtC")
    c.pop()
    qk1 = c.sbuf("qk1", [128, 1024], F32)
    qk2 = c.sbuf("qk2", [128, 1024], F32)
    qk3 = c.sbuf("qk3", [128, 1024], F32)
    qkp = [c.sbuf("qkp", [128, 8, 128], BF16) for _ in range(2)]
    qkT = [c.sbuf("qkT", [128, 8, 128], BF16) for _ in range(2)]
    vbc = [c.sbuf("vbc", [128, 1024], BF16) for _ in range(2)]
    sg = [c.sbuf("sg", [128, 1024], BF16) for _ in range(2)]
    scT = [c.sbuf("scT", [128, 4, 128], BF16) for _ in range(2)]
    sob = [c.sbuf("sob", [128, 1024], BF16) for _ in range(2)]
    bnst = c.sbuf("bnst", [128, NO, 4, 6], F32)
    bnag = c.sbuf("bnag", [128, NO, 4, 2], F32)
    bnr = c.sbuf("bnr", [128, NO, 4, 2], F32)
    onr = [c.sbuf("onr", [128, 1024], F32) for _ in range(2)]
    gat = [c.sbuf("gat", [128, 1024], BF16) for _ in range(2)]
    gT = [c.sbuf("gT", [128, 8, 128], BF16) for _ in range(2)]
    bbt = [c.sbuf("bbt", [128, 1024], BF16) for _ in range(2)]

    def C1_s0(t):
        if t == 0:
            ht.load(0)
            ht.load(1)
        if t + 2 < NO:
            ht.load(t + 2)
        c.dma("sp", sob[t % 2][:], sown_d[t], reads=[], writes=[c.W("sob", t, 2)], key=f"sob{t % 2}")
        ht.norm(t)

    def C1_s0b(t):
        ht.transpose(t)

    def C1_s1(t):
        hT, hk = ht.get(t)
        for (pi, lo) in [(1, 0), (2, 512), (3, 1024), (4, 1536), (5, 2048), (6, 2560)]:
            for k in range(8):
                c.op("pe", lambda e: e.matmul(PS[pi][:], lhsT=hT[:, k, :], rhs=WC[:, k, lo:lo + 512], start=(k == 0),
                                              stop=False), reads=[hk, "WC"], writes=[("pz", pi)])
            bias_mm(PS[pi][:], bwC, lo, 512, "C", [("pz", pi)])
        cb = cosC[:, t, 16:80].unsqueeze(1).unsqueeze(1).broadcast_to([128, 4, 2, 64])
        sb = sinC[:, t, 16:80].unsqueeze(1).unsqueeze(1).broadcast_to([128, 4, 2, 64])
        for j in range(2):
            z4 = PS[1 + j][:].rearrange("p (h t d) -> p h t d", h=4, t=2)
            sl = slice(j * 512, (j + 1) * 512)
            rope(qk3[:, sl].rearrange("p (h t d) -> p h t d", h=4, t=2), z4, cb, sb,
                 qk1[:, sl].rearrange("p (h t d) -> p h t d", h=4, t=2),
                 qk2[:, sl].rearrange("p (h t d) -> p h t d", h=4, t=2), "pool",
                 [("pz", 1 + j), "rtCsin", "rtCcos"], [("qk3", j)], f"qk{j}")
        c.op("pool", lambda e: e.tensor_tensor(out=qkp[t % 2][:], in0=qk3[:].rearrange("p (h d) -> p h d", h=8),
                                               in1=kdecC[:], op=ALU.mult), reads=[("qk3", 0), ("qk3", 1), "kdecC"],
             writes=[c.W("qkp", t, 2)])
        c.W("vbc", t, 2)
        c.W("sg", t, 2)
        for hh in range(2):
            c.op("act", lambda e: e.activation(out=vbc[t % 2][:, hh * 512:(hh + 1) * 512], in_=PS[3 + hh][:],
                                               func=AF.Copy), reads=[("pz", 3 + hh)], writes=[("vbc", t % 2)])
            c.op("act", lambda e: e.activation(out=sg[t % 2][:, hh * 512:(hh + 1) * 512], in_=PS[5 + hh][:],
                                               func=AF.Silu), reads=[("pz", 5 + hh)], writes=[("sg", t % 2)])

    def C1_s2(t):
        pt = PS[7][:].bitcast(BF16)
        for j in range(8):
            c.op("pe", lambda e: e.transpose(out=pt[:, j * 128:(j + 1) * 128], in_=qkp[t % 2][:, j, :],
                                             identity=identb[:]), reads=[c.R("qkp", t, 2), "const"], writes=["ptC"])
        c.op("act", lambda e: e.activation(out=qkT[t % 2][:], in_=pt.rearrange("p (j n) -> p j n", j=8), func=AF.Copy),
             reads=["ptC"], writes=[c.W("qkT", t, 2)])
        for h in range(4):
            c.op("pe", lambda e: e.matmul(PS[1][:, h * 128:(h + 1) * 128], lhsT=qkT[t % 2][:, 4 + h, :],
                                          rhs=qkT[t % 2][:, h, :], start=True, stop=True),
                 reads=[c.R("qkT", t, 2)], writes=[("pz", 1)])
        c.op("dve", lambda e: e.tensor_tensor(out=scT[t % 2][:], in0=PS[1][:].rearrange("p (h n) -> p h n", h=4),
                                              in1=tri[:].unsqueeze(1).broadcast_to([128, 4, 128]), op=ALU.mult),
             reads=[("pz", 1), "const"], writes=[c.W("scT", t, 2)])
        for h in range(4):
            po = PS[3 + h // 2][:, (h % 2) * 256:(h % 2 + 1) * 256]
            c.op("pe", lambda e: e.matmul(po, lhsT=scT[t % 2][:, h, :], rhs=vbc[t % 2][:, h * 256:(h + 1) * 256],
                                          start=True, stop=False), reads=[c.R("scT", t, 2), c.R("vbc", t, 2)],
                 writes=[("pz", 3 + h // 2)])
            c.op("pe", lambda e: e.matmul(po, lhsT=qkT[t % 2][:, h, :], rhs=sob[t % 2][:, h * 256:(h + 1) * 256],
                                          start=False, stop=True), reads=[c.R("qkT", t, 2), c.R("sob", t, 2)],
                 writes=[("pz", 3 + h // 2)])
        for h in range(4):
            po = PS[3 + h // 2][:, (h % 2) * 256:(h % 2 + 1) * 256]
            c.op("dve", lambda e: e.bn_stats(out=bnst[:, t, h, :], in_=po), reads=[("pz", 3 + h // 2)],
                 writes=[("bnst", t)])
            c.op("dve", lambda e: e.bn_aggr(out=bnag[:, t, h, :], in_=bnst[:, t, h, :]), reads=[("bnst", t)],
                 writes=[("bnag", t)])
        c.op("dve", lambda e: e.tensor_scalar(out=bnr[:, t, :, 0], in0=bnag[:, t, :, 1], scalar1=GN_EPS, scalar2=None,
                                              op0=ALU.add), reads=[("bnag", t)], writes=[("bnr0", t)])
        rsqrt_cols(bnr[:, t, :, 0], bnr[:, t, :, 1], 4, [("bnr0", t)], [("bnr1", t)])
        c.W("onr", t, 2)
        for h in range(4):
            po = PS[3 + h // 2][:, (h % 2) * 256:(h % 2 + 1) * 256]
            c.op("dve", lambda e: e.tensor_scalar(out=onr[t % 2][:, h * 256:(h + 1) * 256], in0=po,
                                                  scalar1=bnag[:, t, h, 0:1], scalar2=bnr[:, t, h, 1:2],
                                                  op0=ALU.subtract, op1=ALU.mult),
                 reads=[("pz", 3 + h // 2), ("bnag", t), ("bnr1", t)], writes=[("onr", t % 2)])
        c.op("pool", lambda e: e.tensor_tensor(out=gat[t % 2][:], in0=onr[t % 2][:], in1=sg[t % 2][:], op=ALU.mult),
             reads=[("onr", t % 2), c.R("sg", t, 2)], writes=[c.W("gat", t, 2)])

    def C1_s3(t):
        pt = PS[7][:].bitcast(BF16)
        for k in range(8):
            c.op("pe", lambda e: e.transpose(out=pt[:, k * 128:(k + 1) * 128], in_=gat[t % 2][:, k * 128:(k + 1) * 128],
                                             identity=identb[:]), reads=[c.R("gat", t, 2), "const"], writes=["ptC"])
        c.op("act", lambda e: e.activation(out=gT[t % 2][:], in_=pt.rearrange("p (j n) -> p j n", j=8), func=AF.Copy),
             reads=["ptC"], writes=[c.W("gT", t, 2)])
        c.W("bbt", t, 2)
        for hh in range(2):
            for k in range(8):
                c.op("pe", lambda e: e.matmul(PS[5 + hh][:], lhsT=gT[t % 2][:, k, :], rhs=wor[:, k, hh * 512:(hh + 1) * 512],
                                              start=(k == 0), stop=(k == 7)), reads=[c.R("gT", t, 2), "wor"],
                     writes=[("pz", 5 + hh)])
            c.op("act", lambda e: e.activation(out=bbt[t % 2][:, hh * 512:(hh + 1) * 512], in_=PS[5 + hh][:],
                                               func=AF.Copy), reads=[("pz", 5 + hh)], writes=[("bbt", t % 2)])
        c.dma("sp", bb_d[t], bbt[t % 2][:], reads=[("bbt", t % 2)], writes=["bb_d"], key=f"bbw{t % 2}")

    def C1_s123(t):
        C1_s1(t)
        C1_s2(t)
        C1_s3(t)

    pipeline(NO, [C1_s0, C1_s0b, C1_s123])
    print("sbuf remaining in pass C1:", nc.sbuf_bytes_remaining, "ops", c.nops)
    c.pop()
    if stop == "C1":
        return finish()

    h2T = c.sbuf("h2T", [128, 8, NO * 128], BF16)
    print("sbuf remaining before C2:", nc.sbuf_bytes_remaining)
    c.push()
    PS = [c.psum(f"pd_{i}", [128, 512], F32) for i in range(8)]
    WG = c.sbuf("WG", [128, 8, 2048], BF16)
    load_w(WG[:], w_in[:, O_GA:O_GA + 2048].rearrange("(k p) n -> p k n", p=128), "WG", "wD0")
    bwG = fold_adaln(WG, 2048, "WG", PS[1], ("pz", 1), "G")
    wom = c.sbuf("wom", [64, 8, 1024], BF16)
    load_w(wom[:], w_o_mla.rearrange("(h p) n -> p h n", p=64), "wom", "wD1")
    wout = c.sbuf("wout", [128, 8, 1024], BF16)
    load_w(wout[:], w_out.rearrange("(k p) n -> p k n", p=128), "wout", "wD2")
    wr = c.sbuf("wr", [128, 8, 64], F32)
    c.dma("sp", wr[:], w_router.rearrange("(k p) n -> p k n", p=128), writes=["wr"], key="wD3")
    ht = HT(xown, 4, A1, B1, PS[0])
    tg = [c.sbuf("tg", [128, 2048], BF16)] * 2
    ott = [c.sbuf("ott", [64, 8, 128], BF16) for _ in range(2)]
    bbr = [c.sbuf("bbr", [128, 1024], BF16) for _ in range(2)]
    m1 = c.sbuf("m1", [128, 1024], F32)
    m2 = c.sbuf("m2", [128, 1024], F32)
    mg = [c.sbuf("mg", [128, 1024], BF16) for _ in range(2)]
    mgT = [c.sbuf("mgT", [128, 8, 128], BF16) for _ in range(2)]
    ty = m1
    x1 = [c.sbuf("x1", [128, 1024], F32) for _ in range(2)]
    xs2 = [c.sbuf("xs2", [128, 1024], F32) for _ in range(2)]
    h2f = [c.sbuf("h2f", [128, 8, 128], F32) for _ in range(2)]
    st2 = c.sbuf("st2", [128, 3 * 64], F32)
    rt = c.sbuf("rt", [128, 2, 64 * 4 + 64 + 8 * 4], F32)

    def C2_s0(t):
        if t == 0:
            ht.load(0)
            ht.load(1)
        if t + 2 < NO:
            ht.load(t + 2)
        c.dma("sp", bbr[t % 2][:], bb_d[t], reads=["bb_d"], writes=[c.W("bbr", t, 2)], key=f"bbr{t % 2}")
        c.dma("sp", ott[t % 2][:], ot_d[:, :, t * 128:(t + 1) * 128].rearrange("h p n -> p h n"), reads=["ot_d"],
              writes=[c.W("ott", t, 2)], key=f"ott{t % 2}")
        ht.norm(t)

    def C2_s0b(t):
        ht.transpose(t)

    def C2_s1(t):
        hT, hk = ht.get(t)
        xt = ht.xt[t % 4]
        for j in range(4):
            for k in range(8):
                c.op("pe", lambda e: e.matmul(PS[1 + j][:], lhsT=hT[:, k, :], rhs=WG[:, k, j * 512:(j + 1) * 512],
                                              start=(k == 0), stop=False), reads=[hk, "WG"], writes=[("pz", 1 + j)])
            bias_mm(PS[1 + j][:], bwG, j * 512, 512, "G", [("pz", 1 + j)])
            c.op("act", lambda e: e.activation(out=tg[t % 2][:, j * 512:(j + 1) * 512], in_=PS[1 + j][:], func=AF.Tanh,
                                               scale=0.5), reads=[("pz", 1 + j)], writes=["tg"])
        for hh in range(2):
            for h in range(8):
                c.op("pe", lambda e: e.matmul(PS[5 + hh][:], lhsT=ott[t % 2][:, h, :],
                                              rhs=wom[:, h, hh * 512:(hh + 1) * 512], start=(h == 0), stop=(h == 7)),
                     reads=[c.R("ott", t, 2), "wom"], writes=[("pz", 5 + hh)])
            sl = slice(hh * 512, (hh + 1) * 512)
            c.op("dve", lambda e: e.scalar_tensor_tensor(out=m1[:, sl], in0=tg[t % 2][:, sl], scalar=1.0, in1=PS[5 + hh][:],
                                                         op0=ALU.add, op1=ALU.mult),
                 reads=["tg", ("pz", 5 + hh)], writes=[("m1", hh)])
        c.op("dve", lambda e: e.scalar_tensor_tensor(out=m2[:], in0=tg[t % 2][:, 1024:2048], scalar=1.0, in1=bbr[t % 2][:],
                                                     op0=ALU.add, op1=ALU.mult),
             reads=["tg", c.R("bbr", t, 2)], writes=["m2"])
        c.op("pool", lambda e: e.tensor_tensor(out=mg[t % 2][:], in0=m1[:], in1=m2[:], op=ALU.add),
             reads=[("m1", 0), ("m1", 1), "m2"], writes=[c.W("mg", t, 2)])
        pt = PS[7][:].bitcast(BF16)
        for k in range(8):
            c.op("pe", lambda e: e.transpose(out=pt[:, k * 128:(k + 1) * 128], in_=mg[t % 2][:, k * 128:(k + 1) * 128],
                                             identity=identb[:]), reads=[c.R("mg", t, 2), "const"], writes=["ptD"])
        c.op("act", lambda e: e.activation(out=mgT[t % 2][:], in_=pt.rearrange("p (j n) -> p j n", j=8), func=AF.Copy),
             reads=["ptD"], writes=[c.W("mgT", t, 2)])
        c.W("x1", t, 2)
        for hh in range(2):
            sl = slice(hh * 512, (hh + 1) * 512)
            for k in range(8):
                c.op("pe", lambda e: e.matmul(PS[1 + hh][:], lhsT=mgT[t % 2][:, k, :], rhs=wout[:, k, sl],
                                              start=(k == 0), stop=(k == 7)), reads=[c.R("mgT", t, 2), "wout"],
                     writes=[("pz", 1 + hh)])
            c.op("dve", lambda e: e.tensor_tensor(out=ty[:, sl], in0=PS[1 + hh][:], in1=GT1[:, sl], op=ALU.mult),
                 reads=[("pz", 1 + hh), "GT"], writes=[("m1", hh)])
            c.op("pool", lambda e: e.tensor_tensor(out=x1[t % 2][:, sl], in0=ty[:, sl], in1=xt[:, sl], op=ALU.add),
                 reads=[("m1", hh), c.R("xt", t, 4)], writes=[("x1", t % 2)])
        c.dma("sp", x1_d[t], x1[t % 2][:], reads=[("x1", t % 2)], writes=["x1_d"], key=f"x1w{t % 2}")
        m = t
        a, b, r = st2[:, m:m + 1], st2[:, 64 + m:65 + m], st2[:, 128 + m:129 + m]
        c.op("act", lambda e: e.activation(out=junk[:], in_=x1[t % 2][:], func=AF.Square, accum_out=a),
             reads=[("x1", t % 2)], writes=["junk", ("s2a", m)])
        c.op("dve", lambda e: e.tensor_scalar(out=b, in0=a, scalar1=1.0 / D, scalar2=RMS_EPS, op0=ALU.mult, op1=ALU.add),
             reads=[("s2a", m)], writes=[("s2b", m)])
        rsqrt_cols(b, r, 1, [("s2b", m)], [("s2r", m)])
        c.op("dve", lambda e: e.tensor_scalar(out=xs2[t % 2][:], in0=x1[t % 2][:], scalar1=r, scalar2=None, op0=ALU.mult),
             reads=[("x1", t % 2), ("s2r", m)], writes=[c.W("xs2", t, 2)])

    def C2_s2(t):
        for k in range(8):
            pb = PS[3 + k // 4][:, (k % 4) * 128:(k % 4 + 1) * 128]
            c.op("pe", lambda e: e.transpose(out=pb, in_=xs2[t % 2][:, k * 128:(k + 1) * 128], identity=identf[:]),
                 reads=[c.R("xs2", t, 2), "const"], writes=[("pz", 3 + k // 4)])
        for kk in range(8):
            k = (kk // 2) + 4 * (kk % 2)
            pb = PS[3 + k // 4][:, (k % 4) * 128:(k % 4 + 1) * 128]
            if k < 4:
                c.op("act", lambda e: e.activation(out=h2f[t % 2][:, k, :], in_=pb, func=AF.Identity,
                                                   scale=A2[:, k:k + 1], bias=B2[:, k:k + 1]),
                     reads=[("pz", 3), "AB"], writes=[("h2f", t % 2, 0)])
            else:
                c.op("dve", lambda e: e.tensor_scalar(out=h2f[t % 2][:, k, :], in0=pb, scalar1=A2[:, k:k + 1],
                                                      scalar2=B2[:, k:k + 1], op0=ALU.mult, op1=ALU.add),
                     reads=[("pz", 4), "AB"], writes=[("h2f", t % 2, 1)])
        c.op("dve", lambda e: e.tensor_copy(out=h2T[:, :, t * 128:(t + 1) * 128], in_=h2f[t % 2][:]),
             reads=[("h2f", t % 2, 0), ("h2f", t % 2, 1)], writes=["h2T"])
        for k in range(8):
            c.op("pe", lambda e: e.matmul(PS[5][:, 0:64], lhsT=h2f[t % 2][:, k, :], rhs=wr[:, k, :], start=(k == 0),
                                          stop=(k == 7)), reads=[("h2f", t % 2, 0), ("h2f", t % 2, 1), "wr"], writes=[("pz", 5)])
        R_ = rt[:, t % 2, :]
        s_, bi, mb, sel = R_[:, 0:64], R_[:, 64:128], R_[:, 128:192], R_[:, 192:256]
        m8 = R_[:, 256:320]
        gs, g8, gm, gneg = R_[:, 320:328], R_[:, 328:336], R_[:, 336:344], R_[:, 344:352]
        rk_ = ("rt", t % 2)
        c.op("act", lambda e: e.activation(out=s_, in_=PS[5][:, 0:64], func=AF.Tanh, scale=0.5), reads=[("pz", 5)],
             writes=[rk_])
        c.op("dve", lambda e: e.tensor_scalar(out=s_, in0=s_, scalar1=0.5, scalar2=0.5, op0=ALU.mult, op1=ALU.add),
             reads=[rk_], writes=[rk_])
        c.op("dve", lambda e: e.tensor_tensor(out=bi, in0=s_, in1=brout_t[:], op=ALU.add), reads=[rk_, "const"],
             writes=[rk_])
        for g in range(8):
            c.op("dve", lambda e: e.max(out=m8[:, g * 8:(g + 1) * 8], in_=bi[:, g * 8:(g + 1) * 8]), reads=[rk_],
                 writes=[rk_])
        m83 = m8.rearrange("p (g k) -> p g k", g=8)
        c.op("dve", lambda e: e.tensor_tensor(out=gs, in0=m83[:, :, 0], in1=m83[:, :, 1], op=ALU.add), reads=[rk_],
             writes=[rk_])
        c.op("dve", lambda e: e.max(out=g8, in_=gs), reads=[rk_], writes=[rk_])
        c.op("dve", lambda e: e.tensor_scalar(out=gm, in0=gs, scalar1=g8[:, 3:4], scalar2=None, op0=ALU.is_ge),
             reads=[rk_], writes=[rk_])
        c.op("dve", lambda e: e.tensor_scalar(out=gneg, in0=gm, scalar1=-1.0, scalar2=8.0, op0=ALU.add, op1=ALU.mult),
             reads=[rk_], writes=[rk_])
        bi3, mb3 = bi.rearrange("p (g k) -> p g k", g=8), mb.rearrange("p (g k) -> p g k", g=8)
        c.op("dve", lambda e: e.tensor_tensor(out=mb3, in0=bi3, in1=gm.unsqueeze(2).broadcast_to([128, 8, 8]), op=ALU.mult),
             reads=[rk_], writes=[rk_])
        c.op("dve", lambda e: e.tensor_tensor(out=mb3, in0=mb3, in1=gneg.unsqueeze(2).broadcast_to([128, 8, 8]), op=ALU.add),
             reads=[rk_], writes=[rk_])
        c.op("dve", lambda e: e.max(out=g8, in_=mb), reads=[rk_], writes=[rk_])
        c.op("dve", lambda e: e.tensor_scalar(out=sel, in0=mb, scalar1=g8[:, 7:8], scalar2=None, op0=ALU.is_ge),
             reads=[rk_], writes=[rk_])
        c.op("dve", lambda e: e.tensor_tensor(out=sel, in0=sel, in1=s_, op=ALU.mult), reads=[rk_], writes=[rk_])
        c.op("dve", lambda e: e.tensor_reduce(out=gs[:, 0:1], in_=sel, axis=AX.X, op=ALU.add), reads=[rk_], writes=[rk_])
        c.op("dve", lambda e: e.reciprocal(out=gs[:, 1:2], in_=gs[:, 0:1]), reads=[rk_], writes=[rk_])
        c.op("dve", lambda e: e.tensor_scalar(out=comb[:, t, :], in0=sel, scalar1=gs[:, 1:2], scalar2=2.5, op0=ALU.mult,
                                              op1=ALU.mult), reads=[rk_], writes=["comb"])

    def C2_s12(t):
        C2_s1(t)
        C2_s2(t)

    pipeline(NO, [C2_s0, C2_s0b, C2_s12])
    print("sbuf remaining in pass C2:", nc.sbuf_bytes_remaining, "ops", c.nops)
    c.pop()
    dbg("h2T", h2T[:], [128, 8, NO * 128], BF16)
    dbg("comb", comb[:], [128, NO, 64], F32)
    if stop == "C2":
        return finish()

    c.push()
    PG = c.psum("pg", [128, 2048], F32)
    PY = [c.psum(f"py{i}", [128, 1024], F32) for i in range(2)]
    acc = c.sbuf("acc", [128, NO, 1024], F32)
    wgu = [c.sbuf("wgu", [128, 8, 512], BF16) for _ in range(2)]
    wdn = [c.sbuf("wdn", [128, 2, 1024], BF16) for _ in range(2)]
    sgm = [c.sbuf("sgm", [128, 1024], BF16) for _ in range(2)]
    actT = [c.sbuf("actT", [128, 2, 512], BF16) for _ in range(2)]
    def load_expert(ei):
        e_ = ei - 1
        sl = ei % 2
        srcs = (w_sg, w_su, w_sd) if e_ < 0 else (w_eg[e_], w_eu[e_], w_ed[e_])
        c.W("wexp", ei, 2)
        c.dma("pool", wgu[sl][:, :, 0:256], srcs[0].rearrange("(k p) n -> p k n", p=128), writes=[("wexp", sl)], key=f"we{sl}a")
        c.dma("pool", wgu[sl][:, :, 256:512], srcs[1].rearrange("(k p) n -> p k n", p=128), writes=[("wexp", sl)], key=f"we{sl}b")
        c.dma("pool", wdn[sl][:], srcs[2].rearrange("(k p) n -> p k n", p=128), writes=[("wexp", sl)], key=f"we{sl}c")

    units = [(ei, tc_) for ei in range(NEXP + 1) for tc_ in range(4)]
    load_expert(0)

    def moe_s0(u):
        ei, tc_ = units[u]
        sl, a_ = ei % 2, u % 2
        if tc_ == 1 and ei + 1 <= NEXP:
            load_expert(ei + 1)
        wk = c.R("wexp", ei, 2)
        for j in range(4):
            for k in range(8):
                c.op("pe", lambda e: e.matmul(PG[:, j * 512:(j + 1) * 512], lhsT=wgu[sl][:, k, j * 128:(j + 1) * 128],
                                              rhs=h2T[:, k, tc_ * 512:(tc_ + 1) * 512], start=(k == 0), stop=(k == 7)),
                     reads=[wk, "h2T"], writes=[("pg", j // 2)])
        c.op("act", lambda e: e.activation(out=sgm[a_][:], in_=PG[:, 0:1024], func=AF.Silu), reads=[("pg", 0)],
             writes=[("sgm", a_)])
        c.op("dve", lambda e: e.tensor_tensor(out=actT[a_][:].rearrange("p f n -> p (f n)"), in0=sgm[a_][:],
                                              in1=PG[:, 1024:2048], op=ALU.mult), reads=[("sgm", a_), ("pg", 1)],
             writes=[("actT", a_)])

    def moe_s1(u):
        ei, tc_ = units[u]
        e_ = ei - 1
        sl, a_ = ei % 2, u % 2
        wk = ("wexp", sl)
        for tt in range(4):
            i = tc_ * 4 + tt
            y_ = (u * 4 + tt) % 2
            for hh in range(2):
                for fc in range(2):
                    c.op("pe", lambda e: e.matmul(PY[y_][:, hh * 512:(hh + 1) * 512], lhsT=actT[a_][:, fc, tt * 128:(tt + 1) * 128],
                                                  rhs=wdn[sl][:, fc, hh * 512:(hh + 1) * 512], start=(fc == 0), stop=(fc == 1)),
                         reads=[("actT", a_), wk], writes=[("py", y_)])
            if e_ < 0:
                c.op("act", lambda e: e.activation(out=acc[:, i, :], in_=PY[y_][:], func=AF.Copy), reads=[("py", y_)],
                     writes=[("acc", i)])
            else:
                c.op("dve", lambda e: e.scalar_tensor_tensor(out=acc[:, i, :], in0=PY[y_][:], scalar=comb[:, i, e_:e_ + 1],
                                                             in1=acc[:, i, :], op0=ALU.mult, op1=ALU.add),
                     reads=[("py", y_), ("acc", i), "comb"], writes=[("acc", i)])

    for u in range(len(units) + 1):
        if u < len(units):
            moe_s0(u)
        if u >= 1:
            moe_s1(u - 1)
    x1r = [c.sbuf("x1r", [128, 1024], F32) for _ in range(2)]
    xo = [c.sbuf("xo", [128, 1024], F32) for _ in range(2)]
    yo = [c.sbuf("yo", [128, 1024], F32) for _ in range(2)]
    stf = c.sbuf("stf", [128, 3 * 64], F32)
    for t in range(NO):
        c.dma("sp", x1r[t % 2][:], x1_d[t], reads=["x1_d"], writes=[("x1r", t % 2)], key=f"x1r{t % 2}")
        c.op("dve", lambda e: e.tensor_tensor(out=xo[t % 2][:], in0=acc[:, t, :], in1=GT2[:], op=ALU.mult),
             reads=[("acc", t), "GT"], writes=[("xo", t % 2)])
        c.op("pool", lambda e: e.tensor_tensor(out=xo[t % 2][:], in0=xo[t % 2][:], in1=x1r[t % 2][:], op=ALU.add),
             reads=[("xo", t % 2), ("x1r", t % 2)], writes=[("xo", t % 2)])
        a, b, r = stf[:, t:t + 1], stf[:, 64 + t:65 + t], stf[:, 128 + t:129 + t]
        c.op("act", lambda e: e.activation(out=junk[:], in_=xo[t % 2][:], func=AF.Square, accum_out=a),
             reads=[("xo", t % 2)], writes=["junk", ("sfa", t)])
        c.op("dve", lambda e: e.tensor_scalar(out=b, in0=a, scalar1=1.0 / D, scalar2=RMS_EPS, op0=ALU.mult, op1=ALU.add),
             reads=[("sfa", t)], writes=[("sfb", t)])
        rsqrt_cols(b, r, 1, [("sfb", t)], [("sfr", t)])
        c.op("dve", lambda e: e.scalar_tensor_tensor(out=yo[t % 2][:], in0=xo[t % 2][:], scalar=r, in1=gfin_t[:],
                                                     op0=ALU.mult, op1=ALU.mult),
             reads=[("xo", t % 2), ("sfr", t), "const"], writes=[("yo", t % 2)])
        c.dma("sp", out_d[t * 128:(t + 1) * 128, :], yo[t % 2][:], reads=[("yo", t % 2)], writes=["out"], key=f"out{t % 2}")
    c.pop()
    c.close()
    return nc


_NC_CACHE = {}


def _consts(j):
    bf = ml_dtypes.bfloat16
    k = np.arange(128)
    tri = (k[:, None] <= k[None, :]).astype(np.float32)
    amask = np.zeros((128, 4, 128), np.float32)
    for l in range(4):
        if l < j:
            amask[:, l, :] = 1.0
        elif l == j:
            amask[:, l, :] = tri
    inv_mla = 1.0 / (10000.0 ** (np.arange(0, 32, 2, dtype=np.float32) / np.float32(32)))
    inv_ret = 1.0 / (10000.0 ** (np.arange(0, 128, 2, dtype=np.float32) / np.float32(128)))
    invf = np.broadcast_to(np.concatenate([inv_mla, inv_ret]).astype(np.float32)[None, :], (128, 80)).copy()
    g = np.array(GAMMA, np.float64)
    m = np.arange(128, dtype=np.float64)
    kdecA = (g[None, :] ** (127.0 - m[:, None])) * 128.0 ** -0.5
    kdecC = (g[None, :] ** (-m[:, None])) * 128.0 ** -0.5
    qdec = g[None, :] ** m[:, None]
    rep = lambda a: np.repeat(a[:, :, None], 128, axis=2).astype(np.float32)
    G = g ** 128
    rc = np.zeros((128, 16), np.float64)
    for h in range(4):
        rc[:, h * 4 + 0] = g[h] * G[h] ** j
        for l in range(3):
            rc[:, h * 4 + 1 + l] = g[h] * (G[h] ** (j - 1 - l)) if l < j else 0.0
    return dict(identb=np.eye(128).astype(bf), identf=np.eye(128, dtype=np.float32), tri=tri,
                amask=amask.astype(bf), invf=invf, kdecA=rep(kdecA), kdecC=rep(kdecC), qdec=rep(qdec),
                rcoef=rc.astype(np.float32))


def _col(v, nchunk):
    return np.ascontiguousarray(np.asarray(v, np.float32).reshape(nchunk, 128).T)


_BUILD_ARGS = {}


def kernel(x, c, positions, w_ada, b_ada, g_norm1, w_in, g_cq, w_uq, g_ckv, w_ukv, g_ret, w_o_mla, w_o_ret,
           w_out, g_norm2, w_router, b_router, w_exp_gate, w_exp_up, w_exp_down, w_sh_gate, w_sh_up, w_sh_down,
           g_final):
    f = lambda a: np.ascontiguousarray(np.asarray(a, dtype=np.float32))
    x = f(x)
    positions = np.asarray(positions).astype(np.int32)
    if "nc" not in _NC_CACHE:
        _NC_CACHE["nc"] = build(**_BUILD_ARGS)
    nc = _NC_CACHE["nc"]
    shared = dict(
        w_ada=f(w_ada), bada_row=f(b_ada).reshape(1, -1), g1c=_col(g_norm1, 8), g2c=_col(g_norm2, 8), w_in=f(w_in),
        gcq=_col(g_cq, 3), w_uq=f(w_uq), gckv=_col(g_ckv, 2), w_ukv=f(w_ukv), gret=_col(g_ret, 8), w_o_mla=f(w_o_mla),
        w_o_ret=f(w_o_ret), w_out=f(w_out), w_router=f(w_router),
        brout=np.ascontiguousarray(np.broadcast_to(f(b_router)[None, :], (128, 64))),
        w_exp_gate=f(w_exp_gate), w_exp_up=f(w_exp_up), w_exp_down=f(w_exp_down), w_sh_gate=f(w_sh_gate),
        w_sh_up=f(w_sh_up), w_sh_down=f(w_sh_down),
        gfin=np.ascontiguousarray(np.broadcast_to(f(g_final)[None, :], (128, 1024))),
    )
    in_maps = []
    for core in range(8):
        b, j = core // 4, core % 4
        xb = x[b]
        xt = xb.reshape(NT, 128, D)
        pt = positions[b].reshape(NT, 128)
        m = dict(shared)
        m.update(_consts(j))
        m["xall"] = xb
        m["xown"] = np.ascontiguousarray(xt[j::4].reshape(NO * 128, D))
        m["posall"] = np.ascontiguousarray(pt.T)
        m["posown"] = np.ascontiguousarray(pt[j::4].T)
        m["cvec"] = _col(c[b], 8)
        in_maps.append(m)
    res = run_bass_kernel_spmd(nc, in_maps, core_ids=list(range(8)))
    _NC_CACHE["res"] = res
    out = np.empty((2, S, D), np.float32)
    for core in range(8):
        b, j = core // 4, core % 4
        o = np.asarray(res.results[core]["out"]).reshape(NO, 128, D)
        out[b].reshape(NT, 128, D)[j::4] = o
    return out
```

```python
import math
from contextlib import ExitStack

import numpy as np
import ml_dtypes

import concourse.bass as bass
import concourse.mybir as mybir
from concourse.bass_utils import run_bass_kernel_spmd

F32 = mybir.dt.float32
BF16 = mybir.dt.bfloat16
I32 = mybir.dt.int32
AF = mybir.ActivationFunctionType
ALU = mybir.AluOpType
AX = mybir.AxisListType

D = 1024
S = 8192
NT = 64
NO = 16
D_IN = 5792
RMS_EPS = 1e-6
GN_EPS = 1e-5
TWO_PI = 2.0 * math.pi
MAGIC = 12582912.0
C1 = 6.28125
C2 = float(np.float32(TWO_PI - 6.28125))
C3 = float(TWO_PI - 6.28125 - float(np.float32(TWO_PI - 6.28125)))
SCALE_MLA = 96.0 ** -0.5
GAMMA = [1.0 - 2.0 ** (-5.0 - h) for h in range(4)]
NEXP = 64
import os
NOSAME = False

O_CQ, O_CKV, O_KR, O_RQ, O_RK, O_RV, O_RG, O_GA, O_GB = 0, 384, 640, 672, 1184, 1696, 2720, 3744, 4768


class Ctx:
    def __init__(self, nc):
        self.nc = nc
        self.stacks = [ExitStack()]
        self.eng = {"pe": nc.tensor, "act": nc.scalar, "dve": nc.vector, "pool": nc.gpsimd, "sp": nc.sync}
        self.sem, self.cnt = {}, {}
        for e in ("pe", "act", "dve", "pool"):
            self.sem[e] = self.stacks[0].enter_context(nc.semaphore("s_" + e))
            self.cnt[e] = 0
        self.dsem, self.dcnt = {}, {}
        self.waited = {e: {} for e in self.eng}
        self.last_w, self.readers, self.owner = {}, {}, {}
        self.nops = 0
        self.uid = 0

    def sbuf(self, name, shape, dtype):
        self.uid += 1
        return self.stacks[-1].enter_context(self.nc.sbuf_tensor(f"{name}_{self.uid}", list(shape), dtype))

    def psum(self, name, shape, dtype):
        self.uid += 1
        return self.stacks[-1].enter_context(self.nc.psum_tensor(f"{name}_{self.uid}", list(shape), dtype))

    def push(self):
        self.stacks.append(ExitStack())

    def pop(self):
        self.barrier()
        self.stacks.pop().close()

    def W(self, name, t=0, n=1):
        k = (name, t % n)
        self.owner[k] = t
        return k

    def R(self, name, t=0, n=1):
        k = (name, t % n)
        assert self.owner.get(k) == t, f"stale read {name} tile {t} owner {self.owner.get(k)}"
        return k

    PSUM_NAMES = {"pmod", "pc", "pgt", "pT", "pz", "ptA", "pkv", "pcq", "ptQ", "pq", "ptQ2", "pk", "ps", "po", "ptC",
                  "ptD", "pg", "py"}

    def _split(self, reads, writes):
        rd, wr = [], list(writes)
        for r in reads:
            base = r[0] if isinstance(r, tuple) else r
            if base in self.PSUM_NAMES:
                if r not in wr:
                    wr.append(r)
            else:
                rd.append(r)
        return rd, wr

    def _deps(self, reads, writes):
        deps = []
        for r in reads:
            t = self.last_w.get(r)
            if t is not None:
                deps.append(t)
        for w in writes:
            t = self.last_w.get(w)
            if t is not None:
                deps.append(t)
            deps.extend(self.readers.get(w, ()))
        return deps

    def _wait(self, eng, deps):
        best = {}
        for (skey, sem, val) in deps:
            if eng == "pe" and skey == "pe":
                continue
            if NOSAME and skey == eng:
                continue
            if val > best.get(skey, (None, 0))[1]:
                best[skey] = (sem, val)
        w = self.waited[eng]
        for skey, (sem, val) in best.items():
            if w.get(skey, 0) >= val:
                continue
            self.eng[eng].wait_ge(sem, val)
            w[skey] = val

    def _commit(self, ticket, reads, writes):
        for r in reads:
            self.readers.setdefault(r, []).append(ticket)
        for w in writes:
            self.last_w[w] = ticket
            self.readers[w] = []

    def op(self, eng, fn, reads=(), writes=()):
        reads, writes = self._split(reads, writes)
        self._wait(eng, self._deps(reads, writes))
        ins = fn(self.eng[eng])
        self.cnt[eng] += 1
        ins.then_inc(self.sem[eng], 1)
        t = (eng, self.sem[eng], self.cnt[eng])
        self._commit(t, reads, writes)
        self.nops += 1
        return t

    def dma(self, queue, out, in_, reads=(), writes=(), key=None):
        if key not in self.dsem:
            self.dsem[key] = self.stacks[0].enter_context(self.nc.semaphore("d_" + str(key)))
            self.dcnt[key] = 0
        self._wait(queue, self._deps(reads, writes))
        ins = self.eng[queue].dma_start(out=out, in_=in_)
        self.dcnt[key] += 16
        ins.then_inc(self.dsem[key], 16)
        t = ("d_" + str(key), self.dsem[key], self.dcnt[key])
        self._commit(t, reads, writes)
        return t

    def barrier(self):
        tickets = [(e, self.sem[e], self.cnt[e]) for e in self.sem if self.cnt[e] > 0]
        tickets += [("d_" + str(k), self.dsem[k], self.dcnt[k]) for k in self.dsem]
        for e in self.eng:
            self._wait(e, tickets)
        self.last_w, self.readers = {}, {}

    def close(self):
        self.barrier()
        while self.stacks:
            self.stacks.pop().close()


def pipeline(n, stages):
    ns = len(stages)
    for s in range(n + ns - 1):
        for k in range(ns - 1, -1, -1):
            t = s - k
            if 0 <= t < n:
                stages[k](t)


def build(stop=None, debug=False):
    nc = bass.Bass("TRN2", target_bir_lowering=False)
    dbg_out = {}

    def dbg(name, ap, shape, dt):
        if not debug:
            return
        d_ = nc.dram_tensor("dbg_" + name, list(shape), dt, kind="ExternalOutput").ap()
        dbg_out[name] = d_
        c.barrier()
        c.dma("sp", d_, ap, key="dbg_" + name)
        c.barrier()

    def finish():
        c.close()
        return nc

    def din(name, shape, dt=F32):
        return nc.dram_tensor(name, list(shape), dt, kind="ExternalInput").ap()

    xall = din("xall", [S, D])
    xown = din("xown", [NO * 128, D])
    posall = din("posall", [128, NT], I32)
    posown = din("posown", [128, NO], I32)
    cvec = din("cvec", [128, 8])
    w_ada = din("w_ada", [D, 6 * D])
    bada_row = din("bada_row", [1, 6 * D])
    g1c = din("g1c", [128, 8])
    g2c = din("g2c", [128, 8])
    w_in = din("w_in", [D, D_IN])
    gcq = din("gcq", [128, 3])
    w_uq = din("w_uq", [384, 768])
    gckv = din("gckv", [128, 2])
    w_ukv = din("w_ukv", [256, 1024])
    gret = din("gret", [128, 8])
    w_o_mla = din("w_o_mla", [512, 1024])
    w_o_ret = din("w_o_ret", [1024, 1024])
    w_out = din("w_out", [1024, 1024])
    w_router = din("w_router", [1024, 64])
    brout = din("brout", [128, 64])
    w_eg = din("w_exp_gate", [NEXP, 1024, 256])
    w_eu = din("w_exp_up", [NEXP, 1024, 256])
    w_ed = din("w_exp_down", [NEXP, 256, 1024])
    w_sg = din("w_sh_gate", [1024, 256])
    w_su = din("w_sh_up", [1024, 256])
    w_sd = din("w_sh_down", [256, 1024])
    gfin = din("gfin", [128, 1024])
    identb_d = din("identb", [128, 128], BF16)
    identf_d = din("identf", [128, 128])
    tri_d = din("tri", [128, 128])
    amask_d = din("amask", [128, 4, 128], BF16)
    invf_d = din("invf", [128, 80])
    kdecA_d = din("kdecA", [128, 4, 128])
    kdecC_d = din("kdecC", [128, 4, 128])
    qdec_d = din("qdec", [128, 4, 128])
    rcoef_d = din("rcoef", [128, 16])
    out_d = nc.dram_tensor("out", [NO * 128, D], F32, kind="ExternalOutput").ap()
    sown_d = nc.dram_tensor("sown_s", [NO, 128, 1024], BF16, kind="ExternalOutput").ap()
    bb_d = nc.dram_tensor("bb_s", [NO, 128, 1024], BF16, kind="ExternalOutput").ap()
    x1_d = nc.dram_tensor("x1_s", [NO, 128, 1024], F32, kind="ExternalOutput").ap()
    ot_d = nc.dram_tensor("ot_s", [8, 64, NO * 128], BF16, kind="ExternalOutput").ap()

    c = Ctx(nc)

    identb = c.sbuf("identb", [128, 128], BF16)
    identf = c.sbuf("identf", [128, 128], F32)
    tri = c.sbuf("tri", [128, 128], F32)
    amask = c.sbuf("amask", [128, 4, 128], BF16)
    invf = c.sbuf("invf", [128, 80], F32)
    rcoef = c.sbuf("rcoef", [128, 16], F32)
    nhalf = c.sbuf("nhalf", [128, 64], F32)
    onesf = c.sbuf("onesf", [128, 128], F32)
    AB = c.sbuf("AB", [128, 32], F32)
    GT1 = c.sbuf("GT1", [128, 1024], F32)
    GT2 = c.sbuf("GT2", [128, 1024], F32)
    gfin_t = c.sbuf("gfin_t", [128, 1024], F32)
    brout_t = c.sbuf("brout_t", [128, 64], F32)
    posf = c.sbuf("posf", [128, NT + NO], F32)
    junk = c.sbuf("junk", [128, 1024], BF16)
    comb = c.sbuf("comb", [128, NO, 64], F32)
    B1b = c.sbuf("B1b", [128, 8], BF16)
    onesb = c.sbuf("onesb", [1, 128], BF16)

    for (t_, d_, k_) in [(identb, identb_d, "c0"), (identf, identf_d, "c1"), (tri, tri_d, "c2"),
                         (amask, amask_d, "c3"), (invf, invf_d, "c4"), (rcoef, rcoef_d, "c5"),
                         (gfin_t, gfin, "c6"), (brout_t, brout, "c7")]:
        c.dma("sp", t_[:], d_, writes=["const"], key=k_)
    c.op("pool", lambda e: e.memset(nhalf[:], -0.5), writes=["const"])
    c.op("pool", lambda e: e.memset(onesf[:], 1.0), writes=["const"])
    c.barrier()
    if stop == "c":
        return finish()

    def rsqrt_cols(src_ap, dst_ap, ncols, rd, wr):
        c.op("pool", lambda e: e.tensor_tensor(out=dst_ap, in0=src_ap, in1=nhalf[:, 0:ncols], op=ALU.pow),
             reads=rd, writes=wr)

    c.push()
    PS = [c.psum(f"p0_{i}", [128, 512], F32) for i in range(8)]
    cv = c.sbuf("cv", [128, 8], F32)
    cs = c.sbuf("cs", [128, 8], F32)
    modrow = c.sbuf("modrow", [1, 6 * D], F32)
    brow = c.sbuf("brow", [1, 6 * D], F32)
    gcols = c.sbuf("gcols", [128, 16], F32)
    wab = [c.sbuf(f"wab{i}", [128, 8, 512], F32) for i in range(2)]
    posi = c.sbuf("posi", [128, NT + NO], I32)
    c.dma("sp", cv[:], cvec, writes=["cv"], key="p0a")
    c.dma("sp", brow[:], bada_row, writes=["brow"], key="p0b")
    c.dma("sp", gcols[:, 0:8], g1c, writes=["gcols"], key="p0c")
    c.dma("sp", gcols[:, 8:16], g2c, writes=["gcols"], key="p0d")
    c.dma("sp", posi[:, 0:NT], posall, writes=["posi"], key="p0e")
    c.dma("sp", posi[:, NT:NT + NO], posown, writes=["posi"], key="p0f")
    c.op("dve", lambda e: e.tensor_copy(out=posf[:], in_=posi[:]), reads=["posi"], writes=["posf"])
    c.op("act", lambda e: e.activation(out=cs[:], in_=cv[:], func=AF.Silu), reads=["cv"], writes=["cs"])
    for n in range(12):
        wt = wab[n % 2]
        c.dma("sp", wt[:], w_ada[:, n * 512:(n + 1) * 512].rearrange("(k p) n -> p k n", p=128),
              writes=[c.W("wab", n, 2)], key=f"wab{n % 2}")
        for k in range(8):
            c.op("pe", lambda e: e.matmul(PS[n % 2][0:1, :], lhsT=cs[:, k:k + 1], rhs=wt[:, k, :],
                                          start=(k == 0), stop=(k == 7)),
                 reads=[c.R("wab", n, 2), "cs"], writes=[("pmod", n % 2)])
        c.op("dve", lambda e: e.tensor_tensor(out=modrow[0:1, n * 512:(n + 1) * 512], in0=PS[n % 2][0:1, :],
                                              in1=brow[0:1, n * 512:(n + 1) * 512], op=ALU.add),
             reads=[("pmod", n % 2), "brow"], writes=["modrow"])
    if stop == "0a":
        dbg("modrow", modrow[:], [1, 6 * D], F32)
        c.pop()
        return finish()
    pc = PS[2]
    for idx, ch in enumerate(list(range(0, 16)) + list(range(24, 40))):
        c.op("pe", lambda e: e.matmul(pc[:, idx:idx + 1], lhsT=modrow[0:1, ch * 128:(ch + 1) * 128],
                                      rhs=onesf[0:1, 0:1], start=True, stop=True),
             reads=["modrow", "const"], writes=["pc"])
    c.op("dve", lambda e: e.scalar_tensor_tensor(out=AB[:, 0:8], in0=pc[:, 8:16], scalar=1.0, in1=gcols[:, 0:8],
                                                 op0=ALU.add, op1=ALU.mult), reads=["pc", "gcols"], writes=["AB"])
    c.op("dve", lambda e: e.tensor_copy(out=AB[:, 8:16], in_=pc[:, 0:8]), reads=["pc"], writes=["AB"])
    c.op("dve", lambda e: e.scalar_tensor_tensor(out=AB[:, 16:24], in0=pc[:, 24:32], scalar=1.0, in1=gcols[:, 8:16],
                                                 op0=ALU.add, op1=ALU.mult), reads=["pc", "gcols"], writes=["AB"])
    c.op("dve", lambda e: e.tensor_copy(out=AB[:, 24:32], in_=pc[:, 16:24]), reads=["pc"], writes=["AB"])
    if stop == "0b":
        c.pop()
        dbg("AB", AB[:], [128, 32], F32)
        return finish()
    for (dst, base, scl, pi) in [(GT1, 2048, 0.5, 4), (GT2, 5120, 1.0, 6)]:
        for hh in range(2):
            c.op("pe", lambda e: e.matmul(PS[pi + hh][:], lhsT=onesf[0:1, 0:128],
                                          rhs=modrow[0:1, base + hh * 512: base + (hh + 1) * 512],
                                          start=True, stop=True), reads=["modrow", "const"], writes=[("pgt", pi + hh)])
            c.op("act", lambda e: e.activation(out=dst[:, hh * 512:(hh + 1) * 512], in_=PS[pi + hh][:],
                                               func=AF.Copy, scale=scl), reads=[("pgt", pi + hh)], writes=["GT"])
    c.pop()
    dbg("AB", AB[:], [128, 32], F32)
    dbg("GT1", GT1[:], [128, 1024], F32)
    if stop == "0":
        return finish()

    A1, B1, A2, B2 = AB[:, 0:8], AB[:, 8:16], AB[:, 16:24], AB[:, 24:32]
    c.op("dve", lambda e: e.tensor_copy(out=B1b[:], in_=B1), reads=["AB"], writes=["B1b"])
    c.op("pool", lambda e: e.memset(onesb[:], 1.0), writes=["onesb"])

    def fold_adaln(W, ncols, wname, psb, pkey, tag):
        brow_ = c.sbuf("bw_" + tag, [1, ncols], BF16)
        for lo in range(0, ncols, 512):
            n = min(512, ncols - lo)
            for k in range(8):
                c.op("pe", lambda e: e.matmul(psb[0:1, 0:n], lhsT=B1b[:, k:k + 1], rhs=W[:, k, lo:lo + n], start=(k == 0),
                                              stop=(k == 7)), reads=[wname, "B1b"], writes=[pkey])
            c.op("act", lambda e: e.activation(out=brow_[0:1, lo:lo + n], in_=psb[0:1, 0:n], func=AF.Copy),
                 reads=[pkey], writes=["bw_" + tag])
        for k in range(8):
            c.op("dve", lambda e: e.tensor_scalar(out=W[:, k, :], in0=W[:, k, :], scalar1=A1[:, k:k + 1], scalar2=None,
                                                  op0=ALU.mult), reads=[wname, "AB"], writes=[wname])
        return brow_

    def bias_mm(ps_ap, brow_, lo, n, tag, wr):
        c.op("pe", lambda e: e.matmul(ps_ap, lhsT=onesb[0:1, 0:128], rhs=brow_[0:1, lo:lo + n], start=False, stop=True),
             reads=["onesb", "bw_" + tag], writes=wr)

    def load_w(dst_tile, src_ap, name, key):
        c.dma("pool", dst_tile, src_ap, writes=[name], key=key)

    def rope_tables(pos_ap, G, sinT, cosT, tmp, lo, hi, tag):
        n = hi - lo
        ang, u, r = (tmp[0][:, 0:G, 0:n], tmp[1][:, 0:G, 0:n], tmp[2][:, 0:G, 0:n])
        c.op("dve", lambda e: e.tensor_tensor(out=ang, in0=invf[:, lo:hi].unsqueeze(1).broadcast_to([128, G, n]),
                                              in1=pos_ap.unsqueeze(2).broadcast_to([128, G, n]), op=ALU.mult),
             reads=["posf", "const"], writes=[tag + "t0"])
        c.op("dve", lambda e: e.tensor_scalar(out=u, in0=ang, scalar1=1.0 / TWO_PI, scalar2=MAGIC,
                                              op0=ALU.mult, op1=ALU.add), reads=[tag + "t0"], writes=[tag + "t1"])
        c.op("dve", lambda e: e.tensor_scalar(out=u, in0=u, scalar1=-MAGIC, scalar2=None, op0=ALU.add),
             reads=[tag + "t1"], writes=[tag + "t1"])
        c.op("dve", lambda e: e.scalar_tensor_tensor(out=r, in0=u, scalar=-C1, in1=ang, op0=ALU.mult, op1=ALU.add),
             reads=[tag + "t1", tag + "t0"], writes=[tag + "t2"])
        c.op("dve", lambda e: e.scalar_tensor_tensor(out=r, in0=u, scalar=-C2, in1=r, op0=ALU.mult, op1=ALU.add),
             reads=[tag + "t1", tag + "t2"], writes=[tag + "t2"])
        c.op("dve", lambda e: e.scalar_tensor_tensor(out=r, in0=u, scalar=-C3, in1=r, op0=ALU.mult, op1=ALU.add),
             reads=[tag + "t1", tag + "t2"], writes=[tag + "t2"])
        c.op("dve", lambda e: e.tensor_scalar(out=r, in0=r, scalar1=-math.pi, scalar2=math.pi, op0=ALU.max, op1=ALU.min),
             reads=[tag + "t2"], writes=[tag + "t2"])
        c.op("act", lambda e: e.activation(out=sinT[:, 0:G, lo:hi], in_=r, func=AF.Sin), reads=[tag + "t2"],
             writes=[tag + "sin"])
        c.op("dve", lambda e: e.scalar_tensor_tensor(out=ang, in0=r, scalar=-1.0, in1=r, op0=ALU.mult, op1=ALU.max),
             reads=[tag + "t2"], writes=[tag + "t0"])
        c.op("dve", lambda e: e.tensor_scalar(out=ang, in0=ang, scalar1=-1.0, scalar2=math.pi / 2, op0=ALU.mult,
                                              op1=ALU.add), reads=[tag + "t0"], writes=[tag + "t0"])
        c.op("act", lambda e: e.activation(out=cosT[:, 0:G, lo:hi], in_=ang, func=AF.Sin), reads=[tag + "t0"],
             writes=[tag + "cos"])

    def rope(out4, z4, cos_b, sin_b, t1, t2, eng2, rd, wr, tmpname):
        c.op("dve", lambda e: e.tensor_tensor(out=t1, in0=z4, in1=cos_b, op=ALU.mult), reads=rd, writes=[tmpname + "1"])
        c.op("dve", lambda e: e.tensor_tensor(out=t2, in0=z4, in1=sin_b, op=ALU.mult), reads=rd, writes=[tmpname + "2"])
        c.op(eng2, lambda e: e.tensor_tensor(out=out4[:, :, 0, :], in0=t1[:, :, 0, :], in1=t2[:, :, 1, :], op=ALU.subtract),
             reads=[tmpname + "1", tmpname + "2"], writes=wr)
        c.op(eng2, lambda e: e.tensor_tensor(out=out4[:, :, 1, :], in0=t1[:, :, 1, :], in1=t2[:, :, 0, :], op=ALU.add),
             reads=[tmpname + "1", tmpname + "2"], writes=wr)

    class HT:
        def __init__(self, src, nb, A, B, pT):
            self.src, self.nb, self.A, self.B, self.pT = src, nb, A, B, pT
            self.xt = [c.sbuf("xt", [128, 1024], F32) for _ in range(nb)]
            self.xs = [c.sbuf("xs", [128, 1024], BF16) for _ in range(2)]
            self.hT = [c.sbuf("hT", [128, 8, 128], BF16) for _ in range(2)]
            self.st = c.sbuf("hst", [128, 3 * 64], F32)

        def load(self, t):
            c.dma("sp", self.xt[t % self.nb][:], self.src[t * 128:(t + 1) * 128, :],
                  writes=[c.W("xt", t, self.nb)], key=f"xt{t % self.nb}")

        def norm(self, t):
            xt, st = self.xt[t % self.nb], self.st
            a, b, r = st[:, t % 64:t % 64 + 1], st[:, 64 + t % 64:65 + t % 64], st[:, 128 + t % 64:129 + t % 64]
            c.op("act", lambda e: e.activation(out=junk[:], in_=xt[:], func=AF.Square, accum_out=a),
                 reads=[c.R("xt", t, self.nb)], writes=["junk", ("hsa", t % 64)])
            c.op("dve", lambda e: e.tensor_scalar(out=b, in0=a, scalar1=1.0 / D, scalar2=RMS_EPS, op0=ALU.mult,
                                                  op1=ALU.add), reads=[("hsa", t % 64)], writes=[("hsb", t % 64)])
            rsqrt_cols(b, r, 1, [("hsb", t % 64)], [("hsr", t % 64)])
            c.op("dve", lambda e: e.tensor_scalar(out=self.xs[t % 2][:], in0=xt[:], scalar1=r, scalar2=None,
                                                  op0=ALU.mult), reads=[c.R("xt", t, self.nb), ("hsr", t % 64)],
                 writes=[c.W("xs", t, 2)])

        def transpose(self, t):
            xs, hT, pT = self.xs[t % 2], self.hT[t % 2], self.pT
            pTb = pT[:].bitcast(BF16)
            for k in range(8):
                c.op("pe", lambda e: e.transpose(out=pTb[:, k * 128:(k + 1) * 128], in_=xs[:, k * 128:(k + 1) * 128],
                                                 identity=identb[:]), reads=[c.R("xs", t, 2), "const"], writes=["pT"])
            c.W("hT", t, 2)
            c.op("act", lambda e: e.activation(out=hT[:].rearrange("p k n -> p (k n)"), in_=pTb[:, :], func=AF.Copy),
                 reads=["pT"], writes=[("hT", t % 2)])

        def get(self, t):
            return self.hT[t % 2], c.R("hT", t, 2)

    c.push()
    ckvnT = c.sbuf("ckvnT", [128, 2, S], BF16)
    KT = [c.sbuf(f"KT{i}", [128, S], BF16) for i in range(2)]

    c.push()
    PS = [c.psum(f"pa_{i}", [128, 512], F32) for i in range(8)]
    WA = c.sbuf("WA", [128, 8, 1824], BF16)
    load_w(WA[:, :, 0:288], w_in[:, O_CKV:O_CKV + 288].rearrange("(k p) n -> p k n", p=128), "WA", "wA0")
    load_w(WA[:, :, 288:800], w_in[:, O_RK:O_RK + 512].rearrange("(k p) n -> p k n", p=128), "WA", "wA1")
    load_w(WA[:, :, 800:1824], w_in[:, O_RV:O_RV + 1024].rearrange("(k p) n -> p k n", p=128), "WA", "wA2")
    bwA = fold_adaln(WA, 1824, "WA", PS[1], ("pz", 1), "A")
    kdecA = c.sbuf("kdecA", [128, 4, 128], F32)
    c.dma("sp", kdecA[:], kdecA_d, writes=["kdecA"], key="kdA")
    ht = HT(xall, 3, A1, B1, PS[0])
    sinT = [c.sbuf("sinT", [128, 4, 80], F32) for _ in range(2)]
    cosT = [c.sbuf("cosT", [128, 4, 80], F32) for _ in range(2)]
    rtmp = [c.sbuf("rtmp", [128, 4, 80], F32) for _ in range(3)]
    stA = c.sbuf("stA", [128, 3 * 64], F32)
    ckvs = [c.sbuf("ckvs", [128, 256], BF16) for _ in range(2)]
    kst = [c.sbuf("kst", [128, 96], BF16) for _ in range(2)]
    kr1 = c.sbuf("kr1", [128, 32], F32)
    kr2 = c.sbuf("kr2", [128, 32], F32)
    rk1 = c.sbuf("rk1", [128, 512], F32)
    rk2 = c.sbuf("rk2", [128, 512], F32)
    rk3 = c.sbuf("rk3", [128, 512], F32)
    kp = [c.sbuf("kp", [128, 4, 128], BF16) for _ in range(2)]
    vb = [c.sbuf("vb", [128, 1024], BF16) for _ in range(2)]
    Sst = c.sbuf("Sst", [128, 1024], F32)
    Sown = [c.sbuf("Sown", [128, 1024], F32) for _ in range(2)]
    Sownb = [c.sbuf("Sownb", [128, 1024], BF16) for _ in range(2)]
    for i in range(2):
        c.op("pool", lambda e: e.memset(kst[i][:], 0.0), writes=[("kst", i)])
    c.op("pool", lambda e: e.memset(Sst[:], 0.0), writes=["Sst"])

    def A_s0(t):
        if t == 0:
            ht.load(0)
            ht.load(1)
        if t + 2 < NT:
            ht.load(t + 2)
        if t % 4 == 0:
            g = t // 4
            rope_tables(posf[:, t:t + 4], 4, sinT[g % 2], cosT[g % 2], rtmp, 0, 80, f"rtA{g % 2}")
            c.W("tabA", g, 2)
        ht.norm(t)

    def A_s0b(t):
        ht.transpose(t)

    def A_s1(t):
        hT, hk = ht.get(t)
        for (pi, lo, n) in [(1, 0, 288), (2, 288, 512), (3, 800, 512), (4, 1312, 512)]:
            for k in range(8):
                c.op("pe", lambda e: e.matmul(PS[pi][:, 0:n], lhsT=hT[:, k, :], rhs=WA[:, k, lo:lo + n],
                                              start=(k == 0), stop=False), reads=[hk, "WA"], writes=[("pz", pi)])
            bias_mm(PS[pi][:, 0:n], bwA, lo, n, "A", [("pz", pi)])
        m = t % 64
        a, b, r = stA[:, m:m + 1], stA[:, 64 + m:65 + m], stA[:, 128 + m:129 + m]
        c.op("act", lambda e: e.activation(out=junk[:, 0:256], in_=PS[1][:, 0:256], func=AF.Square, accum_out=a),
             reads=[("pz", 1)], writes=["junk", ("sAa", m)])
        c.op("dve", lambda e: e.tensor_scalar(out=b, in0=a, scalar1=1.0 / 256, scalar2=RMS_EPS, op0=ALU.mult,
                                              op1=ALU.add), reads=[("sAa", m)], writes=[("sAb", m)])
        rsqrt_cols(b, r, 1, [("sAb", m)], [("sAr", m)])
        c.op("dve", lambda e: e.tensor_scalar(out=ckvs[t % 2][:], in0=PS[1][:, 0:256], scalar1=r, scalar2=None,
                                              op0=ALU.mult), reads=[("pz", 1), ("sAr", m)], writes=[c.W("ckvs", t, 2)])
        g = t // 4
        tk = c.R("tabA", g, 2)
        sn, cs_ = sinT[g % 2], cosT[g % 2]
        z4 = PS[1][:, 256:288].rearrange("p (h t d) -> p h t d", h=1, t=2)
        cb = cs_[:, t % 4, 0:16].unsqueeze(1).unsqueeze(1).broadcast_to([128, 1, 2, 16])
        sb = sn[:, t % 4, 0:16].unsqueeze(1).unsqueeze(1).broadcast_to([128, 1, 2, 16])
        o4 = kst[t % 2][:, 64:96].rearrange("p (h t d) -> p h t d", h=1, t=2)
        rope(o4, z4, cb, sb, kr1[:].rearrange("p (h t d) -> p h t d", h=1, t=2),
             kr2[:].rearrange("p (h t d) -> p h t d", h=1, t=2), "pool",
             [("pz", 1), f"rtA{g % 2}sin", f"rtA{g % 2}cos"], [c.W("kst", t, 2)], "kr")
        z4 = PS[2][:].rearrange("p (h t d) -> p h t d", h=4, t=2)
        cb = cs_[:, t % 4, 16:80].unsqueeze(1).unsqueeze(1).broadcast_to([128, 4, 2, 64])
        sb = sn[:, t % 4, 16:80].unsqueeze(1).unsqueeze(1).broadcast_to([128, 4, 2, 64])
        o4 = rk3[:].rearrange("p (h t d) -> p h t d", h=4, t=2)
        rope(o4, z4, cb, sb, rk1[:].rearrange("p (h t d) -> p h t d", h=4, t=2),
             rk2[:].rearrange("p (h t d) -> p h t d", h=4, t=2), "pool",
             [("pz", 2), f"rtA{g % 2}sin", f"rtA{g % 2}cos"], ["rk3"], "rk")
        c.op("pool", lambda e: e.tensor_tensor(out=kp[t % 2][:], in0=rk3[:].rearrange("p (h d) -> p h d", h=4),
                                               in1=kdecA[:], op=ALU.mult), reads=["rk3", "kdecA"],
             writes=[c.W("kp", t, 2)])
        c.W("vb", t, 2)
        for hh in range(2):
            c.op("act", lambda e: e.activation(out=vb[t % 2][:, hh * 512:(hh + 1) * 512], in_=PS[3 + hh][:],
                                               func=AF.Copy), reads=[("pz", 3 + hh)], writes=[("vb", t % 2)])

    import os
    _lv = 9

    def A_s2(t):
        pt = PS[5][:].bitcast(BF16)
        for k in range(2):
            c.op("pe", lambda e: e.transpose(out=pt[:, k * 128:(k + 1) * 128], in_=ckvs[t % 2][:, k * 128:(k + 1) * 128],
                                             identity=identb[:]), reads=[c.R("ckvs", t, 2), "const"], writes=["ptA"])
        c.op("pe", lambda e: e.transpose(out=pt[0:96, 256:384], in_=kst[t % 2][:], identity=identb[:]),
             reads=[c.R("kst", t, 2), "const"], writes=["ptA"])
        if _lv < 2:
            return
        for k in range(2):
            c.op("act", lambda e: e.activation(out=ckvnT[:, k, t * 128:(t + 1) * 128],
                                               in_=pt[:, k * 128:(k + 1) * 128], func=AF.Copy),
                 reads=["ptA"], writes=["ckvnT"])
        if _lv < 3:
            return
        _kt = "both"
        if _kt in ("both", "act"):
            c.op("act", lambda e: e.activation(out=KT[0][64:96, t * 128:(t + 1) * 128], in_=pt[64:96, 256:384],
                                               func=AF.Copy), reads=["ptA"], writes=["KT0r"])
        if _kt in ("both", "dve"):
            c.op("act", lambda e: e.activation(out=KT[1][64:96, t * 128:(t + 1) * 128], in_=pt[64:96, 256:384],
                                               func=AF.Copy), reads=["ptA"], writes=["KT1r"])
        if _lv < 4:
            return
        pkv = [PS[6], PS[7]]
        for h in range(4):
            c.op("pe", lambda e: e.matmul(pkv[h // 2][:, (h % 2) * 256:(h % 2 + 1) * 256], lhsT=kp[t % 2][:, h, :],
                                          rhs=vb[t % 2][:, h * 256:(h + 1) * 256], start=True, stop=True),
                 reads=[c.R("kp", t, 2), c.R("vb", t, 2)], writes=["pkv"])
        if _lv < 5:
            return
        l, g = t % 4, t // 4
        so = Sown[g % 2]
        if l == 0:
            c.W("Sown", g, 2)
        for h in range(4):
            hs = slice(h * 256, (h + 1) * 256)
            pk = pkv[h // 2][:, (h % 2) * 256:(h % 2 + 1) * 256]
            if l == 0:
                c.op("act", lambda e: e.activation(out=so[:, hs], in_=Sst[:, hs], func=AF.Copy,
                                                   scale=rcoef[:, h * 4:h * 4 + 1]), reads=["Sst", "const"],
                     writes=[("Sown", g % 2)])
            if l < 3:
                c.op("dve", lambda e: e.scalar_tensor_tensor(out=so[:, hs], in0=pk, scalar=rcoef[:, h * 4 + 1 + l:h * 4 + 2 + l],
                                                             in1=so[:, hs], op0=ALU.mult, op1=ALU.add),
                     reads=["pkv", ("Sown", g % 2), "const"], writes=[("Sown", g % 2)])
            c.op("dve", lambda e: e.scalar_tensor_tensor(out=Sst[:, hs], in0=Sst[:, hs], scalar=GAMMA[h] ** 128, in1=pk,
                                                         op0=ALU.mult, op1=ALU.add), reads=["pkv", "Sst"], writes=["Sst"])
        if l == 3 and _lv >= 6:
            c.op("act", lambda e: e.activation(out=Sownb[g % 2][:], in_=so[:], func=AF.Copy),
                 reads=[c.R("Sown", g, 2)], writes=[c.W("Sownb", g, 2)])
            c.dma("sp", sown_d[g], Sownb[g % 2][:], reads=[c.R("Sownb", g, 2)], writes=["sown_d"], key=f"so{g % 2}")

    import os
    _na = NT
    _ns = 3
    pipeline(_na, [A_s0, A_s0b, A_s1, A_s2][:_ns + 1])
    print("sbuf remaining in pass A:", nc.sbuf_bytes_remaining, "ops", c.nops)
    dbg("Sst", Sst[:], [128, 1024], F32)
    c.pop()
    dbg("ckvnT", ckvnT[:], [128, 2, S], BF16)
    dbg("KT0", KT[0][:], [128, S], BF16)
    if stop == "A":
        return finish()

    QT = c.sbuf("QT", [128, 8, NO * 128], BF16)
    c.push()
    PS = [c.psum(f"pq_{i}", [128, 512], F32) for i in range(8)]
    WQ = c.sbuf("WQ", [128, 8, 384], BF16)
    load_w(WQ[:], w_in[:, O_CQ:O_CQ + 384].rearrange("(k p) n -> p k n", p=128), "WQ", "wQ0")
    bwQ = fold_adaln(WQ, 384, "WQ", PS[1], "pcq", "Q")
    wuq = c.sbuf("wuq", [128, 3, 768], BF16)
    c.push()
    wuq_f = c.sbuf("wuq_f", [128, 3, 768], F32)
    gq = c.sbuf("gq", [128, 3], F32)
    c.dma("sp", wuq_f[:], w_uq.rearrange("(k p) n -> p k n", p=128), writes=["wuq_f"], key="wQ1")
    c.dma("sp", gq[:], gcq, writes=["gq"], key="wQ2")
    for k in range(3):
        wv = wuq_f[:, k, :].rearrange("p (h d) -> p h d", h=8)
        c.op("dve", lambda e: e.tensor_scalar(out=wuq[:, k, 0:512].rearrange("p (h d) -> p h d", h=8), in0=wv[:, :, 0:64],
                                              scalar1=gq[:, k:k + 1], scalar2=None, op0=ALU.mult),
             reads=["wuq_f", "gq"], writes=["wuq"])
        c.op("dve", lambda e: e.tensor_scalar(out=wuq[:, k, 512:768].rearrange("p (h d) -> p h d", h=8), in0=wv[:, :, 64:96],
                                              scalar1=gq[:, k:k + 1], scalar2=None, op0=ALU.mult),
             reads=["wuq_f", "gq"], writes=["wuq"])
    c.pop()
    ht = HT(xown, 3, A1, B1, PS[0])
    sinQ = c.sbuf("sinQ", [128, NO, 16], F32)
    cosQ = c.sbuf("cosQ", [128, NO, 16], F32)
    c.push()
    rtmpQ = [c.sbuf("rtmpQ", [128, NO, 16], F32) for _ in range(3)]
    rope_tables(posf[:, NT:NT + NO], NO, sinQ, cosQ, rtmpQ, 0, 16, "rtQ")
    c.pop()
    stQ = c.sbuf("stQ", [128, 3 * 64], F32)
    cqs = [c.sbuf("cqs", [128, 384], BF16) for _ in range(2)]
    cqnT = [c.sbuf("cqnT", [128, 3, 128], BF16) for _ in range(2)]
    qsb = [c.sbuf("qsb", [128, 8, 96], BF16) for _ in range(2)]
    qr1 = c.sbuf("qr1", [128, 8, 32], F32)
    qr2 = c.sbuf("qr2", [128, 8, 32], F32)

    def Q_s0(t):
        if t == 0:
            ht.load(0)
            ht.load(1)
        if t + 2 < NO:
            ht.load(t + 2)
        ht.norm(t)

    def Q_s0b(t):
        ht.transpose(t)

    def Q_s1(t):
        hT, hk = ht.get(t)
        for k in range(8):
            c.op("pe", lambda e: e.matmul(PS[1][:, 0:384], lhsT=hT[:, k, :], rhs=WQ[:, k, :], start=(k == 0),
                                          stop=False), reads=[hk, "WQ"], writes=["pcq"])
        bias_mm(PS[1][:, 0:384], bwQ, 0, 384, "Q", ["pcq"])
        m = t
        a, b, r = stQ[:, m:m + 1], stQ[:, 64 + m:65 + m], stQ[:, 128 + m:129 + m]
        c.op("act", lambda e: e.activation(out=junk[:, 0:384], in_=PS[1][:, 0:384], func=AF.Square, accum_out=a),
             reads=["pcq"], writes=["junk", ("sQa", m)])
        c.op("dve", lambda e: e.tensor_scalar(out=b, in0=a, scalar1=1.0 / 384, scalar2=RMS_EPS, op0=ALU.mult,
                                              op1=ALU.add), reads=[("sQa", m)], writes=[("sQb", m)])
        rsqrt_cols(b, r, 1, [("sQb", m)], [("sQr", m)])
        c.op("dve", lambda e: e.tensor_scalar(out=cqs[t % 2][:], in0=PS[1][:, 0:384], scalar1=r, scalar2=None,
                                              op0=ALU.mult), reads=["pcq", ("sQr", m)], writes=[c.W("cqs", t, 2)])
        pt = PS[2][:].bitcast(BF16)
        for k in range(3):
            c.op("pe", lambda e: e.transpose(out=pt[:, k * 128:(k + 1) * 128], in_=cqs[t % 2][:, k * 128:(k + 1) * 128],
                                             identity=identb[:]), reads=[c.R("cqs", t, 2), "const"], writes=["ptQ"])
        c.op("act", lambda e: e.activation(out=cqnT[t % 2][:], in_=pt[:, 0:384].rearrange("p (k n) -> p k n", k=3),
                                           func=AF.Copy), reads=["ptQ"], writes=[c.W("cqnT", t, 2)])

    def Q_s2(t):
        for (pi, lo, n) in [(3, 0, 512), (4, 512, 256)]:
            for k in range(3):
                c.op("pe", lambda e: e.matmul(PS[pi][:, 0:n], lhsT=cqnT[t % 2][:, k, :], rhs=wuq[:, k, lo:lo + n],
                                              start=(k == 0), stop=(k == 2)),
                     reads=[c.R("cqnT", t, 2), "wuq"], writes=[("pq", pi)])
        c.W("qsb", t, 2)
        _ql = 9
        c.op("act", lambda e: e.activation(out=qsb[t % 2][:, :, 0:64], in_=PS[3][:].rearrange("p (h d) -> p h d", h=8),
                                           func=AF.Copy), reads=[("pq", 3)], writes=[("qsb", t % 2)])
        z4 = PS[4][:, 0:256].rearrange("p (h t d) -> p h t d", h=8, t=2)
        cb = cosQ[:, t, 0:16].unsqueeze(1).unsqueeze(1).broadcast_to([128, 8, 2, 16])
        sb = sinQ[:, t, 0:16].unsqueeze(1).unsqueeze(1).broadcast_to([128, 8, 2, 16])
        o4 = qsb[t % 2][:, :, 64:96].rearrange("p h (t d) -> p h t d", t=2)
        rope(o4, z4, cb, sb, qr1[:].rearrange("p h (t d) -> p h t d", t=2),
             qr2[:].rearrange("p h (t d) -> p h t d", t=2), "pool",
             [("pq", 4), "rtQsin", "rtQcos"], [("qsb", t % 2)], "qr")
        if _ql < 3:
            return
        pt = PS[5][:].bitcast(BF16)
        for h in range(8):
            c.op("pe", lambda e: e.transpose(out=pt[0:96, h * 128:(h + 1) * 128], in_=qsb[t % 2][:, h, :],
                                             identity=identb[:]), reads=[("qsb", t % 2), "const"], writes=["ptQ2"])
        if _ql < 4:
            return
        c.op("act", lambda e: e.activation(out=QT[0:96, :, t * 128:(t + 1) * 128],
                                           in_=pt[0:96, :].rearrange("p (h n) -> p h n", h=8), func=AF.Copy),
             reads=["ptQ2"], writes=["QT"])

    pipeline(NO, [Q_s0, Q_s0b, Q_s1, Q_s2])
    print("sbuf remaining in pass Q:", nc.sbuf_bytes_remaining, "ops", c.nops)
    c.pop()
    dbg("QT", QT[:], [128, 8, NO * 128], BF16)
    if stop == "Q":
        return finish()

    c.push()
    PS = [c.psum(f"pt_{i}", [128, 512], F32) for i in range(8)]
    wukv = c.sbuf("wukv", [128, 2, 1024], BF16)
    c.push()
    wukv_f = c.sbuf("wukv_f", [128, 2, 1024], F32)
    gkv = c.sbuf("gkv", [128, 2], F32)
    c.dma("sp", wukv_f[:], w_ukv.rearrange("(k p) n -> p k n", p=128), writes=["wukv_f"], key="wT0")
    c.dma("sp", gkv[:], gckv, writes=["gkv"], key="wT1")
    for k in range(2):
        c.op("dve", lambda e: e.tensor_scalar(out=wukv[:, k, :], in0=wukv_f[:, k, :], scalar1=gkv[:, k:k + 1],
                                              scalar2=None, op0=ALU.mult), reads=["wukv_f", "gkv"], writes=["wukv"])
    c.pop()
    otb = [c.sbuf("otb", [64, 512], BF16) for _ in range(2)]
    Vb = [c.sbuf("Vb", [128, NT, 128], BF16) for _ in range(2)]
    for i in range(2):
        c.op("pool", lambda e: e.memset(Vb[i][:, :, 64:128], 0.0), writes=[("Vb1", i)])
        c.op("pool", lambda e: e.memset(Vb[i][:, :, 64:65], 1.0), writes=[("Vb1", i)])
    PT = [c.sbuf("PT", [128, 512], BF16) for _ in range(4)]
    osb = [c.sbuf("osb", [65, 512], F32) for _ in range(2)]
    rec = [c.sbuf("rec", [64, 512], F32) for _ in range(2)]
    fin = [0]

    def up_units(h):
        kt_buf, v_buf = KT[h % 2], Vb[h % 2]
        units = []

        def k_unit(kc):
            def f():
                pk = PS[kc % 2]
                for k in range(2):
                    c.op("pe", lambda e: e.matmul(pk[0:64, :], lhsT=wukv[:, k, h * 128:h * 128 + 64],
                                                  rhs=ckvnT[:, k, kc * 512:(kc + 1) * 512], start=(k == 0), stop=(k == 1)),
                         reads=["wukv", "ckvnT"], writes=[("pk", kc % 2)])
                c.op("dve", lambda e: e.tensor_copy(out=kt_buf[0:64, kc * 512:(kc + 1) * 512], in_=pk[0:64, :]),
                     reads=[("pk", kc % 2)], writes=[("KTn", h % 2)])
            return f

        def v_unit(kb):
            def f():
                pv = PS[kb % 2]
                for j8 in range(8):
                    kt = kb * 8 + j8
                    for k in range(2):
                        c.op("pe", lambda e: e.matmul(pv[:, j8 * 64:(j8 + 1) * 64], lhsT=ckvnT[:, k, kt * 128:(kt + 1) * 128],
                                                      rhs=wukv[:, k, h * 128 + 64:h * 128 + 128], start=(k == 0),
                                                      stop=(k == 1)), reads=["wukv", "ckvnT"], writes=[("pk", kb % 2)])
                c.op("dve", lambda e: e.tensor_copy(out=v_buf[:, kb * 8:(kb + 1) * 8, 0:64],
                                                    in_=pv[:].rearrange("p (j d) -> p j d", j=8)),
                     reads=[("pk", kb % 2)], writes=[("Vb", h % 2)])
            return f

        for kc in range(16):
            units.append(k_unit(kc))
        for kb in range(8):
            units.append(v_unit(kb))
        return units

    steps = [(h, qc, kt) for h in range(8) for qc in range(4) for kt in range(16 * qc + 16)]
    pending = {}
    c.W("KTn", 0, 2)
    c.W("Vb", 0, 2)
    for u in up_units(0):
        u()

    def att_s0(i):
        h, qc, kt = steps[i]
        kt_buf = KT[h % 2]
        if qc == 0 and kt == 0 and h + 1 < 8:
            pending["units"] = up_units(h + 1)
            pending["local"] = 0
            pending["armed"] = False
        if pending.get("units"):
            pending["local"] += 1
            if pending["local"] >= 4 and (pending["local"] - 4) % 6 == 0:
                if not pending["armed"]:
                    c.W("KTn", h + 1, 2)
                    c.W("Vb", h + 1, 2)
                    pending["armed"] = True
                pending["units"].pop(0)()
        gk, l = kt // 4, kt % 4
        c0 = (max(gk, 4 * qc) - 4 * qc) * 128
        ps, pt_ = PS[2 + i % 4], PT[i % 4]
        c.op("pe", lambda e: e.matmul(ps[:, c0:512], lhsT=kt_buf[0:96, kt * 128:(kt + 1) * 128],
                                      rhs=QT[0:96, h, qc * 512 + c0:(qc + 1) * 512], start=True, stop=True),
             reads=[("KTn", h % 2), f"KT{h % 2}r", "QT"], writes=[("ps", i % 4)])
        c.op("act", lambda e: e.activation(out=pt_[:, c0:512], in_=ps[:, c0:512], func=AF.Exp, scale=SCALE_MLA),
             reads=[("ps", i % 4)], writes=[("PT", i % 4)])
        if gk >= 4 * qc:
            c.op("pool", lambda e: e.tensor_tensor(out=pt_[:, c0:c0 + 128], in0=pt_[:, c0:c0 + 128],
                                                   in1=amask[:, l, :], op=ALU.mult),
                 reads=[("PT", i % 4), "const"], writes=[("PT", i % 4)])

    def att_s1(i):
        pass

    def att_s2(i):
        h, qc, kt = steps[i]
        v_buf = Vb[h % 2]
        nk = 16 * qc + 16
        gk = kt // 4
        c0 = (max(gk, 4 * qc) - 4 * qc) * 128
        po = PS[6 + qc % 2]
        pt_ = PT[i % 4]
        c.op("pe", lambda e: e.matmul(po[:, c0:512], lhsT=v_buf[:, kt, :], rhs=pt_[:, c0:512],
                                      start=(kt == 0), stop=(kt == nk - 1)),
             reads=[("Vb", h % 2), ("Vb1", h % 2), ("PT", i % 4)], writes=[("po", qc % 2)])
        if kt == nk - 1:
            f = fin[0]
            fin[0] += 1
            ob, rc = osb[f % 2], rec[f % 2]
            c.op("dve", lambda e: e.tensor_copy(out=ob[:], in_=po[0:65, :]), reads=[("po", qc % 2)],
                 writes=[("osb", f % 2)])
            pd = PS[f % 2]
            c.op("pe", lambda e: e.matmul(pd[0:64, :], lhsT=onesf[64:65, 0:64], rhs=ob[64:65, :], start=True, stop=True),
                 reads=[("osb", f % 2), "const"], writes=[("pk", f % 2)])
            c.op("dve", lambda e: e.reciprocal(out=rc[:], in_=pd[0:64, :]), reads=[("pk", f % 2)], writes=[("rec", f % 2)])
            c.op("dve", lambda e: e.tensor_tensor(out=otb[f % 2][:], in0=ob[0:64, :], in1=rc[:],
                                                  op=ALU.mult), reads=[("osb", f % 2), ("rec", f % 2)], writes=[("otb", f % 2)])
            c.dma("sp", ot_d[h, :, qc * 512:(qc + 1) * 512], otb[f % 2][:], reads=[("otb", f % 2)], writes=["ot_d"],
                  key=f"otw{f % 2}")

    pipeline(len(steps), [att_s0, att_s1, att_s1, att_s2])
    c.pop()
    c.pop()
    if stop == "T":
        return finish()

    c.push()
    PS = [c.psum(f"pc_{i}", [128, 512], F32) for i in range(8)]
    WC = c.sbuf("WC", [128, 8, 3072], BF16)
    load_w(WC[:, :, 0:1024], w_in[:, O_RQ:O_RQ + 1024].rearrange("(k p) n -> p k n", p=128), "WC", "wC0")
    load_w(WC[:, :, 1024:2048], w_in[:, O_RV:O_RV + 1024].rearrange("(k p) n -> p k n", p=128), "WC", "wC1")
    load_w(WC[:, :, 2048:3072], w_in[:, O_RG:O_RG + 1024].rearrange("(k p) n -> p k n", p=128), "WC", "wC2")
    bwC = fold_adaln(WC, 3072, "WC", PS[1], ("pz", 1), "C")
    wor = c.sbuf("wor", [128, 8, 1024], BF16)
    c.push()
    wor_f = c.sbuf("wor_f", [128, 8, 1024], F32)
    gr = c.sbuf("gr", [128, 8], F32)
    c.dma("sp", wor_f[:], w_o_ret.rearrange("(k p) n -> p k n", p=128), writes=["wor_f"], key="wC3")
    c.dma("sp", gr[:], gret, writes=["gr"], key="wC4")
    for k in range(8):
        c.op("dve", lambda e: e.tensor_scalar(out=wor[:, k, :], in0=wor_f[:, k, :], scalar1=gr[:, k:k + 1],
                                              scalar2=None, op0=ALU.mult), reads=["wor_f", "gr"], writes=["wor"])
    c.pop()
    kdecC = c.sbuf("kdecC", [128, 8, 128], F32)
    c.dma("sp", kdecC[:, 0:4, :], qdec_d, writes=["kdecC"], key="wC5")
    c.dma("sp", kdecC[:, 4:8, :], kdecC_d, writes=["kdecC"], key="wC6")
    ht = HT(xown, 3, A1, B1, PS[0])
    sinC = c.sbuf("sinC", [128, NO, 80], F32)
    cosC = c.sbuf("cosC", [128, NO, 80], F32)
    c.push()
    rtmpC = [c.sbuf("rtmpC", [128, NO, 64], F32) for _ in range(3)]
    rope_tables(posf[:, NT:NT + NO], NO, sinC, cosC, rtmpC, 16, 80, "rtC")
    c.pop()
    qk1 = c.sbuf("qk1", [128, 1024], F32)
    qk2 = c.sbuf("qk2", [128, 1024], F32)
    qk3 = c.sbuf("qk3", [128, 1024], F32)
    qkp = [c.sbuf("qkp", [128, 8, 128], BF16) for _ in range(2)]
    qkT = [c.sbuf("qkT", [128, 8, 128], BF16) for _ in range(2)]
    vbc = [c.sbuf("vbc", [128, 1024], BF16) for _ in range(2)]
    sg = [c.sbuf("sg", [128, 1024], BF16) for _ in range(2)]
    scT = [c.sbuf("scT", [128, 4, 128], BF16) for _ in range(2)]
    sob = [c.sbuf("sob", [128, 1024], BF16) for _ in range(2)]
    bnst = c.sbuf("bnst", [128, NO, 4, 6], F32)
    bnag = c.sbuf("bnag", [128, NO, 4, 2], F32)
    bnr = c.sbuf("bnr", [128, NO, 4, 2], F32)
    onr = [c.sbuf("onr", [128, 1024], F32) for _ in range(2)]
    gat = [c.sbuf("gat", [128, 1024], BF16) for _ in range(2)]
    gT = [c.sbuf("gT", [128, 8, 128], BF16) for _ in range(2)]
    bbt = [c.sbuf("bbt", [128, 1024], BF16) for _ in range(2)]

    def C1_s0(t):
        if t == 0:
            ht.load(0)
            ht.load(1)
        if t + 2 < NO:
            ht.load(t + 2)
        ht.norm(t)

    def C1_s0b(t):
        ht.transpose(t)

    def C1_s1(t):
        hT, hk = ht.get(t)
        c.dma("sp", sob[t % 2][:], sown_d[t], reads=[], writes=[c.W("sob", t, 2)], key=f"sob{t % 2}")
        for (pi, lo) in [(1, 0), (2, 512), (5, 2048), (6, 2560), (3, 1024), (4, 1536)]:
            for k in range(8):
                c.op("pe", lambda e: e.matmul(PS[pi][:], lhsT=hT[:, k, :], rhs=WC[:, k, lo:lo + 512], start=(k == 0),
                                              stop=False), reads=[hk, "WC"], writes=[("pz", pi)])
            bias_mm(PS[pi][:], bwC, lo, 512, "C", [("pz", pi)])
        cb = cosC[:, t, 16:80].unsqueeze(1).unsqueeze(1).broadcast_to([128, 4, 2, 64])
        sb = sinC[:, t, 16:80].unsqueeze(1).unsqueeze(1).broadcast_to([128, 4, 2, 64])
        for j in range(2):
            z4 = PS[1 + j][:].rearrange("p (h t d) -> p h t d", h=4, t=2)
            sl = slice(j * 512, (j + 1) * 512)
            rope(qk3[:, sl].rearrange("p (h t d) -> p h t d", h=4, t=2), z4, cb, sb,
                 qk1[:, sl].rearrange("p (h t d) -> p h t d", h=4, t=2),
                 qk2[:, sl].rearrange("p (h t d) -> p h t d", h=4, t=2), "pool",
                 [("pz", 1 + j), "rtCsin", "rtCcos"], [("qk3", j)], f"qk{j}")
        c.op("pool", lambda e: e.tensor_tensor(out=qkp[t % 2][:], in0=qk3[:].rearrange("p (h d) -> p h d", h=8),
                                               in1=kdecC[:], op=ALU.mult), reads=[("qk3", 0), ("qk3", 1), "kdecC"],
             writes=[c.W("qkp", t, 2)])
        c.W("vbc", t, 2)
        c.W("sg", t, 2)
        for hh in range(2):
            c.op("act", lambda e: e.activation(out=vbc[t % 2][:, hh * 512:(hh + 1) * 512], in_=PS[3 + hh][:],
                                               func=AF.Copy), reads=[("pz", 3 + hh)], writes=[("vbc", t % 2)])
            c.op("act", lambda e: e.activation(out=sg[t % 2][:, hh * 512:(hh + 1) * 512], in_=PS[5 + hh][:],
                                               func=AF.Silu), reads=[("pz", 5 + hh)], writes=[("sg", t % 2)])

    def C1_s2(t):
        pt = PS[7][:].bitcast(BF16)
        for j in range(8):
            c.op("pe", lambda e: e.transpose(out=pt[:, j * 128:(j + 1) * 128], in_=qkp[t % 2][:, j, :],
                                             identity=identb[:]), reads=[c.R("qkp", t, 2), "const"], writes=["ptC"])
        c.op("act", lambda e: e.activation(out=qkT[t % 2][:], in_=pt.rearrange("p (j n) -> p j n", j=8), func=AF.Copy),
             reads=["ptC"], writes=[c.W("qkT", t, 2)])
        for h in range(4):
            c.op("pe", lambda e: e.matmul(PS[1][:, h * 128:(h + 1) * 128], lhsT=qkT[t % 2][:, 4 + h, :],
                                          rhs=qkT[t % 2][:, h, :], start=True, stop=True),
                 reads=[c.R("qkT", t, 2)], writes=[("pz", 1)])
        c.op("dve", lambda e: e.tensor_tensor(out=scT[t % 2][:], in0=PS[1][:].rearrange("p (h n) -> p h n", h=4),
                                              in1=tri[:].unsqueeze(1).broadcast_to([128, 4, 128]), op=ALU.mult),
             reads=[("pz", 1), "const"], writes=[c.W("scT", t, 2)])
        for h in range(4):
            po = PS[3 + h // 2][:, (h % 2) * 256:(h % 2 + 1) * 256]
            c.op("pe", lambda e: e.matmul(po, lhsT=scT[t % 2][:, h, :], rhs=vbc[t % 2][:, h * 256:(h + 1) * 256],
                                          start=True, stop=False), reads=[c.R("scT", t, 2), c.R("vbc", t, 2)],
                 writes=[("pz", 3 + h // 2)])
            c.op("pe", lambda e: e.matmul(po, lhsT=qkT[t % 2][:, h, :], rhs=sob[t % 2][:, h * 256:(h + 1) * 256],
                                          start=False, stop=True), reads=[c.R("qkT", t, 2), c.R("sob", t, 2)],
                 writes=[("pz", 3 + h // 2)])
        for h in range(4):
            po = PS[3 + h // 2][:, (h % 2) * 256:(h % 2 + 1) * 256]
            c.op("dve", lambda e: e.bn_stats(out=bnst[:, t, h, :], in_=po), reads=[("pz", 3 + h // 2)],
                 writes=[("bnst", t)])
            c.op("dve", lambda e: e.bn_aggr(out=bnag[:, t, h, :], in_=bnst[:, t, h, :]), reads=[("bnst", t)],
                 writes=[("bnag", t)])
        c.op("dve", lambda e: e.tensor_scalar(out=bnr[:, t, :, 0], in0=bnag[:, t, :, 1], scalar1=GN_EPS, scalar2=None,
                                              op0=ALU.add), reads=[("bnag", t)], writes=[("bnr0", t)])
        rsqrt_cols(bnr[:, t, :, 0], bnr[:, t, :, 1], 4, [("bnr0", t)], [("bnr1", t)])
        c.W("onr", t, 2)
        for h in range(4):
            po = PS[3 + h // 2][:, (h % 2) * 256:(h % 2 + 1) * 256]
            c.op("dve", lambda e: e.tensor_scalar(out=onr[t % 2][:, h * 256:(h + 1) * 256], in0=po,
                                                  scalar1=bnag[:, t, h, 0:1], scalar2=bnr[:, t, h, 1:2],
                                                  op0=ALU.subtract, op1=ALU.mult),
                 reads=[("pz", 3 + h // 2), ("bnag", t), ("bnr1", t)], writes=[("onr", t % 2)])
        c.op("pool", lambda e: e.tensor_tensor(out=gat[t % 2][:], in0=onr[t % 2][:], in1=sg[t % 2][:], op=ALU.mult),
             reads=[("onr", t % 2), c.R("sg", t, 2)], writes=[c.W("gat", t, 2)])

    def C1_s3(t):
        pt = PS[7][:].bitcast(BF16)
        for k in range(8):
            c.op("pe", lambda e: e.transpose(out=pt[:, k * 128:(k + 1) * 128], in_=gat[t % 2][:, k * 128:(k + 1) * 128],
                                             identity=identb[:]), reads=[c.R("gat", t, 2), "const"], writes=["ptC"])
        c.op("act", lambda e: e.activation(out=gT[t % 2][:], in_=pt.rearrange("p (j n) -> p j n", j=8), func=AF.Copy),
             reads=["ptC"], writes=[c.W("gT", t, 2)])
        c.W("bbt", t, 2)
        for hh in range(2):
            for k in range(8):
                c.op("pe", lambda e: e.matmul(PS[5 + hh][:], lhsT=gT[t % 2][:, k, :], rhs=wor[:, k, hh * 512:(hh + 1) * 512],
                                              start=(k == 0), stop=(k == 7)), reads=[c.R("gT", t, 2), "wor"],
                     writes=[("pz", 5 + hh)])
            c.op("act", lambda e: e.activation(out=bbt[t % 2][:, hh * 512:(hh + 1) * 512], in_=PS[5 + hh][:],
                                               func=AF.Copy), reads=[("pz", 5 + hh)], writes=[("bbt", t % 2)])
        c.dma("sp", bb_d[t], bbt[t % 2][:], reads=[("bbt", t % 2)], writes=["bb_d"], key=f"bbw{t % 2}")

    def C1_s123(t):
        C1_s1(t)
        C1_s2(t)
        C1_s3(t)

    C1_s0(0)
    C1_s0(1)
    C1_s0b(0)
    C1_s0(2)
    C1_s0b(1)
    C1_s1(0)
    for t in range(NO):
        C1_s2(t)
        if t + 3 < NO:
            C1_s0(t + 3)
        if t + 2 < NO:
            C1_s0b(t + 2)
        if t + 1 < NO:
            C1_s1(t + 1)
        C1_s3(t)
    print("sbuf remaining in pass C1:", nc.sbuf_bytes_remaining, "ops", c.nops)
    c.pop()
    if stop == "C1":
        return finish()

    h2T = c.sbuf("h2T", [128, 8, NO * 128], BF16)
    print("sbuf remaining before C2:", nc.sbuf_bytes_remaining)
    c.push()
    PS = [c.psum(f"pd_{i}", [128, 512], F32) for i in range(8)]
    WG = c.sbuf("WG", [128, 8, 2048], BF16)
    load_w(WG[:], w_in[:, O_GA:O_GA + 2048].rearrange("(k p) n -> p k n", p=128), "WG", "wD0")
    bwG = fold_adaln(WG, 2048, "WG", PS[1], ("pz", 1), "G")
    wom = c.sbuf("wom", [64, 8, 1024], BF16)
    load_w(wom[:], w_o_mla.rearrange("(h p) n -> p h n", p=64), "wom", "wD1")
    wout = c.sbuf("wout", [128, 8, 1024], BF16)
    load_w(wout[:], w_out.rearrange("(k p) n -> p k n", p=128), "wout", "wD2")
    wr = c.sbuf("wr", [128, 8, 64], F32)
    c.dma("sp", wr[:], w_router.rearrange("(k p) n -> p k n", p=128), writes=["wr"], key="wD3")
    ht = HT(xown, 4, A1, B1, PS[0])
    tg = [c.sbuf("tg", [128, 2048], BF16)] * 2
    ott = [c.sbuf("ott", [64, 8, 128], BF16) for _ in range(2)]
    bbr = [c.sbuf("bbr", [128, 1024], BF16) for _ in range(2)]
    m1 = c.sbuf("m1", [128, 1024], F32)
    m2 = c.sbuf("m2", [128, 1024], F32)
    mg = [c.sbuf("mg", [128, 1024], BF16) for _ in range(2)]
    mgT = [c.sbuf("mgT", [128, 8, 128], BF16) for _ in range(2)]
    ty = m1
    x1 = [c.sbuf("x1", [128, 1024], F32) for _ in range(2)]
    xs2 = [c.sbuf("xs2", [128, 1024], F32) for _ in range(2)]
    h2f = [c.sbuf("h2f", [128, 8, 128], F32) for _ in range(2)]
    st2 = c.sbuf("st2", [128, 3 * 64], F32)
    rt = c.sbuf("rt", [128, 2, 64 * 4 + 64 + 8 * 4], F32)

    def C2_s0(t):
        if t == 0:
            ht.load(0)
            ht.load(1)
        if t + 2 < NO:
            ht.load(t + 2)
        c.dma("sp", bbr[t % 2][:], bb_d[t], reads=["bb_d"], writes=[c.W("bbr", t, 2)], key=f"bbr{t % 2}")
        c.dma("sp", ott[t % 2][:], ot_d[:, :, t * 128:(t + 1) * 128].rearrange("h p n -> p h n"), reads=["ot_d"],
              writes=[c.W("ott", t, 2)], key=f"ott{t % 2}")
        ht.norm(t)

    def C2_s0b(t):
        ht.transpose(t)

    def C2_s1(t):
        hT, hk = ht.get(t)
        xt = ht.xt[t % 4]
        for j in range(4):
            for k in range(8):
                c.op("pe", lambda e: e.matmul(PS[1 + j][:], lhsT=hT[:, k, :], rhs=WG[:, k, j * 512:(j + 1) * 512],
                                              start=(k == 0), stop=False), reads=[hk, "WG"], writes=[("pz", 1 + j)])
            bias_mm(PS[1 + j][:], bwG, j * 512, 512, "G", [("pz", 1 + j)])
            c.op("act", lambda e: e.activation(out=tg[t % 2][:, j * 512:(j + 1) * 512], in_=PS[1 + j][:], func=AF.Tanh,
                                               scale=0.5), reads=[("pz", 1 + j)], writes=["tg"])
    def C2_s1b(t):
        xt = ht.xt[t % 4]
        for hh in range(2):
            for h in range(8):
                c.op("pe", lambda e: e.matmul(PS[5 + hh][:], lhsT=ott[t % 2][:, h, :],
                                              rhs=wom[:, h, hh * 512:(hh + 1) * 512], start=(h == 0), stop=(h == 7)),
                     reads=[c.R("ott", t, 2), "wom"], writes=[("pz", 5 + hh)])
            sl = slice(hh * 512, (hh + 1) * 512)
            c.op("dve", lambda e: e.scalar_tensor_tensor(out=m1[:, sl], in0=tg[t % 2][:, sl], scalar=1.0, in1=PS[5 + hh][:],
                                                         op0=ALU.add, op1=ALU.mult),
                 reads=["tg", ("pz", 5 + hh)], writes=[("m1", hh)])
        c.op("dve", lambda e: e.scalar_tensor_tensor(out=m2[:], in0=tg[t % 2][:, 1024:2048], scalar=1.0, in1=bbr[t % 2][:],
                                                     op0=ALU.add, op1=ALU.mult),
             reads=["tg", c.R("bbr", t, 2)], writes=["m2"])
        c.op("pool", lambda e: e.tensor_tensor(out=mg[t % 2][:], in0=m1[:], in1=m2[:], op=ALU.add),
             reads=[("m1", 0), ("m1", 1), "m2"], writes=[c.W("mg", t, 2)])
        pt = PS[7][:].bitcast(BF16)
        for k in range(8):
            c.op("pe", lambda e: e.transpose(out=pt[:, k * 128:(k + 1) * 128], in_=mg[t % 2][:, k * 128:(k + 1) * 128],
                                             identity=identb[:]), reads=[c.R("mg", t, 2), "const"], writes=["ptD"])
        c.op("act", lambda e: e.activation(out=mgT[t % 2][:], in_=pt.rearrange("p (j n) -> p j n", j=8), func=AF.Copy),
             reads=["ptD"], writes=[c.W("mgT", t, 2)])
        c.W("x1", t, 2)
        for hh in range(2):
            sl = slice(hh * 512, (hh + 1) * 512)
            for k in range(8):
                c.op("pe", lambda e: e.matmul(PS[1 + hh][:], lhsT=mgT[t % 2][:, k, :], rhs=wout[:, k, sl],
                                              start=(k == 0), stop=(k == 7)), reads=[c.R("mgT", t, 2), "wout"],
                     writes=[("pz", 1 + hh)])
            c.op("dve", lambda e: e.tensor_tensor(out=ty[:, sl], in0=PS[1 + hh][:], in1=GT1[:, sl], op=ALU.mult),
                 reads=[("pz", 1 + hh), "GT"], writes=[("m1", hh)])
            c.op("pool", lambda e: e.tensor_tensor(out=x1[t % 2][:, sl], in0=ty[:, sl], in1=xt[:, sl], op=ALU.add),
                 reads=[("m1", hh), c.R("xt", t, 4)], writes=[("x1", t % 2)])
        c.dma("sp", x1_d[t], x1[t % 2][:], reads=[("x1", t % 2)], writes=["x1_d"], key=f"x1w{t % 2}")
        m = t
        a, b, r = st2[:, m:m + 1], st2[:, 64 + m:65 + m], st2[:, 128 + m:129 + m]
        c.op("act", lambda e: e.activation(out=junk[:], in_=x1[t % 2][:], func=AF.Square, accum_out=a),
             reads=[("x1", t % 2)], writes=["junk", ("s2a", m)])
        c.op("dve", lambda e: e.tensor_scalar(out=b, in0=a, scalar1=1.0 / D, scalar2=RMS_EPS, op0=ALU.mult, op1=ALU.add),
             reads=[("s2a", m)], writes=[("s2b", m)])
        rsqrt_cols(b, r, 1, [("s2b", m)], [("s2r", m)])
        c.op("dve", lambda e: e.tensor_scalar(out=xs2[t % 2][:], in0=x1[t % 2][:], scalar1=r, scalar2=None, op0=ALU.mult),
             reads=[("x1", t % 2), ("s2r", m)], writes=[c.W("xs2", t, 2)])

    def C2_s2(t):
        for k in range(8):
            pb = PS[3 + k // 4][:, (k % 4) * 128:(k % 4 + 1) * 128]
            c.op("pe", lambda e: e.transpose(out=pb, in_=xs2[t % 2][:, k * 128:(k + 1) * 128], identity=identf[:]),
                 reads=[c.R("xs2", t, 2), "const"], writes=[("pz", 3 + k // 4)])
        for kk in range(8):
            k = (kk // 2) + 4 * (kk % 2)
            pb = PS[3 + k // 4][:, (k % 4) * 128:(k % 4 + 1) * 128]
            if k < 4:
                c.op("act", lambda e: e.activation(out=h2f[t % 2][:, k, :], in_=pb, func=AF.Identity,
                                                   scale=A2[:, k:k + 1], bias=B2[:, k:k + 1]),
                     reads=[("pz", 3), "AB"], writes=[("h2f", t % 2, 0)])
            else:
                c.op("dve", lambda e: e.tensor_scalar(out=h2f[t % 2][:, k, :], in0=pb, scalar1=A2[:, k:k + 1],
                                                      scalar2=B2[:, k:k + 1], op0=ALU.mult, op1=ALU.add),
                     reads=[("pz", 4), "AB"], writes=[("h2f", t % 2, 1)])
        c.op("dve", lambda e: e.tensor_copy(out=h2T[:, :, t * 128:(t + 1) * 128], in_=h2f[t % 2][:]),
             reads=[("h2f", t % 2, 0), ("h2f", t % 2, 1)], writes=["h2T"])
        for k in range(8):
            c.op("pe", lambda e: e.matmul(PS[5][:, 0:64], lhsT=h2f[t % 2][:, k, :], rhs=wr[:, k, :], start=(k == 0),
                                          stop=(k == 7)), reads=[("h2f", t % 2, 0), ("h2f", t % 2, 1), "wr"], writes=[("pz", 5)])
        R_ = rt[:, t % 2, :]
        s_, bi, mb, sel = R_[:, 0:64], R_[:, 64:128], R_[:, 128:192], R_[:, 192:256]
        m8 = R_[:, 256:320]
        gs, g8, gm, gneg = R_[:, 320:328], R_[:, 328:336], R_[:, 336:344], R_[:, 344:352]
        rk_ = ("rt", t % 2)
        c.op("act", lambda e: e.activation(out=s_, in_=PS[5][:, 0:64], func=AF.Tanh, scale=0.5), reads=[("pz", 5)],
             writes=[rk_])
        c.op("dve", lambda e: e.tensor_scalar(out=s_, in0=s_, scalar1=0.5, scalar2=0.5, op0=ALU.mult, op1=ALU.add),
             reads=[rk_], writes=[rk_])
        c.op("dve", lambda e: e.tensor_tensor(out=bi, in0=s_, in1=brout_t[:], op=ALU.add), reads=[rk_, "const"],
             writes=[rk_])
        for g in range(8):
            c.op("dve", lambda e: e.max(out=m8[:, g * 8:(g + 1) * 8], in_=bi[:, g * 8:(g + 1) * 8]), reads=[rk_],
                 writes=[rk_])
        m83 = m8.rearrange("p (g k) -> p g k", g=8)
        c.op("dve", lambda e: e.tensor_tensor(out=gs, in0=m83[:, :, 0], in1=m83[:, :, 1], op=ALU.add), reads=[rk_],
             writes=[rk_])
        c.op("dve", lambda e: e.max(out=g8, in_=gs), reads=[rk_], writes=[rk_])
        c.op("dve", lambda e: e.tensor_scalar(out=gm, in0=gs, scalar1=g8[:, 3:4], scalar2=None, op0=ALU.is_ge),
             reads=[rk_], writes=[rk_])
        c.op("dve", lambda e: e.tensor_scalar(out=gneg, in0=gm, scalar1=-1.0, scalar2=8.0, op0=ALU.add, op1=ALU.mult),
             reads=[rk_], writes=[rk_])
        bi3, mb3 = bi.rearrange("p (g k) -> p g k", g=8), mb.rearrange("p (g k) -> p g k", g=8)
        c.op("dve", lambda e: e.tensor_tensor(out=mb3, in0=bi3, in1=gm.unsqueeze(2).broadcast_to([128, 8, 8]), op=ALU.mult),
             reads=[rk_], writes=[rk_])
        c.op("dve", lambda e: e.tensor_tensor(out=mb3, in0=mb3, in1=gneg.unsqueeze(2).broadcast_to([128, 8, 8]), op=ALU.add),
             reads=[rk_], writes=[rk_])
        c.op("dve", lambda e: e.max(out=g8, in_=mb), reads=[rk_], writes=[rk_])
        c.op("dve", lambda e: e.tensor_scalar(out=sel, in0=mb, scalar1=g8[:, 7:8], scalar2=None, op0=ALU.is_ge),
             reads=[rk_], writes=[rk_])
        c.op("dve", lambda e: e.tensor_tensor(out=sel, in0=sel, in1=s_, op=ALU.mult), reads=[rk_], writes=[rk_])
        c.op("dve", lambda e: e.tensor_reduce(out=gs[:, 0:1], in_=sel, axis=AX.X, op=ALU.add), reads=[rk_], writes=[rk_])
        c.op("dve", lambda e: e.reciprocal(out=gs[:, 1:2], in_=gs[:, 0:1]), reads=[rk_], writes=[rk_])
        c.op("dve", lambda e: e.tensor_scalar(out=comb[:, t, :], in0=sel, scalar1=gs[:, 1:2], scalar2=2.5, op0=ALU.mult,
                                              op1=ALU.mult), reads=[rk_], writes=["comb"])

    def C2_s12(t):
        C2_s1(t)
        C2_s2(t)

    C2_s0(0)
    C2_s0(1)
    C2_s0b(0)
    C2_s1(0)
    for t in range(NO):
        C2_s1b(t)
        if t + 2 < NO:
            C2_s0(t + 2)
        if t + 1 < NO:
            C2_s0b(t + 1)
            C2_s1(t + 1)
        C2_s2(t)
    print("sbuf remaining in pass C2:", nc.sbuf_bytes_remaining, "ops", c.nops)
    c.pop()
    dbg("h2T", h2T[:], [128, 8, NO * 128], BF16)
    dbg("comb", comb[:], [128, NO, 64], F32)
    if stop == "C2":
        return finish()

    c.push()
    PG = c.psum("pg", [128, 2048], F32)
    PY = [c.psum(f"py{i}", [128, 1024], F32) for i in range(2)]
    acc = c.sbuf("acc", [128, NO, 1024], F32)
    wgu = [c.sbuf("wgu", [128, 8, 512], BF16) for _ in range(2)]
    wdn = [c.sbuf("wdn", [128, 2, 1024], BF16) for _ in range(2)]
    sgm = [c.sbuf("sgm", [128, 1024], BF16) for _ in range(2)]
    actT = [c.sbuf("actT", [128, 2, 512], BF16) for _ in range(2)]
    def load_expert(ei):
        e_ = ei - 1
        sl = ei % 2
        srcs = (w_sg, w_su, w_sd) if e_ < 0 else (w_eg[e_], w_eu[e_], w_ed[e_])
        c.W("wexp", ei, 2)
        c.dma("pool", wgu[sl][:, :, 0:256], srcs[0].rearrange("(k p) n -> p k n", p=128), writes=[("wexp", sl)], key=f"we{sl}a")
        c.dma("pool", wgu[sl][:, :, 256:512], srcs[1].rearrange("(k p) n -> p k n", p=128), writes=[("wexp", sl)], key=f"we{sl}b")
        c.dma("pool", wdn[sl][:], srcs[2].rearrange("(k p) n -> p k n", p=128), writes=[("wexp", sl)], key=f"we{sl}c")

    units = [(ei, tc_) for ei in range(NEXP + 1) for tc_ in range(4)]
    load_expert(0)

    def moe_s0(u):
        ei, tc_ = units[u]
        sl, a_ = ei % 2, u % 2
        if tc_ == 1 and ei + 1 <= NEXP:
            load_expert(ei + 1)
        wk = c.R("wexp", ei, 2)
        for j in range(4):
            for k in range(8):
                c.op("pe", lambda e: e.matmul(PG[:, j * 512:(j + 1) * 512], lhsT=wgu[sl][:, k, j * 128:(j + 1) * 128],
                                              rhs=h2T[:, k, tc_ * 512:(tc_ + 1) * 512], start=(k == 0), stop=(k == 7)),
                     reads=[wk, "h2T"], writes=[("pg", j // 2)])
        c.op("act", lambda e: e.activation(out=sgm[a_][:], in_=PG[:, 0:1024], func=AF.Silu), reads=[("pg", 0)],
             writes=[("sgm", a_)])
        c.op("dve", lambda e: e.tensor_tensor(out=actT[a_][:].rearrange("p f n -> p (f n)"), in0=sgm[a_][:],
                                              in1=PG[:, 1024:2048], op=ALU.mult), reads=[("sgm", a_), ("pg", 1)],
             writes=[("actT", a_)])

    def moe_s1(u):
        ei, tc_ = units[u]
        e_ = ei - 1
        sl, a_ = ei % 2, u % 2
        wk = ("wexp", sl)
        for tt in range(4):
            i = tc_ * 4 + tt
            y_ = (u * 4 + tt) % 2
            for hh in range(2):
                for fc in range(2):
                    c.op("pe", lambda e: e.matmul(PY[y_][:, hh * 512:(hh + 1) * 512], lhsT=actT[a_][:, fc, tt * 128:(tt + 1) * 128],
                                                  rhs=wdn[sl][:, fc, hh * 512:(hh + 1) * 512], start=(fc == 0), stop=(fc == 1)),
                         reads=[("actT", a_), wk], writes=[("py", y_)])
            if e_ < 0:
                c.op("act", lambda e: e.activation(out=acc[:, i, :], in_=PY[y_][:], func=AF.Copy), reads=[("py", y_)],
                     writes=[("acc", i)])
            else:
                c.op("dve", lambda e: e.scalar_tensor_tensor(out=acc[:, i, :], in0=PY[y_][:], scalar=comb[:, i, e_:e_ + 1],
                                                             in1=acc[:, i, :], op0=ALU.mult, op1=ALU.add),
                     reads=[("py", y_), ("acc", i), "comb"], writes=[("acc", i)])

    for u in range(len(units) + 1):
        if u < len(units):
            moe_s0(u)
        if u >= 1:
            moe_s1(u - 1)
    x1r = [c.sbuf("x1r", [128, 1024], F32) for _ in range(2)]
    xo = [c.sbuf("xo", [128, 1024], F32) for _ in range(2)]
    yo = [c.sbuf("yo", [128, 1024], F32) for _ in range(2)]
    stf = c.sbuf("stf", [128, 3 * 64], F32)
    for t in range(NO):
        c.dma("sp", x1r[t % 2][:], x1_d[t], reads=["x1_d"], writes=[("x1r", t % 2)], key=f"x1r{t % 2}")
        c.op("dve", lambda e: e.tensor_tensor(out=xo[t % 2][:], in0=acc[:, t, :], in1=GT2[:], op=ALU.mult),
             reads=[("acc", t), "GT"], writes=[("xo", t % 2)])
        c.op("pool", lambda e: e.tensor_tensor(out=xo[t % 2][:], in0=xo[t % 2][:], in1=x1r[t % 2][:], op=ALU.add),
             reads=[("xo", t % 2), ("x1r", t % 2)], writes=[("xo", t % 2)])
        a, b, r = stf[:, t:t + 1], stf[:, 64 + t:65 + t], stf[:, 128 + t:129 + t]
        c.op("act", lambda e: e.activation(out=junk[:], in_=xo[t % 2][:], func=AF.Square, accum_out=a),
             reads=[("xo", t % 2)], writes=["junk", ("sfa", t)])
        c.op("dve", lambda e: e.tensor_scalar(out=b, in0=a, scalar1=1.0 / D, scalar2=RMS_EPS, op0=ALU.mult, op1=ALU.add),
             reads=[("sfa", t)], writes=[("sfb", t)])
        rsqrt_cols(b, r, 1, [("sfb", t)], [("sfr", t)])
        c.op("dve", lambda e: e.scalar_tensor_tensor(out=yo[t % 2][:], in0=xo[t % 2][:], scalar=r, in1=gfin_t[:],
                                                     op0=ALU.mult, op1=ALU.mult),
             reads=[("xo", t % 2), ("sfr", t), "const"], writes=[("yo", t % 2)])
        c.dma("sp", out_d[t * 128:(t + 1) * 128, :], yo[t % 2][:], reads=[("yo", t % 2)], writes=["out"], key=f"out{t % 2}")
    c.pop()
    c.close()
    return nc


_NC_CACHE = {}


def _consts(j):
    bf = ml_dtypes.bfloat16
    k = np.arange(128)
    tri = (k[:, None] <= k[None, :]).astype(np.float32)
    amask = np.zeros((128, 4, 128), np.float32)
    for l in range(4):
        if l < j:
            amask[:, l, :] = 1.0
        elif l == j:
            amask[:, l, :] = tri
    inv_mla = 1.0 / (10000.0 ** (np.arange(0, 32, 2, dtype=np.float32) / np.float32(32)))
    inv_ret = 1.0 / (10000.0 ** (np.arange(0, 128, 2, dtype=np.float32) / np.float32(128)))
    invf = np.broadcast_to(np.concatenate([inv_mla, inv_ret]).astype(np.float32)[None, :], (128, 80)).copy()
    g = np.array(GAMMA, np.float64)
    m = np.arange(128, dtype=np.float64)
    kdecA = (g[None, :] ** (127.0 - m[:, None])) * 128.0 ** -0.5
    kdecC = (g[None, :] ** (-m[:, None])) * 128.0 ** -0.5
    qdec = g[None, :] ** m[:, None]
    rep = lambda a: np.repeat(a[:, :, None], 128, axis=2).astype(np.float32)
    G = g ** 128
    rc = np.zeros((128, 16), np.float64)
    for h in range(4):
        rc[:, h * 4 + 0] = g[h] * G[h] ** j
        for l in range(3):
            rc[:, h * 4 + 1 + l] = g[h] * (G[h] ** (j - 1 - l)) if l < j else 0.0
    return dict(identb=np.eye(128).astype(bf), identf=np.eye(128, dtype=np.float32), tri=tri,
                amask=amask.astype(bf), invf=invf, kdecA=rep(kdecA), kdecC=rep(kdecC), qdec=rep(qdec),
                rcoef=rc.astype(np.float32))


def _col(v, nchunk):
    return np.ascontiguousarray(np.asarray(v, np.float32).reshape(nchunk, 128).T)


_BUILD_ARGS = {}


def kernel(x, c, positions, w_ada, b_ada, g_norm1, w_in, g_cq, w_uq, g_ckv, w_ukv, g_ret, w_o_mla, w_o_ret,
           w_out, g_norm2, w_router, b_router, w_exp_gate, w_exp_up, w_exp_down, w_sh_gate, w_sh_up, w_sh_down,
           g_final):
    f = lambda a: np.ascontiguousarray(np.asarray(a, dtype=np.float32))
    x = f(x)
    positions = np.asarray(positions).astype(np.int32)
    if "nc" not in _NC_CACHE:
        _NC_CACHE["nc"] = build(**_BUILD_ARGS)
    nc = _NC_CACHE["nc"]
    shared = dict(
        w_ada=f(w_ada), bada_row=f(b_ada).reshape(1, -1), g1c=_col(g_norm1, 8), g2c=_col(g_norm2, 8), w_in=f(w_in),
        gcq=_col(g_cq, 3), w_uq=f(w_uq), gckv=_col(g_ckv, 2), w_ukv=f(w_ukv), gret=_col(g_ret, 8), w_o_mla=f(w_o_mla),
        w_o_ret=f(w_o_ret), w_out=f(w_out), w_router=f(w_router),
        brout=np.ascontiguousarray(np.broadcast_to(f(b_router)[None, :], (128, 64))),
        w_exp_gate=f(w_exp_gate), w_exp_up=f(w_exp_up), w_exp_down=f(w_exp_down), w_sh_gate=f(w_sh_gate),
        w_sh_up=f(w_sh_up), w_sh_down=f(w_sh_down),
        gfin=np.ascontiguousarray(np.broadcast_to(f(g_final)[None, :], (128, 1024))),
    )
    in_maps = []
    for core in range(8):
        b, j = core // 4, core % 4
        xb = x[b]
        xt = xb.reshape(NT, 128, D)
        pt = positions[b].reshape(NT, 128)
        m = dict(shared)
        m.update(_consts(j))
        m["xall"] = xb
        m["xown"] = np.ascontiguousarray(xt[j::4].reshape(NO * 128, D))
        m["posall"] = np.ascontiguousarray(pt.T)
        m["posown"] = np.ascontiguousarray(pt[j::4].T)
        m["cvec"] = _col(c[b], 8)
        in_maps.append(m)
    res = run_bass_kernel_spmd(nc, in_maps, core_ids=list(range(8)))
    _NC_CACHE["res"] = res
    out = np.empty((2, S, D), np.float32)
    for core in range(8):
        b, j = core // 4, core % 4
        o = np.asarray(res.results[core]["out"]).reshape(NO, 128, D)
        out[b].reshape(NT, 128, D)[j::4] = o
    return out
```
